# Optimizing a Trainium2 kernel written in Bass

```python
import math
import jax
import jax.numpy as jnp
from jax import lax
import numpy as np

D_MODEL = 1024
BATCH = 4
SEQ = 4096
DEPTH = 4

N_HEADS_RWKV = 8
HEAD_RWKV = 64
RWKV_WIDTH = N_HEADS_RWKV * HEAD_RWKV
LORA_W = 64
LORA_A = 64
LORA_G = 128
RWKV_GN_EPS = 64e-5

N_HEADS_NSA = 8
N_KV_GROUPS = 2
HEADS_PER_GROUP = N_HEADS_NSA // N_KV_GROUPS
HEAD_NSA = 64
NSA_Q_WIDTH = N_HEADS_NSA * HEAD_NSA
NSA_KV_WIDTH = N_KV_GROUPS * HEAD_NSA
CMP_BLOCK = 32
CMP_STRIDE = 16
CMP_HIDDEN = 128
SEL_BLOCK = 64
N_SELECT = 16
WINDOW = 512
Q_BLOCK = 128
N_BAND = WINDOW // Q_BLOCK + 1
NEG_INF = -1e30
FORCED_SCORE = 1e4

NUM_BUCKETS = 32
MAX_DISTANCE = 1024

N_GROUPS = 4
EXPERTS_PER_GROUP = 8
N_EXPERTS = N_GROUPS * EXPERTS_PER_GROUP
TOP_K_INNER = 2
D_EXPERT = 512
MOE_BLOCK = 128

ALPHA = (2 * DEPTH) ** 0.25
BETA = (8 * DEPTH) ** -0.25
LN_EPS = 1e-5

SHIFT_SPLITS = (RWKV_WIDTH, RWKV_WIDTH, RWKV_WIDTH, LORA_W, LORA_A, LORA_G)
SHIFT_WIDTH = 3 * RWKV_WIDTH + LORA_W + LORA_A + LORA_G
REST_SPLITS = (NSA_Q_WIDTH,) + (NSA_KV_WIDTH,) * 6 + (3 * N_HEADS_NSA, D_MODEL, D_MODEL)
IN_WIDTH = SHIFT_WIDTH + NSA_Q_WIDTH + 6 * NSA_KV_WIDTH + 3 * N_HEADS_NSA + 2 * D_MODEL

kernel_name = 'hybrid_rwkv7_nsa_hmoe_deepnorm'


def _split_cols(z, sizes):
    return jnp.split(z, np.cumsum(sizes)[:-1].tolist(), axis=-1)


def _layer_norm(x, g, b, eps=LN_EPS):
    xf = x.astype(jnp.float32)
    mu = jnp.mean(xf, axis=-1, keepdims=True)
    var = jnp.mean(jnp.square(xf - mu), axis=-1, keepdims=True)
    return ((xf - mu) * lax.rsqrt(var + eps) * g + b).astype(x.dtype)


def _t5_bucket(dist):
    n = jnp.maximum(dist, 0)
    max_exact = NUM_BUCKETS // 2
    nf = jnp.maximum(n, 1).astype(jnp.float32)
    large = max_exact + (jnp.log(nf / max_exact) / math.log(MAX_DISTANCE / max_exact)
                         * (NUM_BUCKETS - max_exact)).astype(jnp.int32)
    large = jnp.minimum(large, NUM_BUCKETS - 1)
    return jnp.where(n < max_exact, n, large)


def _bias_from_buckets(rel_bias, bucket):
    b = jnp.moveaxis(rel_bias.astype(jnp.float32)[bucket], -1, 0)
    return b.reshape((N_KV_GROUPS, HEADS_PER_GROUP) + bucket.shape)


def _nsa_positional(rel_bias, seq):
    n_cmp = seq // CMP_STRIDE - CMP_BLOCK // CMP_STRIDE + 1
    n_sel_blocks = seq // SEL_BLOCK
    t = jnp.arange(seq)[:, None]
    c = jnp.arange(n_cmp)[None, :]
    d_cmp = t - (c * CMP_STRIDE + CMP_BLOCK - 1)
    mask_cmp = d_cmp >= 0
    bias_cmp = _bias_from_buckets(rel_bias, _t5_bucket(d_cmp))
    qo = jnp.arange(Q_BLOCK)[:, None]
    m = jnp.arange(WINDOW + Q_BLOCK)[None, :]
    d_win = qo + WINDOW - m
    bias_win = _bias_from_buckets(rel_bias, _t5_bucket(d_win))
    blk = jnp.arange(seq // Q_BLOCK)[:, None, None]
    mask_win = (d_win >= 0) & (d_win < WINDOW) & (blk * Q_BLOCK - WINDOW + m >= 0)
    cs = jnp.arange(n_cmp)[:, None] * CMP_STRIDE
    ss = jnp.arange(n_sel_blocks)[None, :] * SEL_BLOCK
    overlap = jnp.clip(jnp.minimum(cs + CMP_BLOCK, ss + SEL_BLOCK) - jnp.maximum(cs, ss), 0, None)
    cmp_to_sel = overlap.astype(jnp.float32) / CMP_BLOCK
    return bias_cmp, mask_cmp, bias_win, mask_win, cmp_to_sel


def _rwkv7_time_mix(r, k, v, wl, al, gl, w0, w2, a0, a2, g2, k_k, k_a, r_k, ln_g, ln_b):
    B, S, C = r.shape
    H, N = N_HEADS_RWKV, HEAD_RWKV
    f32 = jnp.float32
    logw = -jax.nn.softplus(-(w0 + jnp.tanh(wl) @ w2).astype(f32)) - 0.5
    decay = jnp.exp(-jnp.exp(logw))
    a = jax.nn.sigmoid((a0 + al @ a2).astype(f32))
    g = jax.nn.sigmoid(gl) @ g2
    r, k, v = r.astype(f32), k.astype(f32), v.astype(f32)
    heads = lambda t: t.reshape(B, S, H, N)
    kk = heads(k * k_k)
    kk = kk / jnp.maximum(jnp.linalg.norm(kk, axis=-1, keepdims=True), 1e-12)
    k = k * (1.0 + (a - 1.0) * k_a)
    rh, kh, vh, ah, wh = heads(r), heads(k), heads(v), heads(a), heads(decay)

    def step(state, inp):
        r_t, w_t, k_t, v_t, kk_t, a_t = inp
        sa = jnp.einsum('bhvk,bhk->bhv', state, -kk_t)
        state = (state * w_t[:, :, None, :] + sa[..., None] * (kk_t * a_t)[:, :, None, :]
                 + v_t[..., None] * k_t[:, :, None, :])
        return state, jnp.einsum('bhvk,bhk->bhv', state, r_t)

    xs = tuple(jnp.moveaxis(t, 1, 0) for t in (rh, wh, kh, vh, kk, ah))
    _, y = lax.scan(step, jnp.zeros((B, H, N, N), f32), xs)
    y = jnp.moveaxis(y, 0, 1)
    mu = jnp.mean(y, axis=-1, keepdims=True)
    var = jnp.mean(jnp.square(y - mu), axis=-1, keepdims=True)
    y = ((y - mu) * lax.rsqrt(var + RWKV_GN_EPS)).reshape(B, S, C) * ln_g + ln_b
    bonus = jnp.sum(rh * kh * r_k, axis=-1, keepdims=True) * vh
    return (y + bonus.reshape(B, S, C)) * g


def _compress(t, pe, w1, w2):
    B, S, G, Dh = t.shape
    rep = CMP_BLOCK // CMP_STRIDE
    nc = S // CMP_STRIDE - rep + 1
    sub = t.reshape(B, S // CMP_STRIDE, CMP_STRIDE, G, Dh)
    blk = jnp.concatenate([sub[:, j:j + nc] for j in range(rep)], axis=2)
    blk = blk + pe[:, None, :]
    blk = blk.transpose(0, 1, 3, 2, 4).reshape(B, nc, G, CMP_BLOCK * Dh)
    out = jax.nn.gelu(blk @ w1) @ w2
    return out.transpose(0, 2, 1, 3)


def _nsa_attention(q, k_cmp, v_cmp, k_slc, v_slc, k_win, v_win, gate_logits,
                   pe_k, w1_k, w2_k, pe_v, w1_v, w2_v, rel_bias, pos):
    bias_cmp, mask_cmp, bias_win, mask_win, cmp_to_sel = pos
    B, S, _ = q.shape
    G, Hg, Dh = N_KV_GROUPS, HEADS_PER_GROUP, HEAD_NSA
    f32 = jnp.float32
    scale = HEAD_NSA ** -0.5
    qh = q.reshape(B, S, G, Hg, Dh).transpose(0, 2, 3, 1, 4)
    kv = lambda t: t.reshape(B, S, G, Dh).transpose(0, 2, 1, 3)

    kc = _compress(k_cmp.reshape(B, S, G, Dh), pe_k, w1_k, w2_k)
    vc = _compress(v_cmp.reshape(B, S, G, Dh), pe_v, w1_v, w2_v)
    lg = jnp.einsum('bghqd,bgcd->bghqc', qh, kc).astype(f32) * scale + bias_cmp
    p_cmp = jax.nn.softmax(jnp.where(mask_cmp, lg, NEG_INF), axis=-1) * mask_cmp
    o_cmp = jnp.einsum('bghqc,bgcd->bghqd', p_cmp, vc.astype(f32))

    n_sel_blocks = S // SEL_BLOCK
    n_sel = min(N_SELECT, n_sel_blocks)
    score = jnp.einsum('bgqc,cj->bgqj', jnp.sum(p_cmp, axis=2), cmp_to_sel)
    t = jnp.arange(S)[:, None]
    j = jnp.arange(n_sel_blocks)[None, :]
    cur = t // SEL_BLOCK
    forced = (j == 0) | (j == cur) | (j == cur - 1)
    score = jnp.where(forced, FORCED_SCORE, jnp.where(j <= cur, score, -1.0))
    _, sel_idx = lax.top_k(score, n_sel)
    ks_b = kv(k_slc).reshape(B, G, n_sel_blocks, SEL_BLOCK, Dh)
    vs_b = kv(v_slc).reshape(B, G, n_sel_blocks, SEL_BLOCK, Dh)
    table = rel_bias.astype(f32).reshape(NUM_BUCKETS, G, Hg).transpose(1, 2, 0)
    bi = jnp.arange(B)[:, None, None, None]
    gi = jnp.arange(G)[None, :, None, None]
    gi6 = jnp.arange(G)[None, :, None, None, None, None]
    hi6 = jnp.arange(Hg)[None, None, :, None, None, None]

    def sel_chunk(c):
        t0 = c * Q_BLOCK
        qc = lax.dynamic_slice_in_dim(qh, t0, Q_BLOCK, axis=3)
        ic = lax.dynamic_slice_in_dim(sel_idx, t0, Q_BLOCK, axis=2)
        kg = ks_b[bi, gi, ic]
        vg = vs_b[bi, gi, ic]
        kpos = ic[..., None] * SEL_BLOCK + jnp.arange(SEL_BLOCK)
        dist = (t0 + jnp.arange(Q_BLOCK))[:, None, None] - kpos
        bias = table[gi6, hi6, _t5_bucket(dist)[:, :, None]]
        lgs = jnp.einsum('bghqd,bgqnld->bghqnl', qc, kg).astype(f32) * scale + bias
        lgs = jnp.where((dist >= 0)[:, :, None], lgs, NEG_INF)
        p = jax.nn.softmax(lgs.reshape(B, G, Hg, Q_BLOCK, n_sel * SEL_BLOCK), axis=-1).reshape(lgs.shape)
        return jnp.einsum('bghqnl,bgqnld->bghqd', p, vg.astype(f32))

    o_slc = lax.map(sel_chunk, jnp.arange(S // Q_BLOCK))
    o_slc = jnp.moveaxis(o_slc, 0, 3).reshape(B, G, Hg, S, Dh)

    nb = S // Q_BLOCK

    def band(t):
        tp = jnp.pad(kv(t), ((0, 0), (0, 0), (WINDOW, 0), (0, 0))).reshape(B, G, nb + N_BAND - 1, Q_BLOCK, Dh)
        return jnp.concatenate([tp[:, :, o:o + nb] for o in range(N_BAND)], axis=3)

    kw_b, vw_b = band(k_win), band(v_win)
    qb = qh.reshape(B, G, Hg, nb, Q_BLOCK, Dh)
    lgw = jnp.einsum('bghnqd,bgnkd->bghnqk', qb, kw_b).astype(f32) * scale + bias_win[:, :, None]
    pw = jax.nn.softmax(jnp.where(mask_win, lgw, NEG_INF), axis=-1)
    o_win = jnp.einsum('bghnqk,bgnkd->bghnqd', pw, vw_b.astype(f32)).reshape(B, G, Hg, S, Dh)

    gates = jax.nn.sigmoid(gate_logits.astype(f32)).reshape(B, S, G, Hg, 3).transpose(0, 2, 3, 1, 4)
    o = gates[..., 0:1] * o_cmp + gates[..., 1:2] * o_slc + gates[..., 2:3] * o_win
    return o.transpose(0, 3, 1, 2, 4).reshape(B, S, NSA_Q_WIDTH)


def _token_mixer(x, w_in, shift_mu, rw_w0, rw_w2, rw_a0, rw_a2, rw_g2, rw_kk, rw_ka, rw_rk,
                 rw_ln_g, rw_ln_b, cmp_pe_k, cmp_w1_k, cmp_w2_k, cmp_pe_v, cmp_w1_v, cmp_w2_v,
                 w_up_rwkv, w_up_nsa, w_out, rel_bias, pos):
    z = x @ w_in
    z_shift, z_rest = z[..., :SHIFT_WIDTH], z[..., SHIFT_WIDTH:]
    z_prev = jnp.pad(z_shift, ((0, 0), (1, 0), (0, 0)))[:, :-1]
    z_shift = z_shift + (z_prev - z_shift) * shift_mu
    r, k, v, wl, al, gl = _split_cols(z_shift, SHIFT_SPLITS)
    q, kc, vc, ks, vs, kw, vw, nsa_g, g_rw, g_nsa = _split_cols(z_rest, REST_SPLITS)
    y_rw = _rwkv7_time_mix(r, k, v, wl, al, gl, rw_w0, rw_w2, rw_a0, rw_a2, rw_g2,
                           rw_kk, rw_ka, rw_rk, rw_ln_g, rw_ln_b).astype(x.dtype)
    y_nsa = _nsa_attention(q, kc, vc, ks, vs, kw, vw, nsa_g, cmp_pe_k, cmp_w1_k, cmp_w2_k,
                           cmp_pe_v, cmp_w1_v, cmp_w2_v, rel_bias, pos).astype(x.dtype)
    merged = jax.nn.sigmoid(g_rw) * (y_rw @ w_up_rwkv) + jax.nn.sigmoid(g_nsa) * (y_nsa @ w_up_nsa)
    return merged @ w_out


def _hier_moe(x, wg, bg, we, be, w1, w3, w2):
    B, S, D = x.shape
    N = B * S
    f32 = jnp.float32
    xf = x.reshape(N, D)
    g_prob = jax.nn.softmax((xf @ wg + bg).astype(f32), axis=-1)
    grp = jnp.argmax(g_prob, axis=-1)
    p_grp = jnp.take_along_axis(g_prob, grp[:, None], axis=1)[:, 0]
    e_logits = (xf @ we + be).astype(f32).reshape(N, N_GROUPS, EXPERTS_PER_GROUP)
    e_logits = jnp.take_along_axis(e_logits, grp[:, None, None], axis=1)[:, 0]
    top_v, top_i = lax.top_k(e_logits, TOP_K_INNER)
    top_w = jax.nn.softmax(top_v, axis=-1) * p_grp[:, None]
    eid = (grp[:, None] * EXPERTS_PER_GROUP + top_i).reshape(-1).astype(jnp.int32)
    slot_w = top_w.reshape(-1)
    slot_tok = jnp.repeat(jnp.arange(N, dtype=jnp.int32), TOP_K_INNER)
    n_slots = N * TOP_K_INNER
    order = jnp.argsort(eid)
    eid_s = eid[order]
    counts = jax.ops.segment_sum(jnp.ones_like(eid), eid, num_segments=N_EXPERTS)
    starts = jnp.cumsum(counts) - counts
    pcounts = (counts + MOE_BLOCK - 1) // MOE_BLOCK * MOE_BLOCK
    pends = jnp.cumsum(pcounts)
    pstarts = pends - pcounts
    dest = pstarts[eid_s] + (jnp.arange(n_slots) - starts[eid_s])
    n_rows = n_slots + N_EXPERTS * MOE_BLOCK
    n_blk = n_rows // MOE_BLOCK
    row_tok = jnp.full((n_rows,), N, jnp.int32).at[dest].set(slot_tok[order])
    row_w = jnp.zeros((n_rows,), f32).at[dest].set(slot_w[order])
    blk_exp = jnp.minimum(jnp.sum(jnp.arange(n_blk)[:, None] * MOE_BLOCK >= pends[None, :], axis=1),
                          N_EXPERTS - 1)
    xpad = jnp.concatenate([xf, jnp.zeros((1, D), xf.dtype)], axis=0)
    xs = xpad[row_tok].reshape(n_blk, MOE_BLOCK, D)

    def expert_block(args):
        xb, e = args
        h = jax.nn.silu(xb @ w1[e]) * (xb @ w3[e])
        return h @ w2[e]

    ys = lax.map(expert_block, (xs, blk_exp)).reshape(n_rows, D)
    out = jnp.zeros((N + 1, D), f32).at[row_tok].add(ys.astype(f32) * row_w[:, None])
    return out[:N].reshape(B, S, D).astype(x.dtype)


def setup_inputs(seed: int = 0) -> dict:
    key = jax.random.key(seed)
    keys = iter(jax.random.split(key, 48))
    f32 = jnp.float32

    def nrm(shape, scale):
        return jax.random.normal(next(keys), shape, f32) * scale

    L, D, C = DEPTH, D_MODEL, RWKV_WIDTH
    return {
        'x': nrm((BATCH, SEQ, D), 1.0),
        'rel_bias': nrm((NUM_BUCKETS, N_HEADS_NSA), 0.3),
        'w_in': nrm((L, D, IN_WIDTH), D ** -0.5),
        'shift_mu': jax.random.uniform(next(keys), (L, SHIFT_WIDTH), f32),
        'rw_w0': jnp.linspace(-6.0, -1.0, C, dtype=f32)[None, :] + nrm((L, C), 0.1),
        'rw_w2': nrm((L, LORA_W, C), 0.1 * LORA_W ** -0.5),
        'rw_a0': nrm((L, C), 0.1),
        'rw_a2': nrm((L, LORA_A, C), 0.3 * LORA_A ** -0.5),
        'rw_g2': nrm((L, LORA_G, C), LORA_G ** -0.5),
        'rw_kk': 0.85 + nrm((L, C), 0.05),
        'rw_ka': 1.0 + nrm((L, C), 0.05),
        'rw_rk': nrm((L, N_HEADS_RWKV, HEAD_RWKV), 0.1),
        'rw_ln_g': 1.0 + nrm((L, C), 0.02),
        'rw_ln_b': nrm((L, C), 0.02),
        'cmp_pe_k': nrm((L, CMP_BLOCK, HEAD_NSA), 0.1),
        'cmp_w1_k': nrm((L, CMP_BLOCK * HEAD_NSA, CMP_HIDDEN), (CMP_BLOCK * HEAD_NSA) ** -0.5),
        'cmp_w2_k': nrm((L, CMP_HIDDEN, HEAD_NSA), CMP_HIDDEN ** -0.5),
        'cmp_pe_v': nrm((L, CMP_BLOCK, HEAD_NSA), 0.1),
        'cmp_w1_v': nrm((L, CMP_BLOCK * HEAD_NSA, CMP_HIDDEN), (CMP_BLOCK * HEAD_NSA) ** -0.5),
        'cmp_w2_v': nrm((L, CMP_HIDDEN, HEAD_NSA), CMP_HIDDEN ** -0.5),
        'w_up_rwkv': nrm((L, C, D), C ** -0.5),
        'w_up_nsa': nrm((L, NSA_Q_WIDTH, D), NSA_Q_WIDTH ** -0.5),
        'w_out': nrm((L, D, D), BETA * D ** -0.5),
        'ln1_g': 1.0 + nrm((L, D), 0.02),
        'ln1_b': nrm((L, D), 0.02),
        'router_group_w': nrm((L, D, N_GROUPS), D ** -0.5),
        'router_group_b': nrm((L, N_GROUPS), 0.01),
        'router_expert_w': nrm((L, D, N_EXPERTS), D ** -0.5),
        'router_expert_b': nrm((L, N_EXPERTS), 0.01),
        'exp_w1': nrm((L, N_EXPERTS, D, D_EXPERT), D ** -0.5),
        'exp_w3': nrm((L, N_EXPERTS, D, D_EXPERT), D ** -0.5),
        'exp_w2': nrm((L, N_EXPERTS, D_EXPERT, D), BETA * D_EXPERT ** -0.5),
        'ln2_g': 1.0 + nrm((L, D), 0.02),
        'ln2_b': nrm((L, D), 0.02),
    }


def reference(x, rel_bias, w_in, shift_mu, rw_w0, rw_w2, rw_a0, rw_a2, rw_g2, rw_kk, rw_ka, rw_rk,
              rw_ln_g, rw_ln_b, cmp_pe_k, cmp_w1_k, cmp_w2_k, cmp_pe_v, cmp_w1_v, cmp_w2_v,
              w_up_rwkv, w_up_nsa, w_out, ln1_g, ln1_b, router_group_w, router_group_b,
              router_expert_w, router_expert_b, exp_w1, exp_w3, exp_w2, ln2_g, ln2_b):
    pos = _nsa_positional(rel_bias, x.shape[1])
    for l in range(DEPTH):
        h = _token_mixer(x, w_in[l], shift_mu[l], rw_w0[l], rw_w2[l], rw_a0[l], rw_a2[l], rw_g2[l],
                         rw_kk[l], rw_ka[l], rw_rk[l], rw_ln_g[l], rw_ln_b[l],
                         cmp_pe_k[l], cmp_w1_k[l], cmp_w2_k[l], cmp_pe_v[l], cmp_w1_v[l], cmp_w2_v[l],
                         w_up_rwkv[l], w_up_nsa[l], w_out[l], rel_bias, pos)
        x = _layer_norm(ALPHA * x + h, ln1_g[l], ln1_b[l])
        h = _hier_moe(x, router_group_w[l], router_group_b[l], router_expert_w[l], router_expert_b[l],
                      exp_w1[l], exp_w3[l], exp_w2[l])
        x = _layer_norm(ALPHA * x + h, ln2_g[l], ln2_b[l])
    return x
```

```python
import numpy as np
from contextlib import ExitStack
import concourse.bass as bass
import concourse.mybir as mybir
from concourse.bass_utils import run_bass_kernel_spmd

F32 = mybir.dt.float32
BF16 = mybir.dt.bfloat16
AF = mybir.ActivationFunctionType
ALU = mybir.AluOpType
AX = mybir.AxisListType


class Buf:
    __slots__ = ("name", "w", "r", "excl")

    def __init__(self, name="", excl=False):
        self.name = name
        self.excl = excl
        self.w = None
        self.r = {}


class KB:
    def __init__(self, nc, stack):
        self.nc = nc
        self.stack = stack
        self.names = ["pe", "act", "dve", "pool", "sp"]
        self.prog = {e: [] for e in self.names}
        self.sem = {e: stack.enter_context(nc.semaphore("s_" + e)) for e in self.names}
        self.cnt = {e: 0 for e in self.names}
        self.seen = {e: {} for e in self.names}
        self.dsem = {}
        self.n_ins = 0
        self.nw = {e: 0 for e in self.names}

    def sb(self, name, shape, dt=F32):
        return self.stack.enter_context(self.nc.sbuf_tensor("sb_" + name, list(shape), dt))

    def ps(self, name, shape, dt=F32):
        return self.stack.enter_context(self.nc.psum_tensor("ps_" + name, list(shape), dt))

    def _semh(self, key):
        if key in self.sem:
            return self.sem[key]
        return self.dsem[key][0]

    def _waits(self, eng, reads, writes):
        need = {}

        def add(d):
            if d is None:
                return
            k, v = d
            if need.get(k, 0) < v:
                need[k] = v
        for b in reads:
            add(b.w)
        for b in writes:
            add(b.w)
            for k, v in b.r.items():
                add((k, v))
        out = []
        seen = self.seen[eng]
        for k, v in need.items():
            if k == "pe" and eng == "pe":
                continue
            if seen.get(k, 0) >= v:
                continue
            seen[k] = v
            out.append((self._semh(k), v))
        return out

    def _mark(self, tok, reads, writes):
        for b in writes:
            b.w = tok
            b.r = {}
        k, v = tok
        for b in reads:
            if b.r.get(k, 0) < v:
                b.r[k] = v

    def op(self, eng, fn, reads=(), writes=()):
        ex = [b for b in reads if b.excl]
        if ex:
            writes = list(writes) + ex
        waits = self._waits(eng, reads, writes)
        self.nw[eng] += len(waits)
        self.cnt[eng] += 1
        tok = (eng, self.cnt[eng])
        sem = self.sem[eng]

        def run(e, waits=waits, fn=fn, sem=sem):
            for s, v in waits:
                e.wait_ge(s, v)
            fn(e).then_inc(sem, 1)
        self.prog[eng].append(run)
        self._mark(tok, reads, writes)
        self.n_ins += 1

    def dma(self, q, key, fn, reads=(), writes=(), n=1):
        key = "d_" + key
        if key not in self.dsem:
            self.dsem[key] = [self.stack.enter_context(self.nc.semaphore(key)), 0]
        waits = self._waits(q, reads, writes)
        self.dsem[key][1] += 16 * n
        tok = (key, self.dsem[key][1])
        sem = self.dsem[key][0]

        def run(e, waits=waits, fn=fn, sem=sem):
            for s, v in waits:
                e.wait_ge(s, v)
            fn(e, sem)
        self.prog[q].append(run)
        self._mark(tok, reads, writes)
        self.n_ins += n

    def finish(self, bufs):
        waits = self._waits("sp", bufs, bufs)

        def run(e, waits=waits):
            for s, v in waits:
                e.wait_ge(s, v)
        self.prog["sp"].append(run)

    def emit(self):
        nc = self.nc
        with nc.Block() as block:
            @block.sync
            def _(e):
                for f in self.prog["sp"]:
                    f(e)

            @block.tensor
            def _(e):
                for f in self.prog["pe"]:
                    f(e)

            @block.scalar
            def _(e):
                for f in self.prog["act"]:
                    f(e)

            @block.vector
            def _(e):
                for f in self.prog["dve"]:
                    f(e)

            @block.gpsimd
            def _(e):
                for f in self.prog["pool"]:
                    f(e)


S = 4096
NS = 512
NSEG = S // NS
CH = 128
NCH = NS // CH
GN_EPS = 64e-5


def build_a1(nseg=NSEG, debug=False):
    nc = bass.Bass("TRN2", target_bir_lowering=False)
    xT_d = nc.dram_tensor("xT", [1024, S], F32, kind="ExternalInput").ap()
    w_d = nc.dram_tensor("w", [1024, 1024], F32, kind="ExternalInput").ap()
    vec_d = nc.dram_tensor("vec", [128, 22], F32, kind="ExternalInput").ap()
    lw_d = nc.dram_tensor("lw", [128, 256], F32, kind="ExternalInput").ap()
    g2_d = nc.dram_tensor("g2", [128, 256], F32, kind="ExternalInput").ap()
    cst_d = nc.dram_tensor("cst", [128, 512 + 256 + 128 + 128], F32, kind="ExternalInput").ap()
    yT_d = nc.dram_tensor("yT", [256, S], F32, kind="ExternalOutput").ap()
    xT_v = xT_d.rearrange("(kc p) t -> p kc t", p=128)
    w_v = w_d.rearrange("(kc p) n -> p kc n", p=128)

    with ExitStack() as st:
        kb = KB(nc, st)
        sb, ps = kb.sb, kb.ps
        NXS = 3
        xs = [sb(f"xs{i}", [128, 8, NS], BF16) for i in range(NXS)]; XS = [Buf() for _ in range(NXS)]
        wsb = sb("wsb", [128, 8, 1024], BF16); WSB = Buf()
        vec = sb("vec", [128, 22]); VEC = Buf()
        vx = sb("vx", [128, 8]); VX = Buf()
        lw = sb("lw", [128, 256]); LW = Buf()
        g2 = sb("g2", [128, 256]); G2 = Buf()
        cst = sb("cst", [128, 1024]); CST = Buf()
        MASK1 = cst[:, 0:512]; MASK2 = cst[:, 512:768]; MSL = cst[:, 768:896]; BONES = cst[:, 896:1024]
        ident = sb("ident", [128, 128]); IDENT = Buf()
        car = sb("car", [128, 8]); CAR = Buf()
        zr = [sb(f"zr{i}", [128, NS + 1]) for i in range(2)]; ZR = [Buf() for _ in range(2)]
        dtmp = sb("dtmp", [128, NS]); DTMP = Buf()
        zs = [sb(f"zs{j}", [128, NS]) for j in range(8)]; ZS = [Buf() for _ in range(8)]
        tw = sb("tw", [128, NS]); TW = Buf()
        sg = sb("sg", [128, NS]); SG = Buf()
        tnames = ["nld", "cw", "ew", "ewi", "ewx", "aa", "kkn", "sq", "t1", "k2", "bh", "kh", "e1"]
        T = {n: sb("t_" + n, [128, NS]) for n in tnames}; TB = {n: Buf() for n in tnames}
        ar = [sb(f"ar{h}", [128, NCH, 2 * CH]) for h in range(2)]; AR = [Buf() for _ in range(2)]
        bt = [sb(f"bt{h}", [128, NS]) for h in range(2)]; BT = [Buf() for _ in range(2)]
        kt = [sb(f"kt{h}", [128, NS]) for h in range(2)]; KT = [Buf() for _ in range(2)]
        gg = [sb(f"gg{h}", [128, NS]) for h in range(2)]; GG = [Buf() for _ in range(2)]
        bon = [sb(f"bon{h}", [128, NS]) for h in range(2)]; BON = [Buf() for _ in range(2)]
        yf = [sb(f"yf{h}", [128, NS]) for h in range(2)]; YF = [Buf() for _ in range(2)]
        wc = [sb(f"wc{h}", [128, NCH]) for h in range(2)]; WC = [Buf() for _ in range(2)]
        bhT = sb("bhT", [128, NCH, 256]); BHT = [Buf() for _ in range(NCH)]
        khT = sb("khT", [128, NCH, 256]); KHT = [Buf() for _ in range(NCH)]
        vT = sb("vT", [128, NCH, 256]); VT = [Buf() for _ in range(NCH)]
        mab = [sb(f"mab{h}", [128, 256]) for h in range(4)]; MAB = [Buf() for _ in range(4)]
        mak = [sb(f"mak{h}", [128, 256]) for h in range(4)]; MAK = [Buf() for _ in range(4)]
        mm = [[sb(f"mm{h}_{i}", [128, 128]) for i in range(2)] for h in range(4)]; MM = [[Buf(), Buf()] for _ in range(4)]
        nn = [[sb(f"nn{h}_{i}", [128, 128]) for i in range(2)] for h in range(4)]; NN = [[Buf(), Buf()] for _ in range(4)]
        qq = [[sb(f"qq{h}_{i}", [128, 128]) for i in range(2)] for h in range(4)]; QQ = [[Buf(), Buf()] for _ in range(4)]
        xsb = [sb(f"xsb{h}", [128, 64]) for h in range(4)]; XSB = [Buf() for _ in range(4)]
        usb = [sb(f"usb{h}", [128, 64]) for h in range(4)]; USB = [Buf() for _ in range(4)]
        stt = [[sb(f"st{hp}_{i}", [128, 64]) for i in range(2)] for hp in range(2)]
        STT = [[[Buf(), Buf()] for _ in range(2)] for hp in range(2)]
        ytok = sb("ytok", [128, 256]); YTOK = Buf()
        ysq = sb("ysq", [128, 256]); YSQ = Buf()
        yn = sb("yn", [128, 256]); YN = Buf()
        sts = sb("sts", [128, 32]); STS = Buf()
        osb = [sb(f"osb{h}", [128, NS]) for h in range(2)]; OSB = [Buf() for _ in range(2)]
        pa = [ps(f"pa{i}", [128, 512]) for i in range(2)]; PA = [Buf(excl=True) for _ in range(2)]
        pab = [ps(f"pab{i}", [128, 512]) for i in range(2)]; PAB = [Buf(excl=True) for _ in range(2)]
        pq = [ps(f"pq{i}", [128, 512]) for i in range(2)]; PQB = [Buf(excl=True) for _ in range(2)]
        PQ = [[PQB[i]] * 4 for i in range(2)]
        psts = [ps(f"pst{i}", [128, 512]) for i in range(2)]; PSTB = [Buf(excl=True) for _ in range(2)]

        def ld(q, key, out, in_, B):
            kb.dma(q, key, lambda e, s: e.dma_start(out=out, in_=in_).then_inc(s, 16), writes=[B])
        ld("sp", "vec", vec[:], vec_d[:, :], VEC)
        ld("sp", "lw", lw[:], lw_d[:, :], LW)
        ld("sp", "g2", g2[:], g2_d[:, :], G2)
        ld("sp", "cst", cst[:], cst_d[:, :], CST)
        for kc in range(0, 8, 4):
            kb.dma("pool", "wsb", lambda e, s, kc=kc: e.dma_start(out=wsb[:, kc:kc + 4, :], in_=w_v[:, kc:kc + 4, :]).then_inc(s, 16), writes=[WSB])
        kb.op("pool", lambda e: e.memset(ident[:], 1.0), writes=[IDENT])
        kb.op("pool", lambda e: e.affine_select(out=ident[:], in_=ident[:], pattern=[[-1, 128]], compare_op=ALU.is_equal,
                                                fill=0.0, base=0, channel_multiplier=1), reads=[IDENT], writes=[IDENT])
        kb.op("dve", lambda e: e.memset(car[:], 0.0), writes=[CAR])
        kb.op("dve", lambda e: e.tensor_scalar(vx[:, 0:2], vec[:, 8:10], -1.0, None, ALU.mult), reads=[VEC], writes=[VX])
        kb.op("dve", lambda e: e.tensor_scalar(vx[:, 2:4], vec[:, 14:16], -1.0, 1.0, ALU.mult, ALU.add), reads=[VEC, VX], writes=[VX])
        for hp in range(2):
            for i in range(2):
                kb.op("dve", lambda e, hp=hp, i=i: e.memset(stt[hp][i][:], 0.0), writes=STT[hp][i])

        def load_x(sgi):
            sl = sgi % NXS
            kb.dma("pool", f"xs{sl}", lambda e, s, sl=sl, sgi=sgi: e.dma_start(
                out=xs[sl][:, :, :], in_=xT_v[:, :, sgi * NS:(sgi + 1) * NS]).then_inc(s, 16), writes=[XS[sl]])

        load_x(0)
        pai = [0]

        def next_pa():
            i = pai[0] % 2
            pai[0] += 1
            return pa[i], PA[i]

        def mm512(lhsT_fn, rhs_fn, nk, reads, M=128):
            p, P = next_pa()
            for k in range(nk):
                a_, b_ = lhsT_fn(k), rhs_fn(k)
                kb.op("pe", lambda e, k=k, p=p, a_=a_, b_=b_: e.matmul(p[0:M, :], a_, b_, start=(k == 0), stop=(k == nk - 1)),
                      reads=reads, writes=[P])
            return p, P

        ping = [0, 0]
        dbg_n = [0]

        def dbg(name, ap, B):
            if not debug:
                return
            shp = list(ap.shape)
            d = nc.dram_tensor("dbg_" + name, shp, F32, kind="ExternalOutput").ap()
            dbg_n[0] += 1
            cntv = dbg_n[0] * 16

            def f(e, s, d=d, ap=ap, cntv=cntv):
                e.dma_start(out=d, in_=ap).then_inc(s, 16)
                e.wait_ge(s, cntv)
            kb.dma("sp", "dbg", f, reads=[B])
        for sgi in range(nseg):
            sl = sgi % NXS
            if sgi + 1 < nseg:
                load_x(sgi + 1)
            for j in range(8):
                p, P = mm512(lambda k, j=j: wsb[:, k, j * 128:(j + 1) * 128], lambda k, sl=sl: xs[sl][:, k, :], 8, [WSB, XS[sl]])
                z, Z = zr[j % 2], ZR[j % 2]
                kb.op("pool", lambda e, z=z, j=j: e.tensor_copy(z[:, 0:1], car[:, j:j + 1]), reads=[CAR], writes=[Z])
                kb.op("act", lambda e, z=z, p=p: e.activation(out=z[:, 1:NS + 1], in_=p[:, :], func=AF.Copy), reads=[P], writes=[Z])
                kb.op("pool", lambda e, z=z, j=j: e.tensor_copy(car[:, j:j + 1], z[:, NS:NS + 1]), reads=[Z], writes=[CAR])
                kb.op("dve", lambda e, z=z: e.tensor_tensor(dtmp[:], z[:, 0:NS], z[:, 1:NS + 1], ALU.subtract), reads=[Z], writes=[DTMP])
                kb.op("dve", lambda e, z=z, j=j: e.scalar_tensor_tensor(zs[j][:], dtmp[:], vec[:, j:j + 1], z[:, 1:NS + 1], ALU.mult, ALU.add),
                      reads=[DTMP, Z, VEC], writes=[ZS[j]])
            L1, L2 = zs[6], zs[7]
            for j in range(8):
                dbg(f"zs{j}", zs[j][:], ZS[j])
            kb.op("act", lambda e: e.activation(out=tw[0:64, :], in_=L1[0:64, :], func=AF.Tanh), reads=[ZS[6]], writes=[TW])
            kb.op("act", lambda e: e.activation(out=sg[:], in_=L2[:], func=AF.Sigmoid), reads=[ZS[7]], writes=[SG])
            for hp in range(2):
                Rz, Kz, Vz = zs[0 + hp], zs[2 + hp], zs[4 + hp]
                RZ, KZ, VZ = ZS[0 + hp], ZS[2 + hp], ZS[4 + hp]
                cs = slice(hp * 128, (hp + 1) * 128)
                p, P = mm512(lambda k: lw[64:128, cs], lambda k: L1[64:128, :], 1, [LW, ZS[6]])
                kb.op("act", lambda e, p=p, hp=hp: e.activation(out=T["aa"][:], in_=p[:, :], func=AF.Sigmoid, bias=vec[:, 10 + hp:11 + hp]),
                      reads=[P, VEC], writes=[TB["aa"]])
                p, P = mm512(lambda k: g2[:, cs], lambda k: sg[:], 1, [G2, SG])
                kb.op("act", lambda e, p=p, hp=hp: e.activation(out=gg[hp][:], in_=p[:, :], func=AF.Copy), reads=[P], writes=[GG[hp]])
                p, P = mm512(lambda k: lw[0:64, cs], lambda k: tw[0:64, :], 1, [LW, TW])
                kb.op("act", lambda e, p=p, hp=hp: e.activation(out=T["e1"][:], in_=p[:, :], func=AF.Exp, bias=vx[:, hp:hp + 1], scale=-1.0),
                      reads=[P, VX], writes=[TB["e1"]])
                kb.op("act", lambda e: e.activation(out=T["e1"][:], in_=T["e1"][:], func=AF.Ln, bias=1.0), reads=[TB["e1"]], writes=[TB["e1"]])
                kb.op("act", lambda e: e.activation(out=T["nld"][:], in_=T["e1"][:], func=AF.Exp, bias=-0.5, scale=-1.0),
                      reads=[TB["e1"]], writes=[TB["nld"]])
                kb.op("dve", lambda e: e.tensor_tensor_scan(T["cw"][:], MASK1, T["nld"][:], 0.0, ALU.mult, ALU.add),
                      reads=[CST, TB["nld"]], writes=[TB["cw"]])
                kb.op("act", lambda e: e.activation(out=T["ew"][:], in_=T["cw"][:], func=AF.Exp, scale=-1.0), reads=[TB["cw"]], writes=[TB["ew"]])
                kb.op("act", lambda e: e.activation(out=T["ewi"][:], in_=T["cw"][:], func=AF.Exp), reads=[TB["cw"]], writes=[TB["ewi"]])
                kb.op("pool", lambda e: e.tensor_tensor(T["ewx"][:], T["cw"][:], T["nld"][:], ALU.subtract), reads=[TB["cw"], TB["nld"]], writes=[TB["ewx"]])
                kb.op("act", lambda e: e.activation(out=T["ewx"][:], in_=T["ewx"][:], func=AF.Exp, scale=-1.0), reads=[TB["ewx"]], writes=[TB["ewx"]])
                kb.op("pool", lambda e, hp=hp: e.tensor_copy(wc[hp][:], T["ew"][:].rearrange("p (c t) -> p c t", t=CH)[:, :, CH - 1]),
                      reads=[TB["ew"]], writes=[WC[hp]])
                kb.op("dve", lambda e, hp=hp, Kz=Kz: e.tensor_scalar(T["kkn"][:], Kz[:], vec[:, 12 + hp:13 + hp], None, ALU.mult),
                      reads=[KZ, VEC], writes=[TB["kkn"]])
                kb.op("pool", lambda e: e.tensor_tensor(T["sq"][:], T["kkn"][:], T["kkn"][:], ALU.mult), reads=[TB["kkn"]], writes=[TB["sq"]])
                p, P = mm512(lambda k: BONES, lambda k: T["sq"][:], 1, [CST, TB["sq"]])
                kb.op("act", lambda e, p=p: e.activation(out=T["sq"][:], in_=p[:, :], func=AF.Sqrt), reads=[P], writes=[TB["sq"]])
                kb.op("dve", lambda e: e.tensor_scalar(T["sq"][:], T["sq"][:], 1e-12, None, ALU.max), reads=[TB["sq"]], writes=[TB["sq"]])
                kb.op("dve", lambda e: e.reciprocal(T["sq"][:], T["sq"][:]), reads=[TB["sq"]], writes=[TB["sq"]])
                kb.op("dve", lambda e: e.tensor_tensor(T["kkn"][:], T["kkn"][:], T["sq"][:], ALU.mult), reads=[TB["kkn"], TB["sq"]], writes=[TB["kkn"]])
                kb.op("pool", lambda e, hp=hp: e.tensor_scalar(T["t1"][:], T["aa"][:], vec[:, 14 + hp:15 + hp], vx[:, 2 + hp:3 + hp], ALU.mult, ALU.add),
                      reads=[TB["aa"], VEC, VX], writes=[TB["t1"]])
                kb.op("pool", lambda e, Kz=Kz: e.tensor_tensor(T["k2"][:], Kz[:], T["t1"][:], ALU.mult), reads=[KZ, TB["t1"]], writes=[TB["k2"]])
                arv = ar[hp]
                kb.op("dve", lambda e, arv=arv: e.scalar_tensor_tensor(arv[:, :, 0:CH], T["kkn"][:].rearrange("p (c t) -> p c t", t=CH), -1.0,
                                                                      T["ewx"][:].rearrange("p (c t) -> p c t", t=CH), ALU.mult, ALU.mult),
                      reads=[TB["kkn"], TB["ewx"]], writes=[AR[hp]])
                kb.op("pool", lambda e, arv=arv, Rz=Rz: e.tensor_tensor(arv[:, :, CH:2 * CH], Rz[:].rearrange("p (c t) -> p c t", t=CH),
                                                                       T["ew"][:].rearrange("p (c t) -> p c t", t=CH), ALU.mult),
                      reads=[RZ, TB["ew"]], writes=[AR[hp]])
                kb.op("dve", lambda e: e.tensor_tensor(T["t1"][:], T["kkn"][:], T["aa"][:], ALU.mult), reads=[TB["kkn"], TB["aa"]], writes=[TB["t1"]])
                kb.op("dve", lambda e, hp=hp: e.tensor_tensor(bt[hp][:], T["t1"][:], T["ewi"][:], ALU.mult), reads=[TB["t1"], TB["ewi"]], writes=[BT[hp]])
                kb.op("pool", lambda e, hp=hp: e.tensor_tensor(kt[hp][:], T["k2"][:], T["ewi"][:], ALU.mult), reads=[TB["k2"], TB["ewi"]], writes=[KT[hp]])
                wcb = wc[hp][:].unsqueeze(2).to_broadcast([128, NCH, CH])
                kb.op("dve", lambda e, hp=hp, wcb=wcb: e.tensor_tensor(T["bh"][:].rearrange("p (c t) -> p c t", t=CH),
                                                                      bt[hp][:].rearrange("p (c t) -> p c t", t=CH), wcb, ALU.mult),
                      reads=[BT[hp], WC[hp]], writes=[TB["bh"]])
                kb.op("pool", lambda e, hp=hp, wcb=wcb: e.tensor_tensor(T["kh"][:].rearrange("p (c t) -> p c t", t=CH),
                                                                       kt[hp][:].rearrange("p (c t) -> p c t", t=CH), wcb, ALU.mult),
                      reads=[KT[hp], WC[hp]], writes=[TB["kh"]])
                kb.op("dve", lambda e, hp=hp, Rz=Rz: e.scalar_tensor_tensor(T["t1"][:], Rz[:], vec[:, 16 + hp:17 + hp], T["k2"][:], ALU.mult, ALU.mult),
                      reads=[RZ, VEC, TB["k2"], TB["t1"]], writes=[TB["t1"]])
                p, P = mm512(lambda k: BONES, lambda k: T["t1"][:], 1, [CST, TB["t1"]])
                kb.op("dve", lambda e, p=p, hp=hp, Vz=Vz: e.tensor_tensor(bon[hp][:], p[:, :], Vz[:], ALU.mult), reads=[P, VZ], writes=[BON[hp]])
                for n_ in ("nld", "cw", "ew", "ewi", "ewx", "aa", "kkn", "k2", "bh", "kh"):
                    dbg(f"{n_}{hp}", T[n_][:], TB[n_])
                dbg(f"bt{hp}", bt[hp][:], BT[hp]); dbg(f"kt{hp}", kt[hp][:], KT[hp]); dbg(f"ar{hp}", ar[hp][:].rearrange("p c t -> p (c t)"), AR[hp])
                dbg(f"gg{hp}", gg[hp][:], GG[hp]); dbg(f"bon{hp}", bon[hp][:], BON[hp]); dbg(f"wc{hp}", wc[hp][:], WC[hp])
                for c in range(NCH):
                    for (src, SRC, dst, DST) in ((T["bh"], TB["bh"], bhT, BHT), (T["kh"], TB["kh"], khT, KHT), (Vz, VZ, vT, VT)):
                        pbank, PT = next_pa()
                        pt = pbank[:, 0:128]
                        kb.op("pe", lambda e, pt=pt, src=src, c=c: e.transpose(pt, src[:, c * CH:(c + 1) * CH], ident[:]),
                              reads=[SRC, IDENT], writes=[PT])
                        kb.op("act", lambda e, pt=pt, dst=dst, c=c, cs=cs: e.activation(out=dst[:, c, cs], in_=pt, func=AF.Copy),
                              reads=[PT], writes=[DST[c]])
            import os as _os
            for c in range(NCH if str(sgi) in _os.environ.get('CHSEG', '01234567') else 0):
                csl = slice(c * CH, (c + 1) * CH)
                for h in range(4):
                    hp, rows = h // 2, slice(64 * (h % 2), 64 * (h % 2) + 64)
                    pp, PP = pab[h % 2], PAB[h % 2]
                    kb.op("pe", lambda e, pp=pp, hp=hp, rows=rows, c=c, csl=csl: e.matmul(pp[:, 0:256], bt[hp][rows, csl], ar[hp][rows, c, :], start=True, stop=True),
                          reads=[BT[hp], AR[hp]], writes=[PP])
                    kb.op("pe", lambda e, pp=pp, hp=hp, rows=rows, c=c, csl=csl: e.matmul(pp[:, 256:512], kt[hp][rows, csl], ar[hp][rows, c, :], start=True, stop=True),
                          reads=[KT[hp], AR[hp]], writes=[PP])
                    kb.op("dve", lambda e, pp=pp, h=h: e.tensor_tensor(mab[h][:], pp[:, 0:256], MASK2, ALU.mult), reads=[PP, CST], writes=[MAB[h]])
                    kb.op("dve", lambda e, pp=pp, h=h: e.tensor_tensor(mak[h][:], pp[:, 256:512], MASK2, ALU.mult), reads=[PP, CST], writes=[MAK[h]])
                    q, Q = pq[h % 2], PQ[h % 2]
                    kb.op("pe", lambda e, q=q, hp=hp, rows=rows, c=c, csl=csl: e.matmul(q[:, 0:128], ar[hp][rows, c, 0:CH], bt[hp][rows, csl], start=True, stop=True),
                          reads=[BT[hp], AR[hp]], writes=[Q[0]])
                    kb.op("dve", lambda e, q=q, h=h: e.tensor_tensor(nn[h][0][:], q[:, 0:128], MSL, ALU.mult), reads=[Q[0], CST], writes=[NN[h][0]])
                    kb.op("pool", lambda e, h=h: e.tensor_copy(mm[h][0][:], mab[h][:, 0:128]), reads=[MAB[h]], writes=[MM[h][0]])
                    kb.op("pool", lambda e, h=h: e.tensor_tensor(qq[h][0][:], mab[h][:, 0:128], ident[:], ALU.add), reads=[MAB[h], IDENT], writes=[QQ[h][0]])
                for k in range(int(_os.environ.get('NEU', '6'))):
                    a_, b_ = k % 2, (k + 1) % 2
                    for h in range(4):
                        q, Q = pq[h % 2], PQ[h % 2]
                        kb.op("pe", lambda e, q=q, h=h, a_=a_: e.matmul(q[:, 128:256], mm[h][a_][:], nn[h][a_][:], start=True, stop=True),
                              reads=[MM[h][a_], NN[h][a_]], writes=[Q[1]])
                        if _os.environ.get("NEUENG", "dve") == "act":
                            kb.op("act", lambda e, q=q, h=h, b_=b_: e.activation(out=nn[h][b_][:], in_=q[:, 128:256], func=AF.Copy), reads=[Q[1]], writes=[NN[h][b_]])
                        else:
                            kb.op("dve", lambda e, q=q, h=h, b_=b_: e.tensor_copy(nn[h][b_][:], q[:, 128:256]), reads=[Q[1]], writes=[NN[h][b_]])
                        if k < 5:
                            kb.op("pe", lambda e, q=q, h=h, a_=a_: e.matmul(q[:, 256:384], nn[h][a_][:], mm[h][a_][:], start=True, stop=True),
                                  reads=[MM[h][a_], NN[h][a_]], writes=[Q[2]])
                            kb.op("act", lambda e, q=q, h=h, b_=b_: e.activation(out=mm[h][b_][:], in_=q[:, 256:384], func=AF.Copy), reads=[Q[2]], writes=[MM[h][b_]])
                        kb.op("pe", lambda e, q=q, h=h, a_=a_, b_=b_: e.matmul(q[:, 384:512], nn[h][b_][:], qq[h][a_][:], start=True, stop=True),
                              reads=[NN[h][b_], QQ[h][a_]], writes=[Q[3]])
                        kb.op("dve", lambda e, q=q, h=h, a_=a_, b_=b_: e.tensor_tensor(qq[h][b_][:], q[:, 384:512], qq[h][a_][:], ALU.add),
                              reads=[Q[3], QQ[h][a_]], writes=[QQ[h][b_]])
                qf = 0
                if c == 0:
                    for h in range(4):
                        dbg(f"mab{h}", mab[h][:], MAB[h]); dbg(f"mak{h}", mak[h][:], MAK[h]); dbg(f"q{h}", qq[h][0][:], QQ[h][0])
                        dbg(f"n6_{h}", nn[h][0][:], NN[h][0])
                for h in range(4 if _os.environ.get('STATE', '1') == '1' else 0):
                    hp, hh = h // 2, h % 2
                    rows = slice(64 * hh, 64 * hh + 64)
                    hc = slice(h * 64, (h + 1) * 64)
                    so, sn = ping[hp], 1 - ping[hp]
                    So, Sn = STT[hp][so][hh], STT[hp][sn][hh]
                    pst = psts[hh]
                    PX = PU = PS_ = PY = PSTB[hh]
                    kb.op("pe", lambda e, h=h, c=c, hc=hc, pst=pst: e.matmul(pst[:, 0:64], mak[h][:, 0:128], vT[:, c, hc], start=True, stop=False),
                          reads=[MAK[h], VT[c]], writes=[PX])
                    kb.op("pe", lambda e, hp=hp, rows=rows, c=c, so=so, pst=pst: e.matmul(pst[:, 0:64], ar[hp][rows, c, 0:CH], stt[hp][so][rows, :], start=False, stop=True),
                          reads=[AR[hp], So], writes=[PX])
                    kb.op("act", lambda e, h=h, pst=pst: e.activation(out=xsb[h][:], in_=pst[:, 0:64], func=AF.Copy), reads=[PX], writes=[XSB[h]])
                    kb.op("pe", lambda e, h=h, pst=pst: e.matmul(pst[:, 64:128], qq[h][qf][:], xsb[h][:], start=True, stop=True), reads=[QQ[h][qf], XSB[h]], writes=[PU])
                    kb.op("act", lambda e, h=h, pst=pst: e.activation(out=usb[h][:], in_=pst[:, 64:128], func=AF.Copy), reads=[PU], writes=[USB[h]])
                    yo = slice(256 + hp * 64, 256 + (hp + 1) * 64)
                    kb.op("pe", lambda e, hp=hp, rows=rows, c=c, so=so, yo=yo, pst=pst: e.matmul(pst[:, yo], ar[hp][rows, c, CH:2 * CH], stt[hp][so][rows, :], start=True, stop=False),
                          reads=[AR[hp], So], writes=[PY])
                    kb.op("pe", lambda e, h=h, yo=yo, pst=pst: e.matmul(pst[:, yo], mab[h][:, 128:256], usb[h][:], start=False, stop=False), reads=[MAB[h], USB[h]], writes=[PY])
                    kb.op("pe", lambda e, h=h, c=c, hc=hc, yo=yo, pst=pst: e.matmul(pst[:, yo], mak[h][:, 128:256], vT[:, c, hc], start=False, stop=True),
                          reads=[MAK[h], VT[c]], writes=[PY])
                    so_ = slice(128 + 64 * hh, 128 + 64 * hh + 64)
                    kb.op("pe", lambda e, h=h, c=c, hc=hc, rows=rows, pst=pst: e.matmul(pst[rows, 128:192], bhT[:, c, hc], usb[h][:], start=True, stop=False),
                          reads=[BHT[c], USB[h]], writes=[PS_])
                    kb.op("pe", lambda e, h=h, c=c, hc=hc, rows=rows, pst=pst: e.matmul(pst[rows, 128:192], khT[:, c, hc], vT[:, c, hc], start=False, stop=True),
                          reads=[KHT[c], VT[c]], writes=[PS_])
                    kb.op("dve", lambda e, hp=hp, rows=rows, c=c, so=so, sn=sn, pst=pst: e.scalar_tensor_tensor(
                        stt[hp][sn][rows, :], stt[hp][so][rows, :], wc[hp][rows, c:c + 1], pst[rows, 128:192], ALU.mult, ALU.add),
                        reads=[So, WC[hp], PS_], writes=[Sn])
                    if hh == 1:
                        ping[hp] = sn
                for hh in range(2):
                    ypv = psts[hh][:, 256:384].rearrange("p (hp v) -> p hp v", v=64)
                    kb.op("act", lambda e, hh=hh, ypv=ypv: e.activation(out=ytok[:].rearrange("p (hp hh v) -> p hp hh v", hh=2, v=64)[:, :, hh, :], in_=ypv, func=AF.Copy),
                          reads=[PSTB[hh]], writes=[YTOK])
                    kb.op("act", lambda e, hh=hh, ypv=ypv: e.activation(out=ysq[:].rearrange("p (hp hh v) -> p hp hh v", hh=2, v=64)[:, :, hh, :], in_=ypv, func=AF.Square),
                          reads=[PSTB[hh]], writes=[YSQ])
                kb.op("dve", lambda e: e.tensor_reduce(sts[:, 0:4], ytok[:].rearrange("p (h v) -> p h v", v=64), AX.X, ALU.add), reads=[YTOK], writes=[STS])
                kb.op("dve", lambda e: e.tensor_reduce(sts[:, 4:8], ysq[:].rearrange("p (h v) -> p h v", v=64), AX.X, ALU.add), reads=[YSQ, STS], writes=[STS])
                kb.op("dve", lambda e: e.tensor_scalar(sts[:, 8:12], sts[:, 0:4], 1.0 / 64, None, ALU.mult), reads=[STS], writes=[STS])
                kb.op("dve", lambda e: e.tensor_tensor(sts[:, 12:16], sts[:, 8:12], sts[:, 8:12], ALU.mult), reads=[STS], writes=[STS])
                kb.op("dve", lambda e: e.scalar_tensor_tensor(sts[:, 16:20], sts[:, 4:8], 1.0 / 64, sts[:, 12:16], ALU.mult, ALU.subtract),
                      reads=[STS], writes=[STS])
                kb.op("dve", lambda e: e.tensor_scalar(sts[:, 16:20], sts[:, 16:20], GN_EPS, None, ALU.add), reads=[STS], writes=[STS])
                kb.op("act", lambda e: e.activation(out=sts[:, 20:24], in_=sts[:, 16:20], func=AF.Sqrt), reads=[STS], writes=[STS])
                kb.op("dve", lambda e: e.reciprocal(sts[:, 24:28], sts[:, 20:24]), reads=[STS], writes=[STS])
                kb.op("dve", lambda e: e.tensor_tensor(yn[:].rearrange("p (h v) -> p h v", v=64), ytok[:].rearrange("p (h v) -> p h v", v=64),
                                                       sts[:, 8:12].unsqueeze(2).to_broadcast([128, 4, 64]), ALU.subtract), reads=[YTOK, STS], writes=[YN])
                kb.op("dve", lambda e: e.tensor_tensor(yn[:].rearrange("p (h v) -> p h v", v=64), yn[:].rearrange("p (h v) -> p h v", v=64),
                                                       sts[:, 24:28].unsqueeze(2).to_broadcast([128, 4, 64]), ALU.mult), reads=[YN, STS], writes=[YN])
                if c == 0:
                    dbg("ytok", ytok[:], YTOK); dbg("yn", yn[:], YN); dbg("sts", sts[:, 0:28], STS)
                    for h in range(4):
                        dbg(f"x{h}", xsb[h][:], XSB[h]); dbg(f"u{h}", usb[h][:], USB[h])
                    dbg("vT0", vT[:, 0, :], VT[0]); dbg("bhT0", bhT[:, 0, :], BHT[0]); dbg("khT0", khT[:, 0, :], KHT[0])
                    for hp in range(2):
                        dbg(f"st{hp}", stt[hp][ping[hp]][:], STT[hp][ping[hp]][0])
                for hp in range(2):
                    pbank, PT = next_pa()
                    pt = pbank[:, 0:128]
                    kb.op("pe", lambda e, pt=pt, hp=hp: e.transpose(pt, yn[:, hp * 128:(hp + 1) * 128], ident[:]), reads=[YN, IDENT], writes=[PT])
                    kb.op("dve", lambda e, pt=pt, hp=hp, csl=csl: e.tensor_scalar(yf[hp][:, csl], pt, vec[:, 18 + hp:19 + hp], vec[:, 20 + hp:21 + hp], ALU.mult, ALU.add),
                          reads=[PT, VEC], writes=[YF[hp]])
            for hp in range(2):
                kb.op("pool", lambda e, hp=hp: e.tensor_tensor(osb[hp][:], yf[hp][:], bon[hp][:], ALU.add), reads=[YF[hp], BON[hp]], writes=[OSB[hp]])
                kb.op("pool", lambda e, hp=hp: e.tensor_tensor(osb[hp][:], osb[hp][:], gg[hp][:], ALU.mult), reads=[OSB[hp], GG[hp]], writes=[OSB[hp]])
                kb.dma("sp", f"out{hp}", lambda e, s, hp=hp, sgi=sgi: e.dma_start(out=yT_d[hp * 128:(hp + 1) * 128, sgi * NS:(sgi + 1) * NS], in_=osb[hp][:]).then_inc(s, 16),
                       reads=[OSB[hp]])
        kb.finish(OSB)
        kb.emit()
        print("A1 instructions:", kb.n_ins, kb.cnt, "waits", kb.nw, {k: v[1] for k, v in kb.dsem.items()})
    return nc


def a1_consts():
    m1 = np.ones((128, 512), np.float32); m1[:, ::CH] = 0.0
    s = np.arange(128)[:, None]; t = np.arange(128)[None, :]
    msu = (t > s).astype(np.float32); miu = (t >= s).astype(np.float32)
    msl = (t < s).astype(np.float32)
    bones = (s // 64 == t // 64).astype(np.float32)
    return np.concatenate([m1, msu, miu, msl, bones], axis=1)


def a1_inputs(inp, l, b, hh):
    ch = slice(256 * hh, 256 * hh + 256)
    w_in = inp["w_in"][l]
    w = np.concatenate([w_in[:, 0:512][:, ch], w_in[:, 512:1024][:, ch], w_in[:, 1024:1536][:, ch], w_in[:, 1536:1792]], axis=1)
    mu = inp["shift_mu"][l]
    mu_cols = np.concatenate([mu[0:512][ch], mu[512:1024][ch], mu[1024:1536][ch], mu[1536:1792]])
    vec = np.zeros((128, 22), np.float32)
    vec[:, 0:8] = mu_cols.reshape(8, 128).T
    def two(v):
        return v[ch].reshape(2, 128).T
    vec[:, 8:10] = two(inp["rw_w0"][l]); vec[:, 10:12] = two(inp["rw_a0"][l]); vec[:, 12:14] = two(inp["rw_kk"][l])
    vec[:, 14:16] = two(inp["rw_ka"][l]); vec[:, 16:18] = two(inp["rw_rk"][l].reshape(512))
    vec[:, 18:20] = two(inp["rw_ln_g"][l]); vec[:, 20:22] = two(inp["rw_ln_b"][l])
    lw = np.concatenate([inp["rw_w2"][l][:, ch], inp["rw_a2"][l][:, ch]], axis=0)
    g2 = inp["rw_g2"][l][:, ch]
    return dict(w=np.ascontiguousarray(w), vec=vec, lw=np.ascontiguousarray(lw), g2=np.ascontiguousarray(g2), cst=a1_consts())


import math

S = 4096
NEG = -30000.0
TW = 2304
DCL = 1280


def t5_bucket(n):
    n = np.maximum(n, 0); me = 16
    nf = np.maximum(n, 1).astype(np.float32)
    large = me + (np.log(nf / np.float32(me)) / np.float32(math.log(1024 / me)) * np.float32(32 - me)).astype(np.int32)
    large = np.minimum(large, 31)
    return np.where(n < me, n, large)


class Em:
    def __init__(self, kb):
        self.kb = kb

    def mm(self, out, lhsT, rhs, start, stop, reads, writes):
        self.kb.op("pe", lambda e, o=out, a=lhsT, b=rhs, s=start, t=stop: e.matmul(o, a, b, start=s, stop=t), reads, writes)

    def tr(self, out, in_, ident, reads, writes):
        self.kb.op("pe", lambda e, o=out, a=in_, b=ident: e.transpose(o, a, b), reads, writes)

    def act(self, out, in_, func, reads, writes, **kw):
        self.kb.op("act", lambda e, o=out, i=in_, f=func, kw=kw: e.activation(out=o, in_=i, func=f, **kw), reads, writes)

    def tt(self, eng, out, a, b, op, reads, writes):
        self.kb.op(eng, lambda e, o=out, a=a, b=b, op=op: e.tensor_tensor(o, a, b, op), reads, writes)

    def ts(self, eng, out, a, s1, s2, op0, op1, reads, writes):
        if op1 is None:
            self.kb.op(eng, lambda e, o=out, a=a, s1=s1, op0=op0: e.tensor_scalar(o, a, s1, None, op0), reads, writes)
        else:
            self.kb.op(eng, lambda e, o=out, a=a, s1=s1, s2=s2, op0=op0, op1=op1: e.tensor_scalar(o, a, s1, s2, op0, op1), reads, writes)

    def stt(self, eng, out, a, s, b, op0, op1, reads, writes):
        self.kb.op(eng, lambda e, o=out, a=a, s=s, b=b, op0=op0, op1=op1: e.scalar_tensor_tensor(o, a, s, b, op0, op1), reads, writes)

    def cp(self, eng, out, in_, reads, writes):
        self.kb.op(eng, lambda e, o=out, i=in_: e.tensor_copy(o, i), reads, writes)

    def ms(self, eng, out, val, writes):
        self.kb.op(eng, lambda e, o=out, v=val: e.memset(o, v), (), writes)

    def red(self, out, in_, op, reads, writes):
        self.kb.op("dve", lambda e, o=out, i=in_, op=op: e.tensor_reduce(o, i, AX.X, op), reads, writes)

    def rcp(self, out, in_, reads, writes):
        self.kb.op("dve", lambda e, o=out, i=in_: e.reciprocal(o, i), reads, writes)

    def dma(self, q, key, out, in_, reads, writes):
        self.kb.dma(q, key, lambda e, s, o=out, i=in_: e.dma_start(out=o, in_=i).then_inc(s, 16), reads, writes)


def build_a2(nq=8, debug=False):
    nc = bass.Bass("TRN2", target_bir_lowering=False)
    D = lambda n, shp: nc.dram_tensor(n, shp, F32, kind="ExternalInput").ap()
    xT_d = D("xT", [1024, S]); wf_d = D("wf", [1024, 640]); wt_d = D("wt", [1024, 140])
    w1_d = D("w1", [128, 32 * 128]); pe_d = D("peT", [128, 32]); w2_d = D("w2", [128, 192])
    btc_d = D("btc", [128, 4 * 256]); mkc_d = D("mkc", [128, 256])
    bts_d = D("bts", [128, 4 * TW]); mks_d = D("mks", [128, TW]); mkw_d = D("mkw", [128, TW])
    m12_d = D("m12", [128, 256]); rv_d = D("rv", [128, 1]); c2s_d = D("c2s", [128, 128])
    exd_d = D("exd", [64, 32 * 128]); hsel_d = D("hsel", [128, 256])
    y_d = nc.dram_tensor("y", [S, 256], F32, kind="ExternalOutput").ap()
    xT_v = xT_d.rearrange("(kc p) t -> p kc t", p=128)
    y_v = y_d.rearrange("(a p) c -> p a c", p=128)

    with ExitStack() as st:
        kb = KB(nc, st); em = Em(kb)
        sb, ps = kb.sb, kb.ps
        xs = [sb(f"xs{i}", [128, 8, 512], BF16) for i in range(2)]; XS = [Buf() for _ in range(2)]
        wf = sb("wf", [128, 8, 640], BF16); WF = Buf()
        wt = sb("wt", [128, 8, 256], BF16); WT = Buf()
        w1 = sb("w1", [128, 32, 128], BF16); W1 = Buf()
        peT = sb("peT", [128, 32], BF16); PET = Buf()
        w2f = sb("w2f", [128, 192]); w2 = sb("w2", [128, 192], BF16); W2 = Buf()
        qT = [sb(f"qT{i}", [128, S], BF16) for i in range(2)]; QT = [Buf() for _ in range(2)]
        ksT = sb("ksT", [128, S], BF16); KST = Buf()
        kwT = sb("kwT", [128, S], BF16); KWT = Buf()
        kcv = sb("kcv", [128, S], BF16); KCV = Buf()
        vs = sb("vs", [128, 32, 96], BF16); VS = Buf()
        vw = sb("vw", [128, 32, 96], BF16); VW = Buf()
        gt = sb("gt", [128, 32, 16]); GT = Buf()
        btc = sb("btc", [128, 4, 256]); BTC = Buf()
        mkc = sb("mkc", [128, 256]); MKC = Buf()
        HW_ = TW // 2
        stg = sb("stg", [128, HW_]); STG = Buf()
        stg2 = sb("stg2", [128, HW_]); STG2 = Buf()
        mks = sb("mks", [128, HW_]); MKS = Buf()
        mkw = sb("mkw", [128, HW_]); MKW = Buf()
        ebs = [sb(f"ebs{h}", [128, TW], BF16) for h in range(4)]; EBS = [Buf() for _ in range(4)]
        ebw = [sb(f"ebw{h}", [128, TW], BF16) for h in range(4)]; EBW = [Buf() for _ in range(4)]
        m12 = sb("m12", [128, 256]); M12 = Buf()
        rv = sb("rv", [128, 1]); RV = Buf()
        vca = sb("vca", [128, 2, 128], BF16); VCA = Buf()
        c2sf = sb("c2sf", [128, 128]); C2SF = Buf()
        exd = sb("exd", [128, 32, 128], BF16); EXD = Buf()
        hself = sb("hself", [128, 256]); hsel = sb("hsel", [128, 2, 128], BF16); HSEL = Buf()
        identf = sb("identf", [128, 128]); ident = sb("ident", [128, 128], BF16); IDENT = Buf()
        kct = sb("kct", [128, 256], BF16); KCT = Buf()
        gh = [sb(f"gh{i}", [128, 256], BF16) for i in range(2)]; GH = [Buf() for _ in range(2)]
        hx = sb("hx", [128, 256]); HX = Buf()
        hy = sb("hy", [128, 256]); HY = Buf()
        hb = sb("hb", [128, 2]); HB = Buf()
        sm = sb("sm", [128, 64]); SM = Buf()
        cst = sb("cst", [128, 16]); CST = Buf()
        qsq = sb("qsq", [128, 512], BF16); QSQ = Buf()
        mxc = sb("mxc", [128, 4, 8]); MXC = Buf()
        kxc = sb("kxc", [128, 2, 8]); KXC = Buf()
        lg = sb("lg", [128, 256]); LG = Buf()
        pc = sb("pc", [128, 256], BF16); PC = Buf()
        pct = sb("pct", [128, 2, 128], BF16); PCT = Buf()
        sc = sb("sc", [128, 64]); SC = Buf()
        sc2 = sb("sc2", [128, 64]); SC2 = Buf()
        mx8 = sb("mx8", [128, 16]); MX8 = Buf()
        nst = sb("nst", [128, 64], BF16); NST = Buf()
        nsh = sb("nsh", [128, 512], BF16); NSH = Buf()
        yg = sb("yg", [128, 4, 256]); YG = Buf()
        pt = [sb(f"pt{i}", [128, 512], BF16) for i in range(3)]; PT = [Buf() for _ in range(3)]
        p2 = [sb(f"p2{i}", [128, 512], BF16) for i in range(3)]; P2 = [Buf() for _ in range(3)]
        rin = sb("rin", [128, 8]); RIN = Buf()
        zb = sb("zb", [128, 512], BF16); ZB = Buf()
        otmp = sb("otmp", [128, 4, 64]); OTMP = Buf()
        pg = [ps(f"pg{i}", [128, 512]) for i in range(3)]; PG = [Buf(excl=True) for _ in range(3)]
        ptb = ps("ptb", [128, 1024], BF16); PTB = Buf(excl=True)
        pl = [ps(f"pl{i}", [128, 512]) for i in range(2)]; PL = [Buf(excl=True) for _ in range(2)]
        pacc = [ps(f"pacc{i}", [128, 4, 128]) for i in range(2)]; PACC = [Buf(excl=True) for _ in range(2)]

        gi = [0]

        def gbank():
            i = gi[0] % 3; gi[0] += 1
            return pg[i], PG[i]

        dbg_n = [0]

        def dbg(name, ap, B):
            if not debug:
                return
            d = nc.dram_tensor("dbg_" + name, list(ap.shape), F32, kind="ExternalOutput").ap()
            dbg_n[0] += 1
            cntv = dbg_n[0] * 16

            def f(e, s, d=d, ap=ap, cntv=cntv):
                e.dma_start(out=d, in_=ap).then_inc(s, 16)
                e.wait_ge(s, cntv)
            kb.dma("pool", "dbg", f, reads=[B])

        em.dma("pool", "wf", wf[:, :, :], wf_d.rearrange("(kc p) n -> p kc n", p=128), [], [WF])
        em.dma("pool", "wt", wt[:, :, 0:140], wt_d.rearrange("(kc p) n -> p kc n", p=128), [], [WT])
        em.dma("pool", "w1", w1[:, :, :], w1_d.rearrange("p (a b) -> p a b", b=128), [], [W1])
        em.dma("pool", "pe", peT[:], pe_d[:, :], [], [PET])
        em.dma("sp", "w2", w2f[:], w2_d[:, :], [], [W2])
        em.dma("sp", "btc", btc[:].rearrange("p h w -> p (h w)"), btc_d[:, :], [], [BTC])
        em.dma("sp", "mkc", mkc[:], mkc_d[:, :], [], [MKC])
        em.dma("sp", "m12", m12[:], m12_d[:, :], [], [M12])
        em.dma("sp", "rv", rv[:], rv_d[:, :], [], [RV])
        em.dma("sp", "c2s", c2sf[:], c2s_d[:, :], [], [C2SF])
        em.ms("pool", exd[:], 0.0, [EXD])
        em.dma("pool", "exd", exd[0:64, :, :].rearrange("p a b -> p (a b)"), exd_d[:, :], [EXD], [EXD])
        em.dma("sp", "hsel", hself[:], hsel_d[:, :], [], [HSEL])
        em.cp("dve", w2[:], w2f[:], [W2], [W2])
        em.cp("dve", hsel[:].rearrange("p a b -> p (a b)"), hself[:], [HSEL], [HSEL])
        em.ms("pool", identf[:], 1.0, [IDENT])
        kb.op("pool", lambda e: e.affine_select(out=identf[:], in_=identf[:], pattern=[[-1, 128]], compare_op=ALU.is_equal,
                                                fill=0.0, base=0, channel_multiplier=1), [IDENT], [IDENT])
        em.cp("pool", ident[:], identf[:], [IDENT], [IDENT])
        em.ms("dve", vs[:, :, 64:65], 1.0, [VS])
        em.ms("dve", vw[:, :, 64:65], 1.0, [VW])
        em.ms("dve", vca[:], 0.0, [VCA])
        em.ms("dve", zb[:], 0.0, [ZB])
        em.ms("dve", nsh[:], 0.0, [NSH])
        em.ms("dve", pc[:], 0.0, [PC])
        em.ms("dve", kct[:], 0.0, [KCT])
        for h in range(4):
            em.tt("dve", btc[:, h, :], btc[:, h, :], mkc[:], ALU.add, [BTC, MKC], [BTC])
        first = True
        for hf in range(2):
            cs_ = slice(hf * HW_, (hf + 1) * HW_)
            em.dma("sp", "mks", mks[:], mks_d[:, cs_], [], [MKS])
            em.dma("sp", "mkw", mkw[:], mkw_d[:, cs_], [], [MKW])
            for h in range(4):
                em.dma("sp", "stg", stg[:], bts_d[:, h * TW + hf * HW_:h * TW + (hf + 1) * HW_], [], [STG])
                if first:
                    em.red(cst[:, 8:9], stg[:], ALU.max, [STG], [CST])
                    first = False
                else:
                    em.red(cst[:, 9:10], stg[:], ALU.max, [STG], [CST])
                    em.tt("dve", cst[:, 8:9], cst[:, 8:9], cst[:, 9:10], ALU.max, [CST], [CST])
                em.tt("dve", stg2[:], stg[:], mks[:], ALU.add, [STG, MKS], [STG2])
                em.act(ebs[h][:, cs_], stg2[:], AF.Exp, [STG2], [EBS[h]])
                em.tt("dve", stg2[:], stg[:], mkw[:], ALU.add, [STG, MKW], [STG2])
                em.act(ebw[h][:, cs_], stg2[:], AF.Exp, [STG2], [EBW[h]])

        import os as _os
        PH = int(_os.environ.get('PH', '9'))
        def load_x(sg):
            sl = sg % 2
            em.dma("pool", f"xs{sl}", xs[sl][:, :, :], xT_v[:, :, sg * 512:(sg + 1) * 512], [], [XS[sl]])

        load_x(0)
        for sg in range(8 if PH >= 2 else 0):
            sl = sg % 2
            if sg + 1 < 8:
                load_x(sg + 1)
            seg = slice(sg * 512, (sg + 1) * 512)
            dsts = [(qT[0], QT[0], 0.125), (qT[1], QT[1], 0.125), (ksT, KST, 1.0), (kwT, KWT, 1.0), (kcv, KCV, 1.0)]
            for j, (dst, DST, scl) in enumerate(dsts):
                p, P = gbank()
                for k in range(8):
                    em.mm(p[:, :], wf[:, k, j * 128:(j + 1) * 128], xs[sl][:, k, :], k == 0, k == 7, [WF, XS[sl]], [P])
                em.act(dst[:, seg], p[:, :], AF.Copy, [P], [DST], scale=scl)
            PJ = _os.environ.get('PJ', 'abc12')
            for tt_ in range(4 if 'b' in PJ else 0):
                tile_i = sg * 4 + tt_
                p, P = gbank()
                for k in range(8):
                    em.mm(p[:, 0:140], xs[sl][:, k, tt_ * 128:(tt_ + 1) * 128], wt[:, k, 0:140], k == 0, k == 7, [WT, XS[sl]], [P])
                if '1' in PJ:
                    em.cp("dve", vs[:, tile_i, 0:64], p[:, 0:64], [P], [VS])
                    em.cp("dve", vw[:, tile_i, 0:64], p[:, 64:128], [P], [VW])
                if '2' in PJ:
                    em.act(gt[:, tile_i, 0:12], p[:, 128:140], AF.Sigmoid, [P], [GT])
            for i in range(2 if 'c' in PJ else 0):
                em.tt("dve", qsq[:], qT[i][:, seg], qT[i][:, seg], ALU.mult, [QT[i]], [QSQ])
                for hh in range(2):
                    p, P = gbank()
                    em.mm(p[:, :], hsel[:, hh, :], qsq[:], True, True, [HSEL, QSQ], [P])
                    em.red(mxc[:, 2 * i + hh, sg:sg + 1], p[:, :], ALU.max, [P], [MXC])
            for i, (src, SRC) in enumerate(((ksT, KST), (kwT, KWT)) if 'c' in PJ else ()):
                em.tt("dve", qsq[:], src[:, seg], src[:, seg], ALU.mult, [SRC], [QSQ])
                p, P = gbank()
                em.mm(p[:, :], hsel[:, 0, :], qsq[:], True, True, [HSEL, QSQ], [P])
                em.red(kxc[:, i, sg:sg + 1], p[:, :], ALU.max, [P], [KXC])
        if PH < 3:
            nq = 0
        em.red(sm[:, 0:4], mxc[:], ALU.max, [MXC], [SM])
        em.red(sm[:, 4:6], kxc[:], ALU.max, [KXC], [SM])
        for br in range(2):
            em.ts("dve", sm[:, 8 + 4 * br:12 + 4 * br], sm[:, 0:4], sm[:, 4 + br:5 + br], None, ALU.mult, None, [SM], [SM])
        em.act(sm[:, 16:24], sm[:, 8:16], AF.Sqrt, [SM], [SM])
        em.ts("dve", cst[:, 0:8], sm[:, 16:24], cst[:, 8:9], -1.0, ALU.add, ALU.mult, [SM, CST], [CST])

        for br in range(2 if PH >= 3 else 0):
            rows = slice(64 * br, 64 * br + 64)
            p, P = gbank()
            for pp in range(32):
                em.mm(p[:, 0:255], w1[rows, pp, :], kcv[rows, pp:pp + 16 * 254 + 1:16], pp == 0, pp == 31, [W1, KCV], [P])
            pb, PB = gbank()
            for pp in range(32):
                em.mm(pb[:, 0:1], w1[rows, pp, :], peT[rows, pp:pp + 1], pp == 0, pp == 31, [W1, PET], [PB])
            em.cp("dve", hb[:, br:br + 1], pb[:, 0:1], [PB], [HB])
            em.ts("dve", hx[:, 0:255], p[:, 0:255], hb[:, br:br + 1], None, ALU.add, None, [P, HB], [HX])
            em.tt("dve", hy[:, 0:255], hx[:, 0:255], hx[:, 0:255], ALU.mult, [HX], [HY])
            em.ts("dve", hy[:, 0:255], hy[:, 0:255], 0.044715, 1.0, ALU.mult, ALU.add, [HY], [HY])
            em.tt("dve", hy[:, 0:255], hy[:, 0:255], hx[:, 0:255], ALU.mult, [HY, HX], [HY])
            em.act(hy[:, 0:255], hy[:, 0:255], AF.Tanh, [HY], [HY], scale=0.7978845608028654)
            em.ts("dve", hy[:, 0:255], hy[:, 0:255], 1.0, 0.5, ALU.add, ALU.mult, [HY], [HY])
            em.tt("dve", gh[br][:, 0:255], hy[:, 0:255], hx[:, 0:255], ALU.mult, [HY, HX], [GH[br]])
        p, P = gbank()
        em.mm(p[:, 0:255], w2[:, 0:128], gh[0][:, 0:255], True, True, [W2, GH[0]], [P])
        em.act(kct[:, 0:255], p[:, 0:255], AF.Copy, [P], [KCT])
        for ct in range(2):
            ncs = 128 if ct == 0 else 127
            p, P = gbank()
            em.mm(p[0:ncs, 0:64], gh[1][:, ct * 128:ct * 128 + ncs], w2[:, 128:192], True, True, [W2, GH[1]], [P])
            em.act(vca[0:ncs, ct, 0:64], p[0:ncs, 0:64], AF.Copy, [P], [VCA])
        em.cp("dve", vca[:, :, 64:128], c2sf[:].rearrange("p (a b) -> p a b", b=64), [C2SF], [VCA])
        dbg("kct", kct[:, 0:255], KCT); dbg("vca", vca[:].rearrange("p a b -> p (a b)"), VCA)
        dbg("cst", cst[:, 0:9], CST)

        pti = [0]
        for Q in range(nq):
            qs = slice(Q * 512, (Q + 1) * 512)
            for a in range(4):
                n = 4 * Q + a
                t0 = 128 * n
                ncol = min(255, 8 * n + 7)
                off = 248 - 8 * n
                for h in range(4):
                    rows = slice(64 * (h % 2), 64 * (h % 2) + 64)
                    p, P = gbank()
                    ncp = min(256, (ncol + 31) // 32 * 32)
                    em.mm(p[:, 0:ncp], qT[h // 2][rows, t0:t0 + 128], kct[rows, 0:ncp], True, True, [QT[h // 2], KCT], [P])
                    em.tt("dve", lg[:, 0:ncol], p[:, 0:ncol], btc[:, h, off:off + ncol], ALU.add, [P, BTC], [LG])
                    em.red(sm[:, 32:33], lg[:, 0:ncol], ALU.max, [LG], [SM])
                    em.ts("dve", sm[:, 33:34], sm[:, 32:33], -1.0, None, ALU.mult, None, [SM], [SM])
                    em.ms("dve", sm[:, 34:35], 0.0, [SM])
                    em.act(pc[:, 0:ncol], lg[:, 0:ncol], AF.Exp, [LG, SM], [PC, SM], bias=sm[:, 33:34], accum_out=sm[:, 34:35])
                    nct = 1 if ncol <= 128 else 2
                    for ct in range(nct):
                        em.tr(ptb[:, ct * 128:(ct + 1) * 128], pc[:, ct * 128:(ct + 1) * 128], ident[:], [PC, IDENT], [PTB])
                    for ct in range(nct):
                        em.cp("dve", pct[:, ct, :], ptb[:, ct * 128:(ct + 1) * 128], [PTB], [PCT])
                    po, PO = gbank()
                    for ct in range(nct):
                        em.mm(po[:, 0:128], pct[:, ct, :], vca[:, ct, :], ct == 0, ct == nct - 1, [PCT, VCA], [PO])
                    em.rcp(sm[:, 35:36], sm[:, 34:35], [SM], [SM])
                    if n == 0:
                        em.tt("dve", sm[:, 35:36], sm[:, 35:36], rv[:], ALU.mult, [SM, RV], [SM])
                    em.tt("dve", sm[:, 36:37], sm[:, 35:36], gt[:, n, 3 * h:3 * h + 1], ALU.mult, [SM, GT], [SM])
                    em.ts("dve", yg[:, a, h * 64:(h + 1) * 64], po[:, 0:64], sm[:, 36:37], None, ALU.mult, None, [PO, SM], [YG])
                    if h == 0:
                        em.ts("dve", sc[:], po[:, 64:128], sm[:, 35:36], None, ALU.mult, None, [PO, SM], [SC])
                    else:
                        em.stt("dve", sc[:], po[:, 64:128], sm[:, 35:36], sc[:], ALU.mult, ALU.add, [PO, SM, SC], [SC])
                w0 = 64 - 2 * n
                em.tt("dve", sc[:], sc[:], m12[:, w0:w0 + 64], ALU.mult, [SC, M12], [SC])
                em.tt("dve", sc[:], sc[:], m12[:, 128 + w0:128 + w0 + 64], ALU.add, [SC, M12], [SC])
                em.ms("dve", sc[:, 0:1], 1.0e4, [SC])
                kb.op("dve", lambda e: e.max(out=mx8[:, 0:8], in_=sc[:]), [SC], [MX8])
                kb.op("dve", lambda e: e.match_replace(out=sc2[:], in_to_replace=mx8[:, 0:8], in_values=sc[:], imm_value=-1.0e9), [SC, MX8], [SC2])
                kb.op("dve", lambda e: e.max(out=mx8[:, 8:16], in_=sc2[:]), [SC2], [MX8])
                em.red(sm[:, 40:41], mx8[:, 8:16], ALU.min, [MX8], [SM])
                em.ts("dve", nst[:], sc[:], sm[:, 40:41], NEG, ALU.is_lt, ALU.mult, [SC, SM], [NST])
                em.tr(ptb[0:64, 256:384], nst[:], ident[:], [NST, IDENT], [PTB])
                em.cp("dve", nsh[0:64, a * 128:(a + 1) * 128], ptb[0:64, 256:384], [PTB], [NSH])
                if Q == 0 and a == 1:
                    dbg("sc", sc[:], SC); dbg("yg1", yg[:, 1, :], YG)
            QP = _os.environ.get('QP', 'abc')
            for br in [b_ for b_ in range(2) if 'bc'[b_] in QP]:
                kT, KT_, vv, VV, eb, EB = (ksT, KST, vs, VS, ebs, EBS) if br == 0 else (kwT, KWT, vw, VW, ebw, EBW)
                m_lo = 0 if br == 0 else max(0, 4 * Q - 4)
                m_hi = 4 * Q + 3
                for h in range(4):
                    rows = slice(64 * (h % 2), 64 * (h % 2) + 64)
                    acc, ACC = pacc[h % 2], PACC[h % 2]
                    em.mm(acc[:].rearrange("p a b -> p (a b)"), zb[:, 0:128], zb[:, 0:512], True, False, [ZB], [ACC])
                    for m in range(m_lo, m_hi + 1):
                        D0 = 512 * Q - 128 * m
                        wst = min(D0, DCL) + 512
                        pli = pti[0] % 2
                        pl_, PL_ = pl[pli], PL[pli]
                        bi = pti[0] % 3
                        pti[0] += 1
                        em.mm(pl_[:, :], kT[rows, m * 128:(m + 1) * 128], qT[h // 2][rows, qs], True, br == 1, [KT_, QT[h // 2]], [PL_])
                        if br == 0:
                            em.mm(pl_[:, :], exd[:, m, :], nsh[:, :], False, True, [EXD, NSH], [PL_])
                        em.act(pt[bi][:], pl_[:, :], AF.Exp, [PL_, CST], [PT[bi]], bias=cst[:, 4 * br + h:4 * br + h + 1])
                        em.tt("dve", p2[bi][:], pt[bi][:], eb[h][:, wst:wst + 512], ALU.mult, [PT[bi], EB[h]], [P2[bi]])
                        for a in range(4):
                            last_m = min(m_hi, 4 * Q + a)
                            if m > last_m:
                                continue
                            a_lo = m_lo if br == 0 else max(m_lo, 4 * Q + a - 4)
                            if m < a_lo:
                                continue
                            em.mm(acc[:, a, 0:65], p2[bi][:, a * 128:(a + 1) * 128], vv[:, m, 0:65], False, (m == m_hi and a == 3), [P2[bi], VV], [ACC])
                    em.rcp(rin[:, 0:4], acc[:, :, 64], [ACC], [RIN])
                    em.tt("dve", rin[:, 4:8], rin[:, 0:4], gt[:, 4 * Q:4 * Q + 4, 3 * h + 1 + br], ALU.mult, [RIN, GT], [RIN])
                    em.tt("dve", otmp[:], acc[:, :, 0:64], rin[:, 4:8].unsqueeze(2).to_broadcast([128, 4, 64]), ALU.mult, [ACC, RIN], [OTMP])
                    em.tt("pool", yg[:, :, h * 64:(h + 1) * 64], yg[:, :, h * 64:(h + 1) * 64], otmp[:], ALU.add, [YG, OTMP], [YG])
            em.dma("sp", "y", y_v[:, 4 * Q:4 * Q + 4, :], yg[:], [YG], [])
        kb.finish([YG])
        kb.emit()
        print("A2 instructions:", kb.n_ins, kb.cnt, "waits", kb.nw)
    return nc


def a2_consts():
    i = np.arange(128)[:, None]
    j = np.arange(256)[None, :]
    dc = i - 16 * (j - 248) - 31
    mkc = np.where(dc >= 0, 0.0, NEG).astype(np.float32)
    w = np.arange(TW)[None, :]
    ds = w - i - 512
    mks = np.where(ds >= 0, 0.0, NEG).astype(np.float32)
    mkw = np.where((ds >= 0) & (ds < 512), 0.0, NEG).astype(np.float32)
    wv = np.arange(128)[None, :] - 64
    cur = (i >= 64).astype(np.int64)
    forced = (wv == cur) | (wv == cur - 1)
    valid = wv <= cur
    m1 = (valid & ~forced).astype(np.float32)
    m2 = np.where(forced, 1.0e4, np.where(valid, 0.0, -1.0)).astype(np.float32)
    m12 = np.concatenate([m1, m2], axis=1)
    rv = (np.arange(128) >= 31).astype(np.float32)[:, None]
    cs = np.arange(255)[:, None] * 16; ss = np.arange(64)[None, :] * 64
    ov = np.clip(np.minimum(cs + 32, ss + 64) - np.maximum(cs, ss), 0, None).astype(np.float32) / 32
    c2s = np.zeros((256, 64), np.float32); c2s[:255] = ov
    c2s = c2s.reshape(2, 128, 64).transpose(1, 0, 2).reshape(128, 128)
    exd = np.zeros((64, 32, 128), np.float32)
    for m in range(32):
        exd[2 * m, m, 0:64] = 1.0; exd[2 * m + 1, m, 64:128] = 1.0
    hsel = np.zeros((128, 2, 128), np.float32); hsel[0:64, 0, :] = 1.0; hsel[64:128, 1, :] = 1.0
    return dict(mkc=mkc, mks=mks, mkw=mkw, m12=m12, rv=rv, c2s=c2s, exd=exd.reshape(64, 32 * 128), hsel=hsel.reshape(128, 256),
                dc=dc, ds=ds)


def a2_inputs(inp, l, g, consts):
    w_in = inp["w_in"][l]; zr = 1792
    q = w_in[:, zr + 256 * g: zr + 256 * g + 256]
    def kvc(off):
        return w_in[:, zr + off + 64 * g: zr + off + 64 * g + 64]
    kc, vc, ks, vs, kw, vw = (kvc(o) for o in (512, 640, 768, 896, 1024, 1152))
    gates = w_in[:, zr + 1280 + 12 * g: zr + 1280 + 12 * g + 12]
    wf = np.concatenate([q, ks, ks, kw, kw, kc, vc], axis=1)
    wt = np.concatenate([vs, vw, gates], axis=1)
    def w1r(w):
        return w.reshape(32, 64, 128).transpose(1, 0, 2)
    w1 = np.concatenate([w1r(inp["cmp_w1_k"][l]), w1r(inp["cmp_w1_v"][l])], axis=0).reshape(128, 32 * 128)
    peT = np.concatenate([inp["cmp_pe_k"][l].T, inp["cmp_pe_v"][l].T], axis=0)
    w2 = np.concatenate([inp["cmp_w2_k"][l], inp["cmp_w2_k"][l], inp["cmp_w2_v"][l]], axis=1)
    rb = inp["rel_bias"][:, 4 * g:4 * g + 4]
    btc = np.take(rb, t5_bucket(consts["dc"]), axis=0).transpose(0, 2, 1).reshape(128, 4 * 256)
    bts = np.take(rb, t5_bucket(consts["ds"]), axis=0).transpose(0, 2, 1).reshape(128, 4 * TW)
    out = dict(wf=wf, wt=wt, w1=w1, peT=peT, w2=w2, btc=btc, bts=bts)
    for k in ("mkc", "mks", "mkw", "m12", "rv", "c2s", "exd", "hsel"):
        out[k] = consts[k]
    return {k: np.ascontiguousarray(v, dtype=np.float32) for k, v in out.items()}


NT = 2048
ALPHA = 8 ** 0.25
LN_EPS = 1e-5


def layer_norm_fm(kb, em, gbank, R, RB, out_fn, g_ap, b_ap, GB, tmp, TMP, ones, ONES, sq, SQ, mean, MEAN, rstd, RSTD):
    pm, PM = gbank()
    for i in range(8):
        em.mm(pm[:, :], ones[:], R[:, i, :], i == 0, i == 7, [ONES, RB], [PM])
    em.act(mean[:], pm[:, :], AF.Copy, [PM], [MEAN], scale=1.0 / 1024)
    pv, PV = gbank()
    for i in range(8):
        em.tt("pool" if i % 2 else "dve", sq[i % 2][:], R[:, i, :], R[:, i, :], ALU.mult, [RB], [SQ[i % 2]])
        em.mm(pv[:, :], ones[:], sq[i % 2][:], i == 0, i == 7, [ONES, SQ[i % 2]], [PV])
    em.tt("dve", tmp[:], mean[:], mean[:], ALU.mult, [MEAN], [TMP])
    em.stt("dve", rstd[:], pv[:, :], 1.0 / 1024, tmp[:], ALU.mult, ALU.subtract, [PV, TMP], [RSTD])
    em.ts("dve", rstd[:], rstd[:], LN_EPS, None, ALU.add, None, [RSTD], [RSTD])
    em.act(rstd[:], rstd[:], AF.Sqrt, [RSTD], [RSTD])
    em.rcp(rstd[:], rstd[:], [RSTD], [RSTD])
    for i in range(8):
        eng = "pool" if i % 2 else "dve"
        em.tt(eng, tmp[:], R[:, i, :], mean[:], ALU.subtract, [RB, MEAN], [TMP])
        em.tt(eng, tmp[:], tmp[:], rstd[:], ALU.mult, [TMP, RSTD], [TMP])
        o, O = out_fn(i)
        em.ts(eng, o, tmp[:], g_ap[:, i:i + 1], b_ap[:, i:i + 1], ALU.mult, ALU.add, [TMP, GB], [O])


def build_b1():
    nc = bass.Bass("TRN2", target_bir_lowering=False)
    D = lambda n, shp: nc.dram_tensor(n, shp, F32, kind="ExternalInput").ap()
    xT_d = D("xT", [1024, NT]); yr_d = D("yrT", [512, NT]); yn_d = D("ynT", [512, NT])
    wg_d = D("wg", [1024, 2048]); wur_d = D("wur", [512, 1024]); wun_d = D("wun", [512, 1024]); wo_d = D("wo", [1024, 1024])
    ln_d = D("ln", [128, 16])
    o_d = nc.dram_tensor("x1T", [1024, NT], F32, kind="ExternalOutput").ap()
    v3 = lambda ap: ap.rearrange("(kc p) n -> p kc n", p=128)
    with ExitStack() as st:
        kb = KB(nc, st); em = Em(kb); sb, ps = kb.sb, kb.ps
        wg = sb("wg", [128, 8, 2048], BF16); WG = Buf()
        wur = sb("wur", [128, 4, 1024], BF16); WUR = Buf()
        wun = sb("wun", [128, 4, 1024], BF16); WUN = Buf()
        wo = sb("wo", [128, 8, 1024], BF16); WO = Buf()
        ln = sb("ln", [128, 16]); LN = Buf()
        ones = sb("ones", [128, 128]); ONES = Buf()
        xf = sb("xf", [128, 8, 512]); XF = Buf()
        xb = sb("xb", [128, 8, 512], BF16); XB = Buf()
        yr = sb("yr", [128, 4, 512], BF16); YR = Buf()
        yn = sb("yn", [128, 4, 512], BF16); YN = Buf()
        sg = [sb(f"sg{i}", [128, 512]) for i in range(2)]; SG = [Buf() for _ in range(2)]
        m1 = sb("m1", [128, 512]); M1 = Buf()
        mg = sb("mg", [128, 8, 512], BF16); MG = Buf()
        R = sb("R", [128, 8, 512]); RB = Buf()
        ob = sb("ob", [128, 8, 512]); OB = Buf()
        tmp = sb("tmp", [128, 512]); TMP = Buf()
        sq = [sb(f"sq{i}", [128, 512]) for i in range(2)]; SQ = [Buf() for _ in range(2)]
        mean = sb("mean", [128, 512]); MEAN = Buf()
        rstd = sb("rstd", [128, 512]); RSTD = Buf()
        pg = [ps(f"pg{i}", [128, 512]) for i in range(6)]; PG = [Buf(excl=True) for _ in range(6)]
        gi = [0]

        def gbank():
            i = gi[0] % 6; gi[0] += 1
            return pg[i], PG[i]
        for k0 in range(0, 8, 2):
            em.dma("pool", "wg", wg[:, k0:k0 + 2, :], v3(wg_d)[:, k0:k0 + 2, :], [], [WG])
        em.dma("pool", "wur", wur[:, :, :], v3(wur_d), [], [WUR])
        em.dma("pool", "wun", wun[:, :, :], v3(wun_d), [], [WUN])
        em.dma("pool", "wo", wo[:, :, :], v3(wo_d), [], [WO])
        em.dma("sp", "ln", ln[:], ln_d[:, :], [], [LN])
        em.ms("dve", ones[:], 1.0, [ONES])
        for tg in range(NT // 512):
            ts_ = slice(tg * 512, (tg + 1) * 512)
            em.dma("sp", "xf", xf[:, :, :], v3(xT_d)[:, :, ts_], [], [XF])
            em.dma("pool", "yr", yr[:, :, :], v3(yr_d)[:, :, ts_], [], [YR])
            em.dma("pool", "yn", yn[:, :, :], v3(yn_d)[:, :, ts_], [], [YN])
            for i in range(8):
                em.cp("pool" if i % 2 else "dve", xb[:, i, :], xf[:, i, :], [XF], [XB])
            for j in range(8):
                cs = slice(j * 128, (j + 1) * 128)
                for br, (wu, WU, yy, YY) in enumerate(((wur, WUR, yr, YR), (wun, WUN, yn, YN))):
                    p, P = gbank()
                    for k in range(8):
                        em.mm(p[:, :], wg[:, k, br * 1024 + j * 128: br * 1024 + (j + 1) * 128], xb[:, k, :], k == 0, k == 7, [WG, XB], [P])
                    em.act(sg[br][:], p[:, :], AF.Sigmoid, [P], [SG[br]])
                    p2, P2 = gbank()
                    for k in range(4):
                        em.mm(p2[:, :], wu[:, k, cs], yy[:, k, :], k == 0, k == 3, [WU, YY], [P2])
                    if br == 0:
                        em.tt("dve", m1[:], sg[0][:], p2[:, :], ALU.mult, [SG[0], P2], [M1])
                    else:
                        em.tt("dve", sg[1][:], sg[1][:], p2[:, :], ALU.mult, [SG[1], P2], [SG[1]])
                        em.tt("pool", mg[:, j, :], m1[:], sg[1][:], ALU.add, [M1, SG[1]], [MG])
            for i in range(8):
                p, P = gbank()
                for k in range(8):
                    em.mm(p[:, :], wo[:, k, i * 128:(i + 1) * 128], mg[:, k, :], k == 0, k == 7, [WO, MG], [P])
                em.stt("dve", R[:, i, :], xf[:, i, :], ALPHA, p[:, :], ALU.mult, ALU.add, [XF, P], [RB])
            layer_norm_fm(kb, em, gbank, R, RB, lambda i: (ob[:, i, :], OB), ln[:, 0:8], ln[:, 8:16], LN, tmp, TMP, ones, ONES, sq, SQ, mean, MEAN, rstd, RSTD)
            em.dma("sp", "ob", v3(o_d)[:, :, ts_], ob[:, :, :], [OB], [])
        kb.finish([OB])
        kb.emit()
        print("B1 instructions:", kb.n_ins, kb.cnt)
    return nc


def b1_inputs(inp, l):
    w_in = inp["w_in"][l]
    lnp = np.concatenate([inp["ln1_g"][l].reshape(8, 128).T, inp["ln1_b"][l].reshape(8, 128).T], axis=1)
    return dict(wg=np.ascontiguousarray(w_in[:, 1792 + 1304: 1792 + 1304 + 2048]), wur=inp["w_up_rwkv"][l], wun=inp["w_up_nsa"][l],
                wo=inp["w_out"][l], ln=np.ascontiguousarray(lnp, dtype=np.float32))


def build_b2(nexp=32):
    nc = bass.Bass("TRN2", target_bir_lowering=False)
    D = lambda n, shp: nc.dram_tensor(n, shp, F32, kind="ExternalInput").ap()
    x1_d = D("x1T", [1024, NT]); wr_d = D("wr", [1024, 36]); br_d = D("brr", [1, 36])
    w1_d = D("ew1", [32, 1024, 512]); w3_d = D("ew3", [32, 1024, 512]); w2_d = D("ew2", [32, 512, 1024])
    ln_d = D("ln", [128, 16]); selb_d = D("selb", [32, 32 * 128]); g2e_d = D("g2e", [128, 4 * 32])
    o_d = nc.dram_tensor("x2T", [1024, NT], F32, kind="ExternalOutput").ap()
    v3 = lambda ap: ap.rearrange("(kc p) n -> p kc n", p=128)
    with ExitStack() as st:
        kb = KB(nc, st); em = Em(kb); sb, ps = kb.sb, kb.ps
        wr = sb("wr", [128, 8, 64]); WR = Buf()
        brr = sb("brr", [128, 36]); BRR = Buf()
        ln = sb("ln", [128, 16]); LN = Buf()
        selb = sb("selb", [32, 32, 128], BF16); SELB = Buf()
        g2e = sb("g2e", [128, 4, 32]); G2E = Buf()
        ones = sb("ones", [128, 128]); ONES = Buf()
        ident = sb("ident", [128, 128]); IDENT = Buf()
        xf = sb("xf", [128, 8, 512]); XF = Buf()
        x1b = sb("x1b", [128, 8, NT], BF16); X1B = [Buf() for _ in range(4)]
        out = sb("out", [128, 8, NT]); OUT = [Buf() for _ in range(4)]
        cwt = sb("cwt", [32, NT], BF16); CWT = [Buf() for _ in range(4)]
        lgt = sb("lgt", [128, 36]); LGT = Buf()
        rs = sb("rs", [128, 64]); RS = Buf()
        em32 = sb("em32", [128, 32]); EM32 = Buf()
        em2 = sb("em2", [128, 32]); EM2 = Buf()
        cw = sb("cw", [128, 32]); CW = Buf()
        w1 = [sb(f"w1_{i}", [128, 8, 512], BF16) for i in range(2)]; W1 = [Buf() for _ in range(2)]
        w3 = [sb(f"w3_{i}", [128, 8, 512], BF16) for i in range(2)]; W3 = [Buf() for _ in range(2)]
        w2 = [sb(f"w2_{i}", [128, 4, 1024], BF16) for i in range(2)]; W2 = [Buf() for _ in range(2)]
        cwb = sb("cwb", [128, 512]); CWB = Buf()
        sl_ = [sb(f"sl{i}", [128, 512]) for i in range(2)]; SL = [Buf() for _ in range(2)]
        hb = sb("hb", [128, 4, 512], BF16); HB = [Buf() for _ in range(4)]
        ob = xf; OB = XF
        tmp = sb("tmp", [128, 512]); TMP = Buf()
        sq = [sb(f"sq{i}", [128, 512]) for i in range(2)]; SQ = [Buf() for _ in range(2)]
        mean = sb("mean", [128, 512]); MEAN = Buf()
        rstd = sb("rstd", [128, 512]); RSTD = Buf()
        pg = [ps(f"pg{i}", [128, 512]) for i in range(8)]; PG = [Buf(excl=True) for _ in range(8)]
        gi = [0]

        def gbank():
            i = gi[0] % 8; gi[0] += 1
            return pg[i], PG[i]
        em.ms("dve", wr[:], 0.0, [WR])
        em.dma("sp", "wr", wr[:, :, 0:36], v3(wr_d), [WR], [WR])
        em.dma("sp", "brr", brr[:], br_d[0:1, :].partition_broadcast(128), [], [BRR])
        em.dma("sp", "ln", ln[:], ln_d[:, :], [], [LN])
        em.dma("pool", "selb", selb[:].rearrange("p a b -> p (a b)"), selb_d[:, :], [], [SELB])
        em.dma("sp", "g2e", g2e[:].rearrange("p a b -> p (a b)"), g2e_d[:, :], [], [G2E])
        em.ms("dve", ones[:], 1.0, [ONES])
        em.ms("pool", ident[:], 1.0, [IDENT])
        kb.op("pool", lambda e: e.affine_select(out=ident[:], in_=ident[:], pattern=[[-1, 128]], compare_op=ALU.is_equal,
                                                fill=0.0, base=0, channel_multiplier=1), [IDENT], [IDENT])

        def load_w(e):
            s = e % 2
            for k0 in range(0, 8, 4):
                em.dma("pool", f"w1_{s}", w1[s][:, k0:k0 + 4, :], w1_d[e].rearrange("(kc p) n -> p kc n", p=128)[:, k0:k0 + 4, :], [], [W1[s]])
                em.dma("pool", f"w3_{s}", w3[s][:, k0:k0 + 4, :], w3_d[e].rearrange("(kc p) n -> p kc n", p=128)[:, k0:k0 + 4, :], [], [W3[s]])
            for k0 in range(0, 4, 2):
                em.dma("pool", f"w2_{s}", w2[s][:, k0:k0 + 2, :], w2_d[e].rearrange("(kc p) n -> p kc n", p=128)[:, k0:k0 + 2, :], [], [W2[s]])

        load_w(0)
        for tg in range(4):
            ts_ = slice(tg * 512, (tg + 1) * 512)
            em.dma("sp", "xf", xf[:, :, :], v3(x1_d)[:, :, ts_], [], [XF])
            for i in range(8):
                em.cp("pool" if i % 2 else "dve", x1b[:, i, ts_], xf[:, i, :], [XF], [X1B[tg]])
                em.ts("dve" if i % 2 else "pool", out[:, i, ts_], xf[:, i, :], ALPHA, None, ALU.mult, None, [XF], [OUT[tg]])
            for tt_ in range(4):
                p, P = gbank()
                for k in range(8):
                    em.mm(p[:, 0:36], xf[:, k, tt_ * 128:(tt_ + 1) * 128], wr[:, k, 0:36], k == 0, k == 7, [XF, WR], [P])
                em.tt("dve", lgt[:], p[:, 0:36], brr[:], ALU.add, [P, BRR], [LGT])
                em.red(rs[:, 0:1], lgt[:, 0:4], ALU.max, [LGT], [RS])
                em.ts("dve", rs[:, 1:2], rs[:, 0:1], -1.0, None, ALU.mult, None, [RS], [RS])
                em.ms("dve", rs[:, 2:3], 0.0, [RS])
                em.act(rs[:, 4:8], lgt[:, 0:4], AF.Exp, [LGT, RS], [RS], bias=rs[:, 1:2], accum_out=rs[:, 2:3])
                em.rcp(rs[:, 3:4], rs[:, 2:3], [RS], [RS])
                em.ts("dve", rs[:, 8:12], lgt[:, 0:4], rs[:, 0:1], None, ALU.is_ge, None, [LGT, RS], [RS])
                em.ts("dve", em32[:], g2e[:, 0, :], rs[:, 8:9], None, ALU.mult, None, [G2E, RS], [EM32])
                for g in range(1, 4):
                    em.stt("dve", em32[:], g2e[:, g, :], rs[:, 8 + g:9 + g], em32[:], ALU.mult, ALU.add, [G2E, RS, EM32], [EM32])
                em.tt("dve", em2[:], lgt[:, 4:36], em32[:], ALU.mult, [LGT, EM32], [EM2])
                em.ts("dve", em32[:], em32[:], -1.0, 1.0e9, ALU.add, ALU.mult, [EM32], [EM32])
                em.tt("dve", em2[:], em2[:], em32[:], ALU.add, [EM2, EM32], [EM2])
                em.red(rs[:, 12:13], em2[:], ALU.max, [EM2], [RS])
                em.ts("dve", cw[:], em2[:], rs[:, 12:13], None, ALU.is_ge, None, [EM2, RS], [CW])
                em.stt("dve", em32[:], cw[:], -2.0e9, em2[:], ALU.mult, ALU.add, [CW, EM2], [EM32])
                em.red(rs[:, 13:14], em32[:], ALU.max, [EM32], [RS])
                em.ts("dve", em32[:], em32[:], rs[:, 13:14], None, ALU.is_ge, None, [EM32, RS], [EM32])
                em.tt("dve", rs[:, 14:15], rs[:, 13:14], rs[:, 12:13], ALU.subtract, [RS], [RS])
                em.act(rs[:, 15:16], rs[:, 14:15], AF.Exp, [RS], [RS])
                em.ts("dve", rs[:, 15:16], rs[:, 15:16], 1.0, None, ALU.add, None, [RS], [RS])
                em.rcp(rs[:, 16:17], rs[:, 15:16], [RS], [RS])
                em.tt("dve", rs[:, 17:18], rs[:, 16:17], rs[:, 3:4], ALU.mult, [RS], [RS])
                em.tt("dve", rs[:, 18:19], rs[:, 3:4], rs[:, 17:18], ALU.subtract, [RS], [RS])
                em.ts("dve", cw[:], cw[:], rs[:, 17:18], None, ALU.mult, None, [CW, RS], [CW])
                em.stt("dve", cw[:], em32[:], rs[:, 18:19], cw[:], ALU.mult, ALU.add, [EM32, RS, CW], [CW])
                pt_, PT_ = gbank()
                em.tr(pt_[0:32, 0:128], cw[:], ident[:], [CW, IDENT], [PT_])
                em.cp("dve", cwt[:, tg * 512 + tt_ * 128: tg * 512 + (tt_ + 1) * 128], pt_[0:32, 0:128], [PT_], [CWT[tg]])
        for e in range(nexp):
            s = e % 2
            if e + 1 < nexp:
                load_w(e + 1)
            for tg in range(4):
                ts_ = slice(tg * 512, (tg + 1) * 512)
                pc_, PC_ = gbank()
                em.mm(pc_[:, :], selb[:, e, :], cwt[:, ts_], True, True, [SELB, CWT[tg]], [PC_])
                em.act(cwb[:], pc_[:, :], AF.Copy, [PC_], [CWB])
                for f in range(4):
                    fs = slice(f * 128, (f + 1) * 128)
                    pa, PA = gbank()
                    for k in range(8):
                        em.mm(pa[:, :], w1[s][:, k, fs], x1b[:, k, ts_], k == 0, k == 7, [W1[s], X1B[tg]], [PA])
                    pb, PB = gbank()
                    for k in range(8):
                        em.mm(pb[:, :], w3[s][:, k, fs], x1b[:, k, ts_], k == 0, k == 7, [W3[s], X1B[tg]], [PB])
                    em.act(sl_[f % 2][:], pa[:, :], AF.Silu, [PA], [SL[f % 2]])
                    em.tt("dve", sl_[f % 2][:], sl_[f % 2][:], pb[:, :], ALU.mult, [SL[f % 2], PB], [SL[f % 2]])
                    em.tt("pool", hb[:, f, :], sl_[f % 2][:], cwb[:], ALU.mult, [SL[f % 2], CWB], [HB[f]])
                for i in range(8):
                    po, PO = gbank()
                    for f in range(4):
                        em.mm(po[:, :], w2[s][:, f, i * 128:(i + 1) * 128], hb[:, f, :], f == 0, f == 3, [W2[s], HB[f]], [PO])
                    em.tt("dve", out[:, i, ts_], out[:, i, ts_], po[:, :], ALU.add, [OUT[tg], PO], [OUT[tg]])
        for tg in range(4):
            ts_ = slice(tg * 512, (tg + 1) * 512)
            layer_norm_fm(kb, em, gbank, out[:, :, ts_], OUT[tg], lambda i: (ob[:, i, :], OB), ln[:, 0:8], ln[:, 8:16], LN, tmp, TMP,
                          ones, ONES, sq, SQ, mean, MEAN, rstd, RSTD)
            em.dma("sp", "ob", v3(o_d)[:, :, ts_], ob[:, :, :], [OB], [])
        kb.finish([OB])
        kb.emit()
        print("B2 instructions:", kb.n_ins, kb.cnt)
    return nc


def b2_consts():
    selb = np.zeros((32, 32, 128), np.float32)
    for e in range(32):
        selb[e, e, :] = 1.0
    g2e = np.zeros((128, 4, 32), np.float32)
    for g in range(4):
        g2e[:, g, g * 8:(g + 1) * 8] = 1.0
    return dict(selb=selb.reshape(32, 32 * 128), g2e=g2e.reshape(128, 128))


def b2_inputs(inp, l, consts):
    wr = np.concatenate([inp["router_group_w"][l], inp["router_expert_w"][l]], axis=1)
    brr = np.concatenate([inp["router_group_b"][l], inp["router_expert_b"][l]])[None, :]
    lnp = np.concatenate([inp["ln2_g"][l].reshape(8, 128).T, inp["ln2_b"][l].reshape(8, 128).T], axis=1)
    return dict(wr=np.ascontiguousarray(wr), brr=np.ascontiguousarray(brr), ew1=inp["exp_w1"][l], ew3=inp["exp_w3"][l], ew2=inp["exp_w2"][l],
                ln=np.ascontiguousarray(lnp, dtype=np.float32), selb=consts["selb"], g2e=consts["g2e"])

_PROGS = {}


def _prog(name):
    if name not in _PROGS:
        _PROGS[name] = {"a1": build_a1, "a2": build_a2, "b1": build_b1, "b2": build_b2}[name]()
    return _PROGS[name]


def _run(name, maps):
    res = run_bass_kernel_spmd(_prog(name), maps, core_ids=list(range(8)))
    return res.results


def kernel(**inputs):
    inp = {k: np.asarray(v) for k, v in inputs.items()}
    x = inp["x"].astype(np.float32, copy=False)
    B = x.shape[0]
    xT = [np.ascontiguousarray(x[b].T) for b in range(B)]
    c2 = a2_consts()
    cb2 = b2_consts()
    for l in range(4):
        per_hh = [a1_inputs(inp, l, 0, hh) for hh in range(2)]
        maps = []
        for c in range(8):
            m = dict(per_hh[c % 2]); m["xT"] = xT[c // 2]; maps.append(m)
        r1 = _run("a1", maps)
        yrT = [np.concatenate([r1[2 * b]["yT"], r1[2 * b + 1]["yT"]], axis=0) for b in range(B)]
        per_g = [a2_inputs(inp, l, g, c2) for g in range(2)]
        maps = []
        for c in range(8):
            m = dict(per_g[c % 2]); m["xT"] = xT[c // 2]; maps.append(m)
        r2 = _run("a2", maps)
        ynT = [np.ascontiguousarray(np.concatenate([r2[2 * b]["y"], r2[2 * b + 1]["y"]], axis=1).T) for b in range(B)]
        base = b1_inputs(inp, l)
        maps = []
        for c in range(8):
            b, hf = c // 2, c % 2
            ts = slice(hf * NT, (hf + 1) * NT)
            m = dict(base)
            m["xT"] = np.ascontiguousarray(xT[b][:, ts]); m["yrT"] = np.ascontiguousarray(yrT[b][:, ts]); m["ynT"] = np.ascontiguousarray(ynT[b][:, ts])
            maps.append(m)
        r3 = _run("b1", maps)
        base = b2_inputs(inp, l, cb2)
        maps = []
        for c in range(8):
            m = dict(base); m["x1T"] = r3[c]["x1T"]; maps.append(m)
        r4 = _run("b2", maps)
        xT = [np.ascontiguousarray(np.concatenate([r4[2 * b]["x2T"], r4[2 * b + 1]["x2T"]], axis=1)) for b in range(B)]
    out = np.stack([xT[b].T for b in range(B)], axis=0)
    return np.ascontiguousarray(out, dtype=np.float32)
```

```python
import numpy as np
from contextlib import ExitStack
import concourse.bass as bass
import concourse.mybir as mybir
from concourse.bass_utils import run_bass_kernel_spmd

F32 = mybir.dt.float32
BF16 = mybir.dt.bfloat16
AF = mybir.ActivationFunctionType
ALU = mybir.AluOpType
AX = mybir.AxisListType


class Buf:
    __slots__ = ("name", "w", "r", "excl")

    def __init__(self, name="", excl=False):
        self.name = name
        self.excl = excl
        self.w = None
        self.r = {}


class KB:
    def __init__(self, nc, stack):
        self.nc = nc
        self.stack = stack
        self.pstack = stack
        self.prefix = ""
        self.names = ["pe", "act", "dve", "pool", "sp"]
        self.prog = {e: [] for e in self.names}
        self.sem = {e: stack.enter_context(nc.semaphore("s_" + e)) for e in self.names}
        self.cnt = {e: 0 for e in self.names}
        self.seen = {e: {} for e in self.names}
        self.dsem = {}
        self.n_ins = 0
        self.nw = {e: 0 for e in self.names}

    def sb(self, name, shape, dt=F32):
        return self.pstack.enter_context(self.nc.sbuf_tensor(self.prefix + "sb_" + name, list(shape), dt))

    def ps(self, name, shape, dt=F32):
        return self.pstack.enter_context(self.nc.psum_tensor(self.prefix + "ps_" + name, list(shape), dt))

    def _semh(self, key):
        if key in self.sem:
            return self.sem[key]
        return self.dsem[key][0]

    def _waits(self, eng, reads, writes):
        need = {}

        def add(d):
            if d is None:
                return
            k, v = d
            if need.get(k, 0) < v:
                need[k] = v
        for b in reads:
            add(b.w)
        for b in writes:
            add(b.w)
            for k, v in b.r.items():
                add((k, v))
        out = []
        seen = self.seen[eng]
        for k, v in need.items():
            if k == "pe" and eng == "pe":
                continue
            if seen.get(k, 0) >= v:
                continue
            seen[k] = v
            out.append((self._semh(k), v))
        return out

    def _mark(self, tok, reads, writes):
        for b in writes:
            b.w = tok
            b.r = {}
        k, v = tok
        for b in reads:
            if b.r.get(k, 0) < v:
                b.r[k] = v

    def op(self, eng, fn, reads=(), writes=()):
        ex = [b for b in reads if b.excl]
        if ex:
            writes = list(writes) + ex
        waits = self._waits(eng, reads, writes)
        self.nw[eng] += len(waits)
        self.cnt[eng] += 1
        tok = (eng, self.cnt[eng])
        sem = self.sem[eng]

        def run(e, waits=waits, fn=fn, sem=sem):
            for s, v in waits:
                e.wait_ge(s, v)
            fn(e).then_inc(sem, 1)
        self.prog[eng].append(run)
        self._mark(tok, reads, writes)
        self.n_ins += 1

    def dma(self, q, key, fn, reads=(), writes=(), n=1):
        key = "d_" + key
        if key not in self.dsem:
            self.dsem[key] = [self.stack.enter_context(self.nc.semaphore(key)), 0]
        waits = self._waits(q, reads, writes)
        self.dsem[key][1] += 16 * n
        tok = (key, self.dsem[key][1])
        sem = self.dsem[key][0]

        def run(e, waits=waits, fn=fn, sem=sem):
            for s, v in waits:
                e.wait_ge(s, v)
            fn(e, sem)
        self.prog[q].append(run)
        self._mark(tok, reads, writes)
        self.n_ins += n

    def finish(self, bufs):
        waits = self._waits("sp", bufs, bufs)

        def run(e, waits=waits):
            for s, v in waits:
                e.wait_ge(s, v)
        self.prog["sp"].append(run)

    def emit(self):
        nc = self.nc
        prog = self.prog
        self.prog = {e: [] for e in self.names}
        with nc.Block() as block:
            @block.sync
            def _(e):
                for f in prog["sp"]:
                    f(e)

            @block.tensor
            def _(e):
                for f in prog["pe"]:
                    f(e)

            @block.scalar
            def _(e):
                for f in prog["act"]:
                    f(e)

            @block.vector
            def _(e):
                for f in prog["dve"]:
                    f(e)

            @block.gpsimd
            def _(e):
                for f in prog["pool"]:
                    f(e)


S = 4096
NS = 512
NSEG = S // NS
CH = 128
NCH = NS // CH
GN_EPS = 64e-5


def a1_body(nc, kb, D, nseg=NSEG, debug=False):
    xT_d, w_d, vec_d, lw_d, g2_d, cst_d, yT_d = (D[k] for k in ("xT", "w", "vec", "lw", "g2", "cst", "yT"))
    xT_v = xT_d.rearrange("(kc p) t -> p kc t", p=128)
    w_v = w_d.rearrange("(kc p) n -> p kc n", p=128)
    if True:
        sb, ps = kb.sb, kb.ps
        NXS = 3
        xs = [sb(f"xs{i}", [128, 8, NS], BF16) for i in range(NXS)]; XS = [Buf() for _ in range(NXS)]
        wsb = sb("wsb", [128, 8, 1024], BF16); WSB = Buf()
        vec = sb("vec", [128, 22]); VEC = Buf()
        vx = sb("vx", [128, 8]); VX = Buf()
        lw = sb("lw", [128, 256]); LW = Buf()
        g2 = sb("g2", [128, 256]); G2 = Buf()
        cst = sb("cst", [128, 1024]); CST = Buf()
        MASK1 = cst[:, 0:512]; MASK2 = cst[:, 512:768]; MSL = cst[:, 768:896]; BONES = cst[:, 896:1024]
        ident = sb("ident", [128, 128]); IDENT = Buf()
        car = sb("car", [128, 8]); CAR = Buf()
        zr = [sb(f"zr{i}", [128, NS + 1]) for i in range(2)]; ZR = [Buf() for _ in range(2)]
        dtmp = sb("dtmp", [128, NS]); DTMP = Buf()
        zs = [sb(f"zs{j}", [128, NS]) for j in range(8)]; ZS = [Buf() for _ in range(8)]
        tw = sb("tw", [128, NS]); TW = Buf()
        sg = sb("sg", [128, NS]); SG = Buf()
        tnames = ["nld", "cw", "ew", "ewi", "ewx", "aa", "kkn", "sq", "t1", "k2", "bh", "kh", "e1"]
        T = {n: sb("t_" + n, [128, NS]) for n in tnames}; TB = {n: Buf() for n in tnames}
        ar = [sb(f"ar{h}", [128, NCH, 2 * CH]) for h in range(2)]; AR = [Buf() for _ in range(2)]
        bt = [sb(f"bt{h}", [128, NS]) for h in range(2)]; BT = [Buf() for _ in range(2)]
        kt = [sb(f"kt{h}", [128, NS]) for h in range(2)]; KT = [Buf() for _ in range(2)]
        gg = [sb(f"gg{h}", [128, NS]) for h in range(2)]; GG = [Buf() for _ in range(2)]
        bon = [sb(f"bon{h}", [128, NS]) for h in range(2)]; BON = [Buf() for _ in range(2)]
        yf = [sb(f"yf{h}", [128, NS]) for h in range(2)]; YF = [Buf() for _ in range(2)]
        wc = [sb(f"wc{h}", [128, NCH]) for h in range(2)]; WC = [Buf() for _ in range(2)]
        bhT = sb("bhT", [128, NCH, 256]); BHT = [Buf() for _ in range(NCH)]
        khT = sb("khT", [128, NCH, 256]); KHT = [Buf() for _ in range(NCH)]
        vT = sb("vT", [128, NCH, 256]); VT = [Buf() for _ in range(NCH)]
        mab = [sb(f"mab{h}", [128, 256]) for h in range(4)]; MAB = [Buf() for _ in range(4)]
        mak = [sb(f"mak{h}", [128, 256]) for h in range(4)]; MAK = [Buf() for _ in range(4)]
        mm = [[sb(f"mm{h}_{i}", [128, 128]) for i in range(2)] for h in range(4)]; MM = [[Buf(), Buf()] for _ in range(4)]
        nn = [[sb(f"nn{h}_{i}", [128, 128]) for i in range(2)] for h in range(4)]; NN = [[Buf(), Buf()] for _ in range(4)]
        qq = [[sb(f"qq{h}_{i}", [128, 128]) for i in range(2)] for h in range(4)]; QQ = [[Buf(), Buf()] for _ in range(4)]
        xsb = [sb(f"xsb{h}", [128, 64]) for h in range(4)]; XSB = [Buf() for _ in range(4)]
        usb = [sb(f"usb{h}", [128, 64]) for h in range(4)]; USB = [Buf() for _ in range(4)]
        stt = [[sb(f"st{hp}_{i}", [128, 64]) for i in range(2)] for hp in range(2)]
        STT = [[[Buf(), Buf()] for _ in range(2)] for hp in range(2)]
        ytok = sb("ytok", [128, 256]); YTOK = Buf()
        ysq = sb("ysq", [128, 256]); YSQ = Buf()
        yn = sb("yn", [128, 256]); YN = Buf()
        sts = sb("sts", [128, 32]); STS = Buf()
        osb = [sb(f"osb{h}", [128, NS]) for h in range(2)]; OSB = [Buf() for _ in range(2)]
        pa = [ps(f"pa{i}", [128, 512]) for i in range(2)]; PA = [Buf(excl=True) for _ in range(2)]
        pab = [ps(f"pab{i}", [128, 512]) for i in range(2)]; PAB = [Buf(excl=True) for _ in range(2)]
        pq = [ps(f"pq{i}", [128, 512]) for i in range(2)]; PQB = [Buf(excl=True) for _ in range(2)]
        PQ = [[PQB[i]] * 4 for i in range(2)]
        psts = [ps(f"pst{i}", [128, 512]) for i in range(2)]; PSTB = [Buf(excl=True) for _ in range(2)]

        def ld(q, key, out, in_, B):
            kb.dma(q, key, lambda e, s: e.dma_start(out=out, in_=in_).then_inc(s, 16), writes=[B])
        ld("sp", "vec", vec[:], vec_d[:, :], VEC)
        ld("sp", "lw", lw[:], lw_d[:, :], LW)
        ld("sp", "g2", g2[:], g2_d[:, :], G2)
        ld("sp", "cst", cst[:], cst_d[:, :], CST)
        for kc in range(0, 8, 4):
            kb.dma("pool", "wsb", lambda e, s, kc=kc: e.dma_start(out=wsb[:, kc:kc + 4, :], in_=w_v[:, kc:kc + 4, :]).then_inc(s, 16), writes=[WSB])
        kb.op("pool", lambda e: e.memset(ident[:], 1.0), writes=[IDENT])
        kb.op("pool", lambda e: e.affine_select(out=ident[:], in_=ident[:], pattern=[[-1, 128]], compare_op=ALU.is_equal,
                                                fill=0.0, base=0, channel_multiplier=1), reads=[IDENT], writes=[IDENT])
        kb.op("dve", lambda e: e.memset(car[:], 0.0), writes=[CAR])
        kb.op("dve", lambda e: e.tensor_scalar(vx[:, 0:2], vec[:, 8:10], -1.0, None, ALU.mult), reads=[VEC], writes=[VX])
        kb.op("dve", lambda e: e.tensor_scalar(vx[:, 2:4], vec[:, 14:16], -1.0, 1.0, ALU.mult, ALU.add), reads=[VEC, VX], writes=[VX])
        for hp in range(2):
            for i in range(2):
                kb.op("dve", lambda e, hp=hp, i=i: e.memset(stt[hp][i][:], 0.0), writes=STT[hp][i])

        def load_x(sgi):
            sl = sgi % NXS
            kb.dma("pool", f"xs{sl}", lambda e, s, sl=sl, sgi=sgi: e.dma_start(
                out=xs[sl][:, :, :], in_=xT_v[:, :, sgi * NS:(sgi + 1) * NS]).then_inc(s, 16), writes=[XS[sl]])

        load_x(0)
        pai = [0]

        def next_pa():
            i = pai[0] % 2
            pai[0] += 1
            return pa[i], PA[i]

        def mm512(lhsT_fn, rhs_fn, nk, reads, M=128):
            p, P = next_pa()
            for k in range(nk):
                a_, b_ = lhsT_fn(k), rhs_fn(k)
                kb.op("pe", lambda e, k=k, p=p, a_=a_, b_=b_: e.matmul(p[0:M, :], a_, b_, start=(k == 0), stop=(k == nk - 1)),
                      reads=reads, writes=[P])
            return p, P

        ping = [0, 0]
        dbg_n = [0]

        def dbg(name, ap, B):
            if not debug:
                return
            shp = list(ap.shape)
            d = nc.dram_tensor("dbg_" + name, shp, F32, kind="ExternalOutput").ap()
            dbg_n[0] += 1
            cntv = dbg_n[0] * 16

            def f(e, s, d=d, ap=ap, cntv=cntv):
                e.dma_start(out=d, in_=ap).then_inc(s, 16)
                e.wait_ge(s, cntv)
            kb.dma("sp", "dbg", f, reads=[B])
        for sgi in range(nseg):
            sl = sgi % NXS
            if sgi + 1 < nseg:
                load_x(sgi + 1)
            for j in range(8):
                p, P = mm512(lambda k, j=j: wsb[:, k, j * 128:(j + 1) * 128], lambda k, sl=sl: xs[sl][:, k, :], 8, [WSB, XS[sl]])
                z, Z = zr[j % 2], ZR[j % 2]
                kb.op("pool", lambda e, z=z, j=j: e.tensor_copy(z[:, 0:1], car[:, j:j + 1]), reads=[CAR], writes=[Z])
                kb.op("act", lambda e, z=z, p=p: e.activation(out=z[:, 1:NS + 1], in_=p[:, :], func=AF.Copy), reads=[P], writes=[Z])
                kb.op("pool", lambda e, z=z, j=j: e.tensor_copy(car[:, j:j + 1], z[:, NS:NS + 1]), reads=[Z], writes=[CAR])
                kb.op("dve", lambda e, z=z: e.tensor_tensor(dtmp[:], z[:, 0:NS], z[:, 1:NS + 1], ALU.subtract), reads=[Z], writes=[DTMP])
                kb.op("dve", lambda e, z=z, j=j: e.scalar_tensor_tensor(zs[j][:], dtmp[:], vec[:, j:j + 1], z[:, 1:NS + 1], ALU.mult, ALU.add),
                      reads=[DTMP, Z, VEC], writes=[ZS[j]])
            L1, L2 = zs[6], zs[7]
            for j in range(8):
                dbg(f"zs{j}", zs[j][:], ZS[j])
            kb.op("act", lambda e: e.activation(out=tw[0:64, :], in_=L1[0:64, :], func=AF.Tanh), reads=[ZS[6]], writes=[TW])
            kb.op("act", lambda e: e.activation(out=sg[:], in_=L2[:], func=AF.Sigmoid), reads=[ZS[7]], writes=[SG])
            for hp in range(2):
                Rz, Kz, Vz = zs[0 + hp], zs[2 + hp], zs[4 + hp]
                RZ, KZ, VZ = ZS[0 + hp], ZS[2 + hp], ZS[4 + hp]
                cs = slice(hp * 128, (hp + 1) * 128)
                p, P = mm512(lambda k: lw[64:128, cs], lambda k: L1[64:128, :], 1, [LW, ZS[6]])
                kb.op("act", lambda e, p=p, hp=hp: e.activation(out=T["aa"][:], in_=p[:, :], func=AF.Sigmoid, bias=vec[:, 10 + hp:11 + hp]),
                      reads=[P, VEC], writes=[TB["aa"]])
                p, P = mm512(lambda k: g2[:, cs], lambda k: sg[:], 1, [G2, SG])
                kb.op("act", lambda e, p=p, hp=hp: e.activation(out=gg[hp][:], in_=p[:, :], func=AF.Copy), reads=[P], writes=[GG[hp]])
                p, P = mm512(lambda k: lw[0:64, cs], lambda k: tw[0:64, :], 1, [LW, TW])
                kb.op("act", lambda e, p=p, hp=hp: e.activation(out=T["e1"][:], in_=p[:, :], func=AF.Exp, bias=vx[:, hp:hp + 1], scale=-1.0),
                      reads=[P, VX], writes=[TB["e1"]])
                kb.op("act", lambda e: e.activation(out=T["e1"][:], in_=T["e1"][:], func=AF.Ln, bias=1.0), reads=[TB["e1"]], writes=[TB["e1"]])
                kb.op("act", lambda e: e.activation(out=T["nld"][:], in_=T["e1"][:], func=AF.Exp, bias=-0.5, scale=-1.0),
                      reads=[TB["e1"]], writes=[TB["nld"]])
                kb.op("dve", lambda e: e.tensor_tensor_scan(T["cw"][:], MASK1, T["nld"][:], 0.0, ALU.mult, ALU.add),
                      reads=[CST, TB["nld"]], writes=[TB["cw"]])
                kb.op("act", lambda e: e.activation(out=T["ew"][:], in_=T["cw"][:], func=AF.Exp, scale=-1.0), reads=[TB["cw"]], writes=[TB["ew"]])
                kb.op("act", lambda e: e.activation(out=T["ewi"][:], in_=T["cw"][:], func=AF.Exp), reads=[TB["cw"]], writes=[TB["ewi"]])
                kb.op("pool", lambda e: e.tensor_tensor(T["ewx"][:], T["cw"][:], T["nld"][:], ALU.subtract), reads=[TB["cw"], TB["nld"]], writes=[TB["ewx"]])
                kb.op("act", lambda e: e.activation(out=T["ewx"][:], in_=T["ewx"][:], func=AF.Exp, scale=-1.0), reads=[TB["ewx"]], writes=[TB["ewx"]])
                kb.op("pool", lambda e, hp=hp: e.tensor_copy(wc[hp][:], T["ew"][:].rearrange("p (c t) -> p c t", t=CH)[:, :, CH - 1]),
                      reads=[TB["ew"]], writes=[WC[hp]])
                kb.op("dve", lambda e, hp=hp, Kz=Kz: e.tensor_scalar(T["kkn"][:], Kz[:], vec[:, 12 + hp:13 + hp], None, ALU.mult),
                      reads=[KZ, VEC], writes=[TB["kkn"]])
                kb.op("pool", lambda e: e.tensor_tensor(T["sq"][:], T["kkn"][:], T["kkn"][:], ALU.mult), reads=[TB["kkn"]], writes=[TB["sq"]])
                p, P = mm512(lambda k: BONES, lambda k: T["sq"][:], 1, [CST, TB["sq"]])
                kb.op("act", lambda e, p=p: e.activation(out=T["sq"][:], in_=p[:, :], func=AF.Sqrt), reads=[P], writes=[TB["sq"]])
                kb.op("dve", lambda e: e.tensor_scalar(T["sq"][:], T["sq"][:], 1e-12, None, ALU.max), reads=[TB["sq"]], writes=[TB["sq"]])
                kb.op("dve", lambda e: e.reciprocal(T["sq"][:], T["sq"][:]), reads=[TB["sq"]], writes=[TB["sq"]])
                kb.op("dve", lambda e: e.tensor_tensor(T["kkn"][:], T["kkn"][:], T["sq"][:], ALU.mult), reads=[TB["kkn"], TB["sq"]], writes=[TB["kkn"]])
                kb.op("pool", lambda e, hp=hp: e.tensor_scalar(T["t1"][:], T["aa"][:], vec[:, 14 + hp:15 + hp], vx[:, 2 + hp:3 + hp], ALU.mult, ALU.add),
                      reads=[TB["aa"], VEC, VX], writes=[TB["t1"]])
                kb.op("pool", lambda e, Kz=Kz: e.tensor_tensor(T["k2"][:], Kz[:], T["t1"][:], ALU.mult), reads=[KZ, TB["t1"]], writes=[TB["k2"]])
                arv = ar[hp]
                kb.op("dve", lambda e, arv=arv: e.scalar_tensor_tensor(arv[:, :, 0:CH], T["kkn"][:].rearrange("p (c t) -> p c t", t=CH), -1.0,
                                                                      T["ewx"][:].rearrange("p (c t) -> p c t", t=CH), ALU.mult, ALU.mult),
                      reads=[TB["kkn"], TB["ewx"]], writes=[AR[hp]])
                kb.op("pool", lambda e, arv=arv, Rz=Rz: e.tensor_tensor(arv[:, :, CH:2 * CH], Rz[:].rearrange("p (c t) -> p c t", t=CH),
                                                                       T["ew"][:].rearrange("p (c t) -> p c t", t=CH), ALU.mult),
                      reads=[RZ, TB["ew"]], writes=[AR[hp]])
                kb.op("dve", lambda e: e.tensor_tensor(T["t1"][:], T["kkn"][:], T["aa"][:], ALU.mult), reads=[TB["kkn"], TB["aa"]], writes=[TB["t1"]])
                kb.op("dve", lambda e, hp=hp: e.tensor_tensor(bt[hp][:], T["t1"][:], T["ewi"][:], ALU.mult), reads=[TB["t1"], TB["ewi"]], writes=[BT[hp]])
                kb.op("pool", lambda e, hp=hp: e.tensor_tensor(kt[hp][:], T["k2"][:], T["ewi"][:], ALU.mult), reads=[TB["k2"], TB["ewi"]], writes=[KT[hp]])
                wcb = wc[hp][:].unsqueeze(2).to_broadcast([128, NCH, CH])
                kb.op("dve", lambda e, hp=hp, wcb=wcb: e.tensor_tensor(T["bh"][:].rearrange("p (c t) -> p c t", t=CH),
                                                                      bt[hp][:].rearrange("p (c t) -> p c t", t=CH), wcb, ALU.mult),
                      reads=[BT[hp], WC[hp]], writes=[TB["bh"]])
                kb.op("pool", lambda e, hp=hp, wcb=wcb: e.tensor_tensor(T["kh"][:].rearrange("p (c t) -> p c t", t=CH),
                                                                       kt[hp][:].rearrange("p (c t) -> p c t", t=CH), wcb, ALU.mult),
                      reads=[KT[hp], WC[hp]], writes=[TB["kh"]])
                kb.op("dve", lambda e, hp=hp, Rz=Rz: e.scalar_tensor_tensor(T["t1"][:], Rz[:], vec[:, 16 + hp:17 + hp], T["k2"][:], ALU.mult, ALU.mult),
                      reads=[RZ, VEC, TB["k2"], TB["t1"]], writes=[TB["t1"]])
                p, P = mm512(lambda k: BONES, lambda k: T["t1"][:], 1, [CST, TB["t1"]])
                kb.op("dve", lambda e, p=p, hp=hp, Vz=Vz: e.tensor_tensor(bon[hp][:], p[:, :], Vz[:], ALU.mult), reads=[P, VZ], writes=[BON[hp]])
                for n_ in ("nld", "cw", "ew", "ewi", "ewx", "aa", "kkn", "k2", "bh", "kh"):
                    dbg(f"{n_}{hp}", T[n_][:], TB[n_])
                dbg(f"bt{hp}", bt[hp][:], BT[hp]); dbg(f"kt{hp}", kt[hp][:], KT[hp]); dbg(f"ar{hp}", ar[hp][:].rearrange("p c t -> p (c t)"), AR[hp])
                dbg(f"gg{hp}", gg[hp][:], GG[hp]); dbg(f"bon{hp}", bon[hp][:], BON[hp]); dbg(f"wc{hp}", wc[hp][:], WC[hp])
                for c in range(NCH):
                    for (src, SRC, dst, DST) in ((T["bh"], TB["bh"], bhT, BHT), (T["kh"], TB["kh"], khT, KHT), (Vz, VZ, vT, VT)):
                        pbank, PT = next_pa()
                        pt = pbank[:, 0:128]
                        kb.op("pe", lambda e, pt=pt, src=src, c=c: e.transpose(pt, src[:, c * CH:(c + 1) * CH], ident[:]),
                              reads=[SRC, IDENT], writes=[PT])
                        kb.op("act", lambda e, pt=pt, dst=dst, c=c, cs=cs: e.activation(out=dst[:, c, cs], in_=pt, func=AF.Copy),
                              reads=[PT], writes=[DST[c]])
            import os as _os
            for c in range(NCH if str(sgi) in _os.environ.get('CHSEG', '01234567') else 0):
                csl = slice(c * CH, (c + 1) * CH)
                for h in range(4):
                    hp, rows = h // 2, slice(64 * (h % 2), 64 * (h % 2) + 64)
                    pp, PP = pab[h % 2], PAB[h % 2]
                    kb.op("pe", lambda e, pp=pp, hp=hp, rows=rows, c=c, csl=csl: e.matmul(pp[:, 0:256], bt[hp][rows, csl], ar[hp][rows, c, :], start=True, stop=True),
                          reads=[BT[hp], AR[hp]], writes=[PP])
                    kb.op("pe", lambda e, pp=pp, hp=hp, rows=rows, c=c, csl=csl: e.matmul(pp[:, 256:512], kt[hp][rows, csl], ar[hp][rows, c, :], start=True, stop=True),
                          reads=[KT[hp], AR[hp]], writes=[PP])
                    kb.op("dve", lambda e, pp=pp, h=h: e.tensor_tensor(mab[h][:], pp[:, 0:256], MASK2, ALU.mult), reads=[PP, CST], writes=[MAB[h]])
                    kb.op("dve", lambda e, pp=pp, h=h: e.tensor_tensor(mak[h][:], pp[:, 256:512], MASK2, ALU.mult), reads=[PP, CST], writes=[MAK[h]])
                    q, Q = pq[h % 2], PQ[h % 2]
                    kb.op("pe", lambda e, q=q, hp=hp, rows=rows, c=c, csl=csl: e.matmul(q[:, 0:128], ar[hp][rows, c, 0:CH], bt[hp][rows, csl], start=True, stop=True),
                          reads=[BT[hp], AR[hp]], writes=[Q[0]])
                    kb.op("dve", lambda e, q=q, h=h: e.tensor_tensor(nn[h][0][:], q[:, 0:128], MSL, ALU.mult), reads=[Q[0], CST], writes=[NN[h][0]])
                    kb.op("pool", lambda e, h=h: e.tensor_copy(mm[h][0][:], mab[h][:, 0:128]), reads=[MAB[h]], writes=[MM[h][0]])
                    kb.op("pool", lambda e, h=h: e.tensor_tensor(qq[h][0][:], mab[h][:, 0:128], ident[:], ALU.add), reads=[MAB[h], IDENT], writes=[QQ[h][0]])
                for k in range(int(_os.environ.get('NEU', '6'))):
                    a_, b_ = k % 2, (k + 1) % 2
                    for h in range(4):
                        q, Q = pq[h % 2], PQ[h % 2]
                        kb.op("pe", lambda e, q=q, h=h, a_=a_: e.matmul(q[:, 128:256], mm[h][a_][:], nn[h][a_][:], start=True, stop=True),
                              reads=[MM[h][a_], NN[h][a_]], writes=[Q[1]])
                        if _os.environ.get("NEUENG", "dve") == "act":
                            kb.op("act", lambda e, q=q, h=h, b_=b_: e.activation(out=nn[h][b_][:], in_=q[:, 128:256], func=AF.Copy), reads=[Q[1]], writes=[NN[h][b_]])
                        else:
                            kb.op("dve", lambda e, q=q, h=h, b_=b_: e.tensor_copy(nn[h][b_][:], q[:, 128:256]), reads=[Q[1]], writes=[NN[h][b_]])
                        if k < 5:
                            kb.op("pe", lambda e, q=q, h=h, a_=a_: e.matmul(q[:, 256:384], nn[h][a_][:], mm[h][a_][:], start=True, stop=True),
                                  reads=[MM[h][a_], NN[h][a_]], writes=[Q[2]])
                            kb.op("act", lambda e, q=q, h=h, b_=b_: e.activation(out=mm[h][b_][:], in_=q[:, 256:384], func=AF.Copy), reads=[Q[2]], writes=[MM[h][b_]])
                        kb.op("pe", lambda e, q=q, h=h, a_=a_, b_=b_: e.matmul(q[:, 384:512], nn[h][b_][:], qq[h][a_][:], start=True, stop=True),
                              reads=[NN[h][b_], QQ[h][a_]], writes=[Q[3]])
                        kb.op("dve", lambda e, q=q, h=h, a_=a_, b_=b_: e.tensor_tensor(qq[h][b_][:], q[:, 384:512], qq[h][a_][:], ALU.add),
                              reads=[Q[3], QQ[h][a_]], writes=[QQ[h][b_]])
                qf = 0
                if c == 0:
                    for h in range(4):
                        dbg(f"mab{h}", mab[h][:], MAB[h]); dbg(f"mak{h}", mak[h][:], MAK[h]); dbg(f"q{h}", qq[h][0][:], QQ[h][0])
                        dbg(f"n6_{h}", nn[h][0][:], NN[h][0])
                for h in range(4 if _os.environ.get('STATE', '1') == '1' else 0):
                    hp, hh = h // 2, h % 2
                    rows = slice(64 * hh, 64 * hh + 64)
                    hc = slice(h * 64, (h + 1) * 64)
                    so, sn = ping[hp], 1 - ping[hp]
                    So, Sn = STT[hp][so][hh], STT[hp][sn][hh]
                    pst = psts[hh]
                    PX = PU = PS_ = PY = PSTB[hh]
                    kb.op("pe", lambda e, h=h, c=c, hc=hc, pst=pst: e.matmul(pst[:, 0:64], mak[h][:, 0:128], vT[:, c, hc], start=True, stop=False),
                          reads=[MAK[h], VT[c]], writes=[PX])
                    kb.op("pe", lambda e, hp=hp, rows=rows, c=c, so=so, pst=pst: e.matmul(pst[:, 0:64], ar[hp][rows, c, 0:CH], stt[hp][so][rows, :], start=False, stop=True),
                          reads=[AR[hp], So], writes=[PX])
                    kb.op("act", lambda e, h=h, pst=pst: e.activation(out=xsb[h][:], in_=pst[:, 0:64], func=AF.Copy), reads=[PX], writes=[XSB[h]])
                    kb.op("pe", lambda e, h=h, pst=pst: e.matmul(pst[:, 64:128], qq[h][qf][:], xsb[h][:], start=True, stop=True), reads=[QQ[h][qf], XSB[h]], writes=[PU])
                    kb.op("act", lambda e, h=h, pst=pst: e.activation(out=usb[h][:], in_=pst[:, 64:128], func=AF.Copy), reads=[PU], writes=[USB[h]])
                    yo = slice(256 + hp * 64, 256 + (hp + 1) * 64)
                    kb.op("pe", lambda e, hp=hp, rows=rows, c=c, so=so, yo=yo, pst=pst: e.matmul(pst[:, yo], ar[hp][rows, c, CH:2 * CH], stt[hp][so][rows, :], start=True, stop=False),
                          reads=[AR[hp], So], writes=[PY])
                    kb.op("pe", lambda e, h=h, yo=yo, pst=pst: e.matmul(pst[:, yo], mab[h][:, 128:256], usb[h][:], start=False, stop=False), reads=[MAB[h], USB[h]], writes=[PY])
                    kb.op("pe", lambda e, h=h, c=c, hc=hc, yo=yo, pst=pst: e.matmul(pst[:, yo], mak[h][:, 128:256], vT[:, c, hc], start=False, stop=True),
                          reads=[MAK[h], VT[c]], writes=[PY])
                    so_ = slice(128 + 64 * hh, 128 + 64 * hh + 64)
                    kb.op("pe", lambda e, h=h, c=c, hc=hc, rows=rows, pst=pst: e.matmul(pst[rows, 128:192], bhT[:, c, hc], usb[h][:], start=True, stop=False),
                          reads=[BHT[c], USB[h]], writes=[PS_])
                    kb.op("pe", lambda e, h=h, c=c, hc=hc, rows=rows, pst=pst: e.matmul(pst[rows, 128:192], khT[:, c, hc], vT[:, c, hc], start=False, stop=True),
                          reads=[KHT[c], VT[c]], writes=[PS_])
                    kb.op("dve", lambda e, hp=hp, rows=rows, c=c, so=so, sn=sn, pst=pst: e.scalar_tensor_tensor(
                        stt[hp][sn][rows, :], stt[hp][so][rows, :], wc[hp][rows, c:c + 1], pst[rows, 128:192], ALU.mult, ALU.add),
                        reads=[So, WC[hp], PS_], writes=[Sn])
                    if hh == 1:
                        ping[hp] = sn
                for hh in range(2):
                    ypv = psts[hh][:, 256:384].rearrange("p (hp v) -> p hp v", v=64)
                    kb.op("act", lambda e, hh=hh, ypv=ypv: e.activation(out=ytok[:].rearrange("p (hp hh v) -> p hp hh v", hh=2, v=64)[:, :, hh, :], in_=ypv, func=AF.Copy),
                          reads=[PSTB[hh]], writes=[YTOK])
                    kb.op("act", lambda e, hh=hh, ypv=ypv: e.activation(out=ysq[:].rearrange("p (hp hh v) -> p hp hh v", hh=2, v=64)[:, :, hh, :], in_=ypv, func=AF.Square),
                          reads=[PSTB[hh]], writes=[YSQ])
                kb.op("dve", lambda e: e.tensor_reduce(sts[:, 0:4], ytok[:].rearrange("p (h v) -> p h v", v=64), AX.X, ALU.add), reads=[YTOK], writes=[STS])
                kb.op("dve", lambda e: e.tensor_reduce(sts[:, 4:8], ysq[:].rearrange("p (h v) -> p h v", v=64), AX.X, ALU.add), reads=[YSQ, STS], writes=[STS])
                kb.op("dve", lambda e: e.tensor_scalar(sts[:, 8:12], sts[:, 0:4], 1.0 / 64, None, ALU.mult), reads=[STS], writes=[STS])
                kb.op("dve", lambda e: e.tensor_tensor(sts[:, 12:16], sts[:, 8:12], sts[:, 8:12], ALU.mult), reads=[STS], writes=[STS])
                kb.op("dve", lambda e: e.scalar_tensor_tensor(sts[:, 16:20], sts[:, 4:8], 1.0 / 64, sts[:, 12:16], ALU.mult, ALU.subtract),
                      reads=[STS], writes=[STS])
                kb.op("dve", lambda e: e.tensor_scalar(sts[:, 16:20], sts[:, 16:20], GN_EPS, None, ALU.add), reads=[STS], writes=[STS])
                kb.op("act", lambda e: e.activation(out=sts[:, 20:24], in_=sts[:, 16:20], func=AF.Sqrt), reads=[STS], writes=[STS])
                kb.op("dve", lambda e: e.reciprocal(sts[:, 24:28], sts[:, 20:24]), reads=[STS], writes=[STS])
                kb.op("dve", lambda e: e.tensor_tensor(yn[:].rearrange("p (h v) -> p h v", v=64), ytok[:].rearrange("p (h v) -> p h v", v=64),
                                                       sts[:, 8:12].unsqueeze(2).to_broadcast([128, 4, 64]), ALU.subtract), reads=[YTOK, STS], writes=[YN])
                kb.op("dve", lambda e: e.tensor_tensor(yn[:].rearrange("p (h v) -> p h v", v=64), yn[:].rearrange("p (h v) -> p h v", v=64),
                                                       sts[:, 24:28].unsqueeze(2).to_broadcast([128, 4, 64]), ALU.mult), reads=[YN, STS], writes=[YN])
                if c == 0:
                    dbg("ytok", ytok[:], YTOK); dbg("yn", yn[:], YN); dbg("sts", sts[:, 0:28], STS)
                    for h in range(4):
                        dbg(f"x{h}", xsb[h][:], XSB[h]); dbg(f"u{h}", usb[h][:], USB[h])
                    dbg("vT0", vT[:, 0, :], VT[0]); dbg("bhT0", bhT[:, 0, :], BHT[0]); dbg("khT0", khT[:, 0, :], KHT[0])
                    for hp in range(2):
                        dbg(f"st{hp}", stt[hp][ping[hp]][:], STT[hp][ping[hp]][0])
                for hp in range(2):
                    pbank, PT = next_pa()
                    pt = pbank[:, 0:128]
                    kb.op("pe", lambda e, pt=pt, hp=hp: e.transpose(pt, yn[:, hp * 128:(hp + 1) * 128], ident[:]), reads=[YN, IDENT], writes=[PT])
                    kb.op("dve", lambda e, pt=pt, hp=hp, csl=csl: e.tensor_scalar(yf[hp][:, csl], pt, vec[:, 18 + hp:19 + hp], vec[:, 20 + hp:21 + hp], ALU.mult, ALU.add),
                          reads=[PT, VEC], writes=[YF[hp]])
            for hp in range(2):
                kb.op("pool", lambda e, hp=hp: e.tensor_tensor(osb[hp][:], yf[hp][:], bon[hp][:], ALU.add), reads=[YF[hp], BON[hp]], writes=[OSB[hp]])
                kb.op("pool", lambda e, hp=hp: e.tensor_tensor(osb[hp][:], osb[hp][:], gg[hp][:], ALU.mult), reads=[OSB[hp], GG[hp]], writes=[OSB[hp]])
                kb.dma("sp", f"out{hp}", lambda e, s, hp=hp, sgi=sgi: e.dma_start(out=yT_d[hp * 128:(hp + 1) * 128, sgi * NS:(sgi + 1) * NS], in_=osb[hp][:]).then_inc(s, 16),
                       reads=[OSB[hp]])
        kb.finish(OSB)


def build_a1(nseg=NSEG, debug=False):
    nc = bass.Bass("TRN2", target_bir_lowering=False)
    I = lambda n, shp: nc.dram_tensor(n, shp, F32, kind="ExternalInput").ap()
    D = dict(xT=I("xT", [1024, S]), w=I("w", [1024, 1024]), vec=I("vec", [128, 22]), lw=I("lw", [128, 256]), g2=I("g2", [128, 256]),
             cst=I("cst", [128, 1024]), yT=nc.dram_tensor("yT", [256, S], F32, kind="ExternalOutput").ap())
    with ExitStack() as st:
        kb = KB(nc, st)
        a1_body(nc, kb, D, nseg, debug)
        kb.emit()
    return nc


def a1_consts():
    m1 = np.ones((128, 512), np.float32); m1[:, ::CH] = 0.0
    s = np.arange(128)[:, None]; t = np.arange(128)[None, :]
    msu = (t > s).astype(np.float32); miu = (t >= s).astype(np.float32)
    msl = (t < s).astype(np.float32)
    bones = (s // 64 == t // 64).astype(np.float32)
    return np.concatenate([m1, msu, miu, msl, bones], axis=1)


def a1_inputs(inp, l, b, hh):
    ch = slice(256 * hh, 256 * hh + 256)
    w_in = inp["w_in"][l]
    w = np.concatenate([w_in[:, 0:512][:, ch], w_in[:, 512:1024][:, ch], w_in[:, 1024:1536][:, ch], w_in[:, 1536:1792]], axis=1)
    mu = inp["shift_mu"][l]
    mu_cols = np.concatenate([mu[0:512][ch], mu[512:1024][ch], mu[1024:1536][ch], mu[1536:1792]])
    vec = np.zeros((128, 22), np.float32)
    vec[:, 0:8] = mu_cols.reshape(8, 128).T
    def two(v):
        return v[ch].reshape(2, 128).T
    vec[:, 8:10] = two(inp["rw_w0"][l]); vec[:, 10:12] = two(inp["rw_a0"][l]); vec[:, 12:14] = two(inp["rw_kk"][l])
    vec[:, 14:16] = two(inp["rw_ka"][l]); vec[:, 16:18] = two(inp["rw_rk"][l].reshape(512))
    vec[:, 18:20] = two(inp["rw_ln_g"][l]); vec[:, 20:22] = two(inp["rw_ln_b"][l])
    lw = np.concatenate([inp["rw_w2"][l][:, ch], inp["rw_a2"][l][:, ch]], axis=0)
    g2 = inp["rw_g2"][l][:, ch]
    return dict(w=np.ascontiguousarray(w), vec=vec, lw=np.ascontiguousarray(lw), g2=np.ascontiguousarray(g2), cst=a1_consts())


import math

S = 4096
NEG = -30000.0
TW = 2304
DCL = 1280


def t5_bucket(n):
    n = np.maximum(n, 0); me = 16
    nf = np.maximum(n, 1).astype(np.float32)
    large = me + (np.log(nf / np.float32(me)) / np.float32(math.log(1024 / me)) * np.float32(32 - me)).astype(np.int32)
    large = np.minimum(large, 31)
    return np.where(n < me, n, large)


class Em:
    def __init__(self, kb):
        self.kb = kb

    def mm(self, out, lhsT, rhs, start, stop, reads, writes):
        self.kb.op("pe", lambda e, o=out, a=lhsT, b=rhs, s=start, t=stop: e.matmul(o, a, b, start=s, stop=t), reads, writes)

    def tr(self, out, in_, ident, reads, writes):
        self.kb.op("pe", lambda e, o=out, a=in_, b=ident: e.transpose(o, a, b), reads, writes)

    def act(self, out, in_, func, reads, writes, **kw):
        self.kb.op("act", lambda e, o=out, i=in_, f=func, kw=kw: e.activation(out=o, in_=i, func=f, **kw), reads, writes)

    def tt(self, eng, out, a, b, op, reads, writes):
        self.kb.op(eng, lambda e, o=out, a=a, b=b, op=op: e.tensor_tensor(o, a, b, op), reads, writes)

    def ts(self, eng, out, a, s1, s2, op0, op1, reads, writes):
        if op1 is None:
            self.kb.op(eng, lambda e, o=out, a=a, s1=s1, op0=op0: e.tensor_scalar(o, a, s1, None, op0), reads, writes)
        else:
            self.kb.op(eng, lambda e, o=out, a=a, s1=s1, s2=s2, op0=op0, op1=op1: e.tensor_scalar(o, a, s1, s2, op0, op1), reads, writes)

    def stt(self, eng, out, a, s, b, op0, op1, reads, writes):
        self.kb.op(eng, lambda e, o=out, a=a, s=s, b=b, op0=op0, op1=op1: e.scalar_tensor_tensor(o, a, s, b, op0, op1), reads, writes)

    def cp(self, eng, out, in_, reads, writes):
        self.kb.op(eng, lambda e, o=out, i=in_: e.tensor_copy(o, i), reads, writes)

    def ms(self, eng, out, val, writes):
        self.kb.op(eng, lambda e, o=out, v=val: e.memset(o, v), (), writes)

    def red(self, out, in_, op, reads, writes):
        self.kb.op("dve", lambda e, o=out, i=in_, op=op: e.tensor_reduce(o, i, AX.X, op), reads, writes)

    def rcp(self, out, in_, reads, writes):
        self.kb.op("dve", lambda e, o=out, i=in_: e.reciprocal(o, i), reads, writes)

    def dma(self, q, key, out, in_, reads, writes):
        self.kb.dma(q, key, lambda e, s, o=out, i=in_: e.dma_start(out=o, in_=i).then_inc(s, 16), reads, writes)


def a2_body(nc, kb, D, nq=8, debug=False):
    xT_d, wf_d, wt_d, w1_d, pe_d, w2_d, btc_d, mkc_d, bts_d, mks_d, mkw_d, m12_d, rv_d, c2s_d, exd_d, hsel_d = (D[k] for k in ['xT', 'wf', 'wt', 'w1', 'peT', 'w2', 'btc', 'mkc', 'bts', 'mks', 'mkw', 'm12', 'rv', 'c2s', 'exd', 'hsel'])
    yT_d = D["yT"]
    xT_v = xT_d.rearrange("(kc p) t -> p kc t", p=128)
    if True:
        em = Em(kb)
        sb, ps = kb.sb, kb.ps
        xs = [sb(f"xs{i}", [128, 8, 512], BF16) for i in range(2)]; XS = [Buf() for _ in range(2)]
        wf = sb("wf", [128, 8, 640], BF16); WF = Buf()
        wt = sb("wt", [128, 8, 256], BF16); WT = Buf()
        w1 = sb("w1", [128, 32, 128], BF16); W1 = Buf()
        peT = sb("peT", [128, 32], BF16); PET = Buf()
        w2f = sb("w2f", [128, 192]); w2 = sb("w2", [128, 192], BF16); W2 = Buf()
        qT = [sb(f"qT{i}", [128, S], BF16) for i in range(2)]; QT = [Buf() for _ in range(2)]
        ksT = sb("ksT", [128, S], BF16); KST = Buf()
        kwT = sb("kwT", [128, S], BF16); KWT = Buf()
        kcv = sb("kcv", [128, S], BF16); KCV = Buf()
        vs = sb("vs", [128, 32, 96], BF16); VS = Buf()
        vw = sb("vw", [128, 32, 96], BF16); VW = Buf()
        gt = sb("gt", [128, 32, 16]); GT = Buf()
        btc = sb("btc", [128, 4, 256]); BTC = Buf()
        mkc = sb("mkc", [128, 256]); MKC = Buf()
        HW_ = TW // 2
        stg = sb("stg", [128, HW_]); STG = Buf()
        stg2 = sb("stg2", [128, HW_]); STG2 = Buf()
        mks = sb("mks", [128, HW_]); MKS = Buf()
        mkw = sb("mkw", [128, HW_]); MKW = Buf()
        ebs = [sb(f"ebs{h}", [128, TW], BF16) for h in range(4)]; EBS = [Buf() for _ in range(4)]
        ebw = [sb(f"ebw{h}", [128, TW], BF16) for h in range(4)]; EBW = [Buf() for _ in range(4)]
        m12 = sb("m12", [128, 256]); M12 = Buf()
        rv = sb("rv", [128, 1]); RV = Buf()
        vca = sb("vca", [128, 2, 128], BF16); VCA = Buf()
        c2sf = sb("c2sf", [128, 128]); C2SF = Buf()
        exd = sb("exd", [128, 32, 128], BF16); EXD = Buf()
        hself = sb("hself", [128, 256]); hsel = sb("hsel", [128, 2, 128], BF16); HSEL = Buf()
        identf = sb("identf", [128, 128]); ident = sb("ident", [128, 128], BF16); IDENT = Buf()
        kct = sb("kct", [128, 256], BF16); KCT = Buf()
        gh = [sb(f"gh{i}", [128, 256], BF16) for i in range(2)]; GH = [Buf() for _ in range(2)]
        hx = sb("hx", [128, 256]); HX = Buf()
        hy = sb("hy", [128, 256]); HY = Buf()
        hb = sb("hb", [128, 2]); HB = Buf()
        sm = sb("sm", [128, 64]); SM = Buf()
        cst = sb("cst", [128, 16]); CST = Buf()
        qsq = sb("qsq", [128, 512], BF16); QSQ = Buf()
        mxc = sb("mxc", [128, 4, 8]); MXC = Buf()
        kxc = sb("kxc", [128, 2, 8]); KXC = Buf()
        lg = sb("lg", [128, 256]); LG = Buf()
        pc = sb("pc", [128, 256], BF16); PC = Buf()
        pct = sb("pct", [128, 2, 128], BF16); PCT = Buf()
        sc = sb("sc", [128, 64]); SC = Buf()
        sc2 = sb("sc2", [128, 64]); SC2 = Buf()
        mx8 = sb("mx8", [128, 16]); MX8 = Buf()
        nst = sb("nst", [128, 64], BF16); NST = Buf()
        nsh = sb("nsh", [128, 512], BF16); NSH = Buf()
        yg = sb("yg", [128, 4, 256]); YG = Buf()
        ygt = sb("ygt", [128, 2, 512]); YGT = Buf()
        pt = [sb(f"pt{i}", [128, 512], BF16) for i in range(3)]; PT = [Buf() for _ in range(3)]
        p2 = [sb(f"p2{i}", [128, 512], BF16) for i in range(3)]; P2 = [Buf() for _ in range(3)]
        rin = sb("rin", [128, 8]); RIN = Buf()
        zb = sb("zb", [128, 512], BF16); ZB = Buf()
        otmp = sb("otmp", [128, 4, 64]); OTMP = Buf()
        pg = [ps(f"pg{i}", [128, 512]) for i in range(3)]; PG = [Buf(excl=True) for _ in range(3)]
        ptb = ps("ptb", [128, 1024], BF16); PTB = Buf(excl=True)
        pl = [ps(f"pl{i}", [128, 512]) for i in range(2)]; PL = [Buf(excl=True) for _ in range(2)]
        pacc = [ps(f"pacc{i}", [128, 4, 128]) for i in range(2)]; PACC = [Buf(excl=True) for _ in range(2)]

        gi = [0]

        def gbank():
            i = gi[0] % 3; gi[0] += 1
            return pg[i], PG[i]

        dbg_n = [0]

        def dbg(name, ap, B):
            if not debug:
                return
            d = nc.dram_tensor("dbg_" + name, list(ap.shape), F32, kind="ExternalOutput").ap()
            dbg_n[0] += 1
            cntv = dbg_n[0] * 16

            def f(e, s, d=d, ap=ap, cntv=cntv):
                e.dma_start(out=d, in_=ap).then_inc(s, 16)
                e.wait_ge(s, cntv)
            kb.dma("pool", "dbg", f, reads=[B])

        em.dma("pool", "wf", wf[:, :, :], wf_d.rearrange("(kc p) n -> p kc n", p=128), [], [WF])
        em.dma("pool", "wt", wt[:, :, 0:140], wt_d.rearrange("(kc p) n -> p kc n", p=128), [], [WT])
        em.dma("pool", "w1", w1[:, :, :], w1_d.rearrange("p (a b) -> p a b", b=128), [], [W1])
        em.dma("pool", "pe", peT[:], pe_d[:, :], [], [PET])
        em.dma("sp", "w2", w2f[:], w2_d[:, :], [], [W2])
        em.dma("sp", "btc", btc[:].rearrange("p h w -> p (h w)"), btc_d[:, :], [], [BTC])
        em.dma("sp", "mkc", mkc[:], mkc_d[:, :], [], [MKC])
        em.dma("sp", "m12", m12[:], m12_d[:, :], [], [M12])
        em.dma("sp", "rv", rv[:], rv_d[:, :], [], [RV])
        em.dma("sp", "c2s", c2sf[:], c2s_d[:, :], [], [C2SF])
        em.ms("pool", exd[:], 0.0, [EXD])
        em.dma("pool", "exd", exd[0:64, :, :].rearrange("p a b -> p (a b)"), exd_d[:, :], [EXD], [EXD])
        em.dma("sp", "hsel", hself[:], hsel_d[:, :], [], [HSEL])
        em.cp("dve", w2[:], w2f[:], [W2], [W2])
        em.cp("dve", hsel[:].rearrange("p a b -> p (a b)"), hself[:], [HSEL], [HSEL])
        em.ms("pool", identf[:], 1.0, [IDENT])
        kb.op("pool", lambda e: e.affine_select(out=identf[:], in_=identf[:], pattern=[[-1, 128]], compare_op=ALU.is_equal,
                                                fill=0.0, base=0, channel_multiplier=1), [IDENT], [IDENT])
        em.cp("pool", ident[:], identf[:], [IDENT], [IDENT])
        em.ms("dve", vs[:, :, 64:65], 1.0, [VS])
        em.ms("dve", vw[:, :, 64:65], 1.0, [VW])
        em.ms("dve", vca[:], 0.0, [VCA])
        em.ms("dve", zb[:], 0.0, [ZB])
        em.ms("dve", nsh[:], 0.0, [NSH])
        em.ms("dve", pc[:], 0.0, [PC])
        em.ms("dve", kct[:], 0.0, [KCT])
        for h in range(4):
            em.tt("dve", btc[:, h, :], btc[:, h, :], mkc[:], ALU.add, [BTC, MKC], [BTC])
        first = True
        for hf in range(2):
            cs_ = slice(hf * HW_, (hf + 1) * HW_)
            em.dma("sp", "mks", mks[:], mks_d[:, cs_], [], [MKS])
            em.dma("sp", "mkw", mkw[:], mkw_d[:, cs_], [], [MKW])
            for h in range(4):
                em.dma("sp", "stg", stg[:], bts_d[:, h * TW + hf * HW_:h * TW + (hf + 1) * HW_], [], [STG])
                if first:
                    em.red(cst[:, 8:9], stg[:], ALU.max, [STG], [CST])
                    first = False
                else:
                    em.red(cst[:, 9:10], stg[:], ALU.max, [STG], [CST])
                    em.tt("dve", cst[:, 8:9], cst[:, 8:9], cst[:, 9:10], ALU.max, [CST], [CST])
                em.tt("dve", stg2[:], stg[:], mks[:], ALU.add, [STG, MKS], [STG2])
                em.act(ebs[h][:, cs_], stg2[:], AF.Exp, [STG2], [EBS[h]])
                em.tt("dve", stg2[:], stg[:], mkw[:], ALU.add, [STG, MKW], [STG2])
                em.act(ebw[h][:, cs_], stg2[:], AF.Exp, [STG2], [EBW[h]])

        import os as _os
        PH = int(_os.environ.get('PH', '9'))
        def load_x(sg):
            sl = sg % 2
            em.dma("pool", f"xs{sl}", xs[sl][:, :, :], xT_v[:, :, sg * 512:(sg + 1) * 512], [], [XS[sl]])

        load_x(0)
        for sg in range(8 if PH >= 2 else 0):
            sl = sg % 2
            if sg + 1 < 8:
                load_x(sg + 1)
            seg = slice(sg * 512, (sg + 1) * 512)
            dsts = [(qT[0], QT[0], 0.125), (qT[1], QT[1], 0.125), (ksT, KST, 1.0), (kwT, KWT, 1.0), (kcv, KCV, 1.0)]
            for j, (dst, DST, scl) in enumerate(dsts):
                p, P = gbank()
                for k in range(8):
                    em.mm(p[:, :], wf[:, k, j * 128:(j + 1) * 128], xs[sl][:, k, :], k == 0, k == 7, [WF, XS[sl]], [P])
                em.act(dst[:, seg], p[:, :], AF.Copy, [P], [DST], scale=scl)
            PJ = _os.environ.get('PJ', 'abc12')
            for tt_ in range(4 if 'b' in PJ else 0):
                tile_i = sg * 4 + tt_
                p, P = gbank()
                for k in range(8):
                    em.mm(p[:, 0:140], xs[sl][:, k, tt_ * 128:(tt_ + 1) * 128], wt[:, k, 0:140], k == 0, k == 7, [WT, XS[sl]], [P])
                if '1' in PJ:
                    em.cp("dve", vs[:, tile_i, 0:64], p[:, 0:64], [P], [VS])
                    em.cp("dve", vw[:, tile_i, 0:64], p[:, 64:128], [P], [VW])
                if '2' in PJ:
                    em.act(gt[:, tile_i, 0:12], p[:, 128:140], AF.Sigmoid, [P], [GT])
            for i in range(2 if 'c' in PJ else 0):
                em.tt("dve", qsq[:], qT[i][:, seg], qT[i][:, seg], ALU.mult, [QT[i]], [QSQ])
                for hh in range(2):
                    p, P = gbank()
                    em.mm(p[:, :], hsel[:, hh, :], qsq[:], True, True, [HSEL, QSQ], [P])
                    em.red(mxc[:, 2 * i + hh, sg:sg + 1], p[:, :], ALU.max, [P], [MXC])
            for i, (src, SRC) in enumerate(((ksT, KST), (kwT, KWT)) if 'c' in PJ else ()):
                em.tt("dve", qsq[:], src[:, seg], src[:, seg], ALU.mult, [SRC], [QSQ])
                p, P = gbank()
                em.mm(p[:, :], hsel[:, 0, :], qsq[:], True, True, [HSEL, QSQ], [P])
                em.red(kxc[:, i, sg:sg + 1], p[:, :], ALU.max, [P], [KXC])
        if PH < 3:
            nq = 0
        em.red(sm[:, 0:4], mxc[:], ALU.max, [MXC], [SM])
        em.red(sm[:, 4:6], kxc[:], ALU.max, [KXC], [SM])
        for br in range(2):
            em.ts("dve", sm[:, 8 + 4 * br:12 + 4 * br], sm[:, 0:4], sm[:, 4 + br:5 + br], None, ALU.mult, None, [SM], [SM])
        em.act(sm[:, 16:24], sm[:, 8:16], AF.Sqrt, [SM], [SM])
        em.ts("dve", cst[:, 0:8], sm[:, 16:24], cst[:, 8:9], -1.0, ALU.add, ALU.mult, [SM, CST], [CST])

        for br in range(2 if PH >= 3 else 0):
            rows = slice(64 * br, 64 * br + 64)
            p, P = gbank()
            for pp in range(32):
                em.mm(p[:, 0:255], w1[rows, pp, :], kcv[rows, pp:pp + 16 * 254 + 1:16], pp == 0, pp == 31, [W1, KCV], [P])
            pb, PB = gbank()
            for pp in range(32):
                em.mm(pb[:, 0:1], w1[rows, pp, :], peT[rows, pp:pp + 1], pp == 0, pp == 31, [W1, PET], [PB])
            em.cp("dve", hb[:, br:br + 1], pb[:, 0:1], [PB], [HB])
            em.ts("dve", hx[:, 0:255], p[:, 0:255], hb[:, br:br + 1], None, ALU.add, None, [P, HB], [HX])
            em.tt("dve", hy[:, 0:255], hx[:, 0:255], hx[:, 0:255], ALU.mult, [HX], [HY])
            em.ts("dve", hy[:, 0:255], hy[:, 0:255], 0.044715, 1.0, ALU.mult, ALU.add, [HY], [HY])
            em.tt("dve", hy[:, 0:255], hy[:, 0:255], hx[:, 0:255], ALU.mult, [HY, HX], [HY])
            em.act(hy[:, 0:255], hy[:, 0:255], AF.Tanh, [HY], [HY], scale=0.7978845608028654)
            em.ts("dve", hy[:, 0:255], hy[:, 0:255], 1.0, 0.5, ALU.add, ALU.mult, [HY], [HY])
            em.tt("dve", gh[br][:, 0:255], hy[:, 0:255], hx[:, 0:255], ALU.mult, [HY, HX], [GH[br]])
        p, P = gbank()
        em.mm(p[:, 0:255], w2[:, 0:128], gh[0][:, 0:255], True, True, [W2, GH[0]], [P])
        em.act(kct[:, 0:255], p[:, 0:255], AF.Copy, [P], [KCT])
        for ct in range(2):
            ncs = 128 if ct == 0 else 127
            p, P = gbank()
            em.mm(p[0:ncs, 0:64], gh[1][:, ct * 128:ct * 128 + ncs], w2[:, 128:192], True, True, [W2, GH[1]], [P])
            em.act(vca[0:ncs, ct, 0:64], p[0:ncs, 0:64], AF.Copy, [P], [VCA])
        em.cp("dve", vca[:, :, 64:128], c2sf[:].rearrange("p (a b) -> p a b", b=64), [C2SF], [VCA])
        dbg("kct", kct[:, 0:255], KCT); dbg("vca", vca[:].rearrange("p a b -> p (a b)"), VCA)
        dbg("cst", cst[:, 0:9], CST)

        pti = [0]
        for Q in range(nq):
            qs = slice(Q * 512, (Q + 1) * 512)
            for a in range(4):
                n = 4 * Q + a
                t0 = 128 * n
                ncol = min(255, 8 * n + 7)
                off = 248 - 8 * n
                for h in range(4):
                    rows = slice(64 * (h % 2), 64 * (h % 2) + 64)
                    p, P = gbank()
                    ncp = min(256, (ncol + 31) // 32 * 32)
                    em.mm(p[:, 0:ncp], qT[h // 2][rows, t0:t0 + 128], kct[rows, 0:ncp], True, True, [QT[h // 2], KCT], [P])
                    em.tt("dve", lg[:, 0:ncol], p[:, 0:ncol], btc[:, h, off:off + ncol], ALU.add, [P, BTC], [LG])
                    em.red(sm[:, 32:33], lg[:, 0:ncol], ALU.max, [LG], [SM])
                    em.ts("dve", sm[:, 33:34], sm[:, 32:33], -1.0, None, ALU.mult, None, [SM], [SM])
                    em.ms("dve", sm[:, 34:35], 0.0, [SM])
                    em.act(pc[:, 0:ncol], lg[:, 0:ncol], AF.Exp, [LG, SM], [PC, SM], bias=sm[:, 33:34], accum_out=sm[:, 34:35])
                    nct = 1 if ncol <= 128 else 2
                    for ct in range(nct):
                        em.tr(ptb[:, ct * 128:(ct + 1) * 128], pc[:, ct * 128:(ct + 1) * 128], ident[:], [PC, IDENT], [PTB])
                    for ct in range(nct):
                        em.cp("dve", pct[:, ct, :], ptb[:, ct * 128:(ct + 1) * 128], [PTB], [PCT])
                    po, PO = gbank()
                    for ct in range(nct):
                        em.mm(po[:, 0:128], pct[:, ct, :], vca[:, ct, :], ct == 0, ct == nct - 1, [PCT, VCA], [PO])
                    em.rcp(sm[:, 35:36], sm[:, 34:35], [SM], [SM])
                    if n == 0:
                        em.tt("dve", sm[:, 35:36], sm[:, 35:36], rv[:], ALU.mult, [SM, RV], [SM])
                    em.tt("dve", sm[:, 36:37], sm[:, 35:36], gt[:, n, 3 * h:3 * h + 1], ALU.mult, [SM, GT], [SM])
                    em.ts("dve", yg[:, a, h * 64:(h + 1) * 64], po[:, 0:64], sm[:, 36:37], None, ALU.mult, None, [PO, SM], [YG])
                    if h == 0:
                        em.ts("dve", sc[:], po[:, 64:128], sm[:, 35:36], None, ALU.mult, None, [PO, SM], [SC])
                    else:
                        em.stt("dve", sc[:], po[:, 64:128], sm[:, 35:36], sc[:], ALU.mult, ALU.add, [PO, SM, SC], [SC])
                w0 = 64 - 2 * n
                em.tt("dve", sc[:], sc[:], m12[:, w0:w0 + 64], ALU.mult, [SC, M12], [SC])
                em.tt("dve", sc[:], sc[:], m12[:, 128 + w0:128 + w0 + 64], ALU.add, [SC, M12], [SC])
                em.ms("dve", sc[:, 0:1], 1.0e4, [SC])
                kb.op("dve", lambda e: e.max(out=mx8[:, 0:8], in_=sc[:]), [SC], [MX8])
                kb.op("dve", lambda e: e.match_replace(out=sc2[:], in_to_replace=mx8[:, 0:8], in_values=sc[:], imm_value=-1.0e9), [SC, MX8], [SC2])
                kb.op("dve", lambda e: e.max(out=mx8[:, 8:16], in_=sc2[:]), [SC2], [MX8])
                em.red(sm[:, 40:41], mx8[:, 8:16], ALU.min, [MX8], [SM])
                em.ts("dve", nst[:], sc[:], sm[:, 40:41], NEG, ALU.is_lt, ALU.mult, [SC, SM], [NST])
                em.tr(ptb[0:64, 256:384], nst[:], ident[:], [NST, IDENT], [PTB])
                em.cp("dve", nsh[0:64, a * 128:(a + 1) * 128], ptb[0:64, 256:384], [PTB], [NSH])
                if Q == 0 and a == 1:
                    dbg("sc", sc[:], SC); dbg("yg1", yg[:, 1, :], YG)
            QP = _os.environ.get('QP', 'abc')
            for br in [b_ for b_ in range(2) if 'bc'[b_] in QP]:
                kT, KT_, vv, VV, eb, EB = (ksT, KST, vs, VS, ebs, EBS) if br == 0 else (kwT, KWT, vw, VW, ebw, EBW)
                m_lo = 0 if br == 0 else max(0, 4 * Q - 4)
                m_hi = 4 * Q + 3
                for h in range(4):
                    rows = slice(64 * (h % 2), 64 * (h % 2) + 64)
                    acc, ACC = pacc[h % 2], PACC[h % 2]
                    em.mm(acc[:].rearrange("p a b -> p (a b)"), zb[:, 0:128], zb[:, 0:512], True, False, [ZB], [ACC])
                    for m in range(m_lo, m_hi + 1):
                        D0 = 512 * Q - 128 * m
                        wst = min(D0, DCL) + 512
                        pli = pti[0] % 2
                        pl_, PL_ = pl[pli], PL[pli]
                        bi = pti[0] % 3
                        pti[0] += 1
                        em.mm(pl_[:, :], kT[rows, m * 128:(m + 1) * 128], qT[h // 2][rows, qs], True, br == 1, [KT_, QT[h // 2]], [PL_])
                        if br == 0:
                            em.mm(pl_[:, :], exd[:, m, :], nsh[:, :], False, True, [EXD, NSH], [PL_])
                        em.act(pt[bi][:], pl_[:, :], AF.Exp, [PL_, CST], [PT[bi]], bias=cst[:, 4 * br + h:4 * br + h + 1])
                        em.tt("dve", p2[bi][:], pt[bi][:], eb[h][:, wst:wst + 512], ALU.mult, [PT[bi], EB[h]], [P2[bi]])
                        for a in range(4):
                            last_m = min(m_hi, 4 * Q + a)
                            if m > last_m:
                                continue
                            a_lo = m_lo if br == 0 else max(m_lo, 4 * Q + a - 4)
                            if m < a_lo:
                                continue
                            em.mm(acc[:, a, 0:65], p2[bi][:, a * 128:(a + 1) * 128], vv[:, m, 0:65], False, (m == m_hi and a == 3), [P2[bi], VV], [ACC])
                    em.rcp(rin[:, 0:4], acc[:, :, 64], [ACC], [RIN])
                    em.tt("dve", rin[:, 4:8], rin[:, 0:4], gt[:, 4 * Q:4 * Q + 4, 3 * h + 1 + br], ALU.mult, [RIN, GT], [RIN])
                    em.tt("dve", otmp[:], acc[:, :, 0:64], rin[:, 4:8].unsqueeze(2).to_broadcast([128, 4, 64]), ALU.mult, [ACC, RIN], [OTMP])
                    em.tt("pool", yg[:, :, h * 64:(h + 1) * 64], yg[:, :, h * 64:(h + 1) * 64], otmp[:], ALU.add, [YG, OTMP], [YG])
            for a in range(4):
                for hp in range(2):
                    pz, PZ = gbank()
                    em.tr(pz[:, 0:128], yg[:, a, hp * 128:(hp + 1) * 128], identf[:], [YG, IDENT], [PZ])
                    em.cp("dve" if hp else "pool_never", ygt[:, hp, a * 128:(a + 1) * 128], pz[:, 0:128], [PZ], [YGT]) if False else em.cp("dve", ygt[:, hp, a * 128:(a + 1) * 128], pz[:, 0:128], [PZ], [YGT])
            for hp in range(2):
                em.dma("sp", "y", yT_d[hp * 128:(hp + 1) * 128, Q * 512:(Q + 1) * 512], ygt[:, hp, :], [YGT], [])
        kb.finish([YG, YGT])


def build_a2(nq=8, debug=False):
    nc = bass.Bass("TRN2", target_bir_lowering=False)
    I = lambda n, shp: nc.dram_tensor(n, shp, F32, kind="ExternalInput").ap()
    D = {"xT": I("xT", [1024, S]), "wf": I("wf", [1024, 640]), "wt": I("wt", [1024, 140]), "w1": I("w1", [128, 32 * 128]), "peT": I("peT", [128, 32]), "w2": I("w2", [128, 192]), "btc": I("btc", [128, 4 * 256]), "mkc": I("mkc", [128, 256]), "bts": I("bts", [128, 4 * TW]), "mks": I("mks", [128, TW]), "mkw": I("mkw", [128, TW]), "m12": I("m12", [128, 256]), "rv": I("rv", [128, 1]), "c2s": I("c2s", [128, 128]), "exd": I("exd", [64, 32 * 128]), "hsel": I("hsel", [128, 256])}
    D["yT"] = nc.dram_tensor("yT", [256, S], F32, kind="ExternalOutput").ap()
    with ExitStack() as st:
        kb = KB(nc, st)
        a2_body(nc, kb, D, nq, debug)
        kb.emit()
    return nc


def a2_consts():
    i = np.arange(128)[:, None]
    j = np.arange(256)[None, :]
    dc = i - 16 * (j - 248) - 31
    mkc = np.where(dc >= 0, 0.0, NEG).astype(np.float32)
    w = np.arange(TW)[None, :]
    ds = w - i - 512
    mks = np.where(ds >= 0, 0.0, NEG).astype(np.float32)
    mkw = np.where((ds >= 0) & (ds < 512), 0.0, NEG).astype(np.float32)
    wv = np.arange(128)[None, :] - 64
    cur = (i >= 64).astype(np.int64)
    forced = (wv == cur) | (wv == cur - 1)
    valid = wv <= cur
    m1 = (valid & ~forced).astype(np.float32)
    m2 = np.where(forced, 1.0e4, np.where(valid, 0.0, -1.0)).astype(np.float32)
    m12 = np.concatenate([m1, m2], axis=1)
    rv = (np.arange(128) >= 31).astype(np.float32)[:, None]
    cs = np.arange(255)[:, None] * 16; ss = np.arange(64)[None, :] * 64
    ov = np.clip(np.minimum(cs + 32, ss + 64) - np.maximum(cs, ss), 0, None).astype(np.float32) / 32
    c2s = np.zeros((256, 64), np.float32); c2s[:255] = ov
    c2s = c2s.reshape(2, 128, 64).transpose(1, 0, 2).reshape(128, 128)
    exd = np.zeros((64, 32, 128), np.float32)
    for m in range(32):
        exd[2 * m, m, 0:64] = 1.0; exd[2 * m + 1, m, 64:128] = 1.0
    hsel = np.zeros((128, 2, 128), np.float32); hsel[0:64, 0, :] = 1.0; hsel[64:128, 1, :] = 1.0
    return dict(mkc=mkc, mks=mks, mkw=mkw, m12=m12, rv=rv, c2s=c2s, exd=exd.reshape(64, 32 * 128), hsel=hsel.reshape(128, 256),
                dc=dc, ds=ds)


def a2_inputs(inp, l, g, consts):
    w_in = inp["w_in"][l]; zr = 1792
    q = w_in[:, zr + 256 * g: zr + 256 * g + 256]
    def kvc(off):
        return w_in[:, zr + off + 64 * g: zr + off + 64 * g + 64]
    kc, vc, ks, vs, kw, vw = (kvc(o) for o in (512, 640, 768, 896, 1024, 1152))
    gates = w_in[:, zr + 1280 + 12 * g: zr + 1280 + 12 * g + 12]
    wf = np.concatenate([q, ks, ks, kw, kw, kc, vc], axis=1)
    wt = np.concatenate([vs, vw, gates], axis=1)
    def w1r(w):
        return w.reshape(32, 64, 128).transpose(1, 0, 2)
    w1 = np.concatenate([w1r(inp["cmp_w1_k"][l]), w1r(inp["cmp_w1_v"][l])], axis=0).reshape(128, 32 * 128)
    peT = np.concatenate([inp["cmp_pe_k"][l].T, inp["cmp_pe_v"][l].T], axis=0)
    w2 = np.concatenate([inp["cmp_w2_k"][l], inp["cmp_w2_k"][l], inp["cmp_w2_v"][l]], axis=1)
    rb = inp["rel_bias"][:, 4 * g:4 * g + 4]
    btc = np.take(rb, t5_bucket(consts["dc"]), axis=0).transpose(0, 2, 1).reshape(128, 4 * 256)
    bts = np.take(rb, t5_bucket(consts["ds"]), axis=0).transpose(0, 2, 1).reshape(128, 4 * TW)
    out = dict(wf=wf, wt=wt, w1=w1, peT=peT, w2=w2, btc=btc, bts=bts)
    for k in ("mkc", "mks", "mkw", "m12", "rv", "c2s", "exd", "hsel"):
        out[k] = consts[k]
    return {k: np.ascontiguousarray(v, dtype=np.float32) for k, v in out.items()}


NT = 2048
ALPHA = 8 ** 0.25
LN_EPS = 1e-5


def layer_norm_fm(kb, em, gbank, R, RB, out_fn, g_ap, b_ap, GB, tmp, TMP, ones, ONES, sq, SQ, mean, MEAN, rstd, RSTD):
    pm, PM = gbank()
    for i in range(8):
        em.mm(pm[:, :], ones[:], R[:, i, :], i == 0, i == 7, [ONES, RB], [PM])
    em.act(mean[:], pm[:, :], AF.Copy, [PM], [MEAN], scale=1.0 / 1024)
    pv, PV = gbank()
    for i in range(8):
        em.tt("pool" if i % 2 else "dve", sq[i % 2][:], R[:, i, :], R[:, i, :], ALU.mult, [RB], [SQ[i % 2]])
        em.mm(pv[:, :], ones[:], sq[i % 2][:], i == 0, i == 7, [ONES, SQ[i % 2]], [PV])
    em.tt("dve", tmp[:], mean[:], mean[:], ALU.mult, [MEAN], [TMP])
    em.stt("dve", rstd[:], pv[:, :], 1.0 / 1024, tmp[:], ALU.mult, ALU.subtract, [PV, TMP], [RSTD])
    em.ts("dve", rstd[:], rstd[:], LN_EPS, None, ALU.add, None, [RSTD], [RSTD])
    em.act(rstd[:], rstd[:], AF.Sqrt, [RSTD], [RSTD])
    em.rcp(rstd[:], rstd[:], [RSTD], [RSTD])
    for i in range(8):
        eng = "pool" if i % 2 else "dve"
        em.tt(eng, tmp[:], R[:, i, :], mean[:], ALU.subtract, [RB, MEAN], [TMP])
        em.tt(eng, tmp[:], tmp[:], rstd[:], ALU.mult, [TMP, RSTD], [TMP])
        o, O = out_fn(i)
        em.ts(eng, o, tmp[:], g_ap[:, i:i + 1], b_ap[:, i:i + 1], ALU.mult, ALU.add, [TMP, GB], [O])


def b1_body(nc, kb, D):
    xT_d, yr_d, yn_d, wg_d, wur_d, wun_d, wo_d, ln_d, o_d = (D[k] for k in ("xT", "yrT", "ynT", "wg", "wur", "wun", "wo", "ln", "x1T"))
    v3 = lambda ap: ap.rearrange("(kc p) n -> p kc n", p=128)
    if True:
        em = Em(kb); sb, ps = kb.sb, kb.ps
        wg = sb("wg", [128, 8, 2048], BF16); WG = Buf()
        wur = sb("wur", [128, 4, 1024], BF16); WUR = Buf()
        wun = sb("wun", [128, 4, 1024], BF16); WUN = Buf()
        wo = sb("wo", [128, 8, 1024], BF16); WO = Buf()
        ln = sb("ln", [128, 16]); LN = Buf()
        ones = sb("ones", [128, 128]); ONES = Buf()
        xf = sb("xf", [128, 8, 512]); XF = Buf()
        xb = sb("xb", [128, 8, 512], BF16); XB = Buf()
        yr = sb("yr", [128, 4, 512], BF16); YR = Buf()
        yn = sb("yn", [128, 4, 512], BF16); YN = Buf()
        sg = [sb(f"sg{i}", [128, 512]) for i in range(2)]; SG = [Buf() for _ in range(2)]
        m1 = sb("m1", [128, 512]); M1 = Buf()
        mg = sb("mg", [128, 8, 512], BF16); MG = Buf()
        R = sb("R", [128, 8, 512]); RB = Buf()
        ob = sb("ob", [128, 8, 512]); OB = Buf()
        tmp = sb("tmp", [128, 512]); TMP = Buf()
        sq = [sb(f"sq{i}", [128, 512]) for i in range(2)]; SQ = [Buf() for _ in range(2)]
        mean = sb("mean", [128, 512]); MEAN = Buf()
        rstd = sb("rstd", [128, 512]); RSTD = Buf()
        pg = [ps(f"pg{i}", [128, 512]) for i in range(6)]; PG = [Buf(excl=True) for _ in range(6)]
        gi = [0]

        def gbank():
            i = gi[0] % 6; gi[0] += 1
            return pg[i], PG[i]
        for k0 in range(0, 8, 2):
            em.dma("pool", "wg", wg[:, k0:k0 + 2, :], v3(wg_d)[:, k0:k0 + 2, :], [], [WG])
        em.dma("pool", "wur", wur[:, :, :], v3(wur_d), [], [WUR])
        em.dma("pool", "wun", wun[:, :, :], v3(wun_d), [], [WUN])
        em.dma("pool", "wo", wo[:, :, :], v3(wo_d), [], [WO])
        em.dma("sp", "ln", ln[:], ln_d[:, :], [], [LN])
        em.ms("dve", ones[:], 1.0, [ONES])
        for tg in range(NT // 512):
            ts_ = slice(tg * 512, (tg + 1) * 512)
            em.dma("sp", "xf", xf[:, :, :], v3(xT_d)[:, :, ts_], [], [XF])
            em.dma("pool", "yr", yr[:, :, :], v3(yr_d)[:, :, ts_], [], [YR])
            em.dma("pool", "yn", yn[:, :, :], v3(yn_d)[:, :, ts_], [], [YN])
            for i in range(8):
                em.cp("pool" if i % 2 else "dve", xb[:, i, :], xf[:, i, :], [XF], [XB])
            for j in range(8):
                cs = slice(j * 128, (j + 1) * 128)
                for br, (wu, WU, yy, YY) in enumerate(((wur, WUR, yr, YR), (wun, WUN, yn, YN))):
                    p, P = gbank()
                    for k in range(8):
                        em.mm(p[:, :], wg[:, k, br * 1024 + j * 128: br * 1024 + (j + 1) * 128], xb[:, k, :], k == 0, k == 7, [WG, XB], [P])
                    em.act(sg[br][:], p[:, :], AF.Sigmoid, [P], [SG[br]])
                    p2, P2 = gbank()
                    for k in range(4):
                        em.mm(p2[:, :], wu[:, k, cs], yy[:, k, :], k == 0, k == 3, [WU, YY], [P2])
                    if br == 0:
                        em.tt("dve", m1[:], sg[0][:], p2[:, :], ALU.mult, [SG[0], P2], [M1])
                    else:
                        em.tt("dve", sg[1][:], sg[1][:], p2[:, :], ALU.mult, [SG[1], P2], [SG[1]])
                        em.tt("pool", mg[:, j, :], m1[:], sg[1][:], ALU.add, [M1, SG[1]], [MG])
            for i in range(8):
                p, P = gbank()
                for k in range(8):
                    em.mm(p[:, :], wo[:, k, i * 128:(i + 1) * 128], mg[:, k, :], k == 0, k == 7, [WO, MG], [P])
                em.stt("dve", R[:, i, :], xf[:, i, :], ALPHA, p[:, :], ALU.mult, ALU.add, [XF, P], [RB])
            layer_norm_fm(kb, em, gbank, R, RB, lambda i: (ob[:, i, :], OB), ln[:, 0:8], ln[:, 8:16], LN, tmp, TMP, ones, ONES, sq, SQ, mean, MEAN, rstd, RSTD)
            em.dma("sp", "ob", v3(o_d)[:, :, ts_], ob[:, :, :], [OB], [])
        kb.finish([OB])


def build_b1():
    nc = bass.Bass("TRN2", target_bir_lowering=False)
    I = lambda n, shp: nc.dram_tensor(n, shp, F32, kind="ExternalInput").ap()
    D = dict(xT=I("xT", [1024, NT]), yrT=I("yrT", [512, NT]), ynT=I("ynT", [512, NT]), wg=I("wg", [1024, 2048]), wur=I("wur", [512, 1024]),
             wun=I("wun", [512, 1024]), wo=I("wo", [1024, 1024]), ln=I("ln", [128, 16]),
             x1T=nc.dram_tensor("x1T", [1024, NT], F32, kind="ExternalOutput").ap())
    with ExitStack() as st:
        kb = KB(nc, st)
        b1_body(nc, kb, D)
        kb.emit()
    return nc


def b1_inputs(inp, l):
    w_in = inp["w_in"][l]
    lnp = np.concatenate([inp["ln1_g"][l].reshape(8, 128).T, inp["ln1_b"][l].reshape(8, 128).T], axis=1)
    return dict(wg=np.ascontiguousarray(w_in[:, 1792 + 1304: 1792 + 1304 + 2048]), wur=inp["w_up_rwkv"][l], wun=inp["w_up_nsa"][l],
                wo=inp["w_out"][l], ln=np.ascontiguousarray(lnp, dtype=np.float32))


def b2_body(nc, kb, D, nexp=32):
    x1_d, wr_d, br_d, w1_d, w3_d, w2_d, ln_d, selb_d, g2e_d, o_d = (D[k] for k in ("x1T", "wr", "brr", "ew1", "ew3", "ew2", "ln", "selb", "g2e", "x2T"))
    v3 = lambda ap: ap.rearrange("(kc p) n -> p kc n", p=128)
    if True:
        em = Em(kb); sb, ps = kb.sb, kb.ps
        wr = sb("wr", [128, 8, 64]); WR = Buf()
        brr = sb("brr", [128, 36]); BRR = Buf()
        ln = sb("ln", [128, 16]); LN = Buf()
        selb = sb("selb", [32, 32, 128], BF16); SELB = Buf()
        g2e = sb("g2e", [128, 4, 32]); G2E = Buf()
        ones = sb("ones", [128, 128]); ONES = Buf()
        ident = sb("ident", [128, 128]); IDENT = Buf()
        xf = sb("xf", [128, 8, 512]); XF = Buf()
        x1b = sb("x1b", [128, 8, NT], BF16); X1B = [Buf() for _ in range(4)]
        out = sb("out", [128, 8, NT]); OUT = [Buf() for _ in range(4)]
        cwt = sb("cwt", [32, NT], BF16); CWT = [Buf() for _ in range(4)]
        lgt = sb("lgt", [128, 36]); LGT = Buf()
        rs = sb("rs", [128, 64]); RS = Buf()
        em32 = sb("em32", [128, 32]); EM32 = Buf()
        em2 = sb("em2", [128, 32]); EM2 = Buf()
        cw = sb("cw", [128, 32]); CW = Buf()
        w1 = [sb(f"w1_{i}", [128, 8, 512], BF16) for i in range(2)]; W1 = [Buf() for _ in range(2)]
        w3 = [sb(f"w3_{i}", [128, 8, 512], BF16) for i in range(2)]; W3 = [Buf() for _ in range(2)]
        w2 = [sb(f"w2_{i}", [128, 4, 1024], BF16) for i in range(2)]; W2 = [Buf() for _ in range(2)]
        cwb = sb("cwb", [128, 512]); CWB = Buf()
        sl_ = [sb(f"sl{i}", [128, 512]) for i in range(2)]; SL = [Buf() for _ in range(2)]
        hb = sb("hb", [128, 4, 512], BF16); HB = [Buf() for _ in range(4)]
        ob = xf; OB = XF
        tmp = sb("tmp", [128, 512]); TMP = Buf()
        sq = [sb(f"sq{i}", [128, 512]) for i in range(2)]; SQ = [Buf() for _ in range(2)]
        mean = sb("mean", [128, 512]); MEAN = Buf()
        rstd = sb("rstd", [128, 512]); RSTD = Buf()
        pg = [ps(f"pg{i}", [128, 512]) for i in range(8)]; PG = [Buf(excl=True) for _ in range(8)]
        gi = [0]

        def gbank():
            i = gi[0] % 8; gi[0] += 1
            return pg[i], PG[i]
        em.ms("dve", wr[:], 0.0, [WR])
        em.dma("sp", "wr", wr[:, :, 0:36], v3(wr_d), [WR], [WR])
        em.dma("sp", "brr", brr[:], br_d[0:1, :].partition_broadcast(128), [], [BRR])
        em.dma("sp", "ln", ln[:], ln_d[:, :], [], [LN])
        em.dma("pool", "selb", selb[:].rearrange("p a b -> p (a b)"), selb_d[:, :], [], [SELB])
        em.dma("sp", "g2e", g2e[:].rearrange("p a b -> p (a b)"), g2e_d[:, :], [], [G2E])
        em.ms("dve", ones[:], 1.0, [ONES])
        em.ms("pool", ident[:], 1.0, [IDENT])
        kb.op("pool", lambda e: e.affine_select(out=ident[:], in_=ident[:], pattern=[[-1, 128]], compare_op=ALU.is_equal,
                                                fill=0.0, base=0, channel_multiplier=1), [IDENT], [IDENT])

        def load_w(e):
            s = e % 2
            for k0 in range(0, 8, 4):
                em.dma("pool", f"w1_{s}", w1[s][:, k0:k0 + 4, :], w1_d[e].rearrange("(kc p) n -> p kc n", p=128)[:, k0:k0 + 4, :], [], [W1[s]])
                em.dma("pool", f"w3_{s}", w3[s][:, k0:k0 + 4, :], w3_d[e].rearrange("(kc p) n -> p kc n", p=128)[:, k0:k0 + 4, :], [], [W3[s]])
            for k0 in range(0, 4, 2):
                em.dma("pool", f"w2_{s}", w2[s][:, k0:k0 + 2, :], w2_d[e].rearrange("(kc p) n -> p kc n", p=128)[:, k0:k0 + 2, :], [], [W2[s]])

        load_w(0)
        for tg in range(4):
            ts_ = slice(tg * 512, (tg + 1) * 512)
            em.dma("sp", "xf", xf[:, :, :], v3(x1_d)[:, :, ts_], [], [XF])
            for i in range(8):
                em.cp("pool" if i % 2 else "dve", x1b[:, i, ts_], xf[:, i, :], [XF], [X1B[tg]])
                em.ts("dve" if i % 2 else "pool", out[:, i, ts_], xf[:, i, :], ALPHA, None, ALU.mult, None, [XF], [OUT[tg]])
            for tt_ in range(4):
                p, P = gbank()
                for k in range(8):
                    em.mm(p[:, 0:36], xf[:, k, tt_ * 128:(tt_ + 1) * 128], wr[:, k, 0:36], k == 0, k == 7, [XF, WR], [P])
                em.tt("dve", lgt[:], p[:, 0:36], brr[:], ALU.add, [P, BRR], [LGT])
                em.red(rs[:, 0:1], lgt[:, 0:4], ALU.max, [LGT], [RS])
                em.ts("dve", rs[:, 1:2], rs[:, 0:1], -1.0, None, ALU.mult, None, [RS], [RS])
                em.ms("dve", rs[:, 2:3], 0.0, [RS])
                em.act(rs[:, 4:8], lgt[:, 0:4], AF.Exp, [LGT, RS], [RS], bias=rs[:, 1:2], accum_out=rs[:, 2:3])
                em.rcp(rs[:, 3:4], rs[:, 2:3], [RS], [RS])
                em.ts("dve", rs[:, 8:12], lgt[:, 0:4], rs[:, 0:1], None, ALU.is_ge, None, [LGT, RS], [RS])
                em.ts("dve", em32[:], g2e[:, 0, :], rs[:, 8:9], None, ALU.mult, None, [G2E, RS], [EM32])
                for g in range(1, 4):
                    em.stt("dve", em32[:], g2e[:, g, :], rs[:, 8 + g:9 + g], em32[:], ALU.mult, ALU.add, [G2E, RS, EM32], [EM32])
                em.tt("dve", em2[:], lgt[:, 4:36], em32[:], ALU.mult, [LGT, EM32], [EM2])
                em.ts("dve", em32[:], em32[:], -1.0, 1.0e9, ALU.add, ALU.mult, [EM32], [EM32])
                em.tt("dve", em2[:], em2[:], em32[:], ALU.add, [EM2, EM32], [EM2])
                em.red(rs[:, 12:13], em2[:], ALU.max, [EM2], [RS])
                em.ts("dve", cw[:], em2[:], rs[:, 12:13], None, ALU.is_ge, None, [EM2, RS], [CW])
                em.stt("dve", em32[:], cw[:], -2.0e9, em2[:], ALU.mult, ALU.add, [CW, EM2], [EM32])
                em.red(rs[:, 13:14], em32[:], ALU.max, [EM32], [RS])
                em.ts("dve", em32[:], em32[:], rs[:, 13:14], None, ALU.is_ge, None, [EM32, RS], [EM32])
                em.tt("dve", rs[:, 14:15], rs[:, 13:14], rs[:, 12:13], ALU.subtract, [RS], [RS])
                em.act(rs[:, 15:16], rs[:, 14:15], AF.Exp, [RS], [RS])
                em.ts("dve", rs[:, 15:16], rs[:, 15:16], 1.0, None, ALU.add, None, [RS], [RS])
                em.rcp(rs[:, 16:17], rs[:, 15:16], [RS], [RS])
                em.tt("dve", rs[:, 17:18], rs[:, 16:17], rs[:, 3:4], ALU.mult, [RS], [RS])
                em.tt("dve", rs[:, 18:19], rs[:, 3:4], rs[:, 17:18], ALU.subtract, [RS], [RS])
                em.ts("dve", cw[:], cw[:], rs[:, 17:18], None, ALU.mult, None, [CW, RS], [CW])
                em.stt("dve", cw[:], em32[:], rs[:, 18:19], cw[:], ALU.mult, ALU.add, [EM32, RS, CW], [CW])
                pt_, PT_ = gbank()
                em.tr(pt_[0:32, 0:128], cw[:], ident[:], [CW, IDENT], [PT_])
                em.cp("dve", cwt[:, tg * 512 + tt_ * 128: tg * 512 + (tt_ + 1) * 128], pt_[0:32, 0:128], [PT_], [CWT[tg]])
        for e in range(nexp):
            s = e % 2
            if e + 1 < nexp:
                load_w(e + 1)
            for tg in range(4):
                ts_ = slice(tg * 512, (tg + 1) * 512)
                pc_, PC_ = gbank()
                em.mm(pc_[:, :], selb[:, e, :], cwt[:, ts_], True, True, [SELB, CWT[tg]], [PC_])
                em.act(cwb[:], pc_[:, :], AF.Copy, [PC_], [CWB])
                for f in range(4):
                    fs = slice(f * 128, (f + 1) * 128)
                    pa, PA = gbank()
                    for k in range(8):
                        em.mm(pa[:, :], w1[s][:, k, fs], x1b[:, k, ts_], k == 0, k == 7, [W1[s], X1B[tg]], [PA])
                    pb, PB = gbank()
                    for k in range(8):
                        em.mm(pb[:, :], w3[s][:, k, fs], x1b[:, k, ts_], k == 0, k == 7, [W3[s], X1B[tg]], [PB])
                    em.act(sl_[f % 2][:], pa[:, :], AF.Silu, [PA], [SL[f % 2]])
                    em.tt("dve", sl_[f % 2][:], sl_[f % 2][:], pb[:, :], ALU.mult, [SL[f % 2], PB], [SL[f % 2]])
                    em.tt("pool", hb[:, f, :], sl_[f % 2][:], cwb[:], ALU.mult, [SL[f % 2], CWB], [HB[f]])
                for i in range(8):
                    po, PO = gbank()
                    for f in range(4):
                        em.mm(po[:, :], w2[s][:, f, i * 128:(i + 1) * 128], hb[:, f, :], f == 0, f == 3, [W2[s], HB[f]], [PO])
                    em.tt("dve", out[:, i, ts_], out[:, i, ts_], po[:, :], ALU.add, [OUT[tg], PO], [OUT[tg]])
        for tg in range(4):
            ts_ = slice(tg * 512, (tg + 1) * 512)
            layer_norm_fm(kb, em, gbank, out[:, :, ts_], OUT[tg], lambda i: (ob[:, i, :], OB), ln[:, 0:8], ln[:, 8:16], LN, tmp, TMP,
                          ones, ONES, sq, SQ, mean, MEAN, rstd, RSTD)
            em.dma("sp", "ob", v3(o_d)[:, :, ts_], ob[:, :, :], [OB], [])
        kb.finish([OB])


def build_b2(nexp=32):
    nc = bass.Bass("TRN2", target_bir_lowering=False)
    I = lambda n, shp: nc.dram_tensor(n, shp, F32, kind="ExternalInput").ap()
    D = dict(x1T=I("x1T", [1024, NT]), wr=I("wr", [1024, 36]), brr=I("brr", [1, 36]), ew1=I("ew1", [32, 1024, 512]), ew3=I("ew3", [32, 1024, 512]),
             ew2=I("ew2", [32, 512, 1024]), ln=I("ln", [128, 16]), selb=I("selb", [32, 32 * 128]), g2e=I("g2e", [128, 4 * 32]),
             x2T=nc.dram_tensor("x2T", [1024, NT], F32, kind="ExternalOutput").ap())
    with ExitStack() as st:
        kb = KB(nc, st)
        b2_body(nc, kb, D, nexp)
        kb.emit()
    return nc


def b2_consts():
    selb = np.zeros((32, 32, 128), np.float32)
    for e in range(32):
        selb[e, e, :] = 1.0
    g2e = np.zeros((128, 4, 32), np.float32)
    for g in range(4):
        g2e[:, g, g * 8:(g + 1) * 8] = 1.0
    return dict(selb=selb.reshape(32, 32 * 128), g2e=g2e.reshape(128, 128))


def b2_inputs(inp, l, consts):
    wr = np.concatenate([inp["router_group_w"][l], inp["router_expert_w"][l]], axis=1)
    brr = np.concatenate([inp["router_group_b"][l], inp["router_expert_b"][l]])[None, :]
    lnp = np.concatenate([inp["ln2_g"][l].reshape(8, 128).T, inp["ln2_b"][l].reshape(8, 128).T], axis=1)
    return dict(wr=np.ascontiguousarray(wr), brr=np.ascontiguousarray(brr), ew1=inp["exp_w1"][l], ew3=inp["exp_w3"][l], ew2=inp["exp_w2"][l],
                ln=np.ascontiguousarray(lnp, dtype=np.float32), selb=consts["selb"], g2e=consts["g2e"])


L_ = 4


def build_fused(nl=L_):
    nc = bass.Bass("TRN2", target_bir_lowering=False)

    def I(n, shp):
        return nc.dram_tensor(n, list(shp), F32, kind="ExternalInput").ap()

    def T(n, shp):
        return nc.dram_tensor(n, list(shp), F32, kind="Internal").ap()
    x0T = I("x0T", [1024, S])
    a1w = I("a1_w", [nl, 2, 1024, 1024]); a1vec = I("a1_vec", [nl, 2, 128, 22]); a1lw = I("a1_lw", [nl, 2, 128, 256])
    a1g2 = I("a1_g2", [nl, 2, 128, 256]); a1cst = I("a1_cst", [128, 1024])
    a2wf = I("a2_wf", [nl, 2, 1024, 640]); a2wt = I("a2_wt", [nl, 2, 1024, 140]); a2w1 = I("a2_w1", [nl, 128, 32 * 128])
    a2pe = I("a2_peT", [nl, 128, 32]); a2w2 = I("a2_w2", [nl, 128, 192]); a2btc = I("a2_btc", [2, 128, 4 * 256]); a2bts = I("a2_bts", [2, 128, 4 * TW])
    a2c = {k: I("a2_" + k, shp) for k, shp in (("mkc", [128, 256]), ("mks", [128, TW]), ("mkw", [128, TW]), ("m12", [128, 256]), ("rv", [128, 1]),
                                                 ("c2s", [128, 128]), ("exd", [64, 32 * 128]), ("hsel", [128, 256]))}
    b1wg = I("b1_wg", [nl, 1024, 2048]); b1wur = I("b1_wur", [nl, 512, 1024]); b1wun = I("b1_wun", [nl, 512, 1024]); b1wo = I("b1_wo", [nl, 1024, 1024])
    b1ln = I("b1_ln", [nl, 128, 16])
    b2wr = I("b2_wr", [nl, 1024, 36]); b2br = I("b2_brr", [nl, 1, 36]); b2e1 = I("b2_ew1", [nl, 32, 1024, 512]); b2e3 = I("b2_ew3", [nl, 32, 1024, 512])
    b2e2 = I("b2_ew2", [nl, 32, 512, 1024]); b2ln = I("b2_ln", [nl, 128, 16]); b2selb = I("b2_selb", [32, 32 * 128]); b2g2e = I("b2_g2e", [128, 128])
    outT = nc.dram_tensor("outT", [1024, S], F32, kind="ExternalOutput").ap()
    XT = [T("xt0", [1024, S]), T("xt1", [1024, S])]
    YR = T("yr", [512, S]); YN = T("yn", [512, S]); X1 = T("x1", [1024, S])

    with ExitStack() as st:
        kb = KB(nc, st)
        pn = [0]

        def phase(fn, D, *args):
            with ExitStack() as pst:
                kb.pstack = pst
                kb.prefix = f"p{pn[0]}_"
                pn[0] += 1
                fn(nc, kb, D, *args)
                kb.emit()
            kb.pstack = st
        for l in range(nl):
            xin = x0T if l == 0 else XT[l % 2]
            xout = outT if l == nl - 1 else XT[(l + 1) % 2]
            for hh in range(2):
                phase(a1_body, dict(xT=xin, w=a1w[l, hh], vec=a1vec[l, hh], lw=a1lw[l, hh], g2=a1g2[l, hh], cst=a1cst,
                                    yT=YR[hh * 256:(hh + 1) * 256, :]))
            for g in range(2):
                D = dict(xT=xin, wf=a2wf[l, g], wt=a2wt[l, g], w1=a2w1[l], peT=a2pe[l], w2=a2w2[l], btc=a2btc[g], bts=a2bts[g],
                         yT=YN[g * 256:(g + 1) * 256, :])
                D.update(a2c)
                phase(a2_body, D)
            for hf in range(2):
                ts = slice(hf * NT, (hf + 1) * NT)
                phase(b1_body, dict(xT=xin[:, ts], yrT=YR[:, ts], ynT=YN[:, ts], wg=b1wg[l], wur=b1wur[l], wun=b1wun[l], wo=b1wo[l], ln=b1ln[l],
                                    x1T=X1[:, ts]))
            for hf in range(2):
                ts = slice(hf * NT, (hf + 1) * NT)
                phase(b2_body, dict(x1T=X1[:, ts], wr=b2wr[l], brr=b2br[l], ew1=b2e1[l], ew3=b2e3[l], ew2=b2e2[l], ln=b2ln[l], selb=b2selb,
                                    g2e=b2g2e, x2T=xout[:, ts]))
        print("FUSED instructions:", kb.n_ins, kb.cnt)
    return nc


def fused_inputs(inp, nl=L_):
    c2 = a2_consts(); cb2 = b2_consts()
    a1 = [[a1_inputs(inp, l, 0, hh) for hh in range(2)] for l in range(nl)]
    a2 = [[a2_inputs(inp, l, g, c2) for g in range(2)] for l in range(nl)]
    b1 = [b1_inputs(inp, l) for l in range(nl)]
    b2 = [b2_inputs(inp, l, cb2) for l in range(nl)]
    st = lambda f: np.ascontiguousarray(np.stack(f, axis=0), dtype=np.float32)
    m = {}
    for k, nm in (("w", "a1_w"), ("vec", "a1_vec"), ("lw", "a1_lw"), ("g2", "a1_g2")):
        m[nm] = st([st([a1[l][hh][k] for hh in range(2)]) for l in range(nl)])
    m["a1_cst"] = a1[0][0]["cst"]
    for k, nm in (("wf", "a2_wf"), ("wt", "a2_wt")):
        m[nm] = st([st([a2[l][g][k] for g in range(2)]) for l in range(nl)])
    for k, nm in (("w1", "a2_w1"), ("peT", "a2_peT"), ("w2", "a2_w2")):
        m[nm] = st([a2[l][0][k] for l in range(nl)])
    m["a2_btc"] = st([a2[0][g]["btc"] for g in range(2)]); m["a2_bts"] = st([a2[0][g]["bts"] for g in range(2)])
    for k in ("mkc", "mks", "mkw", "m12", "rv", "c2s", "exd", "hsel"):
        m["a2_" + k] = a2[0][0][k]
    for k, nm in (("wg", "b1_wg"), ("wur", "b1_wur"), ("wun", "b1_wun"), ("wo", "b1_wo"), ("ln", "b1_ln")):
        m[nm] = st([b1[l][k] for l in range(nl)])
    for k, nm in (("wr", "b2_wr"), ("brr", "b2_brr"), ("ln", "b2_ln")):
        m[nm] = st([b2[l][k] for l in range(nl)])
    m["b2_ew1"] = np.ascontiguousarray(inp["exp_w1"][:nl], dtype=np.float32)
    m["b2_ew3"] = np.ascontiguousarray(inp["exp_w3"][:nl], dtype=np.float32)
    m["b2_ew2"] = np.ascontiguousarray(inp["exp_w2"][:nl], dtype=np.float32)
    m["b2_selb"] = cb2["selb"]; m["b2_g2e"] = cb2["g2e"]
    return m

_NC = {}


def kernel(**inputs):
    inp = {k: np.asarray(v) for k, v in inputs.items()}
    if "nc" not in _NC:
        _NC["nc"] = build_fused(L_)
    m = fused_inputs(inp, L_)
    x = inp["x"].astype(np.float32, copy=False)
    B = x.shape[0]
    xT = [np.ascontiguousarray(x[b].T) for b in range(B)]
    maps = []
    for c in range(8):
        mm_ = dict(m); mm_["x0T"] = xT[c // 2]; maps.append(mm_)
    res = run_bass_kernel_spmd(_NC["nc"], maps, core_ids=list(range(8)))
    out = np.stack([res.results[2 * b]["outT"].T for b in range(B)], axis=0)
    return np.ascontiguousarray(out, dtype=np.float32)
```

```python
import numpy as np
from contextlib import ExitStack
import concourse.bass as bass
import concourse.mybir as mybir
from concourse.bass_utils import run_bass_kernel_spmd

F32 = mybir.dt.float32
BF16 = mybir.dt.bfloat16
AF = mybir.ActivationFunctionType
ALU = mybir.AluOpType
AX = mybir.AxisListType


class Buf:
    __slots__ = ("name", "w", "r", "excl")

    def __init__(self, name="", excl=False):
        self.name = name
        self.excl = excl
        self.w = None
        self.r = {}


class KB:
    def __init__(self, nc, stack):
        self.nc = nc
        self.stack = stack
        self.pstack = stack
        self.prefix = ""
        self.names = ["pe", "act", "dve", "pool", "sp"]
        self.prog = {e: [] for e in self.names}
        self.sem = {e: stack.enter_context(nc.semaphore("s_" + e)) for e in self.names}
        self.cnt = {e: 0 for e in self.names}
        self.seen = {e: {} for e in self.names}
        self.dsem = {}
        self.n_ins = 0
        self.nw = {e: 0 for e in self.names}

    def sb(self, name, shape, dt=F32):
        return self.pstack.enter_context(self.nc.sbuf_tensor(self.prefix + "sb_" + name, list(shape), dt))

    def ps(self, name, shape, dt=F32):
        return self.pstack.enter_context(self.nc.psum_tensor(self.prefix + "ps_" + name, list(shape), dt))

    def _semh(self, key):
        if key in self.sem:
            return self.sem[key]
        return self.dsem[key][0]

    def _waits(self, eng, reads, writes):
        need = {}

        def add(d):
            if d is None:
                return
            k, v = d
            if need.get(k, 0) < v:
                need[k] = v
        for b in reads:
            add(b.w)
        for b in writes:
            add(b.w)
            for k, v in b.r.items():
                add((k, v))
        out = []
        seen = self.seen[eng]
        for k, v in need.items():
            if k == "pe" and eng == "pe":
                continue
            if seen.get(k, 0) >= v:
                continue
            seen[k] = v
            out.append((self._semh(k), v))
        return out

    def _mark(self, tok, reads, writes):
        for b in writes:
            b.w = tok
            b.r = {}
        k, v = tok
        for b in reads:
            if b.r.get(k, 0) < v:
                b.r[k] = v

    def op(self, eng, fn, reads=(), writes=()):
        ex = [b for b in reads if b.excl]
        if ex:
            writes = list(writes) + ex
        waits = self._waits(eng, reads, writes)
        self.nw[eng] += len(waits)
        self.cnt[eng] += 1
        tok = (eng, self.cnt[eng])
        sem = self.sem[eng]

        def run(e, waits=waits, fn=fn, sem=sem):
            for s, v in waits:
                e.wait_ge(s, v)
            fn(e).then_inc(sem, 1)
        self.prog[eng].append(run)
        self._mark(tok, reads, writes)
        self.n_ins += 1

    def dma(self, q, key, fn, reads=(), writes=(), n=1):
        key = "d_" + key
        if key not in self.dsem:
            self.dsem[key] = [self.stack.enter_context(self.nc.semaphore(key)), 0]
        waits = self._waits(q, reads, writes)
        self.dsem[key][1] += 16 * n
        tok = (key, self.dsem[key][1])
        sem = self.dsem[key][0]

        def run(e, waits=waits, fn=fn, sem=sem):
            for s, v in waits:
                e.wait_ge(s, v)
            fn(e, sem)
        self.prog[q].append(run)
        self._mark(tok, reads, writes)
        self.n_ins += n

    def finish(self, bufs):
        waits = self._waits("sp", bufs, bufs)

        def run(e, waits=waits):
            for s, v in waits:
                e.wait_ge(s, v)
        self.prog["sp"].append(run)

    def emit(self):
        nc = self.nc
        prog = self.prog
        self.prog = {e: [] for e in self.names}
        with nc.Block() as block:
            @block.sync
            def _(e):
                for f in prog["sp"]:
                    f(e)

            @block.tensor
            def _(e):
                for f in prog["pe"]:
                    f(e)

            @block.scalar
            def _(e):
                for f in prog["act"]:
                    f(e)

            @block.vector
            def _(e):
                for f in prog["dve"]:
                    f(e)

            @block.gpsimd
            def _(e):
                for f in prog["pool"]:
                    f(e)


S = 4096
NS = 512
NSEG = S // NS
CH = 128
NCH = NS // CH
GN_EPS = 64e-5


def a1_body(nc, kb, D, nseg=NSEG, debug=False):
    Em_ = globals().get('Em')
    if Em_ is None:
        from a2 import Em as Em_
    xT_d, w_d, vec_d, lw_d, g2_d, cst_d, yT_d = (D[k] for k in ("xT", "w", "vec", "lw", "g2", "cst", "yT"))
    xT_v = xT_d.rearrange("(kc p) t -> p kc t", p=128)
    w_v = w_d.rearrange("(kc p) n -> p kc n", p=128)
    if True:
        sb, ps = kb.sb, kb.ps
        NXS = 3
        xs = [sb(f"xs{i}", [128, 8, NS], BF16) for i in range(NXS)]; XS = [Buf() for _ in range(NXS)]
        wsb = sb("wsb", [128, 8, 1024], BF16); WSB = Buf()
        vec = sb("vec", [128, 22]); VEC = Buf()
        vx = sb("vx", [128, 8]); VX = Buf()
        lw = sb("lw", [128, 256]); LW = Buf()
        g2 = sb("g2", [128, 256]); G2 = Buf()
        cst = sb("cst", [128, 1280]); CST = Buf()
        MASK1 = cst[:, 0:512]; MASK4 = cst[:, 512:1024]; MSL = cst[:, 1024:1152]; BONES = cst[:, 1152:1280]
        ident = sb("ident", [128, 128]); IDENT = Buf()
        car = sb("car", [128, 8]); CAR = Buf()
        zr = [sb(f"zr{i}", [128, NS + 1]) for i in range(2)]; ZR = [Buf() for _ in range(2)]
        dtmp = sb("dtmp", [128, NS]); DTMP = Buf()
        zs = [sb(f"zs{j}", [128, NS]) for j in range(8)]; ZS = [Buf() for _ in range(8)]
        tw = sb("tw", [128, NS]); TW = Buf()
        sg = sb("sg", [128, NS]); SG = Buf()
        tnames = ["nld", "cw", "ew", "ewi", "ewx", "aa", "kkn", "sq", "t1", "k2", "bh", "kh", "e1"]
        T = {n: sb("t_" + n, [128, NS]) for n in tnames}; TB = {n: Buf() for n in tnames}
        ar = [sb(f"ar{h}", [128, NCH, 2 * CH]) for h in range(2)]; AR = [Buf() for _ in range(2)]
        bt = [sb(f"bt{h}", [128, NS]) for h in range(2)]; BT = [Buf() for _ in range(2)]
        kt = [sb(f"kt{h}", [128, NS]) for h in range(2)]; KT = [Buf() for _ in range(2)]
        gg = [sb(f"gg{h}", [128, NS]) for h in range(2)]; GG = [Buf() for _ in range(2)]
        bon = [sb(f"bon{h}", [128, NS]) for h in range(2)]; BON = [Buf() for _ in range(2)]
        yf = [sb(f"yf{h}", [128, NS]) for h in range(2)]; YF = [Buf() for _ in range(2)]
        wc = [sb(f"wc{h}", [128, NCH]) for h in range(2)]; WC = [Buf() for _ in range(2)]
        bhT = sb("bhT", [128, NCH, 256]); BHT = [Buf() for _ in range(NCH)]
        khT = sb("khT", [128, NCH, 256]); KHT = [Buf() for _ in range(NCH)]
        vT = sb("vT", [128, NCH, 256]); VT = [Buf() for _ in range(NCH)]
        mabk = [sb(f"mabk{h}", [128, 512]) for h in range(4)]; MABK = [Buf() for _ in range(4)]
        nm = [[sb(f"nm{h}_{i}", [128, 256]) for i in range(2)] for h in range(4)]; NM = [[Buf(), Buf()] for _ in range(4)]
        qq = [[sb(f"qq{h}_{i}", [128, 128]) for i in range(2)] for h in range(4)]; QQ = [[Buf(), Buf()] for _ in range(4)]
        xsb = [sb(f"xsb{h}", [128, 64]) for h in range(4)]; XSB = [Buf() for _ in range(4)]
        usb = [sb(f"usb{h}", [128, 64]) for h in range(4)]; USB = [Buf() for _ in range(4)]
        stt = [[sb(f"st{hp}_{i}", [128, 64]) for i in range(2)] for hp in range(2)]
        STT = [[[Buf(), Buf()] for _ in range(2)] for hp in range(2)]
        ytok = sb("ytok", [128, 256]); YTOK = Buf()
        ysq = sb("ysq", [128, 256]); YSQ = Buf()
        yn = sb("yn", [128, 256]); YN = Buf()
        sts = sb("sts", [128, 32]); STS = Buf()
        osb = [sb(f"osb{h}", [128, NS]) for h in range(2)]; OSB = [Buf() for _ in range(2)]
        NPA = 4
        pa = [ps(f"pa{i}", [128, 512]) for i in range(NPA)]; PA = [Buf(excl=True) for _ in range(NPA)]
        pq = [ps(f"pq{i}", [128, 512]) for i in range(4)]; PQB = [Buf(excl=True) for _ in range(4)]

        def ld(q, key, out, in_, B):
            kb.dma(q, key, lambda e, s: e.dma_start(out=out, in_=in_).then_inc(s, 16), writes=[B])
        ld("sp", "vec", vec[:], vec_d[:, :], VEC)
        ld("sp", "lw", lw[:], lw_d[:, :], LW)
        ld("sp", "g2", g2[:], g2_d[:, :], G2)
        ld("sp", "cst", cst[:], cst_d[:, :], CST)
        for kc in range(0, 8, 4):
            kb.dma("pool", "wsb", lambda e, s, kc=kc: e.dma_start(out=wsb[:, kc:kc + 4, :], in_=w_v[:, kc:kc + 4, :]).then_inc(s, 16), writes=[WSB])
        kb.op("pool", lambda e: e.memset(ident[:], 1.0), writes=[IDENT])
        kb.op("pool", lambda e: e.affine_select(out=ident[:], in_=ident[:], pattern=[[-1, 128]], compare_op=ALU.is_equal,
                                                fill=0.0, base=0, channel_multiplier=1), reads=[IDENT], writes=[IDENT])
        kb.op("dve", lambda e: e.memset(car[:], 0.0), writes=[CAR])
        kb.op("dve", lambda e: e.tensor_scalar(vx[:, 0:2], vec[:, 8:10], -1.0, None, ALU.mult), reads=[VEC], writes=[VX])
        kb.op("dve", lambda e: e.tensor_scalar(vx[:, 2:4], vec[:, 14:16], -1.0, 1.0, ALU.mult, ALU.add), reads=[VEC, VX], writes=[VX])
        for hp in range(2):
            for i in range(2):
                kb.op("dve", lambda e, hp=hp, i=i: e.memset(stt[hp][i][:], 0.0), writes=STT[hp][i])

        def load_x(sgi):
            sl = sgi % NXS
            kb.dma("pool", f"xs{sl}", lambda e, s, sl=sl, sgi=sgi: e.dma_start(
                out=xs[sl][:, :, :], in_=xT_v[:, :, sgi * NS:(sgi + 1) * NS]).then_inc(s, 16), writes=[XS[sl]])

        load_x(0)
        pai = [0]

        def next_pa():
            i = pai[0] % NPA
            pai[0] += 1
            return pa[i], PA[i]

        def mm512(lhsT_fn, rhs_fn, nk, reads, M=128):
            p, P = next_pa()
            for k in range(nk):
                a_, b_ = lhsT_fn(k), rhs_fn(k)
                kb.op("pe", lambda e, k=k, p=p, a_=a_, b_=b_: e.matmul(p[0:M, :], a_, b_, start=(k == 0), stop=(k == nk - 1)),
                      reads=reads, writes=[P])
            return p, P

        ping = [0, 0]
        dbg_n = [0]

        def dbg(name, ap, B):
            if not debug:
                return
            shp = list(ap.shape)
            d = nc.dram_tensor("dbg_" + name, shp, F32, kind="ExternalOutput").ap()
            dbg_n[0] += 1
            cntv = dbg_n[0] * 16

            def f(e, s, d=d, ap=ap, cntv=cntv):
                e.dma_start(out=d, in_=ap).then_inc(s, 16)
                e.wait_ge(s, cntv)
            kb.dma("sp", "dbg", f, reads=[B])
        for sgi in range(nseg):
            sl = sgi % NXS
            if sgi + 1 < nseg:
                load_x(sgi + 1)
            for j in range(8):
                p, P = mm512(lambda k, j=j: wsb[:, k, j * 128:(j + 1) * 128], lambda k, sl=sl: xs[sl][:, k, :], 8, [WSB, XS[sl]])
                z, Z = zr[j % 2], ZR[j % 2]
                kb.op("pool", lambda e, z=z, j=j: e.tensor_copy(z[:, 0:1], car[:, j:j + 1]), reads=[CAR], writes=[Z])
                kb.op("act", lambda e, z=z, p=p: e.activation(out=z[:, 1:NS + 1], in_=p[:, :], func=AF.Copy), reads=[P], writes=[Z])
                kb.op("pool", lambda e, z=z, j=j: e.tensor_copy(car[:, j:j + 1], z[:, NS:NS + 1]), reads=[Z], writes=[CAR])
                kb.op("dve", lambda e, z=z: e.tensor_tensor(dtmp[:], z[:, 0:NS], z[:, 1:NS + 1], ALU.subtract), reads=[Z], writes=[DTMP])
                kb.op("dve", lambda e, z=z, j=j: e.scalar_tensor_tensor(zs[j][:], dtmp[:], vec[:, j:j + 1], z[:, 1:NS + 1], ALU.mult, ALU.add),
                      reads=[DTMP, Z, VEC], writes=[ZS[j]])
            L1, L2 = zs[6], zs[7]
            for j in range(8):
                dbg(f"zs{j}", zs[j][:], ZS[j])
            kb.op("act", lambda e: e.activation(out=tw[0:64, :], in_=L1[0:64, :], func=AF.Tanh), reads=[ZS[6]], writes=[TW])
            kb.op("act", lambda e: e.activation(out=sg[:], in_=L2[:], func=AF.Sigmoid), reads=[ZS[7]], writes=[SG])
            for hp in range(2):
                Rz, Kz, Vz = zs[0 + hp], zs[2 + hp], zs[4 + hp]
                RZ, KZ, VZ = ZS[0 + hp], ZS[2 + hp], ZS[4 + hp]
                cs = slice(hp * 128, (hp + 1) * 128)
                p, P = mm512(lambda k: lw[64:128, cs], lambda k: L1[64:128, :], 1, [LW, ZS[6]])
                kb.op("act", lambda e, p=p, hp=hp: e.activation(out=T["aa"][:], in_=p[:, :], func=AF.Sigmoid, bias=vec[:, 10 + hp:11 + hp]),
                      reads=[P, VEC], writes=[TB["aa"]])
                p, P = mm512(lambda k: g2[:, cs], lambda k: sg[:], 1, [G2, SG])
                kb.op("act", lambda e, p=p, hp=hp: e.activation(out=gg[hp][:], in_=p[:, :], func=AF.Copy), reads=[P], writes=[GG[hp]])
                p, P = mm512(lambda k: lw[0:64, cs], lambda k: tw[0:64, :], 1, [LW, TW])
                kb.op("act", lambda e, p=p, hp=hp: e.activation(out=T["e1"][:], in_=p[:, :], func=AF.Exp, bias=vx[:, hp:hp + 1], scale=-1.0),
                      reads=[P, VX], writes=[TB["e1"]])
                kb.op("act", lambda e: e.activation(out=T["e1"][:], in_=T["e1"][:], func=AF.Ln, bias=1.0), reads=[TB["e1"]], writes=[TB["e1"]])
                kb.op("act", lambda e: e.activation(out=T["nld"][:], in_=T["e1"][:], func=AF.Exp, bias=-0.5, scale=-1.0),
                      reads=[TB["e1"]], writes=[TB["nld"]])
                kb.op("dve", lambda e: e.tensor_tensor_scan(T["cw"][:], MASK1, T["nld"][:], 0.0, ALU.mult, ALU.add),
                      reads=[CST, TB["nld"]], writes=[TB["cw"]])
                kb.op("act", lambda e: e.activation(out=T["ew"][:], in_=T["cw"][:], func=AF.Exp, scale=-1.0), reads=[TB["cw"]], writes=[TB["ew"]])
                kb.op("act", lambda e: e.activation(out=T["ewi"][:], in_=T["cw"][:], func=AF.Exp), reads=[TB["cw"]], writes=[TB["ewi"]])
                kb.op("pool", lambda e: e.tensor_tensor(T["ewx"][:], T["cw"][:], T["nld"][:], ALU.subtract), reads=[TB["cw"], TB["nld"]], writes=[TB["ewx"]])
                kb.op("act", lambda e: e.activation(out=T["ewx"][:], in_=T["ewx"][:], func=AF.Exp, scale=-1.0), reads=[TB["ewx"]], writes=[TB["ewx"]])
                kb.op("pool", lambda e, hp=hp: e.tensor_copy(wc[hp][:], T["ew"][:].rearrange("p (c t) -> p c t", t=CH)[:, :, CH - 1]),
                      reads=[TB["ew"]], writes=[WC[hp]])
                kb.op("dve", lambda e, hp=hp, Kz=Kz: e.tensor_scalar(T["kkn"][:], Kz[:], vec[:, 12 + hp:13 + hp], None, ALU.mult),
                      reads=[KZ, VEC], writes=[TB["kkn"]])
                kb.op("pool", lambda e: e.tensor_tensor(T["sq"][:], T["kkn"][:], T["kkn"][:], ALU.mult), reads=[TB["kkn"]], writes=[TB["sq"]])
                p, P = mm512(lambda k: BONES, lambda k: T["sq"][:], 1, [CST, TB["sq"]])
                kb.op("act", lambda e, p=p: e.activation(out=T["sq"][:], in_=p[:, :], func=AF.Sqrt), reads=[P], writes=[TB["sq"]])
                kb.op("dve", lambda e: e.tensor_scalar(T["sq"][:], T["sq"][:], 1e-12, None, ALU.max), reads=[TB["sq"]], writes=[TB["sq"]])
                kb.op("dve", lambda e: e.reciprocal(T["sq"][:], T["sq"][:]), reads=[TB["sq"]], writes=[TB["sq"]])
                kb.op("dve", lambda e: e.tensor_tensor(T["kkn"][:], T["kkn"][:], T["sq"][:], ALU.mult), reads=[TB["kkn"], TB["sq"]], writes=[TB["kkn"]])
                kb.op("pool", lambda e, hp=hp: e.tensor_scalar(T["t1"][:], T["aa"][:], vec[:, 14 + hp:15 + hp], vx[:, 2 + hp:3 + hp], ALU.mult, ALU.add),
                      reads=[TB["aa"], VEC, VX], writes=[TB["t1"]])
                kb.op("pool", lambda e, Kz=Kz: e.tensor_tensor(T["k2"][:], Kz[:], T["t1"][:], ALU.mult), reads=[KZ, TB["t1"]], writes=[TB["k2"]])
                arv = ar[hp]
                kb.op("dve", lambda e, arv=arv: e.scalar_tensor_tensor(arv[:, :, 0:CH], T["kkn"][:].rearrange("p (c t) -> p c t", t=CH), -1.0,
                                                                      T["ewx"][:].rearrange("p (c t) -> p c t", t=CH), ALU.mult, ALU.mult),
                      reads=[TB["kkn"], TB["ewx"]], writes=[AR[hp]])
                kb.op("pool", lambda e, arv=arv, Rz=Rz: e.tensor_tensor(arv[:, :, CH:2 * CH], Rz[:].rearrange("p (c t) -> p c t", t=CH),
                                                                       T["ew"][:].rearrange("p (c t) -> p c t", t=CH), ALU.mult),
                      reads=[RZ, TB["ew"]], writes=[AR[hp]])
                kb.op("dve", lambda e: e.tensor_tensor(T["t1"][:], T["kkn"][:], T["aa"][:], ALU.mult), reads=[TB["kkn"], TB["aa"]], writes=[TB["t1"]])
                kb.op("dve", lambda e, hp=hp: e.tensor_tensor(bt[hp][:], T["t1"][:], T["ewi"][:], ALU.mult), reads=[TB["t1"], TB["ewi"]], writes=[BT[hp]])
                kb.op("pool", lambda e, hp=hp: e.tensor_tensor(kt[hp][:], T["k2"][:], T["ewi"][:], ALU.mult), reads=[TB["k2"], TB["ewi"]], writes=[KT[hp]])
                wcb = wc[hp][:].unsqueeze(2).to_broadcast([128, NCH, CH])
                kb.op("dve", lambda e, hp=hp, wcb=wcb: e.tensor_tensor(T["bh"][:].rearrange("p (c t) -> p c t", t=CH),
                                                                      bt[hp][:].rearrange("p (c t) -> p c t", t=CH), wcb, ALU.mult),
                      reads=[BT[hp], WC[hp]], writes=[TB["bh"]])
                kb.op("pool", lambda e, hp=hp, wcb=wcb: e.tensor_tensor(T["kh"][:].rearrange("p (c t) -> p c t", t=CH),
                                                                       kt[hp][:].rearrange("p (c t) -> p c t", t=CH), wcb, ALU.mult),
                      reads=[KT[hp], WC[hp]], writes=[TB["kh"]])
                kb.op("dve", lambda e, hp=hp, Rz=Rz: e.scalar_tensor_tensor(T["t1"][:], Rz[:], vec[:, 16 + hp:17 + hp], T["k2"][:], ALU.mult, ALU.mult),
                      reads=[RZ, VEC, TB["k2"], TB["t1"]], writes=[TB["t1"]])
                p, P = mm512(lambda k: BONES, lambda k: T["t1"][:], 1, [CST, TB["t1"]])
                kb.op("dve", lambda e, p=p, hp=hp, Vz=Vz: e.tensor_tensor(bon[hp][:], p[:, :], Vz[:], ALU.mult), reads=[P, VZ], writes=[BON[hp]])
                for n_ in ("nld", "cw", "ew", "ewi", "ewx", "aa", "kkn", "k2", "bh", "kh"):
                    dbg(f"{n_}{hp}", T[n_][:], TB[n_])
                dbg(f"bt{hp}", bt[hp][:], BT[hp]); dbg(f"kt{hp}", kt[hp][:], KT[hp]); dbg(f"ar{hp}", ar[hp][:].rearrange("p c t -> p (c t)"), AR[hp])
                dbg(f"gg{hp}", gg[hp][:], GG[hp]); dbg(f"bon{hp}", bon[hp][:], BON[hp]); dbg(f"wc{hp}", wc[hp][:], WC[hp])
                for c in range(NCH):
                    for (src, SRC, dst, DST) in ((T["bh"], TB["bh"], bhT, BHT), (T["kh"], TB["kh"], khT, KHT), (Vz, VZ, vT, VT)):
                        pbank, PT = next_pa()
                        pt = pbank[:, 0:128]
                        kb.op("pe", lambda e, pt=pt, src=src, c=c: e.transpose(pt, src[:, c * CH:(c + 1) * CH], ident[:]),
                              reads=[SRC, IDENT], writes=[PT])
                        kb.op("act", lambda e, pt=pt, dst=dst, c=c, cs=cs: e.activation(out=dst[:, c, cs], in_=pt, func=AF.Copy),
                              reads=[PT], writes=[DST[c]])
            em = Em_(kb)
            for c in range(NCH):
                csl = slice(c * CH, (c + 1) * CH)
                HD = []
                for h in range(4):
                    hp, hh = h // 2, h % 2
                    HD.append(dict(h=h, hp=hp, hh=hh, rows=slice(64 * hh, 64 * hh + 64), hc=slice(h * 64, (h + 1) * 64), q=pq[h], Q=PQB[h]))
                for d in HD:
                    em.mm(d["q"][:, 0:256], bt[d["hp"]][d["rows"], csl], ar[d["hp"]][d["rows"], c, :], True, True, [BT[d["hp"]], AR[d["hp"]]], [d["Q"]])
                    em.mm(d["q"][:, 256:512], kt[d["hp"]][d["rows"], csl], ar[d["hp"]][d["rows"], c, :], True, True, [KT[d["hp"]], AR[d["hp"]]], [d["Q"]])
                for d in HD:
                    h = d["h"]
                    em.tt("dve", mabk[h][:], d["q"][:, :], MASK4, ALU.mult, [d["Q"], CST], [MABK[h]])
                for d in HD:
                    em.mm(d["q"][:, 0:128], ar[d["hp"]][d["rows"], c, 0:CH], bt[d["hp"]][d["rows"], csl], True, True, [BT[d["hp"]], AR[d["hp"]]], [d["Q"]])
                for d in HD:
                    h = d["h"]
                    em.tt("dve", nm[h][0][:, 0:128], d["q"][:, 0:128], MSL, ALU.mult, [d["Q"], CST], [NM[h][0]])
                    em.cp("pool", nm[h][0][:, 128:256], mabk[h][:, 0:128], [MABK[h]], [NM[h][0]])
                    em.tt("pool", qq[h][0][:], mabk[h][:, 0:128], ident[:], ALU.add, [MABK[h], IDENT], [QQ[h][0]])
                for k in range(6):
                    a_, b_ = k % 2, (k + 1) % 2
                    wdt = 256 if k < 5 else 128
                    for d in HD:
                        h = d["h"]
                        em.mm(d["q"][:, 0:128], nm[h][a_][:, 128:256], nm[h][a_][:, 0:128], True, True, [NM[h][a_]], [d["Q"]])
                        if k < 5:
                            em.mm(d["q"][:, 128:256], nm[h][a_][:, 0:128], nm[h][a_][:, 128:256], True, True, [NM[h][a_]], [d["Q"]])
                    for d in HD:
                        h = d["h"]
                        if h % 2 == 0:
                            em.cp("dve", nm[h][b_][:, 0:wdt], d["q"][:, 0:wdt], [d["Q"]], [NM[h][b_]])
                        else:
                            em.act(nm[h][b_][:, 0:wdt], d["q"][:, 0:wdt], AF.Copy, [d["Q"]], [NM[h][b_]])
                    for d in HD:
                        h = d["h"]
                        em.mm(d["q"][:, 256:384], nm[h][b_][:, 0:128], qq[h][a_][:], True, True, [NM[h][b_], QQ[h][a_]], [d["Q"]])
                    for d in HD:
                        h = d["h"]
                        em.tt("dve", qq[h][b_][:], d["q"][:, 256:384], qq[h][a_][:], ALU.add, [d["Q"], QQ[h][a_]], [QQ[h][b_]])
                qf = 0
                so = [ping[0], ping[1]]
                for d in HD:
                    h, hp, rows, hc = d["h"], d["hp"], d["rows"], d["hc"]
                    So = STT[hp][so[hp]][d["hh"]]
                    em.mm(d["q"][:, 0:64], mabk[h][:, 256:384], vT[:, c, hc], True, False, [MABK[h], VT[c]], [d["Q"]])
                    em.mm(d["q"][:, 0:64], ar[hp][rows, c, 0:CH], stt[hp][so[hp]][rows, :], False, True, [AR[hp], So], [d["Q"]])
                for d in HD:
                    h = d["h"]
                    if h % 2 == 0:
                        em.act(xsb[h][:], d["q"][:, 0:64], AF.Copy, [d["Q"]], [XSB[h]])
                    else:
                        em.cp("dve", xsb[h][:], d["q"][:, 0:64], [d["Q"]], [XSB[h]])
                for d in HD:
                    h = d["h"]
                    em.mm(d["q"][:, 64:128], qq[h][qf][:], xsb[h][:], True, True, [QQ[h][qf], XSB[h]], [d["Q"]])
                for d in HD:
                    h = d["h"]
                    if h % 2 == 0:
                        em.act(usb[h][:], d["q"][:, 64:128], AF.Copy, [d["Q"]], [USB[h]])
                    else:
                        em.cp("dve", usb[h][:], d["q"][:, 64:128], [d["Q"]], [USB[h]])
                for d in HD:
                    h, hp, rows, hc = d["h"], d["hp"], d["rows"], d["hc"]
                    So = STT[hp][so[hp]][d["hh"]]
                    em.mm(d["q"][:, 256:320], ar[hp][rows, c, CH:2 * CH], stt[hp][so[hp]][rows, :], True, False, [AR[hp], So], [d["Q"]])
                    em.mm(d["q"][:, 256:320], mabk[h][:, 128:256], usb[h][:], False, False, [MABK[h], USB[h]], [d["Q"]])
                    em.mm(d["q"][:, 256:320], mabk[h][:, 384:512], vT[:, c, hc], False, True, [MABK[h], VT[c]], [d["Q"]])
                    em.mm(d["q"][rows, 128:192], bhT[:, c, hc], usb[h][:], True, False, [BHT[c], USB[h]], [d["Q"]])
                    em.mm(d["q"][rows, 128:192], khT[:, c, hc], vT[:, c, hc], False, True, [KHT[c], VT[c]], [d["Q"]])
                for d in HD:
                    h, hp, rows, hc = d["h"], d["hp"], d["rows"], d["hc"]
                    sn = 1 - so[hp]
                    em.stt("dve", stt[hp][sn][rows, :], stt[hp][so[hp]][rows, :], wc[hp][rows, c:c + 1], d["q"][rows, 128:192], ALU.mult, ALU.add,
                           [STT[hp][so[hp]][d["hh"]], WC[hp], d["Q"]], [STT[hp][sn][d["hh"]]])
                    em.act(ytok[:, hc], d["q"][:, 256:320], AF.Copy, [d["Q"]], [YTOK])
                    em.act(ysq[:, hc], d["q"][:, 256:320], AF.Square, [d["Q"]], [YSQ])
                ping[0], ping[1] = 1 - so[0], 1 - so[1]
                kb.op("dve", lambda e: e.tensor_reduce(sts[:, 0:4], ytok[:].rearrange("p (h v) -> p h v", v=64), AX.X, ALU.add), reads=[YTOK], writes=[STS])
                kb.op("dve", lambda e: e.tensor_reduce(sts[:, 4:8], ysq[:].rearrange("p (h v) -> p h v", v=64), AX.X, ALU.add), reads=[YSQ, STS], writes=[STS])
                kb.op("dve", lambda e: e.tensor_scalar(sts[:, 8:12], sts[:, 0:4], 1.0 / 64, None, ALU.mult), reads=[STS], writes=[STS])
                kb.op("dve", lambda e: e.tensor_tensor(sts[:, 12:16], sts[:, 8:12], sts[:, 8:12], ALU.mult), reads=[STS], writes=[STS])
                kb.op("dve", lambda e: e.scalar_tensor_tensor(sts[:, 16:20], sts[:, 4:8], 1.0 / 64, sts[:, 12:16], ALU.mult, ALU.subtract),
                      reads=[STS], writes=[STS])
                kb.op("dve", lambda e: e.tensor_scalar(sts[:, 16:20], sts[:, 16:20], GN_EPS, None, ALU.add), reads=[STS], writes=[STS])
                kb.op("act", lambda e: e.activation(out=sts[:, 20:24], in_=sts[:, 16:20], func=AF.Sqrt), reads=[STS], writes=[STS])
                kb.op("dve", lambda e: e.reciprocal(sts[:, 24:28], sts[:, 20:24]), reads=[STS], writes=[STS])
                kb.op("dve", lambda e: e.tensor_tensor(yn[:].rearrange("p (h v) -> p h v", v=64), ytok[:].rearrange("p (h v) -> p h v", v=64),
                                                       sts[:, 8:12].unsqueeze(2).to_broadcast([128, 4, 64]), ALU.subtract), reads=[YTOK, STS], writes=[YN])
                kb.op("dve", lambda e: e.tensor_tensor(yn[:].rearrange("p (h v) -> p h v", v=64), yn[:].rearrange("p (h v) -> p h v", v=64),
                                                       sts[:, 24:28].unsqueeze(2).to_broadcast([128, 4, 64]), ALU.mult), reads=[YN, STS], writes=[YN])
                for hp in range(2):
                    pbank, PT = next_pa()
                    pt = pbank[:, 0:128]
                    em.tr(pt, yn[:, hp * 128:(hp + 1) * 128], ident[:], [YN, IDENT], [PT])
                    em.ts("dve", yf[hp][:, csl], pt, vec[:, 18 + hp:19 + hp], vec[:, 20 + hp:21 + hp], ALU.mult, ALU.add, [PT, VEC], [YF[hp]])
            for hp in range(2):
                kb.op("pool", lambda e, hp=hp: e.tensor_tensor(osb[hp][:], yf[hp][:], bon[hp][:], ALU.add), reads=[YF[hp], BON[hp]], writes=[OSB[hp]])
                kb.op("pool", lambda e, hp=hp: e.tensor_tensor(osb[hp][:], osb[hp][:], gg[hp][:], ALU.mult), reads=[OSB[hp], GG[hp]], writes=[OSB[hp]])
                kb.dma("sp", f"out{hp}", lambda e, s, hp=hp, sgi=sgi: e.dma_start(out=yT_d[hp * 128:(hp + 1) * 128, sgi * NS:(sgi + 1) * NS], in_=osb[hp][:]).then_inc(s, 16),
                       reads=[OSB[hp]])
        kb.finish(OSB)


def build_a1(nseg=NSEG, debug=False):
    nc = bass.Bass("TRN2", target_bir_lowering=False)
    I = lambda n, shp: nc.dram_tensor(n, shp, F32, kind="ExternalInput").ap()
    D = dict(xT=I("xT", [1024, S]), w=I("w", [1024, 1024]), vec=I("vec", [128, 22]), lw=I("lw", [128, 256]), g2=I("g2", [128, 256]),
             cst=I("cst", [128, 1280]), yT=nc.dram_tensor("yT", [256, S], F32, kind="ExternalOutput").ap())
    with ExitStack() as st:
        kb = KB(nc, st)
        a1_body(nc, kb, D, nseg, debug)
        kb.emit()
    return nc


def a1_consts():
    m1 = np.ones((128, 512), np.float32); m1[:, ::CH] = 0.0
    s = np.arange(128)[:, None]; t = np.arange(128)[None, :]
    msu = (t > s).astype(np.float32); miu = (t >= s).astype(np.float32)
    msl = (t < s).astype(np.float32)
    bones = (s // 64 == t // 64).astype(np.float32)
    return np.concatenate([m1, msu, miu, msu, miu, msl, bones], axis=1)


def a1_inputs(inp, l, b, hh):
    ch = slice(256 * hh, 256 * hh + 256)
    w_in = inp["w_in"][l]
    w = np.concatenate([w_in[:, 0:512][:, ch], w_in[:, 512:1024][:, ch], w_in[:, 1024:1536][:, ch], w_in[:, 1536:1792]], axis=1)
    mu = inp["shift_mu"][l]
    mu_cols = np.concatenate([mu[0:512][ch], mu[512:1024][ch], mu[1024:1536][ch], mu[1536:1792]])
    vec = np.zeros((128, 22), np.float32)
    vec[:, 0:8] = mu_cols.reshape(8, 128).T
    def two(v):
        return v[ch].reshape(2, 128).T
    vec[:, 8:10] = two(inp["rw_w0"][l]); vec[:, 10:12] = two(inp["rw_a0"][l]); vec[:, 12:14] = two(inp["rw_kk"][l])
    vec[:, 14:16] = two(inp["rw_ka"][l]); vec[:, 16:18] = two(inp["rw_rk"][l].reshape(512))
    vec[:, 18:20] = two(inp["rw_ln_g"][l]); vec[:, 20:22] = two(inp["rw_ln_b"][l])
    lw = np.concatenate([inp["rw_w2"][l][:, ch], inp["rw_a2"][l][:, ch]], axis=0)
    g2 = inp["rw_g2"][l][:, ch]
    return dict(w=np.ascontiguousarray(w), vec=vec, lw=np.ascontiguousarray(lw), g2=np.ascontiguousarray(g2), cst=a1_consts())


import math

S = 4096
NEG = -30000.0
TW = 2304
DCL = 1280


def t5_bucket(n):
    n = np.maximum(n, 0); me = 16
    nf = np.maximum(n, 1).astype(np.float32)
    large = me + (np.log(nf / np.float32(me)) / np.float32(math.log(1024 / me)) * np.float32(32 - me)).astype(np.int32)
    large = np.minimum(large, 31)
    return np.where(n < me, n, large)


class Em:
    def __init__(self, kb):
        self.kb = kb

    def mm(self, out, lhsT, rhs, start, stop, reads, writes):
        self.kb.op("pe", lambda e, o=out, a=lhsT, b=rhs, s=start, t=stop: e.matmul(o, a, b, start=s, stop=t), reads, writes)

    def tr(self, out, in_, ident, reads, writes):
        self.kb.op("pe", lambda e, o=out, a=in_, b=ident: e.transpose(o, a, b), reads, writes)

    def act(self, out, in_, func, reads, writes, **kw):
        self.kb.op("act", lambda e, o=out, i=in_, f=func, kw=kw: e.activation(out=o, in_=i, func=f, **kw), reads, writes)

    def tt(self, eng, out, a, b, op, reads, writes):
        self.kb.op(eng, lambda e, o=out, a=a, b=b, op=op: e.tensor_tensor(o, a, b, op), reads, writes)

    def ts(self, eng, out, a, s1, s2, op0, op1, reads, writes):
        if op1 is None:
            self.kb.op(eng, lambda e, o=out, a=a, s1=s1, op0=op0: e.tensor_scalar(o, a, s1, None, op0), reads, writes)
        else:
            self.kb.op(eng, lambda e, o=out, a=a, s1=s1, s2=s2, op0=op0, op1=op1: e.tensor_scalar(o, a, s1, s2, op0, op1), reads, writes)

    def stt(self, eng, out, a, s, b, op0, op1, reads, writes):
        self.kb.op(eng, lambda e, o=out, a=a, s=s, b=b, op0=op0, op1=op1: e.scalar_tensor_tensor(o, a, s, b, op0, op1), reads, writes)

    def cp(self, eng, out, in_, reads, writes):
        self.kb.op(eng, lambda e, o=out, i=in_: e.tensor_copy(o, i), reads, writes)

    def ms(self, eng, out, val, writes):
        self.kb.op(eng, lambda e, o=out, v=val: e.memset(o, v), (), writes)

    def red(self, out, in_, op, reads, writes):
        self.kb.op("dve", lambda e, o=out, i=in_, op=op: e.tensor_reduce(o, i, AX.X, op), reads, writes)

    def rcp(self, out, in_, reads, writes):
        self.kb.op("dve", lambda e, o=out, i=in_: e.reciprocal(o, i), reads, writes)

    def dma(self, q, key, out, in_, reads, writes):
        self.kb.dma(q, key, lambda e, s, o=out, i=in_: e.dma_start(out=o, in_=i).then_inc(s, 16), reads, writes)


def a2_body(nc, kb, D, nq=8, debug=False):
    xT_d, wf_d, wt_d, w1_d, pe_d, w2_d, btc_d, mkc_d, bts_d, mks_d, mkw_d, m12_d, rv_d, c2s_d, exd_d, hsel_d = (D[k] for k in ['xT', 'wf', 'wt', 'w1', 'peT', 'w2', 'btc', 'mkc', 'bts', 'mks', 'mkw', 'm12', 'rv', 'c2s', 'exd', 'hsel'])
    yT_d = D["yT"]
    xT_v = xT_d.rearrange("(kc p) t -> p kc t", p=128)
    if True:
        em = Em(kb)
        sb, ps = kb.sb, kb.ps
        xs = [sb(f"xs{i}", [128, 8, 512], BF16) for i in range(2)]; XS = [Buf() for _ in range(2)]
        wf = sb("wf", [128, 8, 640], BF16); WF = Buf()
        wt = sb("wt", [128, 8, 256], BF16); WT = Buf()
        w1 = sb("w1", [128, 32, 128], BF16); W1 = Buf()
        peT = sb("peT", [128, 32], BF16); PET = Buf()
        w2f = sb("w2f", [128, 192]); w2 = sb("w2", [128, 192], BF16); W2 = Buf()
        qT = [sb(f"qT{i}", [128, S], BF16) for i in range(2)]; QT = [Buf() for _ in range(2)]
        ksT = sb("ksT", [128, S], BF16); KST = Buf()
        kwT = sb("kwT", [128, S], BF16); KWT = Buf()
        kcv = sb("kcv", [128, S], BF16); KCV = Buf()
        vs = sb("vs", [128, 32, 96], BF16); VS = Buf()
        vw = sb("vw", [128, 32, 96], BF16); VW = Buf()
        gt = sb("gt", [128, 32, 16]); GT = Buf()
        btc = sb("btc", [128, 4, 256]); BTC = Buf()
        mkc = sb("mkc", [128, 256]); MKC = Buf()
        HW_ = TW // 2
        stg = sb("stg", [128, HW_]); STG = Buf()
        stg2 = sb("stg2", [128, HW_]); STG2 = Buf()
        mks = sb("mks", [128, HW_]); MKS = Buf()
        mkw = sb("mkw", [128, HW_]); MKW = Buf()
        ebs = [sb(f"ebs{h}", [128, TW], BF16) for h in range(4)]; EBS = [Buf() for _ in range(4)]
        ebw = [sb(f"ebw{h}", [128, TW], BF16) for h in range(4)]; EBW = [Buf() for _ in range(4)]
        m12 = sb("m12", [128, 256]); M12 = Buf()
        rv = sb("rv", [128, 1]); RV = Buf()
        vca = sb("vca", [128, 2, 128], BF16); VCA = Buf()
        c2sf = sb("c2sf", [128, 128]); C2SF = Buf()
        exd = sb("exd", [128, 32, 128], BF16); EXD = Buf()
        hself = sb("hself", [128, 256]); hsel = sb("hsel", [128, 2, 128], BF16); HSEL = Buf()
        identf = sb("identf", [128, 128]); ident = sb("ident", [128, 128], BF16); IDENT = Buf()
        kct = sb("kct", [128, 256], BF16); KCT = Buf()
        gh = [sb(f"gh{i}", [128, 256], BF16) for i in range(2)]; GH = [Buf() for _ in range(2)]
        hx = sb("hx", [128, 256]); HX = Buf()
        hy = sb("hy", [128, 256]); HY = Buf()
        hb = sb("hb", [128, 2]); HB = Buf()
        sm = sb("sm", [128, 64]); SM = Buf()
        cst = sb("cst", [128, 16]); CST = Buf()
        qsq = sb("qsq", [128, 512], BF16); QSQ = Buf()
        mxc = sb("mxc", [128, 4, 8]); MXC = Buf()
        kxc = sb("kxc", [128, 2, 8]); KXC = Buf()
        lgs = [sb(f"lg{h}", [128, 256]) for h in range(4)]; LGS = [Buf() for _ in range(4)]
        pcs = [sb(f"pc{h}", [128, 256], BF16) for h in range(4)]; PCS = [Buf() for _ in range(4)]
        pcts = [sb(f"pct{h}", [128, 2, 128], BF16) for h in range(4)]; PCTS = [Buf() for _ in range(4)]
        smh = sb("smh", [128, 32]); SMH = [Buf() for _ in range(4)]
        sc = sb("sc", [128, 64]); SC = Buf()
        sc2 = sb("sc2", [128, 64]); SC2 = Buf()
        mx8 = sb("mx8", [128, 16]); MX8 = Buf()
        nst = sb("nst", [128, 64], BF16); NST = Buf()
        nsh = sb("nsh", [128, 512], BF16); NSH = Buf()
        yg = sb("yg", [128, 4, 256]); YG = Buf()
        ygt = sb("ygt", [128, 2, 512]); YGT = Buf()
        pt = [sb(f"pt{i}", [128, 512], BF16) for i in range(3)]; PT = [Buf() for _ in range(3)]
        p2 = [sb(f"p2{i}", [128, 512], BF16) for i in range(3)]; P2 = [Buf() for _ in range(3)]
        rin = sb("rin", [128, 8]); RIN = Buf()
        zb = sb("zb", [128, 512], BF16); ZB = Buf()
        otmp = sb("otmp", [128, 4, 64]); OTMP = Buf()
        pg = [ps(f"pg{i}", [128, 512]) for i in range(3)]; PG = [Buf(excl=True) for _ in range(3)]
        ptb = ps("ptb", [128, 1024], BF16); PTB = Buf(excl=True)
        pl = [ps(f"pl{i}", [128, 512]) for i in range(2)]; PL = [Buf(excl=True) for _ in range(2)]
        pacc = [ps(f"pacc{i}", [128, 4, 128]) for i in range(2)]; PACC = [Buf(excl=True) for _ in range(2)]

        gi = [0]

        def gbank():
            i = gi[0] % 3; gi[0] += 1
            return pg[i], PG[i]

        dbg_n = [0]

        def dbg(name, ap, B):
            if not debug:
                return
            d = nc.dram_tensor("dbg_" + name, list(ap.shape), F32, kind="ExternalOutput").ap()
            dbg_n[0] += 1
            cntv = dbg_n[0] * 16

            def f(e, s, d=d, ap=ap, cntv=cntv):
                e.dma_start(out=d, in_=ap).then_inc(s, 16)
                e.wait_ge(s, cntv)
            kb.dma("pool", "dbg", f, reads=[B])

        em.dma("pool", "wf", wf[:, :, :], wf_d.rearrange("(kc p) n -> p kc n", p=128), [], [WF])
        em.dma("pool", "wt", wt[:, :, 0:140], wt_d.rearrange("(kc p) n -> p kc n", p=128), [], [WT])
        em.dma("pool", "w1", w1[:, :, :], w1_d.rearrange("p (a b) -> p a b", b=128), [], [W1])
        em.dma("pool", "pe", peT[:], pe_d[:, :], [], [PET])
        em.dma("sp", "w2", w2f[:], w2_d[:, :], [], [W2])
        em.dma("sp", "btc", btc[:].rearrange("p h w -> p (h w)"), btc_d[:, :], [], [BTC])
        em.dma("sp", "mkc", mkc[:], mkc_d[:, :], [], [MKC])
        em.dma("sp", "m12", m12[:], m12_d[:, :], [], [M12])
        em.dma("sp", "rv", rv[:], rv_d[:, :], [], [RV])
        em.dma("sp", "c2s", c2sf[:], c2s_d[:, :], [], [C2SF])
        em.ms("pool", exd[:], 0.0, [EXD])
        em.dma("pool", "exd", exd[0:64, :, :].rearrange("p a b -> p (a b)"), exd_d[:, :], [EXD], [EXD])
        em.dma("sp", "hsel", hself[:], hsel_d[:, :], [], [HSEL])
        em.cp("dve", w2[:], w2f[:], [W2], [W2])
        em.cp("dve", hsel[:].rearrange("p a b -> p (a b)"), hself[:], [HSEL], [HSEL])
        em.ms("pool", identf[:], 1.0, [IDENT])
        kb.op("pool", lambda e: e.affine_select(out=identf[:], in_=identf[:], pattern=[[-1, 128]], compare_op=ALU.is_equal,
                                                fill=0.0, base=0, channel_multiplier=1), [IDENT], [IDENT])
        em.cp("pool", ident[:], identf[:], [IDENT], [IDENT])
        em.ms("dve", vs[:, :, 64:65], 1.0, [VS])
        em.ms("dve", vw[:, :, 64:65], 1.0, [VW])
        em.ms("dve", vca[:], 0.0, [VCA])
        em.ms("dve", zb[:], 0.0, [ZB])
        em.ms("dve", nsh[:], 0.0, [NSH])
        for h_ in range(4):
            em.ms("dve", pcs[h_][:], 0.0, [PCS[h_]])
        em.ms("dve", kct[:], 0.0, [KCT])
        for h in range(4):
            em.tt("dve", btc[:, h, :], btc[:, h, :], mkc[:], ALU.add, [BTC, MKC], [BTC])
        first = True
        for hf in range(2):
            cs_ = slice(hf * HW_, (hf + 1) * HW_)
            em.dma("sp", "mks", mks[:], mks_d[:, cs_], [], [MKS])
            em.dma("sp", "mkw", mkw[:], mkw_d[:, cs_], [], [MKW])
            for h in range(4):
                em.dma("sp", "stg", stg[:], bts_d[:, h * TW + hf * HW_:h * TW + (hf + 1) * HW_], [], [STG])
                if first:
                    em.red(cst[:, 8:9], stg[:], ALU.max, [STG], [CST])
                    first = False
                else:
                    em.red(cst[:, 9:10], stg[:], ALU.max, [STG], [CST])
                    em.tt("dve", cst[:, 8:9], cst[:, 8:9], cst[:, 9:10], ALU.max, [CST], [CST])
                em.tt("dve", stg2[:], stg[:], mks[:], ALU.add, [STG, MKS], [STG2])
                em.act(ebs[h][:, cs_], stg2[:], AF.Exp, [STG2], [EBS[h]])
                em.tt("dve", stg2[:], stg[:], mkw[:], ALU.add, [STG, MKW], [STG2])
                em.act(ebw[h][:, cs_], stg2[:], AF.Exp, [STG2], [EBW[h]])

        import os as _os
        PH = int(_os.environ.get('PH', '9'))
        def load_x(sg):
            sl = sg % 2
            em.dma("pool", f"xs{sl}", xs[sl][:, :, :], xT_v[:, :, sg * 512:(sg + 1) * 512], [], [XS[sl]])

        load_x(0)
        for sg in range(8 if PH >= 2 else 0):
            sl = sg % 2
            if sg + 1 < 8:
                load_x(sg + 1)
            seg = slice(sg * 512, (sg + 1) * 512)
            dsts = [(qT[0], QT[0], 0.125), (qT[1], QT[1], 0.125), (ksT, KST, 1.0), (kwT, KWT, 1.0), (kcv, KCV, 1.0)]
            for j, (dst, DST, scl) in enumerate(dsts):
                p, P = gbank()
                for k in range(8):
                    em.mm(p[:, :], wf[:, k, j * 128:(j + 1) * 128], xs[sl][:, k, :], k == 0, k == 7, [WF, XS[sl]], [P])
                em.act(dst[:, seg], p[:, :], AF.Copy, [P], [DST], scale=scl)
            PJ = _os.environ.get('PJ', 'abc12')
            for tt_ in range(4 if 'b' in PJ else 0):
                tile_i = sg * 4 + tt_
                p, P = gbank()
                for k in range(8):
                    em.mm(p[:, 0:140], xs[sl][:, k, tt_ * 128:(tt_ + 1) * 128], wt[:, k, 0:140], k == 0, k == 7, [WT, XS[sl]], [P])
                if '1' in PJ:
                    em.cp("dve", vs[:, tile_i, 0:64], p[:, 0:64], [P], [VS])
                    em.cp("dve", vw[:, tile_i, 0:64], p[:, 64:128], [P], [VW])
                if '2' in PJ:
                    em.act(gt[:, tile_i, 0:12], p[:, 128:140], AF.Sigmoid, [P], [GT])
            for i in range(2 if 'c' in PJ else 0):
                em.tt("dve", qsq[:], qT[i][:, seg], qT[i][:, seg], ALU.mult, [QT[i]], [QSQ])
                for hh in range(2):
                    p, P = gbank()
                    em.mm(p[:, :], hsel[:, hh, :], qsq[:], True, True, [HSEL, QSQ], [P])
                    em.red(mxc[:, 2 * i + hh, sg:sg + 1], p[:, :], ALU.max, [P], [MXC])
            for i, (src, SRC) in enumerate(((ksT, KST), (kwT, KWT)) if 'c' in PJ else ()):
                em.tt("dve", qsq[:], src[:, seg], src[:, seg], ALU.mult, [SRC], [QSQ])
                p, P = gbank()
                em.mm(p[:, :], hsel[:, 0, :], qsq[:], True, True, [HSEL, QSQ], [P])
                em.red(kxc[:, i, sg:sg + 1], p[:, :], ALU.max, [P], [KXC])
        if PH < 3:
            nq = 0
        em.red(sm[:, 0:4], mxc[:], ALU.max, [MXC], [SM])
        em.red(sm[:, 4:6], kxc[:], ALU.max, [KXC], [SM])
        for br in range(2):
            em.ts("dve", sm[:, 8 + 4 * br:12 + 4 * br], sm[:, 0:4], sm[:, 4 + br:5 + br], None, ALU.mult, None, [SM], [SM])
        em.act(sm[:, 16:24], sm[:, 8:16], AF.Sqrt, [SM], [SM])
        em.ts("dve", cst[:, 0:8], sm[:, 16:24], cst[:, 8:9], -1.0, ALU.add, ALU.mult, [SM, CST], [CST])

        for br in range(2 if PH >= 3 else 0):
            rows = slice(64 * br, 64 * br + 64)
            p, P = gbank()
            for pp in range(32):
                em.mm(p[:, 0:255], w1[rows, pp, :], kcv[rows, pp:pp + 16 * 254 + 1:16], pp == 0, pp == 31, [W1, KCV], [P])
            pb, PB = gbank()
            for pp in range(32):
                em.mm(pb[:, 0:1], w1[rows, pp, :], peT[rows, pp:pp + 1], pp == 0, pp == 31, [W1, PET], [PB])
            em.cp("dve", hb[:, br:br + 1], pb[:, 0:1], [PB], [HB])
            em.ts("dve", hx[:, 0:255], p[:, 0:255], hb[:, br:br + 1], None, ALU.add, None, [P, HB], [HX])
            em.tt("dve", hy[:, 0:255], hx[:, 0:255], hx[:, 0:255], ALU.mult, [HX], [HY])
            em.ts("dve", hy[:, 0:255], hy[:, 0:255], 0.044715, 1.0, ALU.mult, ALU.add, [HY], [HY])
            em.tt("dve", hy[:, 0:255], hy[:, 0:255], hx[:, 0:255], ALU.mult, [HY, HX], [HY])
            em.act(hy[:, 0:255], hy[:, 0:255], AF.Tanh, [HY], [HY], scale=0.7978845608028654)
            em.ts("dve", hy[:, 0:255], hy[:, 0:255], 1.0, 0.5, ALU.add, ALU.mult, [HY], [HY])
            em.tt("dve", gh[br][:, 0:255], hy[:, 0:255], hx[:, 0:255], ALU.mult, [HY, HX], [GH[br]])
        p, P = gbank()
        em.mm(p[:, 0:255], w2[:, 0:128], gh[0][:, 0:255], True, True, [W2, GH[0]], [P])
        em.act(kct[:, 0:255], p[:, 0:255], AF.Copy, [P], [KCT])
        for ct in range(2):
            ncs = 128 if ct == 0 else 127
            p, P = gbank()
            em.mm(p[0:ncs, 0:64], gh[1][:, ct * 128:ct * 128 + ncs], w2[:, 128:192], True, True, [W2, GH[1]], [P])
            em.act(vca[0:ncs, ct, 0:64], p[0:ncs, 0:64], AF.Copy, [P], [VCA])
        em.cp("dve", vca[:, :, 64:128], c2sf[:].rearrange("p (a b) -> p a b", b=64), [C2SF], [VCA])
        dbg("kct", kct[:, 0:255], KCT); dbg("vca", vca[:].rearrange("p a b -> p (a b)"), VCA)
        dbg("cst", cst[:, 0:9], CST)

        pti = [0]
        for Q in range(nq):
            qs = slice(Q * 512, (Q + 1) * 512)
            for a in range(4):
                n = 4 * Q + a
                t0 = 128 * n
                ncol = min(255, 8 * n + 7)
                off = 248 - 8 * n
                ncp = min(256, (ncol + 31) // 32 * 32)
                nct = 1 if ncol <= 128 else 2
                for hpair in ((0, 1), (2, 3)):
                    HP = {}
                    for h in hpair:
                        rows = slice(64 * (h % 2), 64 * (h % 2) + 64)
                        p, P = gbank()
                        em.mm(p[:, 0:ncp], qT[h // 2][rows, t0:t0 + 128], kct[rows, 0:ncp], True, True, [QT[h // 2], KCT], [P])
                        HP[h] = (p, P)
                    for h in hpair:
                        p, P = HP[h]
                        s0 = 8 * h
                        em.tt("dve", lgs[h][:, 0:ncol], p[:, 0:ncol], btc[:, h, off:off + ncol], ALU.add, [P, BTC], [LGS[h]])
                        em.red(smh[:, s0:s0 + 1], lgs[h][:, 0:ncol], ALU.max, [LGS[h]], [SMH[h]])
                        em.ts("dve", smh[:, s0 + 1:s0 + 2], smh[:, s0:s0 + 1], -1.0, None, ALU.mult, None, [SMH[h]], [SMH[h]])
                        em.ms("dve", smh[:, s0 + 2:s0 + 3], 0.0, [SMH[h]])
                    for h in hpair:
                        s0 = 8 * h
                        em.act(pcs[h][:, 0:ncol], lgs[h][:, 0:ncol], AF.Exp, [LGS[h], SMH[h]], [PCS[h], SMH[h]], bias=smh[:, s0 + 1:s0 + 2], accum_out=smh[:, s0 + 2:s0 + 3])
                    for h in hpair:
                        for ct in range(nct):
                            em.tr(ptb[:, ct * 128:(ct + 1) * 128], pcs[h][:, ct * 128:(ct + 1) * 128], ident[:], [PCS[h], IDENT], [PTB])
                        em.cp("dve", pcts[h][:, 0:nct, :], ptb[:, 0:nct * 128].rearrange("p (a b) -> p a b", b=128), [PTB], [PCTS[h]])
                    PO_ = {}
                    for h in hpair:
                        po, PO = gbank()
                        for ct in range(nct):
                            em.mm(po[:, 0:128], pcts[h][:, ct, :], vca[:, ct, :], ct == 0, ct == nct - 1, [PCTS[h], VCA], [PO])
                        PO_[h] = (po, PO)
                    for h in hpair:
                        po, PO = PO_[h]
                        s0 = 8 * h
                        em.rcp(smh[:, s0 + 3:s0 + 4], smh[:, s0 + 2:s0 + 3], [SMH[h]], [SMH[h]])
                        if n == 0:
                            em.tt("dve", smh[:, s0 + 3:s0 + 4], smh[:, s0 + 3:s0 + 4], rv[:], ALU.mult, [SMH[h], RV], [SMH[h]])
                        em.tt("dve", smh[:, s0 + 4:s0 + 5], smh[:, s0 + 3:s0 + 4], gt[:, n, 3 * h:3 * h + 1], ALU.mult, [SMH[h], GT], [SMH[h]])
                        em.ts("dve", yg[:, a, h * 64:(h + 1) * 64], po[:, 0:64], smh[:, s0 + 4:s0 + 5], None, ALU.mult, None, [PO, SMH[h]], [YG])
                        if h == 0:
                            em.ts("dve", sc[:], po[:, 64:128], smh[:, s0 + 3:s0 + 4], None, ALU.mult, None, [PO, SMH[h]], [SC])
                        else:
                            em.stt("dve", sc[:], po[:, 64:128], smh[:, s0 + 3:s0 + 4], sc[:], ALU.mult, ALU.add, [PO, SMH[h], SC], [SC])
                w0 = 64 - 2 * n
                em.tt("dve", sc[:], sc[:], m12[:, w0:w0 + 64], ALU.mult, [SC, M12], [SC])
                em.tt("dve", sc[:], sc[:], m12[:, 128 + w0:128 + w0 + 64], ALU.add, [SC, M12], [SC])
                em.ms("dve", sc[:, 0:1], 1.0e4, [SC])
                kb.op("dve", lambda e: e.max(out=mx8[:, 0:8], in_=sc[:]), [SC], [MX8])
                kb.op("dve", lambda e: e.match_replace(out=sc2[:], in_to_replace=mx8[:, 0:8], in_values=sc[:], imm_value=-1.0e9), [SC, MX8], [SC2])
                kb.op("dve", lambda e: e.max(out=mx8[:, 8:16], in_=sc2[:]), [SC2], [MX8])
                em.red(sm[:, 40:41], mx8[:, 8:16], ALU.min, [MX8], [SM])
                em.ts("dve", nst[:], sc[:], sm[:, 40:41], NEG, ALU.is_lt, ALU.mult, [SC, SM], [NST])
                em.tr(ptb[0:64, 256:384], nst[:], ident[:], [NST, IDENT], [PTB])
                em.cp("dve", nsh[0:64, a * 128:(a + 1) * 128], ptb[0:64, 256:384], [PTB], [NSH])
                if Q == 0 and a == 1:
                    dbg("sc", sc[:], SC); dbg("yg1", yg[:, 1, :], YG)
            QP = _os.environ.get('QP', 'abc')
            for br in [b_ for b_ in range(2) if 'bc'[b_] in QP]:
                kT, KT_, vv, VV, eb, EB = (ksT, KST, vs, VS, ebs, EBS) if br == 0 else (kwT, KWT, vw, VW, ebw, EBW)
                m_lo = 0 if br == 0 else max(0, 4 * Q - 4)
                m_hi = 4 * Q + 3
                tiles = [(h, m) for h in range(4) for m in range(m_lo, m_hi + 1)]
                slot = {}

                def stage1(ti):
                    h, m = tiles[ti]
                    rows = slice(64 * (h % 2), 64 * (h % 2) + 64)
                    pli = pti[0] % 2
                    bi = pti[0] % 3
                    pti[0] += 1
                    slot[ti] = (pli, bi)
                    pl_, PL_ = pl[pli], PL[pli]
                    em.mm(pl_[:, :], kT[rows, m * 128:(m + 1) * 128], qT[h // 2][rows, qs], True, br == 1, [KT_, QT[h // 2]], [PL_])
                    if br == 0:
                        em.mm(pl_[:, :], exd[:, m, :], nsh[:, :], False, True, [EXD, NSH], [PL_])

                def stage23(ti):
                    h, m = tiles[ti]
                    pli, bi = slot.pop(ti)
                    pl_, PL_ = pl[pli], PL[pli]
                    acc, ACC = pacc[h % 2], PACC[h % 2]
                    D0 = 512 * Q - 128 * m
                    wst = min(D0, DCL) + 512
                    em.act(pt[bi][:], pl_[:, :], AF.Exp, [PL_, CST], [PT[bi]], bias=cst[:, 4 * br + h:4 * br + h + 1])
                    em.tt("dve", p2[bi][:], pt[bi][:], eb[h][:, wst:wst + 512], ALU.mult, [PT[bi], EB[h]], [P2[bi]])
                    if m == m_lo:
                        em.mm(acc[:].rearrange("p a b -> p (a b)"), zb[:, 0:128], zb[:, 0:512], True, False, [ZB], [ACC])
                    for a in range(4):
                        last_m = min(m_hi, 4 * Q + a)
                        if m > last_m:
                            continue
                        a_lo = m_lo if br == 0 else max(m_lo, 4 * Q + a - 4)
                        if m < a_lo:
                            continue
                        em.mm(acc[:, a, 0:65], p2[bi][:, a * 128:(a + 1) * 128], vv[:, m, 0:65], False, (m == m_hi and a == 3), [P2[bi], VV], [ACC])
                    if m == m_hi:
                        em.rcp(rin[:, 0:4], acc[:, :, 64], [ACC], [RIN])
                        em.tt("dve", rin[:, 4:8], rin[:, 0:4], gt[:, 4 * Q:4 * Q + 4, 3 * h + 1 + br], ALU.mult, [RIN, GT], [RIN])
                        em.tt("dve", otmp[:], acc[:, :, 0:64], rin[:, 4:8].unsqueeze(2).to_broadcast([128, 4, 64]), ALU.mult, [ACC, RIN], [OTMP])
                        em.tt("pool", yg[:, :, h * 64:(h + 1) * 64], yg[:, :, h * 64:(h + 1) * 64], otmp[:], ALU.add, [YG, OTMP], [YG])
                stage1(0)
                for ti in range(len(tiles)):
                    if ti + 1 < len(tiles):
                        stage1(ti + 1)
                    stage23(ti)
            for a in range(4):
                for hp in range(2):
                    pz, PZ = gbank()
                    em.tr(pz[:, 0:128], yg[:, a, hp * 128:(hp + 1) * 128], identf[:], [YG, IDENT], [PZ])
                    em.cp("dve" if hp else "pool_never", ygt[:, hp, a * 128:(a + 1) * 128], pz[:, 0:128], [PZ], [YGT]) if False else em.cp("dve", ygt[:, hp, a * 128:(a + 1) * 128], pz[:, 0:128], [PZ], [YGT])
            for hp in range(2):
                em.dma("sp", "y", yT_d[hp * 128:(hp + 1) * 128, Q * 512:(Q + 1) * 512], ygt[:, hp, :], [YGT], [])
        kb.finish([YG, YGT])


def build_a2(nq=8, debug=False):
    nc = bass.Bass("TRN2", target_bir_lowering=False)
    I = lambda n, shp: nc.dram_tensor(n, shp, F32, kind="ExternalInput").ap()
    D = {"xT": I("xT", [1024, S]), "wf": I("wf", [1024, 640]), "wt": I("wt", [1024, 140]), "w1": I("w1", [128, 32 * 128]), "peT": I("peT", [128, 32]), "w2": I("w2", [128, 192]), "btc": I("btc", [128, 4 * 256]), "mkc": I("mkc", [128, 256]), "bts": I("bts", [128, 4 * TW]), "mks": I("mks", [128, TW]), "mkw": I("mkw", [128, TW]), "m12": I("m12", [128, 256]), "rv": I("rv", [128, 1]), "c2s": I("c2s", [128, 128]), "exd": I("exd", [64, 32 * 128]), "hsel": I("hsel", [128, 256])}
    D["yT"] = nc.dram_tensor("yT", [256, S], F32, kind="ExternalOutput").ap()
    with ExitStack() as st:
        kb = KB(nc, st)
        a2_body(nc, kb, D, nq, debug)
        kb.emit()
    return nc


def a2_consts():
    i = np.arange(128)[:, None]
    j = np.arange(256)[None, :]
    dc = i - 16 * (j - 248) - 31
    mkc = np.where(dc >= 0, 0.0, NEG).astype(np.float32)
    w = np.arange(TW)[None, :]
    ds = w - i - 512
    mks = np.where(ds >= 0, 0.0, NEG).astype(np.float32)
    mkw = np.where((ds >= 0) & (ds < 512), 0.0, NEG).astype(np.float32)
    wv = np.arange(128)[None, :] - 64
    cur = (i >= 64).astype(np.int64)
    forced = (wv == cur) | (wv == cur - 1)
    valid = wv <= cur
    m1 = (valid & ~forced).astype(np.float32)
    m2 = np.where(forced, 1.0e4, np.where(valid, 0.0, -1.0)).astype(np.float32)
    m12 = np.concatenate([m1, m2], axis=1)
    rv = (np.arange(128) >= 31).astype(np.float32)[:, None]
    cs = np.arange(255)[:, None] * 16; ss = np.arange(64)[None, :] * 64
    ov = np.clip(np.minimum(cs + 32, ss + 64) - np.maximum(cs, ss), 0, None).astype(np.float32) / 32
    c2s = np.zeros((256, 64), np.float32); c2s[:255] = ov
    c2s = c2s.reshape(2, 128, 64).transpose(1, 0, 2).reshape(128, 128)
    exd = np.zeros((64, 32, 128), np.float32)
    for m in range(32):
        exd[2 * m, m, 0:64] = 1.0; exd[2 * m + 1, m, 64:128] = 1.0
    hsel = np.zeros((128, 2, 128), np.float32); hsel[0:64, 0, :] = 1.0; hsel[64:128, 1, :] = 1.0
    return dict(mkc=mkc, mks=mks, mkw=mkw, m12=m12, rv=rv, c2s=c2s, exd=exd.reshape(64, 32 * 128), hsel=hsel.reshape(128, 256),
                dc=dc, ds=ds)


def a2_inputs(inp, l, g, consts):
    w_in = inp["w_in"][l]; zr = 1792
    q = w_in[:, zr + 256 * g: zr + 256 * g + 256]
    def kvc(off):
        return w_in[:, zr + off + 64 * g: zr + off + 64 * g + 64]
    kc, vc, ks, vs, kw, vw = (kvc(o) for o in (512, 640, 768, 896, 1024, 1152))
    gates = w_in[:, zr + 1280 + 12 * g: zr + 1280 + 12 * g + 12]
    wf = np.concatenate([q, ks, ks, kw, kw, kc, vc], axis=1)
    wt = np.concatenate([vs, vw, gates], axis=1)
    def w1r(w):
        return w.reshape(32, 64, 128).transpose(1, 0, 2)
    w1 = np.concatenate([w1r(inp["cmp_w1_k"][l]), w1r(inp["cmp_w1_v"][l])], axis=0).reshape(128, 32 * 128)
    peT = np.concatenate([inp["cmp_pe_k"][l].T, inp["cmp_pe_v"][l].T], axis=0)
    w2 = np.concatenate([inp["cmp_w2_k"][l], inp["cmp_w2_k"][l], inp["cmp_w2_v"][l]], axis=1)
    rb = inp["rel_bias"][:, 4 * g:4 * g + 4]
    btc = np.take(rb, t5_bucket(consts["dc"]), axis=0).transpose(0, 2, 1).reshape(128, 4 * 256)
    bts = np.take(rb, t5_bucket(consts["ds"]), axis=0).transpose(0, 2, 1).reshape(128, 4 * TW)
    out = dict(wf=wf, wt=wt, w1=w1, peT=peT, w2=w2, btc=btc, bts=bts)
    for k in ("mkc", "mks", "mkw", "m12", "rv", "c2s", "exd", "hsel"):
        out[k] = consts[k]
    return {k: np.ascontiguousarray(v, dtype=np.float32) for k, v in out.items()}


NT = 2048
ALPHA = 8 ** 0.25
LN_EPS = 1e-5


def layer_norm_fm(kb, em, gbank, R, RB, out_fn, g_ap, b_ap, GB, tmp, TMP, ones, ONES, sq, SQ, mean, MEAN, rstd, RSTD):
    pm, PM = gbank()
    for i in range(8):
        em.mm(pm[:, :], ones[:], R[:, i, :], i == 0, i == 7, [ONES, RB], [PM])
    em.act(mean[:], pm[:, :], AF.Copy, [PM], [MEAN], scale=1.0 / 1024)
    pv, PV = gbank()
    for i in range(8):
        em.tt("pool" if i % 2 else "dve", sq[i % 2][:], R[:, i, :], R[:, i, :], ALU.mult, [RB], [SQ[i % 2]])
        em.mm(pv[:, :], ones[:], sq[i % 2][:], i == 0, i == 7, [ONES, SQ[i % 2]], [PV])
    em.tt("dve", tmp[:], mean[:], mean[:], ALU.mult, [MEAN], [TMP])
    em.stt("dve", rstd[:], pv[:, :], 1.0 / 1024, tmp[:], ALU.mult, ALU.subtract, [PV, TMP], [RSTD])
    em.ts("dve", rstd[:], rstd[:], LN_EPS, None, ALU.add, None, [RSTD], [RSTD])
    em.act(rstd[:], rstd[:], AF.Sqrt, [RSTD], [RSTD])
    em.rcp(rstd[:], rstd[:], [RSTD], [RSTD])
    for i in range(8):
        eng = "pool" if i % 2 else "dve"
        em.tt(eng, tmp[:], R[:, i, :], mean[:], ALU.subtract, [RB, MEAN], [TMP])
        em.tt(eng, tmp[:], tmp[:], rstd[:], ALU.mult, [TMP, RSTD], [TMP])
        o, O = out_fn(i)
        em.ts(eng, o, tmp[:], g_ap[:, i:i + 1], b_ap[:, i:i + 1], ALU.mult, ALU.add, [TMP, GB], [O])


def b1_body(nc, kb, D):
    xT_d, yr_d, yn_d, wg_d, wur_d, wun_d, wo_d, ln_d, o_d = (D[k] for k in ("xT", "yrT", "ynT", "wg", "wur", "wun", "wo", "ln", "x1T"))
    v3 = lambda ap: ap.rearrange("(kc p) n -> p kc n", p=128)
    if True:
        em = Em(kb); sb, ps = kb.sb, kb.ps
        wg = sb("wg", [128, 8, 2048], BF16); WG = Buf()
        wur = sb("wur", [128, 4, 1024], BF16); WUR = Buf()
        wun = sb("wun", [128, 4, 1024], BF16); WUN = Buf()
        wo = sb("wo", [128, 8, 1024], BF16); WO = Buf()
        ln = sb("ln", [128, 16]); LN = Buf()
        ones = sb("ones", [128, 128]); ONES = Buf()
        xf = sb("xf", [128, 8, 512]); XF = Buf()
        xb = sb("xb", [128, 8, 512], BF16); XB = Buf()
        yr = sb("yr", [128, 4, 512], BF16); YR = Buf()
        yn = sb("yn", [128, 4, 512], BF16); YN = Buf()
        sg = [sb(f"sg{i}", [128, 512]) for i in range(2)]; SG = [Buf() for _ in range(2)]
        m1 = sb("m1", [128, 512]); M1 = Buf()
        mg = sb("mg", [128, 8, 512], BF16); MG = Buf()
        R = sb("R", [128, 8, 512]); RB = Buf()
        ob = sb("ob", [128, 8, 512]); OB = Buf()
        tmp = sb("tmp", [128, 512]); TMP = Buf()
        sq = [sb(f"sq{i}", [128, 512]) for i in range(2)]; SQ = [Buf() for _ in range(2)]
        mean = sb("mean", [128, 512]); MEAN = Buf()
        rstd = sb("rstd", [128, 512]); RSTD = Buf()
        pg = [ps(f"pg{i}", [128, 512]) for i in range(6)]; PG = [Buf(excl=True) for _ in range(6)]
        gi = [0]

        def gbank():
            i = gi[0] % 6; gi[0] += 1
            return pg[i], PG[i]
        for k0 in range(0, 8, 2):
            em.dma("pool", "wg", wg[:, k0:k0 + 2, :], v3(wg_d)[:, k0:k0 + 2, :], [], [WG])
        em.dma("pool", "wur", wur[:, :, :], v3(wur_d), [], [WUR])
        em.dma("pool", "wun", wun[:, :, :], v3(wun_d), [], [WUN])
        em.dma("pool", "wo", wo[:, :, :], v3(wo_d), [], [WO])
        em.dma("sp", "ln", ln[:], ln_d[:, :], [], [LN])
        em.ms("dve", ones[:], 1.0, [ONES])
        for tg in range(NT // 512):
            ts_ = slice(tg * 512, (tg + 1) * 512)
            em.dma("sp", "xf", xf[:, :, :], v3(xT_d)[:, :, ts_], [], [XF])
            em.dma("pool", "yr", yr[:, :, :], v3(yr_d)[:, :, ts_], [], [YR])
            em.dma("pool", "yn", yn[:, :, :], v3(yn_d)[:, :, ts_], [], [YN])
            for i in range(8):
                em.cp("pool" if i % 2 else "dve", xb[:, i, :], xf[:, i, :], [XF], [XB])
            for j in range(8):
                cs = slice(j * 128, (j + 1) * 128)
                for br, (wu, WU, yy, YY) in enumerate(((wur, WUR, yr, YR), (wun, WUN, yn, YN))):
                    p, P = gbank()
                    for k in range(8):
                        em.mm(p[:, :], wg[:, k, br * 1024 + j * 128: br * 1024 + (j + 1) * 128], xb[:, k, :], k == 0, k == 7, [WG, XB], [P])
                    em.act(sg[br][:], p[:, :], AF.Sigmoid, [P], [SG[br]])
                    p2, P2 = gbank()
                    for k in range(4):
                        em.mm(p2[:, :], wu[:, k, cs], yy[:, k, :], k == 0, k == 3, [WU, YY], [P2])
                    if br == 0:
                        em.tt("dve", m1[:], sg[0][:], p2[:, :], ALU.mult, [SG[0], P2], [M1])
                    else:
                        em.tt("dve", sg[1][:], sg[1][:], p2[:, :], ALU.mult, [SG[1], P2], [SG[1]])
                        em.tt("pool", mg[:, j, :], m1[:], sg[1][:], ALU.add, [M1, SG[1]], [MG])
            for i in range(8):
                p, P = gbank()
                for k in range(8):
                    em.mm(p[:, :], wo[:, k, i * 128:(i + 1) * 128], mg[:, k, :], k == 0, k == 7, [WO, MG], [P])
                em.stt("dve", R[:, i, :], xf[:, i, :], ALPHA, p[:, :], ALU.mult, ALU.add, [XF, P], [RB])
            layer_norm_fm(kb, em, gbank, R, RB, lambda i: (ob[:, i, :], OB), ln[:, 0:8], ln[:, 8:16], LN, tmp, TMP, ones, ONES, sq, SQ, mean, MEAN, rstd, RSTD)
            em.dma("sp", "ob", v3(o_d)[:, :, ts_], ob[:, :, :], [OB], [])
        kb.finish([OB])


def build_b1():
    nc = bass.Bass("TRN2", target_bir_lowering=False)
    I = lambda n, shp: nc.dram_tensor(n, shp, F32, kind="ExternalInput").ap()
    D = dict(xT=I("xT", [1024, NT]), yrT=I("yrT", [512, NT]), ynT=I("ynT", [512, NT]), wg=I("wg", [1024, 2048]), wur=I("wur", [512, 1024]),
             wun=I("wun", [512, 1024]), wo=I("wo", [1024, 1024]), ln=I("ln", [128, 16]),
             x1T=nc.dram_tensor("x1T", [1024, NT], F32, kind="ExternalOutput").ap())
    with ExitStack() as st:
        kb = KB(nc, st)
        b1_body(nc, kb, D)
        kb.emit()
    return nc


def b1_inputs(inp, l):
    w_in = inp["w_in"][l]
    lnp = np.concatenate([inp["ln1_g"][l].reshape(8, 128).T, inp["ln1_b"][l].reshape(8, 128).T], axis=1)
    return dict(wg=np.ascontiguousarray(w_in[:, 1792 + 1304: 1792 + 1304 + 2048]), wur=inp["w_up_rwkv"][l], wun=inp["w_up_nsa"][l],
                wo=inp["w_out"][l], ln=np.ascontiguousarray(lnp, dtype=np.float32))


def b2_body(nc, kb, D, nexp=32):
    x1_d, wr_d, br_d, w1_d, w3_d, w2_d, ln_d, selb_d, g2e_d, o_d = (D[k] for k in ("x1T", "wr", "brr", "ew1", "ew3", "ew2", "ln", "selb", "g2e", "x2T"))
    v3 = lambda ap: ap.rearrange("(kc p) n -> p kc n", p=128)
    if True:
        em = Em(kb); sb, ps = kb.sb, kb.ps
        wr = sb("wr", [128, 8, 64]); WR = Buf()
        brr = sb("brr", [128, 36]); BRR = Buf()
        ln = sb("ln", [128, 16]); LN = Buf()
        selb = sb("selb", [32, 32, 128], BF16); SELB = Buf()
        g2e = sb("g2e", [128, 4, 32]); G2E = Buf()
        ones = sb("ones", [128, 128]); ONES = Buf()
        ident = sb("ident", [128, 128]); IDENT = Buf()
        xf = sb("xf", [128, 8, 512]); XF = Buf()
        x1b = sb("x1b", [128, 8, NT], BF16); X1B = [Buf() for _ in range(4)]
        out = sb("out", [128, 8, NT]); OUT = [Buf() for _ in range(4)]
        cwt = sb("cwt", [32, NT], BF16); CWT = [Buf() for _ in range(4)]
        lgt = sb("lgt", [128, 36]); LGT = Buf()
        rs = sb("rs", [128, 64]); RS = Buf()
        em32 = sb("em32", [128, 32]); EM32 = Buf()
        em2 = sb("em2", [128, 32]); EM2 = Buf()
        cw = sb("cw", [128, 32]); CW = Buf()
        w1 = [sb(f"w1_{i}", [128, 8, 512], BF16) for i in range(2)]; W1 = [Buf() for _ in range(2)]
        w3 = [sb(f"w3_{i}", [128, 8, 512], BF16) for i in range(2)]; W3 = [Buf() for _ in range(2)]
        w2 = [sb(f"w2_{i}", [128, 4, 1024], BF16) for i in range(2)]; W2 = [Buf() for _ in range(2)]
        cwb = sb("cwb", [128, 512]); CWB = Buf()
        sl_ = [sb(f"sl{i}", [128, 512]) for i in range(2)]; SL = [Buf() for _ in range(2)]
        hb = sb("hb", [128, 4, 512], BF16); HB = [Buf() for _ in range(4)]
        ob = xf; OB = XF
        tmp = sb("tmp", [128, 512]); TMP = Buf()
        sq = [sb(f"sq{i}", [128, 512]) for i in range(2)]; SQ = [Buf() for _ in range(2)]
        mean = sb("mean", [128, 512]); MEAN = Buf()
        rstd = sb("rstd", [128, 512]); RSTD = Buf()
        pg = [ps(f"pg{i}", [128, 512]) for i in range(8)]; PG = [Buf(excl=True) for _ in range(8)]
        gi = [0]

        def gbank():
            i = gi[0] % 8; gi[0] += 1
            return pg[i], PG[i]
        em.ms("dve", wr[:], 0.0, [WR])
        em.dma("sp", "wr", wr[:, :, 0:36], v3(wr_d), [WR], [WR])
        em.dma("sp", "brr", brr[:], br_d[0:1, :].partition_broadcast(128), [], [BRR])
        em.dma("sp", "ln", ln[:], ln_d[:, :], [], [LN])
        em.dma("pool", "selb", selb[:].rearrange("p a b -> p (a b)"), selb_d[:, :], [], [SELB])
        em.dma("sp", "g2e", g2e[:].rearrange("p a b -> p (a b)"), g2e_d[:, :], [], [G2E])
        em.ms("dve", ones[:], 1.0, [ONES])
        em.ms("pool", ident[:], 1.0, [IDENT])
        kb.op("pool", lambda e: e.affine_select(out=ident[:], in_=ident[:], pattern=[[-1, 128]], compare_op=ALU.is_equal,
                                                fill=0.0, base=0, channel_multiplier=1), [IDENT], [IDENT])

        def load_w(e):
            s = e % 2
            for k0 in range(0, 8, 4):
                em.dma("pool", f"w1_{s}", w1[s][:, k0:k0 + 4, :], w1_d[e].rearrange("(kc p) n -> p kc n", p=128)[:, k0:k0 + 4, :], [], [W1[s]])
                em.dma("pool", f"w3_{s}", w3[s][:, k0:k0 + 4, :], w3_d[e].rearrange("(kc p) n -> p kc n", p=128)[:, k0:k0 + 4, :], [], [W3[s]])
            for k0 in range(0, 4, 2):
                em.dma("pool", f"w2_{s}", w2[s][:, k0:k0 + 2, :], w2_d[e].rearrange("(kc p) n -> p kc n", p=128)[:, k0:k0 + 2, :], [], [W2[s]])

        load_w(0)
        for tg in range(4):
            ts_ = slice(tg * 512, (tg + 1) * 512)
            em.dma("sp", "xf", xf[:, :, :], v3(x1_d)[:, :, ts_], [], [XF])
            for i in range(8):
                em.cp("pool" if i % 2 else "dve", x1b[:, i, ts_], xf[:, i, :], [XF], [X1B[tg]])
                em.ts("dve" if i % 2 else "pool", out[:, i, ts_], xf[:, i, :], ALPHA, None, ALU.mult, None, [XF], [OUT[tg]])
            for tt_ in range(4):
                p, P = gbank()
                for k in range(8):
                    em.mm(p[:, 0:36], xf[:, k, tt_ * 128:(tt_ + 1) * 128], wr[:, k, 0:36], k == 0, k == 7, [XF, WR], [P])
                em.tt("dve", lgt[:], p[:, 0:36], brr[:], ALU.add, [P, BRR], [LGT])
                em.red(rs[:, 0:1], lgt[:, 0:4], ALU.max, [LGT], [RS])
                em.ts("dve", rs[:, 1:2], rs[:, 0:1], -1.0, None, ALU.mult, None, [RS], [RS])
                em.ms("dve", rs[:, 2:3], 0.0, [RS])
                em.act(rs[:, 4:8], lgt[:, 0:4], AF.Exp, [LGT, RS], [RS], bias=rs[:, 1:2], accum_out=rs[:, 2:3])
                em.rcp(rs[:, 3:4], rs[:, 2:3], [RS], [RS])
                em.ts("dve", rs[:, 8:12], lgt[:, 0:4], rs[:, 0:1], None, ALU.is_ge, None, [LGT, RS], [RS])
                em.ts("dve", em32[:], g2e[:, 0, :], rs[:, 8:9], None, ALU.mult, None, [G2E, RS], [EM32])
                for g in range(1, 4):
                    em.stt("dve", em32[:], g2e[:, g, :], rs[:, 8 + g:9 + g], em32[:], ALU.mult, ALU.add, [G2E, RS, EM32], [EM32])
                em.tt("dve", em2[:], lgt[:, 4:36], em32[:], ALU.mult, [LGT, EM32], [EM2])
                em.ts("dve", em32[:], em32[:], -1.0, 1.0e9, ALU.add, ALU.mult, [EM32], [EM32])
                em.tt("dve", em2[:], em2[:], em32[:], ALU.add, [EM2, EM32], [EM2])
                em.red(rs[:, 12:13], em2[:], ALU.max, [EM2], [RS])
                em.ts("dve", cw[:], em2[:], rs[:, 12:13], None, ALU.is_ge, None, [EM2, RS], [CW])
                em.stt("dve", em32[:], cw[:], -2.0e9, em2[:], ALU.mult, ALU.add, [CW, EM2], [EM32])
                em.red(rs[:, 13:14], em32[:], ALU.max, [EM32], [RS])
                em.ts("dve", em32[:], em32[:], rs[:, 13:14], None, ALU.is_ge, None, [EM32, RS], [EM32])
                em.tt("dve", rs[:, 14:15], rs[:, 13:14], rs[:, 12:13], ALU.subtract, [RS], [RS])
                em.act(rs[:, 15:16], rs[:, 14:15], AF.Exp, [RS], [RS])
                em.ts("dve", rs[:, 15:16], rs[:, 15:16], 1.0, None, ALU.add, None, [RS], [RS])
                em.rcp(rs[:, 16:17], rs[:, 15:16], [RS], [RS])
                em.tt("dve", rs[:, 17:18], rs[:, 16:17], rs[:, 3:4], ALU.mult, [RS], [RS])
                em.tt("dve", rs[:, 18:19], rs[:, 3:4], rs[:, 17:18], ALU.subtract, [RS], [RS])
                em.ts("dve", cw[:], cw[:], rs[:, 17:18], None, ALU.mult, None, [CW, RS], [CW])
                em.stt("dve", cw[:], em32[:], rs[:, 18:19], cw[:], ALU.mult, ALU.add, [EM32, RS, CW], [CW])
                pt_, PT_ = gbank()
                em.tr(pt_[0:32, 0:128], cw[:], ident[:], [CW, IDENT], [PT_])
                em.cp("dve", cwt[:, tg * 512 + tt_ * 128: tg * 512 + (tt_ + 1) * 128], pt_[0:32, 0:128], [PT_], [CWT[tg]])
        for e in range(nexp):
            s = e % 2
            if e + 1 < nexp:
                load_w(e + 1)
            for tg in range(4):
                ts_ = slice(tg * 512, (tg + 1) * 512)
                pc_, PC_ = gbank()
                em.mm(pc_[:, :], selb[:, e, :], cwt[:, ts_], True, True, [SELB, CWT[tg]], [PC_])
                em.act(cwb[:], pc_[:, :], AF.Copy, [PC_], [CWB])
                for f in range(4):
                    fs = slice(f * 128, (f + 1) * 128)
                    pa, PA = gbank()
                    for k in range(8):
                        em.mm(pa[:, :], w1[s][:, k, fs], x1b[:, k, ts_], k == 0, k == 7, [W1[s], X1B[tg]], [PA])
                    pb, PB = gbank()
                    for k in range(8):
                        em.mm(pb[:, :], w3[s][:, k, fs], x1b[:, k, ts_], k == 0, k == 7, [W3[s], X1B[tg]], [PB])
                    em.act(sl_[f % 2][:], pa[:, :], AF.Silu, [PA], [SL[f % 2]])
                    em.tt("dve", sl_[f % 2][:], sl_[f % 2][:], pb[:, :], ALU.mult, [SL[f % 2], PB], [SL[f % 2]])
                    em.tt("pool", hb[:, f, :], sl_[f % 2][:], cwb[:], ALU.mult, [SL[f % 2], CWB], [HB[f]])
                for i in range(8):
                    po, PO = gbank()
                    for f in range(4):
                        em.mm(po[:, :], w2[s][:, f, i * 128:(i + 1) * 128], hb[:, f, :], f == 0, f == 3, [W2[s], HB[f]], [PO])
                    em.tt("dve", out[:, i, ts_], out[:, i, ts_], po[:, :], ALU.add, [OUT[tg], PO], [OUT[tg]])
        for tg in range(4):
            ts_ = slice(tg * 512, (tg + 1) * 512)
            layer_norm_fm(kb, em, gbank, out[:, :, ts_], OUT[tg], lambda i: (ob[:, i, :], OB), ln[:, 0:8], ln[:, 8:16], LN, tmp, TMP,
                          ones, ONES, sq, SQ, mean, MEAN, rstd, RSTD)
            em.dma("sp", "ob", v3(o_d)[:, :, ts_], ob[:, :, :], [OB], [])
        kb.finish([OB])


def build_b2(nexp=32):
    nc = bass.Bass("TRN2", target_bir_lowering=False)
    I = lambda n, shp: nc.dram_tensor(n, shp, F32, kind="ExternalInput").ap()
    D = dict(x1T=I("x1T", [1024, NT]), wr=I("wr", [1024, 36]), brr=I("brr", [1, 36]), ew1=I("ew1", [32, 1024, 512]), ew3=I("ew3", [32, 1024, 512]),
             ew2=I("ew2", [32, 512, 1024]), ln=I("ln", [128, 16]), selb=I("selb", [32, 32 * 128]), g2e=I("g2e", [128, 4 * 32]),
             x2T=nc.dram_tensor("x2T", [1024, NT], F32, kind="ExternalOutput").ap())
    with ExitStack() as st:
        kb = KB(nc, st)
        b2_body(nc, kb, D, nexp)
        kb.emit()
    return nc


def b2_consts():
    selb = np.zeros((32, 32, 128), np.float32)
    for e in range(32):
        selb[e, e, :] = 1.0
    g2e = np.zeros((128, 4, 32), np.float32)
    for g in range(4):
        g2e[:, g, g * 8:(g + 1) * 8] = 1.0
    return dict(selb=selb.reshape(32, 32 * 128), g2e=g2e.reshape(128, 128))


def b2_inputs(inp, l, consts):
    wr = np.concatenate([inp["router_group_w"][l], inp["router_expert_w"][l]], axis=1)
    brr = np.concatenate([inp["router_group_b"][l], inp["router_expert_b"][l]])[None, :]
    lnp = np.concatenate([inp["ln2_g"][l].reshape(8, 128).T, inp["ln2_b"][l].reshape(8, 128).T], axis=1)
    return dict(wr=np.ascontiguousarray(wr), brr=np.ascontiguousarray(brr), ew1=inp["exp_w1"][l], ew3=inp["exp_w3"][l], ew2=inp["exp_w2"][l],
                ln=np.ascontiguousarray(lnp, dtype=np.float32), selb=consts["selb"], g2e=consts["g2e"])


L_ = 4


def build_fused(nl=L_):
    nc = bass.Bass("TRN2", target_bir_lowering=False)

    def I(n, shp):
        return nc.dram_tensor(n, list(shp), F32, kind="ExternalInput").ap()

    def T(n, shp):
        return nc.dram_tensor(n, list(shp), F32, kind="Internal").ap()
    x0T = I("x0T", [1024, S])
    a1w = I("a1_w", [nl, 2, 1024, 1024]); a1vec = I("a1_vec", [nl, 2, 128, 22]); a1lw = I("a1_lw", [nl, 2, 128, 256])
    a1g2 = I("a1_g2", [nl, 2, 128, 256]); a1cst = I("a1_cst", [128, 1280])
    a2wf = I("a2_wf", [nl, 2, 1024, 640]); a2wt = I("a2_wt", [nl, 2, 1024, 140]); a2w1 = I("a2_w1", [nl, 128, 32 * 128])
    a2pe = I("a2_peT", [nl, 128, 32]); a2w2 = I("a2_w2", [nl, 128, 192]); a2btc = I("a2_btc", [2, 128, 4 * 256]); a2bts = I("a2_bts", [2, 128, 4 * TW])
    a2c = {k: I("a2_" + k, shp) for k, shp in (("mkc", [128, 256]), ("mks", [128, TW]), ("mkw", [128, TW]), ("m12", [128, 256]), ("rv", [128, 1]),
                                                 ("c2s", [128, 128]), ("exd", [64, 32 * 128]), ("hsel", [128, 256]))}
    b1wg = I("b1_wg", [nl, 1024, 2048]); b1wur = I("b1_wur", [nl, 512, 1024]); b1wun = I("b1_wun", [nl, 512, 1024]); b1wo = I("b1_wo", [nl, 1024, 1024])
    b1ln = I("b1_ln", [nl, 128, 16])
    b2wr = I("b2_wr", [nl, 1024, 36]); b2br = I("b2_brr", [nl, 1, 36]); b2e1 = I("b2_ew1", [nl, 32, 1024, 512]); b2e3 = I("b2_ew3", [nl, 32, 1024, 512])
    b2e2 = I("b2_ew2", [nl, 32, 512, 1024]); b2ln = I("b2_ln", [nl, 128, 16]); b2selb = I("b2_selb", [32, 32 * 128]); b2g2e = I("b2_g2e", [128, 128])
    outT = nc.dram_tensor("outT", [1024, S], F32, kind="ExternalOutput").ap()
    XT = [T("xt0", [1024, S]), T("xt1", [1024, S])]
    YR = T("yr", [512, S]); YN = T("yn", [512, S]); X1 = T("x1", [1024, S])

    with ExitStack() as st:
        kb = KB(nc, st)
        pn = [0]

        def phase(fn, D, *args):
            with ExitStack() as pst:
                kb.pstack = pst
                kb.prefix = f"p{pn[0]}_"
                pn[0] += 1
                fn(nc, kb, D, *args)
                kb.emit()
            kb.pstack = st
        for l in range(nl):
            xin = x0T if l == 0 else XT[l % 2]
            xout = outT if l == nl - 1 else XT[(l + 1) % 2]
            for hh in range(2):
                phase(a1_body, dict(xT=xin, w=a1w[l, hh], vec=a1vec[l, hh], lw=a1lw[l, hh], g2=a1g2[l, hh], cst=a1cst,
                                    yT=YR[hh * 256:(hh + 1) * 256, :]))
            for g in range(2):
                D = dict(xT=xin, wf=a2wf[l, g], wt=a2wt[l, g], w1=a2w1[l], peT=a2pe[l], w2=a2w2[l], btc=a2btc[g], bts=a2bts[g],
                         yT=YN[g * 256:(g + 1) * 256, :])
                D.update(a2c)
                phase(a2_body, D)
            for hf in range(2):
                ts = slice(hf * NT, (hf + 1) * NT)
                phase(b1_body, dict(xT=xin[:, ts], yrT=YR[:, ts], ynT=YN[:, ts], wg=b1wg[l], wur=b1wur[l], wun=b1wun[l], wo=b1wo[l], ln=b1ln[l],
                                    x1T=X1[:, ts]))
            for hf in range(2):
                ts = slice(hf * NT, (hf + 1) * NT)
                phase(b2_body, dict(x1T=X1[:, ts], wr=b2wr[l], brr=b2br[l], ew1=b2e1[l], ew3=b2e3[l], ew2=b2e2[l], ln=b2ln[l], selb=b2selb,
                                    g2e=b2g2e, x2T=xout[:, ts]))
        print("FUSED instructions:", kb.n_ins, kb.cnt)
    return nc


def fused_inputs(inp, nl=L_):
    c2 = a2_consts(); cb2 = b2_consts()
    a1 = [[a1_inputs(inp, l, 0, hh) for hh in range(2)] for l in range(nl)]
    a2 = [[a2_inputs(inp, l, g, c2) for g in range(2)] for l in range(nl)]
    b1 = [b1_inputs(inp, l) for l in range(nl)]
    b2 = [b2_inputs(inp, l, cb2) for l in range(nl)]
    st = lambda f: np.ascontiguousarray(np.stack(f, axis=0), dtype=np.float32)
    m = {}
    for k, nm in (("w", "a1_w"), ("vec", "a1_vec"), ("lw", "a1_lw"), ("g2", "a1_g2")):
        m[nm] = st([st([a1[l][hh][k] for hh in range(2)]) for l in range(nl)])
    m["a1_cst"] = a1[0][0]["cst"]
    for k, nm in (("wf", "a2_wf"), ("wt", "a2_wt")):
        m[nm] = st([st([a2[l][g][k] for g in range(2)]) for l in range(nl)])
    for k, nm in (("w1", "a2_w1"), ("peT", "a2_peT"), ("w2", "a2_w2")):
        m[nm] = st([a2[l][0][k] for l in range(nl)])
    m["a2_btc"] = st([a2[0][g]["btc"] for g in range(2)]); m["a2_bts"] = st([a2[0][g]["bts"] for g in range(2)])
    for k in ("mkc", "mks", "mkw", "m12", "rv", "c2s", "exd", "hsel"):
        m["a2_" + k] = a2[0][0][k]
    for k, nm in (("wg", "b1_wg"), ("wur", "b1_wur"), ("wun", "b1_wun"), ("wo", "b1_wo"), ("ln", "b1_ln")):
        m[nm] = st([b1[l][k] for l in range(nl)])
    for k, nm in (("wr", "b2_wr"), ("brr", "b2_brr"), ("ln", "b2_ln")):
        m[nm] = st([b2[l][k] for l in range(nl)])
    m["b2_ew1"] = np.ascontiguousarray(inp["exp_w1"][:nl], dtype=np.float32)
    m["b2_ew3"] = np.ascontiguousarray(inp["exp_w3"][:nl], dtype=np.float32)
    m["b2_ew2"] = np.ascontiguousarray(inp["exp_w2"][:nl], dtype=np.float32)
    m["b2_selb"] = cb2["selb"]; m["b2_g2e"] = cb2["g2e"]
    return m

_NC = {}


def kernel(**inputs):
    inp = {k: np.asarray(v) for k, v in inputs.items()}
    if "nc" not in _NC:
        _NC["nc"] = build_fused(L_)
    m = fused_inputs(inp, L_)
    x = inp["x"].astype(np.float32, copy=False)
    B = x.shape[0]
    xT = [np.ascontiguousarray(x[b].T) for b in range(B)]
    maps = []
    for c in range(8):
        mm_ = dict(m); mm_["x0T"] = xT[c // 2]; maps.append(mm_)
    res = run_bass_kernel_spmd(_NC["nc"], maps, core_ids=list(range(8)))
    out = np.stack([res.results[2 * b]["outT"].T for b in range(B)], axis=0)
    return np.ascontiguousarray(out, dtype=np.float32)
```

```python
import numpy as np
from contextlib import ExitStack
import concourse.bass as bass
import concourse.mybir as mybir
from concourse.bass_utils import run_bass_kernel_spmd

F32 = mybir.dt.float32
BF16 = mybir.dt.bfloat16
AF = mybir.ActivationFunctionType
ALU = mybir.AluOpType
AX = mybir.AxisListType


class Buf:
    __slots__ = ("name", "w", "r", "excl")

    def __init__(self, name="", excl=False):
        self.name = name
        self.excl = excl
        self.w = None
        self.r = {}


class KB:
    def __init__(self, nc, stack):
        self.nc = nc
        self.stack = stack
        self.pstack = stack
        self.prefix = ""
        self.names = ["pe", "act", "dve", "pool", "sp"]
        self.prog = {e: [] for e in self.names}
        self.sem = {e: stack.enter_context(nc.semaphore("s_" + e)) for e in self.names}
        self.cnt = {e: 0 for e in self.names}
        self.seen = {e: {} for e in self.names}
        self.dsem = {}
        self.n_ins = 0
        self.nw = {e: 0 for e in self.names}

    def sb(self, name, shape, dt=F32):
        return self.pstack.enter_context(self.nc.sbuf_tensor(self.prefix + "sb_" + name, list(shape), dt))

    def ps(self, name, shape, dt=F32):
        return self.pstack.enter_context(self.nc.psum_tensor(self.prefix + "ps_" + name, list(shape), dt))

    def _semh(self, key):
        if key in self.sem:
            return self.sem[key]
        return self.dsem[key][0]

    def _waits(self, eng, reads, writes):
        need = {}

        def add(d):
            if d is None:
                return
            k, v = d
            if need.get(k, 0) < v:
                need[k] = v
        for b in reads:
            add(b.w)
        for b in writes:
            add(b.w)
            for k, v in b.r.items():
                add((k, v))
        out = []
        seen = self.seen[eng]
        for k, v in need.items():
            if k == "pe" and eng == "pe":
                continue
            if seen.get(k, 0) >= v:
                continue
            seen[k] = v
            out.append((self._semh(k), v))
        return out

    def _mark(self, tok, reads, writes):
        for b in writes:
            b.w = tok
            b.r = {}
        k, v = tok
        for b in reads:
            if b.r.get(k, 0) < v:
                b.r[k] = v

    def op(self, eng, fn, reads=(), writes=()):
        ex = [b for b in reads if b.excl]
        if ex:
            writes = list(writes) + ex
        waits = self._waits(eng, reads, writes)
        self.nw[eng] += len(waits)
        self.cnt[eng] += 1
        tok = (eng, self.cnt[eng])
        sem = self.sem[eng]

        def run(e, waits=waits, fn=fn, sem=sem):
            for s, v in waits:
                e.wait_ge(s, v)
            fn(e).then_inc(sem, 1)
        self.prog[eng].append(run)
        self._mark(tok, reads, writes)
        self.n_ins += 1

    def dma(self, q, key, fn, reads=(), writes=(), n=1):
        key = "d_" + key
        if key not in self.dsem:
            self.dsem[key] = [self.stack.enter_context(self.nc.semaphore(key)), 0]
        waits = self._waits(q, reads, writes)
        self.dsem[key][1] += 16 * n
        tok = (key, self.dsem[key][1])
        sem = self.dsem[key][0]

        def run(e, waits=waits, fn=fn, sem=sem):
            for s, v in waits:
                e.wait_ge(s, v)
            fn(e, sem)
        self.prog[q].append(run)
        self._mark(tok, reads, writes)
        self.n_ins += n

    def finish(self, bufs):
        waits = self._waits("sp", bufs, bufs)

        def run(e, waits=waits):
            for s, v in waits:
                e.wait_ge(s, v)
        self.prog["sp"].append(run)

    def emit(self):
        nc = self.nc
        prog = self.prog
        self.prog = {e: [] for e in self.names}
        with nc.Block() as block:
            @block.sync
            def _(e):
                for f in prog["sp"]:
                    f(e)

            @block.tensor
            def _(e):
                for f in prog["pe"]:
                    f(e)

            @block.scalar
            def _(e):
                for f in prog["act"]:
                    f(e)

            @block.vector
            def _(e):
                for f in prog["dve"]:
                    f(e)

            @block.gpsimd
            def _(e):
                for f in prog["pool"]:
                    f(e)


S = 4096
NS = 512
NSEG = S // NS
CH = 128
NCH = NS // CH
GN_EPS = 64e-5


def a1_body(nc, kb, D, nseg=NSEG, debug=False):
    Em_ = globals().get('Em')
    if Em_ is None:
        from a2 import Em as Em_
    xT_d, w_d, vec_d, lw_d, g2_d, cst_d, yT_d = (D[k] for k in ("xT", "w", "vec", "lw", "g2", "cst", "yT"))
    xT_v = xT_d.rearrange("(kc p) t -> p kc t", p=128)
    w_v = w_d.rearrange("(kc p) n -> p kc n", p=128)
    if True:
        sb, ps = kb.sb, kb.ps
        NXS = 3
        xs = [sb(f"xs{i}", [128, 8, NS], BF16) for i in range(NXS)]; XS = [Buf() for _ in range(NXS)]
        wsb = sb("wsb", [128, 8, 1024], BF16); WSB = Buf()
        vec = sb("vec", [128, 22]); VEC = Buf()
        vx = sb("vx", [128, 8]); VX = Buf()
        lw = sb("lw", [128, 256]); LW = Buf()
        g2 = sb("g2", [128, 256]); G2 = Buf()
        cst = sb("cst", [128, 1280]); CST = Buf()
        MASK1 = cst[:, 0:512]; MASK4 = cst[:, 512:1024]; MSL = cst[:, 1024:1152]; BONES = cst[:, 1152:1280]
        ident = sb("ident", [128, 128]); IDENT = Buf()
        car = sb("car", [128, 8]); CAR = Buf()
        zr = [sb(f"zr{i}", [128, NS + 1]) for i in range(2)]; ZR = [Buf() for _ in range(2)]
        dtmp = sb("dtmp", [128, NS]); DTMP = Buf()
        zs = [sb(f"zs{j}", [128, NS]) for j in range(8)]; ZS = [Buf() for _ in range(8)]
        tw = sb("tw", [128, NS]); TW = Buf()
        sg = sb("sg", [128, NS]); SG = Buf()
        tnames = ["nld", "cw", "ew", "ewi", "ewx", "aa", "kkn", "sq", "t1", "k2", "bh", "kh", "e1"]
        T = {n: sb("t_" + n, [128, NS]) for n in tnames}; TB = {n: Buf() for n in tnames}
        ar = [sb(f"ar{h}", [128, NCH, 2 * CH]) for h in range(2)]; AR = [Buf() for _ in range(2)]
        bt = [sb(f"bt{h}", [128, NS]) for h in range(2)]; BT = [Buf() for _ in range(2)]
        kt = [sb(f"kt{h}", [128, NS]) for h in range(2)]; KT = [Buf() for _ in range(2)]
        gg = [sb(f"gg{h}", [128, NS]) for h in range(2)]; GG = [Buf() for _ in range(2)]
        bon = [sb(f"bon{h}", [128, NS]) for h in range(2)]; BON = [Buf() for _ in range(2)]
        yf = [sb(f"yf{h}", [128, NS]) for h in range(2)]; YF = [Buf() for _ in range(2)]
        wc = [sb(f"wc{h}", [128, NCH]) for h in range(2)]; WC = [Buf() for _ in range(2)]
        bhT = sb("bhT", [128, NCH, 256]); BHT = [Buf() for _ in range(NCH)]
        khT = sb("khT", [128, NCH, 256]); KHT = [Buf() for _ in range(NCH)]
        vT = sb("vT", [128, NCH, 256]); VT = [Buf() for _ in range(NCH)]
        mabk = [sb(f"mabk{h}", [128, 512]) for h in range(4)]; MABK = [Buf() for _ in range(4)]
        nm = [[sb(f"nm{h}_{i}", [128, 256]) for i in range(2)] for h in range(4)]; NM = [[Buf(), Buf()] for _ in range(4)]
        qq = [[sb(f"qq{h}_{i}", [128, 128]) for i in range(2)] for h in range(4)]; QQ = [[Buf(), Buf()] for _ in range(4)]
        xsb = [sb(f"xsb{h}", [128, 64]) for h in range(4)]; XSB = [Buf() for _ in range(4)]
        usb = [sb(f"usb{h}", [128, 64]) for h in range(4)]; USB = [Buf() for _ in range(4)]
        stt = [[sb(f"st{hp}_{i}", [128, 64]) for i in range(2)] for hp in range(2)]
        STT = [[[Buf(), Buf()] for _ in range(2)] for hp in range(2)]
        ytok = sb("ytok", [128, 256]); YTOK = Buf()
        ysq = sb("ysq", [128, 256]); YSQ = Buf()
        yn = sb("yn", [128, 256]); YN = Buf()
        sts = sb("sts", [128, 32]); STS = Buf()
        osb = [sb(f"osb{h}", [128, NS]) for h in range(2)]; OSB = [Buf() for _ in range(2)]
        NPA = 4
        pa = [ps(f"pa{i}", [128, 512]) for i in range(NPA)]; PA = [Buf(excl=True) for _ in range(NPA)]
        pq = [ps(f"pq{i}", [128, 512]) for i in range(4)]; PQB = [Buf(excl=True) for _ in range(4)]

        def ld(q, key, out, in_, B):
            kb.dma(q, key, lambda e, s: e.dma_start(out=out, in_=in_).then_inc(s, 16), writes=[B])
        ld("sp", "vec", vec[:], vec_d[:, :], VEC)
        ld("sp", "lw", lw[:], lw_d[:, :], LW)
        ld("sp", "g2", g2[:], g2_d[:, :], G2)
        ld("sp", "cst", cst[:], cst_d[:, :], CST)
        for kc in range(0, 8, 4):
            kb.dma("pool", "wsb", lambda e, s, kc=kc: e.dma_start(out=wsb[:, kc:kc + 4, :], in_=w_v[:, kc:kc + 4, :]).then_inc(s, 16), writes=[WSB])
        kb.op("pool", lambda e: e.memset(ident[:], 1.0), writes=[IDENT])
        kb.op("pool", lambda e: e.affine_select(out=ident[:], in_=ident[:], pattern=[[-1, 128]], compare_op=ALU.is_equal,
                                                fill=0.0, base=0, channel_multiplier=1), reads=[IDENT], writes=[IDENT])
        kb.op("dve", lambda e: e.memset(car[:], 0.0), writes=[CAR])
        kb.op("dve", lambda e: e.tensor_scalar(vx[:, 0:2], vec[:, 8:10], -1.0, None, ALU.mult), reads=[VEC], writes=[VX])
        kb.op("dve", lambda e: e.tensor_scalar(vx[:, 2:4], vec[:, 14:16], -1.0, 1.0, ALU.mult, ALU.add), reads=[VEC, VX], writes=[VX])
        for hp in range(2):
            for i in range(2):
                kb.op("dve", lambda e, hp=hp, i=i: e.memset(stt[hp][i][:], 0.0), writes=STT[hp][i])

        def load_x(sgi):
            sl = sgi % NXS
            kb.dma("pool", f"xs{sl}", lambda e, s, sl=sl, sgi=sgi: e.dma_start(
                out=xs[sl][:, :, :], in_=xT_v[:, :, sgi * NS:(sgi + 1) * NS]).then_inc(s, 16), writes=[XS[sl]])

        load_x(0)
        pai = [0]

        def next_pa():
            i = pai[0] % NPA
            pai[0] += 1
            return pa[i], PA[i]

        def mm512(lhsT_fn, rhs_fn, nk, reads, M=128):
            p, P = next_pa()
            for k in range(nk):
                a_, b_ = lhsT_fn(k), rhs_fn(k)
                kb.op("pe", lambda e, k=k, p=p, a_=a_, b_=b_: e.matmul(p[0:M, :], a_, b_, start=(k == 0), stop=(k == nk - 1)),
                      reads=reads, writes=[P])
            return p, P

        ping = [0, 0]
        dbg_n = [0]

        def dbg(name, ap, B):
            if not debug:
                return
            shp = list(ap.shape)
            d = nc.dram_tensor("dbg_" + name, shp, F32, kind="ExternalOutput").ap()
            dbg_n[0] += 1
            cntv = dbg_n[0] * 16

            def f(e, s, d=d, ap=ap, cntv=cntv):
                e.dma_start(out=d, in_=ap).then_inc(s, 16)
                e.wait_ge(s, cntv)
            kb.dma("sp", "dbg", f, reads=[B])
        for sgi in range(nseg):
            sl = sgi % NXS
            if sgi + 1 < nseg:
                load_x(sgi + 1)
            for j in range(8):
                p, P = mm512(lambda k, j=j: wsb[:, k, j * 128:(j + 1) * 128], lambda k, sl=sl: xs[sl][:, k, :], 8, [WSB, XS[sl]])
                z, Z = zr[j % 2], ZR[j % 2]
                kb.op("pool", lambda e, z=z, j=j: e.tensor_copy(z[:, 0:1], car[:, j:j + 1]), reads=[CAR], writes=[Z])
                kb.op("act", lambda e, z=z, p=p: e.activation(out=z[:, 1:NS + 1], in_=p[:, :], func=AF.Copy), reads=[P], writes=[Z])
                kb.op("pool", lambda e, z=z, j=j: e.tensor_copy(car[:, j:j + 1], z[:, NS:NS + 1]), reads=[Z], writes=[CAR])
                kb.op("dve", lambda e, z=z: e.tensor_tensor(dtmp[:], z[:, 0:NS], z[:, 1:NS + 1], ALU.subtract), reads=[Z], writes=[DTMP])
                kb.op("dve", lambda e, z=z, j=j: e.scalar_tensor_tensor(zs[j][:], dtmp[:], vec[:, j:j + 1], z[:, 1:NS + 1], ALU.mult, ALU.add),
                      reads=[DTMP, Z, VEC], writes=[ZS[j]])
            L1, L2 = zs[6], zs[7]
            for j in range(8):
                dbg(f"zs{j}", zs[j][:], ZS[j])
            kb.op("act", lambda e: e.activation(out=tw[0:64, :], in_=L1[0:64, :], func=AF.Tanh), reads=[ZS[6]], writes=[TW])
            kb.op("act", lambda e: e.activation(out=sg[:], in_=L2[:], func=AF.Sigmoid), reads=[ZS[7]], writes=[SG])
            for hp in range(2):
                Rz, Kz, Vz = zs[0 + hp], zs[2 + hp], zs[4 + hp]
                RZ, KZ, VZ = ZS[0 + hp], ZS[2 + hp], ZS[4 + hp]
                cs = slice(hp * 128, (hp + 1) * 128)
                p, P = mm512(lambda k: lw[64:128, cs], lambda k: L1[64:128, :], 1, [LW, ZS[6]])
                kb.op("act", lambda e, p=p, hp=hp: e.activation(out=T["aa"][:], in_=p[:, :], func=AF.Sigmoid, bias=vec[:, 10 + hp:11 + hp]),
                      reads=[P, VEC], writes=[TB["aa"]])
                p, P = mm512(lambda k: g2[:, cs], lambda k: sg[:], 1, [G2, SG])
                kb.op("act", lambda e, p=p, hp=hp: e.activation(out=gg[hp][:], in_=p[:, :], func=AF.Copy), reads=[P], writes=[GG[hp]])
                p, P = mm512(lambda k: lw[0:64, cs], lambda k: tw[0:64, :], 1, [LW, TW])
                kb.op("act", lambda e, p=p, hp=hp: e.activation(out=T["e1"][:], in_=p[:, :], func=AF.Exp, bias=vx[:, hp:hp + 1], scale=-1.0),
                      reads=[P, VX], writes=[TB["e1"]])
                kb.op("act", lambda e: e.activation(out=T["e1"][:], in_=T["e1"][:], func=AF.Ln, bias=1.0), reads=[TB["e1"]], writes=[TB["e1"]])
                kb.op("act", lambda e: e.activation(out=T["nld"][:], in_=T["e1"][:], func=AF.Exp, bias=-0.5, scale=-1.0),
                      reads=[TB["e1"]], writes=[TB["nld"]])
                kb.op("dve", lambda e: e.tensor_tensor_scan(T["cw"][:], MASK1, T["nld"][:], 0.0, ALU.mult, ALU.add),
                      reads=[CST, TB["nld"]], writes=[TB["cw"]])
                kb.op("act", lambda e: e.activation(out=T["ew"][:], in_=T["cw"][:], func=AF.Exp, scale=-1.0), reads=[TB["cw"]], writes=[TB["ew"]])
                kb.op("act", lambda e: e.activation(out=T["ewi"][:], in_=T["cw"][:], func=AF.Exp), reads=[TB["cw"]], writes=[TB["ewi"]])
                kb.op("pool", lambda e: e.tensor_tensor(T["ewx"][:], T["cw"][:], T["nld"][:], ALU.subtract), reads=[TB["cw"], TB["nld"]], writes=[TB["ewx"]])
                kb.op("act", lambda e: e.activation(out=T["ewx"][:], in_=T["ewx"][:], func=AF.Exp, scale=-1.0), reads=[TB["ewx"]], writes=[TB["ewx"]])
                kb.op("pool", lambda e, hp=hp: e.tensor_copy(wc[hp][:], T["ew"][:].rearrange("p (c t) -> p c t", t=CH)[:, :, CH - 1]),
                      reads=[TB["ew"]], writes=[WC[hp]])
                kb.op("dve", lambda e, hp=hp, Kz=Kz: e.tensor_scalar(T["kkn"][:], Kz[:], vec[:, 12 + hp:13 + hp], None, ALU.mult),
                      reads=[KZ, VEC], writes=[TB["kkn"]])
                kb.op("pool", lambda e: e.tensor_tensor(T["sq"][:], T["kkn"][:], T["kkn"][:], ALU.mult), reads=[TB["kkn"]], writes=[TB["sq"]])
                p, P = mm512(lambda k: BONES, lambda k: T["sq"][:], 1, [CST, TB["sq"]])
                kb.op("act", lambda e, p=p: e.activation(out=T["sq"][:], in_=p[:, :], func=AF.Sqrt), reads=[P], writes=[TB["sq"]])
                kb.op("dve", lambda e: e.tensor_scalar(T["sq"][:], T["sq"][:], 1e-12, None, ALU.max), reads=[TB["sq"]], writes=[TB["sq"]])
                kb.op("dve", lambda e: e.reciprocal(T["sq"][:], T["sq"][:]), reads=[TB["sq"]], writes=[TB["sq"]])
                kb.op("dve", lambda e: e.tensor_tensor(T["kkn"][:], T["kkn"][:], T["sq"][:], ALU.mult), reads=[TB["kkn"], TB["sq"]], writes=[TB["kkn"]])
                kb.op("pool", lambda e, hp=hp: e.tensor_scalar(T["t1"][:], T["aa"][:], vec[:, 14 + hp:15 + hp], vx[:, 2 + hp:3 + hp], ALU.mult, ALU.add),
                      reads=[TB["aa"], VEC, VX], writes=[TB["t1"]])
                kb.op("pool", lambda e, Kz=Kz: e.tensor_tensor(T["k2"][:], Kz[:], T["t1"][:], ALU.mult), reads=[KZ, TB["t1"]], writes=[TB["k2"]])
                arv = ar[hp]
                kb.op("dve", lambda e, arv=arv: e.scalar_tensor_tensor(arv[:, :, 0:CH], T["kkn"][:].rearrange("p (c t) -> p c t", t=CH), -1.0,
                                                                      T["ewx"][:].rearrange("p (c t) -> p c t", t=CH), ALU.mult, ALU.mult),
                      reads=[TB["kkn"], TB["ewx"]], writes=[AR[hp]])
                kb.op("pool", lambda e, arv=arv, Rz=Rz: e.tensor_tensor(arv[:, :, CH:2 * CH], Rz[:].rearrange("p (c t) -> p c t", t=CH),
                                                                       T["ew"][:].rearrange("p (c t) -> p c t", t=CH), ALU.mult),
                      reads=[RZ, TB["ew"]], writes=[AR[hp]])
                kb.op("dve", lambda e: e.tensor_tensor(T["t1"][:], T["kkn"][:], T["aa"][:], ALU.mult), reads=[TB["kkn"], TB["aa"]], writes=[TB["t1"]])
                kb.op("dve", lambda e, hp=hp: e.tensor_tensor(bt[hp][:], T["t1"][:], T["ewi"][:], ALU.mult), reads=[TB["t1"], TB["ewi"]], writes=[BT[hp]])
                kb.op("pool", lambda e, hp=hp: e.tensor_tensor(kt[hp][:], T["k2"][:], T["ewi"][:], ALU.mult), reads=[TB["k2"], TB["ewi"]], writes=[KT[hp]])
                wcb = wc[hp][:].unsqueeze(2).to_broadcast([128, NCH, CH])
                kb.op("dve", lambda e, hp=hp, wcb=wcb: e.tensor_tensor(T["bh"][:].rearrange("p (c t) -> p c t", t=CH),
                                                                      bt[hp][:].rearrange("p (c t) -> p c t", t=CH), wcb, ALU.mult),
                      reads=[BT[hp], WC[hp]], writes=[TB["bh"]])
                kb.op("pool", lambda e, hp=hp, wcb=wcb: e.tensor_tensor(T["kh"][:].rearrange("p (c t) -> p c t", t=CH),
                                                                       kt[hp][:].rearrange("p (c t) -> p c t", t=CH), wcb, ALU.mult),
                      reads=[KT[hp], WC[hp]], writes=[TB["kh"]])
                kb.op("dve", lambda e, hp=hp, Rz=Rz: e.scalar_tensor_tensor(T["t1"][:], Rz[:], vec[:, 16 + hp:17 + hp], T["k2"][:], ALU.mult, ALU.mult),
                      reads=[RZ, VEC, TB["k2"], TB["t1"]], writes=[TB["t1"]])
                p, P = mm512(lambda k: BONES, lambda k: T["t1"][:], 1, [CST, TB["t1"]])
                kb.op("dve", lambda e, p=p, hp=hp, Vz=Vz: e.tensor_tensor(bon[hp][:], p[:, :], Vz[:], ALU.mult), reads=[P, VZ], writes=[BON[hp]])
                for n_ in ("nld", "cw", "ew", "ewi", "ewx", "aa", "kkn", "k2", "bh", "kh"):
                    dbg(f"{n_}{hp}", T[n_][:], TB[n_])
                dbg(f"bt{hp}", bt[hp][:], BT[hp]); dbg(f"kt{hp}", kt[hp][:], KT[hp]); dbg(f"ar{hp}", ar[hp][:].rearrange("p c t -> p (c t)"), AR[hp])
                dbg(f"gg{hp}", gg[hp][:], GG[hp]); dbg(f"bon{hp}", bon[hp][:], BON[hp]); dbg(f"wc{hp}", wc[hp][:], WC[hp])
                for c in range(NCH):
                    for (src, SRC, dst, DST) in ((T["bh"], TB["bh"], bhT, BHT), (T["kh"], TB["kh"], khT, KHT), (Vz, VZ, vT, VT)):
                        pbank, PT = next_pa()
                        pt = pbank[:, 0:128]
                        kb.op("pe", lambda e, pt=pt, src=src, c=c: e.transpose(pt, src[:, c * CH:(c + 1) * CH], ident[:]),
                              reads=[SRC, IDENT], writes=[PT])
                        kb.op("act", lambda e, pt=pt, dst=dst, c=c, cs=cs: e.activation(out=dst[:, c, cs], in_=pt, func=AF.Copy),
                              reads=[PT], writes=[DST[c]])
            em = Em_(kb)
            for c in range(NCH):
                csl = slice(c * CH, (c + 1) * CH)
                HD = []
                for h in range(4):
                    hp, hh = h // 2, h % 2
                    HD.append(dict(h=h, hp=hp, hh=hh, rows=slice(64 * hh, 64 * hh + 64), hc=slice(h * 64, (h + 1) * 64), q=pq[h], Q=PQB[h]))
                for d in HD:
                    em.mm(d["q"][:, 0:256], bt[d["hp"]][d["rows"], csl], ar[d["hp"]][d["rows"], c, :], True, True, [BT[d["hp"]], AR[d["hp"]]], [d["Q"]])
                    em.mm(d["q"][:, 256:512], kt[d["hp"]][d["rows"], csl], ar[d["hp"]][d["rows"], c, :], True, True, [KT[d["hp"]], AR[d["hp"]]], [d["Q"]])
                for d in HD:
                    h = d["h"]
                    em.tt("dve", mabk[h][:], d["q"][:, :], MASK4, ALU.mult, [d["Q"], CST], [MABK[h]])
                for d in HD:
                    em.mm(d["q"][:, 0:128], ar[d["hp"]][d["rows"], c, 0:CH], bt[d["hp"]][d["rows"], csl], True, True, [BT[d["hp"]], AR[d["hp"]]], [d["Q"]])
                for d in HD:
                    h = d["h"]
                    em.tt("dve", nm[h][0][:, 0:128], d["q"][:, 0:128], MSL, ALU.mult, [d["Q"], CST], [NM[h][0]])
                    em.cp("pool", nm[h][0][:, 128:256], mabk[h][:, 0:128], [MABK[h]], [NM[h][0]])
                    em.tt("pool", qq[h][0][:], mabk[h][:, 0:128], ident[:], ALU.add, [MABK[h], IDENT], [QQ[h][0]])
                for k in range(6):
                    a_, b_ = k % 2, (k + 1) % 2
                    wdt = 256 if k < 5 else 128
                    for d in HD:
                        h = d["h"]
                        em.mm(d["q"][:, 0:128], nm[h][a_][:, 128:256], nm[h][a_][:, 0:128], True, True, [NM[h][a_]], [d["Q"]])
                        if k < 5:
                            em.mm(d["q"][:, 128:256], nm[h][a_][:, 0:128], nm[h][a_][:, 128:256], True, True, [NM[h][a_]], [d["Q"]])
                    for d in HD:
                        h = d["h"]
                        if h % 2 == 0:
                            em.cp("dve", nm[h][b_][:, 0:wdt], d["q"][:, 0:wdt], [d["Q"]], [NM[h][b_]])
                        else:
                            em.act(nm[h][b_][:, 0:wdt], d["q"][:, 0:wdt], AF.Copy, [d["Q"]], [NM[h][b_]])
                    for d in HD:
                        h = d["h"]
                        em.mm(d["q"][:, 256:384], nm[h][b_][:, 0:128], qq[h][a_][:], True, True, [NM[h][b_], QQ[h][a_]], [d["Q"]])
                    for d in HD:
                        h = d["h"]
                        em.tt("dve", qq[h][b_][:], d["q"][:, 256:384], qq[h][a_][:], ALU.add, [d["Q"], QQ[h][a_]], [QQ[h][b_]])
                qf = 0
                so = [ping[0], ping[1]]
                for d in HD:
                    h, hp, rows, hc = d["h"], d["hp"], d["rows"], d["hc"]
                    So = STT[hp][so[hp]][d["hh"]]
                    em.mm(d["q"][:, 0:64], mabk[h][:, 256:384], vT[:, c, hc], True, False, [MABK[h], VT[c]], [d["Q"]])
                    em.mm(d["q"][:, 0:64], ar[hp][rows, c, 0:CH], stt[hp][so[hp]][rows, :], False, True, [AR[hp], So], [d["Q"]])
                for d in HD:
                    h = d["h"]
                    if h % 2 == 0:
                        em.act(xsb[h][:], d["q"][:, 0:64], AF.Copy, [d["Q"]], [XSB[h]])
                    else:
                        em.cp("dve", xsb[h][:], d["q"][:, 0:64], [d["Q"]], [XSB[h]])
                for d in HD:
                    h = d["h"]
                    em.mm(d["q"][:, 64:128], qq[h][qf][:], xsb[h][:], True, True, [QQ[h][qf], XSB[h]], [d["Q"]])
                for d in HD:
                    h = d["h"]
                    if h % 2 == 0:
                        em.act(usb[h][:], d["q"][:, 64:128], AF.Copy, [d["Q"]], [USB[h]])
                    else:
                        em.cp("dve", usb[h][:], d["q"][:, 64:128], [d["Q"]], [USB[h]])
                for d in HD:
                    h, hp, rows, hc = d["h"], d["hp"], d["rows"], d["hc"]
                    So = STT[hp][so[hp]][d["hh"]]
                    em.mm(d["q"][:, 256:320], ar[hp][rows, c, CH:2 * CH], stt[hp][so[hp]][rows, :], True, False, [AR[hp], So], [d["Q"]])
                    em.mm(d["q"][:, 256:320], mabk[h][:, 128:256], usb[h][:], False, False, [MABK[h], USB[h]], [d["Q"]])
                    em.mm(d["q"][:, 256:320], mabk[h][:, 384:512], vT[:, c, hc], False, True, [MABK[h], VT[c]], [d["Q"]])
                    em.mm(d["q"][rows, 128:192], bhT[:, c, hc], usb[h][:], True, False, [BHT[c], USB[h]], [d["Q"]])
                    em.mm(d["q"][rows, 128:192], khT[:, c, hc], vT[:, c, hc], False, True, [KHT[c], VT[c]], [d["Q"]])
                for d in HD:
                    h, hp, rows, hc = d["h"], d["hp"], d["rows"], d["hc"]
                    sn = 1 - so[hp]
                    em.stt("dve", stt[hp][sn][rows, :], stt[hp][so[hp]][rows, :], wc[hp][rows, c:c + 1], d["q"][rows, 128:192], ALU.mult, ALU.add,
                           [STT[hp][so[hp]][d["hh"]], WC[hp], d["Q"]], [STT[hp][sn][d["hh"]]])
                    em.act(ytok[:, hc], d["q"][:, 256:320], AF.Copy, [d["Q"]], [YTOK])
                    em.act(ysq[:, hc], d["q"][:, 256:320], AF.Square, [d["Q"]], [YSQ])
                ping[0], ping[1] = 1 - so[0], 1 - so[1]
                kb.op("dve", lambda e: e.tensor_reduce(sts[:, 0:4], ytok[:].rearrange("p (h v) -> p h v", v=64), AX.X, ALU.add), reads=[YTOK], writes=[STS])
                kb.op("dve", lambda e: e.tensor_reduce(sts[:, 4:8], ysq[:].rearrange("p (h v) -> p h v", v=64), AX.X, ALU.add), reads=[YSQ, STS], writes=[STS])
                kb.op("dve", lambda e: e.tensor_scalar(sts[:, 8:12], sts[:, 0:4], 1.0 / 64, None, ALU.mult), reads=[STS], writes=[STS])
                kb.op("dve", lambda e: e.tensor_tensor(sts[:, 12:16], sts[:, 8:12], sts[:, 8:12], ALU.mult), reads=[STS], writes=[STS])
                kb.op("dve", lambda e: e.scalar_tensor_tensor(sts[:, 16:20], sts[:, 4:8], 1.0 / 64, sts[:, 12:16], ALU.mult, ALU.subtract),
                      reads=[STS], writes=[STS])
                kb.op("dve", lambda e: e.tensor_scalar(sts[:, 16:20], sts[:, 16:20], GN_EPS, None, ALU.add), reads=[STS], writes=[STS])
                kb.op("act", lambda e: e.activation(out=sts[:, 20:24], in_=sts[:, 16:20], func=AF.Sqrt), reads=[STS], writes=[STS])
                kb.op("dve", lambda e: e.reciprocal(sts[:, 24:28], sts[:, 20:24]), reads=[STS], writes=[STS])
                kb.op("dve", lambda e: e.tensor_tensor(yn[:].rearrange("p (h v) -> p h v", v=64), ytok[:].rearrange("p (h v) -> p h v", v=64),
                                                       sts[:, 8:12].unsqueeze(2).to_broadcast([128, 4, 64]), ALU.subtract), reads=[YTOK, STS], writes=[YN])
                kb.op("dve", lambda e: e.tensor_tensor(yn[:].rearrange("p (h v) -> p h v", v=64), yn[:].rearrange("p (h v) -> p h v", v=64),
                                                       sts[:, 24:28].unsqueeze(2).to_broadcast([128, 4, 64]), ALU.mult), reads=[YN, STS], writes=[YN])
                for hp in range(2):
                    pbank, PT = next_pa()
                    pt = pbank[:, 0:128]
                    em.tr(pt, yn[:, hp * 128:(hp + 1) * 128], ident[:], [YN, IDENT], [PT])
                    em.ts("dve", yf[hp][:, csl], pt, vec[:, 18 + hp:19 + hp], vec[:, 20 + hp:21 + hp], ALU.mult, ALU.add, [PT, VEC], [YF[hp]])
            for hp in range(2):
                kb.op("pool", lambda e, hp=hp: e.tensor_tensor(osb[hp][:], yf[hp][:], bon[hp][:], ALU.add), reads=[YF[hp], BON[hp]], writes=[OSB[hp]])
                kb.op("pool", lambda e, hp=hp: e.tensor_tensor(osb[hp][:], osb[hp][:], gg[hp][:], ALU.mult), reads=[OSB[hp], GG[hp]], writes=[OSB[hp]])
                kb.dma("sp", f"out{hp}", lambda e, s, hp=hp, sgi=sgi: e.dma_start(out=yT_d[hp * 128:(hp + 1) * 128, sgi * NS:(sgi + 1) * NS], in_=osb[hp][:]).then_inc(s, 16),
                       reads=[OSB[hp]])
        kb.finish(OSB)


def build_a1(nseg=NSEG, debug=False):
    nc = bass.Bass("TRN2", target_bir_lowering=False)
    I = lambda n, shp: nc.dram_tensor(n, shp, F32, kind="ExternalInput").ap()
    D = dict(xT=I("xT", [1024, S]), w=I("w", [1024, 1024]), vec=I("vec", [128, 22]), lw=I("lw", [128, 256]), g2=I("g2", [128, 256]),
             cst=I("cst", [128, 1280]), yT=nc.dram_tensor("yT", [256, S], F32, kind="ExternalOutput").ap())
    with ExitStack() as st:
        kb = KB(nc, st)
        a1_body(nc, kb, D, nseg, debug)
        kb.emit()
    return nc


def a1_consts():
    m1 = np.ones((128, 512), np.float32); m1[:, ::CH] = 0.0
    s = np.arange(128)[:, None]; t = np.arange(128)[None, :]
    msu = (t > s).astype(np.float32); miu = (t >= s).astype(np.float32)
    msl = (t < s).astype(np.float32)
    bones = (s // 64 == t // 64).astype(np.float32)
    return np.concatenate([m1, msu, miu, msu, miu, msl, bones], axis=1)


def a1_inputs(inp, l, b, hh):
    ch = slice(256 * hh, 256 * hh + 256)
    w_in = inp["w_in"][l]
    w = np.concatenate([w_in[:, 0:512][:, ch], w_in[:, 512:1024][:, ch], w_in[:, 1024:1536][:, ch], w_in[:, 1536:1792]], axis=1)
    mu = inp["shift_mu"][l]
    mu_cols = np.concatenate([mu[0:512][ch], mu[512:1024][ch], mu[1024:1536][ch], mu[1536:1792]])
    vec = np.zeros((128, 22), np.float32)
    vec[:, 0:8] = mu_cols.reshape(8, 128).T
    def two(v):
        return v[ch].reshape(2, 128).T
    vec[:, 8:10] = two(inp["rw_w0"][l]); vec[:, 10:12] = two(inp["rw_a0"][l]); vec[:, 12:14] = two(inp["rw_kk"][l])
    vec[:, 14:16] = two(inp["rw_ka"][l]); vec[:, 16:18] = two(inp["rw_rk"][l].reshape(512))
    vec[:, 18:20] = two(inp["rw_ln_g"][l]); vec[:, 20:22] = two(inp["rw_ln_b"][l])
    lw = np.concatenate([inp["rw_w2"][l][:, ch], inp["rw_a2"][l][:, ch]], axis=0)
    g2 = inp["rw_g2"][l][:, ch]
    return dict(w=np.ascontiguousarray(w), vec=vec, lw=np.ascontiguousarray(lw), g2=np.ascontiguousarray(g2), cst=a1_consts())


import math

S = 4096
NEG = -30000.0
TW = 2304
DCL = 1280


def t5_bucket(n):
    n = np.maximum(n, 0); me = 16
    nf = np.maximum(n, 1).astype(np.float32)
    large = me + (np.log(nf / np.float32(me)) / np.float32(math.log(1024 / me)) * np.float32(32 - me)).astype(np.int32)
    large = np.minimum(large, 31)
    return np.where(n < me, n, large)


class Em:
    def __init__(self, kb):
        self.kb = kb

    def mm(self, out, lhsT, rhs, start, stop, reads, writes):
        self.kb.op("pe", lambda e, o=out, a=lhsT, b=rhs, s=start, t=stop: e.matmul(o, a, b, start=s, stop=t), reads, writes)

    def tr(self, out, in_, ident, reads, writes):
        self.kb.op("pe", lambda e, o=out, a=in_, b=ident: e.transpose(o, a, b), reads, writes)

    def act(self, out, in_, func, reads, writes, **kw):
        self.kb.op("act", lambda e, o=out, i=in_, f=func, kw=kw: e.activation(out=o, in_=i, func=f, **kw), reads, writes)

    def tt(self, eng, out, a, b, op, reads, writes):
        self.kb.op(eng, lambda e, o=out, a=a, b=b, op=op: e.tensor_tensor(o, a, b, op), reads, writes)

    def ts(self, eng, out, a, s1, s2, op0, op1, reads, writes):
        if op1 is None:
            self.kb.op(eng, lambda e, o=out, a=a, s1=s1, op0=op0: e.tensor_scalar(o, a, s1, None, op0), reads, writes)
        else:
            self.kb.op(eng, lambda e, o=out, a=a, s1=s1, s2=s2, op0=op0, op1=op1: e.tensor_scalar(o, a, s1, s2, op0, op1), reads, writes)

    def stt(self, eng, out, a, s, b, op0, op1, reads, writes):
        self.kb.op(eng, lambda e, o=out, a=a, s=s, b=b, op0=op0, op1=op1: e.scalar_tensor_tensor(o, a, s, b, op0, op1), reads, writes)

    def cp(self, eng, out, in_, reads, writes):
        self.kb.op(eng, lambda e, o=out, i=in_: e.tensor_copy(o, i), reads, writes)

    def ms(self, eng, out, val, writes):
        self.kb.op(eng, lambda e, o=out, v=val: e.memset(o, v), (), writes)

    def red(self, out, in_, op, reads, writes):
        self.kb.op("dve", lambda e, o=out, i=in_, op=op: e.tensor_reduce(o, i, AX.X, op), reads, writes)

    def rcp(self, out, in_, reads, writes):
        self.kb.op("dve", lambda e, o=out, i=in_: e.reciprocal(o, i), reads, writes)

    def dma(self, q, key, out, in_, reads, writes):
        self.kb.dma(q, key, lambda e, s, o=out, i=in_: e.dma_start(out=o, in_=i).then_inc(s, 16), reads, writes)


def a2_body(nc, kb, D, nq=8, debug=False):
    xT_d, wf_d, wt_d, w1_d, pe_d, w2_d, btc_d, mkc_d, bts_d, mks_d, mkw_d, m12_d, rv_d, c2s_d, exd_d, hsel_d = (D[k] for k in ['xT', 'wf', 'wt', 'w1', 'peT', 'w2', 'btc', 'mkc', 'bts', 'mks', 'mkw', 'm12', 'rv', 'c2s', 'exd', 'hsel'])
    yT_d = D["yT"]
    xT_v = xT_d.rearrange("(kc p) t -> p kc t", p=128)
    if True:
        em = Em(kb)
        sb, ps = kb.sb, kb.ps
        xs = [sb(f"xs{i}", [128, 8, 512], BF16) for i in range(2)]; XS = [Buf() for _ in range(2)]
        wf = sb("wf", [128, 8, 640], BF16); WF = Buf()
        wt = sb("wt", [128, 8, 256], BF16); WT = Buf()
        w1 = sb("w1", [128, 32, 128], BF16); W1 = Buf()
        peT = sb("peT", [128, 32], BF16); PET = Buf()
        w2f = sb("w2f", [128, 192]); w2 = sb("w2", [128, 192], BF16); W2 = Buf()
        qT = [sb(f"qT{i}", [128, S], BF16) for i in range(2)]; QT = [Buf() for _ in range(2)]
        ksT = sb("ksT", [128, S], BF16); KST = Buf()
        kwT = sb("kwT", [128, S], BF16); KWT = Buf()
        kcv = sb("kcv", [128, S], BF16); KCV = Buf()
        vs = sb("vs", [128, 32, 96], BF16); VS = Buf()
        vw = sb("vw", [128, 32, 96], BF16); VW = Buf()
        gt = sb("gt", [128, 32, 16]); GT = Buf()
        btc = sb("btc", [128, 4, 256]); BTC = Buf()
        mkc = sb("mkc", [128, 256]); MKC = Buf()
        HW_ = TW // 2
        stg = sb("stg", [128, HW_]); STG = Buf()
        stg2 = sb("stg2", [128, HW_]); STG2 = Buf()
        mks = sb("mks", [128, HW_]); MKS = Buf()
        mkw = sb("mkw", [128, HW_]); MKW = Buf()
        ebs = [sb(f"ebs{h}", [128, TW], BF16) for h in range(4)]; EBS = [Buf() for _ in range(4)]
        ebw = [sb(f"ebw{h}", [128, TW], BF16) for h in range(4)]; EBW = [Buf() for _ in range(4)]
        m12 = sb("m12", [128, 256]); M12 = Buf()
        rv = sb("rv", [128, 1]); RV = Buf()
        vca = sb("vca", [128, 2, 128], BF16); VCA = Buf()
        c2sf = sb("c2sf", [128, 128]); C2SF = Buf()
        exd = sb("exd", [128, 32, 128], BF16); EXD = Buf()
        hself = sb("hself", [128, 256]); hsel = sb("hsel", [128, 2, 128], BF16); HSEL = Buf()
        identf = sb("identf", [128, 128]); ident = sb("ident", [128, 128], BF16); IDENT = Buf()
        kct = sb("kct", [128, 256], BF16); KCT = Buf()
        gh = [sb(f"gh{i}", [128, 256], BF16) for i in range(2)]; GH = [Buf() for _ in range(2)]
        hx = sb("hx", [128, 256]); HX = Buf()
        hy = sb("hy", [128, 256]); HY = Buf()
        hb = sb("hb", [128, 2]); HB = Buf()
        sm = sb("sm", [128, 64]); SM = Buf()
        cst = sb("cst", [128, 16]); CST = Buf()
        qsq = sb("qsq", [128, 512], BF16); QSQ = Buf()
        mxc = sb("mxc", [128, 4, 8]); MXC = Buf()
        kxc = sb("kxc", [128, 2, 8]); KXC = Buf()
        lgs = [sb(f"lg{h}", [128, 256]) for h in range(4)]; LGS = [Buf() for _ in range(4)]
        pcs = [sb(f"pc{h}", [128, 256], BF16) for h in range(4)]; PCS = [Buf() for _ in range(4)]
        pcts = [sb(f"pct{h}", [128, 2, 128], BF16) for h in range(4)]; PCTS = [Buf() for _ in range(4)]
        smh = sb("smh", [128, 32]); SMH = [Buf() for _ in range(4)]
        sc = sb("sc", [128, 64]); SC = Buf()
        sc2 = sb("sc2", [128, 64]); SC2 = Buf()
        mx8 = sb("mx8", [128, 16]); MX8 = Buf()
        nst = sb("nst", [128, 64], BF16); NST = Buf()
        nsh = sb("nsh", [128, 512], BF16); NSH = Buf()
        yg = sb("yg", [128, 4, 256]); YG = Buf()
        ygt = sb("ygt", [128, 2, 512]); YGT = Buf()
        pt = [sb(f"pt{i}", [128, 512], BF16) for i in range(3)]; PT = [Buf() for _ in range(3)]
        p2 = [sb(f"p2{i}", [128, 512], BF16) for i in range(3)]; P2 = [Buf() for _ in range(3)]
        rin = sb("rin", [128, 8]); RIN = Buf()
        zb = sb("zb", [128, 512], BF16); ZB = Buf()
        otmp = sb("otmp", [128, 4, 64]); OTMP = Buf()
        pg = [ps(f"pg{i}", [128, 512]) for i in range(3)]; PG = [Buf(excl=True) for _ in range(3)]
        ptb = ps("ptb", [128, 1024], BF16); PTB = Buf(excl=True)
        pl = [ps(f"pl{i}", [128, 512]) for i in range(2)]; PL = [Buf(excl=True) for _ in range(2)]
        pacc = [ps(f"pacc{i}", [128, 4, 128]) for i in range(2)]; PACC = [Buf(excl=True) for _ in range(2)]

        gi = [0]

        def gbank():
            i = gi[0] % 3; gi[0] += 1
            return pg[i], PG[i]

        dbg_n = [0]

        def dbg(name, ap, B):
            if not debug:
                return
            d = nc.dram_tensor("dbg_" + name, list(ap.shape), F32, kind="ExternalOutput").ap()
            dbg_n[0] += 1
            cntv = dbg_n[0] * 16

            def f(e, s, d=d, ap=ap, cntv=cntv):
                e.dma_start(out=d, in_=ap).then_inc(s, 16)
                e.wait_ge(s, cntv)
            kb.dma("pool", "dbg", f, reads=[B])

        em.dma("pool", "wf", wf[:, :, :], wf_d.rearrange("(kc p) n -> p kc n", p=128), [], [WF])
        em.dma("pool", "wt", wt[:, :, 0:140], wt_d.rearrange("(kc p) n -> p kc n", p=128), [], [WT])
        em.dma("pool", "w1", w1[:, :, :], w1_d.rearrange("p (a b) -> p a b", b=128), [], [W1])
        em.dma("pool", "pe", peT[:], pe_d[:, :], [], [PET])
        em.dma("sp", "w2", w2f[:], w2_d[:, :], [], [W2])
        em.dma("sp", "btc", btc[:].rearrange("p h w -> p (h w)"), btc_d[:, :], [], [BTC])
        em.dma("sp", "mkc", mkc[:], mkc_d[:, :], [], [MKC])
        em.dma("sp", "m12", m12[:], m12_d[:, :], [], [M12])
        em.dma("sp", "rv", rv[:], rv_d[:, :], [], [RV])
        em.dma("sp", "c2s", c2sf[:], c2s_d[:, :], [], [C2SF])
        em.ms("pool", exd[:], 0.0, [EXD])
        em.dma("pool", "exd", exd[0:64, :, :].rearrange("p a b -> p (a b)"), exd_d[:, :], [EXD], [EXD])
        em.dma("sp", "hsel", hself[:], hsel_d[:, :], [], [HSEL])
        em.cp("dve", w2[:], w2f[:], [W2], [W2])
        em.cp("dve", hsel[:].rearrange("p a b -> p (a b)"), hself[:], [HSEL], [HSEL])
        em.ms("pool", identf[:], 1.0, [IDENT])
        kb.op("pool", lambda e: e.affine_select(out=identf[:], in_=identf[:], pattern=[[-1, 128]], compare_op=ALU.is_equal,
                                                fill=0.0, base=0, channel_multiplier=1), [IDENT], [IDENT])
        em.cp("pool", ident[:], identf[:], [IDENT], [IDENT])
        em.ms("dve", vs[:, :, 64:65], 1.0, [VS])
        em.ms("dve", vw[:, :, 64:65], 1.0, [VW])
        em.ms("dve", vca[:], 0.0, [VCA])
        em.ms("dve", zb[:], 0.0, [ZB])
        em.ms("dve", nsh[:], 0.0, [NSH])
        for h_ in range(4):
            em.ms("dve", pcs[h_][:], 0.0, [PCS[h_]])
        em.ms("dve", kct[:], 0.0, [KCT])
        for h in range(4):
            em.tt("dve", btc[:, h, :], btc[:, h, :], mkc[:], ALU.add, [BTC, MKC], [BTC])
        first = True
        for hf in range(2):
            cs_ = slice(hf * HW_, (hf + 1) * HW_)
            em.dma("sp", "mks", mks[:], mks_d[:, cs_], [], [MKS])
            em.dma("sp", "mkw", mkw[:], mkw_d[:, cs_], [], [MKW])
            for h in range(4):
                em.dma("sp", "stg", stg[:], bts_d[:, h * TW + hf * HW_:h * TW + (hf + 1) * HW_], [], [STG])
                if first:
                    em.red(cst[:, 8:9], stg[:], ALU.max, [STG], [CST])
                    first = False
                else:
                    em.red(cst[:, 9:10], stg[:], ALU.max, [STG], [CST])
                    em.tt("dve", cst[:, 8:9], cst[:, 8:9], cst[:, 9:10], ALU.max, [CST], [CST])
                em.tt("dve", stg2[:], stg[:], mks[:], ALU.add, [STG, MKS], [STG2])
                em.act(ebs[h][:, cs_], stg2[:], AF.Exp, [STG2], [EBS[h]])
                em.tt("dve", stg2[:], stg[:], mkw[:], ALU.add, [STG, MKW], [STG2])
                em.act(ebw[h][:, cs_], stg2[:], AF.Exp, [STG2], [EBW[h]])

        import os as _os
        PH = int(_os.environ.get('PH', '9'))
        def load_x(sg):
            sl = sg % 2
            em.dma("pool", f"xs{sl}", xs[sl][:, :, :], xT_v[:, :, sg * 512:(sg + 1) * 512], [], [XS[sl]])

        load_x(0)
        for sg in range(8 if PH >= 2 else 0):
            sl = sg % 2
            if sg + 1 < 8:
                load_x(sg + 1)
            seg = slice(sg * 512, (sg + 1) * 512)
            dsts = [(qT[0], QT[0], 0.125), (qT[1], QT[1], 0.125), (ksT, KST, 1.0), (kwT, KWT, 1.0), (kcv, KCV, 1.0)]
            for j, (dst, DST, scl) in enumerate(dsts):
                p, P = gbank()
                for k in range(8):
                    em.mm(p[:, :], wf[:, k, j * 128:(j + 1) * 128], xs[sl][:, k, :], k == 0, k == 7, [WF, XS[sl]], [P])
                em.act(dst[:, seg], p[:, :], AF.Copy, [P], [DST], scale=scl)
            PJ = _os.environ.get('PJ', 'abc12')
            for tt_ in range(4 if 'b' in PJ else 0):
                tile_i = sg * 4 + tt_
                p, P = gbank()
                for k in range(8):
                    em.mm(p[:, 0:140], xs[sl][:, k, tt_ * 128:(tt_ + 1) * 128], wt[:, k, 0:140], k == 0, k == 7, [WT, XS[sl]], [P])
                if '1' in PJ:
                    em.cp("dve", vs[:, tile_i, 0:64], p[:, 0:64], [P], [VS])
                    em.cp("dve", vw[:, tile_i, 0:64], p[:, 64:128], [P], [VW])
                if '2' in PJ:
                    em.act(gt[:, tile_i, 0:12], p[:, 128:140], AF.Sigmoid, [P], [GT])
            for i in range(2 if 'c' in PJ else 0):
                em.tt("dve", qsq[:], qT[i][:, seg], qT[i][:, seg], ALU.mult, [QT[i]], [QSQ])
                for hh in range(2):
                    p, P = gbank()
                    em.mm(p[:, :], hsel[:, hh, :], qsq[:], True, True, [HSEL, QSQ], [P])
                    em.red(mxc[:, 2 * i + hh, sg:sg + 1], p[:, :], ALU.max, [P], [MXC])
            for i, (src, SRC) in enumerate(((ksT, KST), (kwT, KWT)) if 'c' in PJ else ()):
                em.tt("dve", qsq[:], src[:, seg], src[:, seg], ALU.mult, [SRC], [QSQ])
                p, P = gbank()
                em.mm(p[:, :], hsel[:, 0, :], qsq[:], True, True, [HSEL, QSQ], [P])
                em.red(kxc[:, i, sg:sg + 1], p[:, :], ALU.max, [P], [KXC])
        if PH < 3:
            nq = 0
        em.red(sm[:, 0:4], mxc[:], ALU.max, [MXC], [SM])
        em.red(sm[:, 4:6], kxc[:], ALU.max, [KXC], [SM])
        for br in range(2):
            em.ts("dve", sm[:, 8 + 4 * br:12 + 4 * br], sm[:, 0:4], sm[:, 4 + br:5 + br], None, ALU.mult, None, [SM], [SM])
        em.act(sm[:, 16:24], sm[:, 8:16], AF.Sqrt, [SM], [SM])
        em.ts("dve", cst[:, 0:8], sm[:, 16:24], cst[:, 8:9], -1.0, ALU.add, ALU.mult, [SM, CST], [CST])

        for br in range(2 if PH >= 3 else 0):
            rows = slice(64 * br, 64 * br + 64)
            p, P = gbank()
            for pp in range(32):
                em.mm(p[:, 0:255], w1[rows, pp, :], kcv[rows, pp:pp + 16 * 254 + 1:16], pp == 0, pp == 31, [W1, KCV], [P])
            pb, PB = gbank()
            for pp in range(32):
                em.mm(pb[:, 0:1], w1[rows, pp, :], peT[rows, pp:pp + 1], pp == 0, pp == 31, [W1, PET], [PB])
            em.cp("dve", hb[:, br:br + 1], pb[:, 0:1], [PB], [HB])
            em.ts("dve", hx[:, 0:255], p[:, 0:255], hb[:, br:br + 1], None, ALU.add, None, [P, HB], [HX])
            em.tt("dve", hy[:, 0:255], hx[:, 0:255], hx[:, 0:255], ALU.mult, [HX], [HY])
            em.ts("dve", hy[:, 0:255], hy[:, 0:255], 0.044715, 1.0, ALU.mult, ALU.add, [HY], [HY])
            em.tt("dve", hy[:, 0:255], hy[:, 0:255], hx[:, 0:255], ALU.mult, [HY, HX], [HY])
            em.act(hy[:, 0:255], hy[:, 0:255], AF.Tanh, [HY], [HY], scale=0.7978845608028654)
            em.ts("dve", hy[:, 0:255], hy[:, 0:255], 1.0, 0.5, ALU.add, ALU.mult, [HY], [HY])
            em.tt("dve", gh[br][:, 0:255], hy[:, 0:255], hx[:, 0:255], ALU.mult, [HY, HX], [GH[br]])
        p, P = gbank()
        em.mm(p[:, 0:255], w2[:, 0:128], gh[0][:, 0:255], True, True, [W2, GH[0]], [P])
        em.act(kct[:, 0:255], p[:, 0:255], AF.Copy, [P], [KCT])
        for ct in range(2):
            ncs = 128 if ct == 0 else 127
            p, P = gbank()
            em.mm(p[0:ncs, 0:64], gh[1][:, ct * 128:ct * 128 + ncs], w2[:, 128:192], True, True, [W2, GH[1]], [P])
            em.act(vca[0:ncs, ct, 0:64], p[0:ncs, 0:64], AF.Copy, [P], [VCA])
        em.cp("dve", vca[:, :, 64:128], c2sf[:].rearrange("p (a b) -> p a b", b=64), [C2SF], [VCA])
        dbg("kct", kct[:, 0:255], KCT); dbg("vca", vca[:].rearrange("p a b -> p (a b)"), VCA)
        dbg("cst", cst[:, 0:9], CST)

        pti = [0]
        for Q in range(nq):
            qs = slice(Q * 512, (Q + 1) * 512)
            for a in range(4):
                n = 4 * Q + a
                t0 = 128 * n
                ncol = min(255, 8 * n + 7)
                off = 248 - 8 * n
                ncp = min(256, (ncol + 31) // 32 * 32)
                nct = 1 if ncol <= 128 else 2
                for hpair in ((0, 1), (2, 3)):
                    HP = {}
                    for h in hpair:
                        rows = slice(64 * (h % 2), 64 * (h % 2) + 64)
                        p, P = gbank()
                        em.mm(p[:, 0:ncp], qT[h // 2][rows, t0:t0 + 128], kct[rows, 0:ncp], True, True, [QT[h // 2], KCT], [P])
                        HP[h] = (p, P)
                    for h in hpair:
                        p, P = HP[h]
                        s0 = 8 * h
                        em.tt("dve", lgs[h][:, 0:ncol], p[:, 0:ncol], btc[:, h, off:off + ncol], ALU.add, [P, BTC], [LGS[h]])
                        em.red(smh[:, s0:s0 + 1], lgs[h][:, 0:ncol], ALU.max, [LGS[h]], [SMH[h]])
                        em.ts("dve", smh[:, s0 + 1:s0 + 2], smh[:, s0:s0 + 1], -1.0, None, ALU.mult, None, [SMH[h]], [SMH[h]])
                        em.ms("dve", smh[:, s0 + 2:s0 + 3], 0.0, [SMH[h]])
                    for h in hpair:
                        s0 = 8 * h
                        em.act(pcs[h][:, 0:ncol], lgs[h][:, 0:ncol], AF.Exp, [LGS[h], SMH[h]], [PCS[h], SMH[h]], bias=smh[:, s0 + 1:s0 + 2], accum_out=smh[:, s0 + 2:s0 + 3])
                    for h in hpair:
                        for ct in range(nct):
                            em.tr(ptb[:, ct * 128:(ct + 1) * 128], pcs[h][:, ct * 128:(ct + 1) * 128], ident[:], [PCS[h], IDENT], [PTB])
                        em.cp("dve", pcts[h][:, 0:nct, :], ptb[:, 0:nct * 128].rearrange("p (a b) -> p a b", b=128), [PTB], [PCTS[h]])
                    PO_ = {}
                    for h in hpair:
                        po, PO = gbank()
                        for ct in range(nct):
                            em.mm(po[:, 0:128], pcts[h][:, ct, :], vca[:, ct, :], ct == 0, ct == nct - 1, [PCTS[h], VCA], [PO])
                        PO_[h] = (po, PO)
                    for h in hpair:
                        po, PO = PO_[h]
                        s0 = 8 * h
                        em.rcp(smh[:, s0 + 3:s0 + 4], smh[:, s0 + 2:s0 + 3], [SMH[h]], [SMH[h]])
                        if n == 0:
                            em.tt("dve", smh[:, s0 + 3:s0 + 4], smh[:, s0 + 3:s0 + 4], rv[:], ALU.mult, [SMH[h], RV], [SMH[h]])
                        em.tt("dve", smh[:, s0 + 4:s0 + 5], smh[:, s0 + 3:s0 + 4], gt[:, n, 3 * h:3 * h + 1], ALU.mult, [SMH[h], GT], [SMH[h]])
                        em.ts("dve", yg[:, a, h * 64:(h + 1) * 64], po[:, 0:64], smh[:, s0 + 4:s0 + 5], None, ALU.mult, None, [PO, SMH[h]], [YG])
                        if h == 0:
                            em.ts("dve", sc[:], po[:, 64:128], smh[:, s0 + 3:s0 + 4], None, ALU.mult, None, [PO, SMH[h]], [SC])
                        else:
                            em.stt("dve", sc[:], po[:, 64:128], smh[:, s0 + 3:s0 + 4], sc[:], ALU.mult, ALU.add, [PO, SMH[h], SC], [SC])
                w0 = 64 - 2 * n
                em.tt("dve", sc[:], sc[:], m12[:, w0:w0 + 64], ALU.mult, [SC, M12], [SC])
                em.tt("dve", sc[:], sc[:], m12[:, 128 + w0:128 + w0 + 64], ALU.add, [SC, M12], [SC])
                em.ms("dve", sc[:, 0:1], 1.0e4, [SC])
                kb.op("dve", lambda e: e.max(out=mx8[:, 0:8], in_=sc[:]), [SC], [MX8])
                kb.op("dve", lambda e: e.match_replace(out=sc2[:], in_to_replace=mx8[:, 0:8], in_values=sc[:], imm_value=-1.0e9), [SC, MX8], [SC2])
                kb.op("dve", lambda e: e.max(out=mx8[:, 8:16], in_=sc2[:]), [SC2], [MX8])
                em.red(sm[:, 40:41], mx8[:, 8:16], ALU.min, [MX8], [SM])
                em.ts("dve", nst[:], sc[:], sm[:, 40:41], NEG, ALU.is_lt, ALU.mult, [SC, SM], [NST])
                em.tr(ptb[0:64, 256:384], nst[:], ident[:], [NST, IDENT], [PTB])
                em.cp("dve", nsh[0:64, a * 128:(a + 1) * 128], ptb[0:64, 256:384], [PTB], [NSH])
                if Q == 0 and a == 1:
                    dbg("sc", sc[:], SC); dbg("yg1", yg[:, 1, :], YG)
            QP = _os.environ.get('QP', 'abc')
            for br in [b_ for b_ in range(2) if 'bc'[b_] in QP]:
                kT, KT_, vv, VV, eb, EB = (ksT, KST, vs, VS, ebs, EBS) if br == 0 else (kwT, KWT, vw, VW, ebw, EBW)
                m_lo = 0 if br == 0 else max(0, 4 * Q - 4)
                m_hi = 4 * Q + 3
                tiles = [(h, m) for h in range(4) for m in range(m_lo, m_hi + 1)]
                slot = {}

                def stage1(ti):
                    h, m = tiles[ti]
                    rows = slice(64 * (h % 2), 64 * (h % 2) + 64)
                    pli = pti[0] % 2
                    bi = pti[0] % 3
                    pti[0] += 1
                    slot[ti] = (pli, bi)
                    pl_, PL_ = pl[pli], PL[pli]
                    em.mm(pl_[:, :], kT[rows, m * 128:(m + 1) * 128], qT[h // 2][rows, qs], True, br == 1, [KT_, QT[h // 2]], [PL_])
                    if br == 0:
                        em.mm(pl_[:, :], exd[:, m, :], nsh[:, :], False, True, [EXD, NSH], [PL_])

                def stage23(ti):
                    h, m = tiles[ti]
                    pli, bi = slot.pop(ti)
                    pl_, PL_ = pl[pli], PL[pli]
                    acc, ACC = pacc[h % 2], PACC[h % 2]
                    D0 = 512 * Q - 128 * m
                    wst = min(D0, DCL) + 512
                    em.act(pt[bi][:], pl_[:, :], AF.Exp, [PL_, CST], [PT[bi]], bias=cst[:, 4 * br + h:4 * br + h + 1])
                    em.tt("dve", p2[bi][:], pt[bi][:], eb[h][:, wst:wst + 512], ALU.mult, [PT[bi], EB[h]], [P2[bi]])
                    if m == m_lo:
                        em.mm(acc[:].rearrange("p a b -> p (a b)"), zb[:, 0:128], zb[:, 0:512], True, False, [ZB], [ACC])
                    for a in range(4):
                        last_m = min(m_hi, 4 * Q + a)
                        if m > last_m:
                            continue
                        a_lo = m_lo if br == 0 else max(m_lo, 4 * Q + a - 4)
                        if m < a_lo:
                            continue
                        em.mm(acc[:, a, 0:65], p2[bi][:, a * 128:(a + 1) * 128], vv[:, m, 0:65], False, (m == m_hi and a == 3), [P2[bi], VV], [ACC])
                    if m == m_hi:
                        em.rcp(rin[:, 0:4], acc[:, :, 64], [ACC], [RIN])
                        em.tt("dve", rin[:, 4:8], rin[:, 0:4], gt[:, 4 * Q:4 * Q + 4, 3 * h + 1 + br], ALU.mult, [RIN, GT], [RIN])
                        em.tt("dve", otmp[:], acc[:, :, 0:64], rin[:, 4:8].unsqueeze(2).to_broadcast([128, 4, 64]), ALU.mult, [ACC, RIN], [OTMP])
                        em.tt("pool", yg[:, :, h * 64:(h + 1) * 64], yg[:, :, h * 64:(h + 1) * 64], otmp[:], ALU.add, [YG, OTMP], [YG])
                stage1(0)
                for ti in range(len(tiles)):
                    if ti + 1 < len(tiles):
                        stage1(ti + 1)
                    stage23(ti)
            for a in range(4):
                for hp in range(2):
                    pz, PZ = gbank()
                    em.tr(pz[:, 0:128], yg[:, a, hp * 128:(hp + 1) * 128], identf[:], [YG, IDENT], [PZ])
                    em.cp("dve" if hp else "pool_never", ygt[:, hp, a * 128:(a + 1) * 128], pz[:, 0:128], [PZ], [YGT]) if False else em.cp("dve", ygt[:, hp, a * 128:(a + 1) * 128], pz[:, 0:128], [PZ], [YGT])
            for hp in range(2):
                em.dma("sp", "y", yT_d[hp * 128:(hp + 1) * 128, Q * 512:(Q + 1) * 512], ygt[:, hp, :], [YGT], [])
        kb.finish([YG, YGT])


def build_a2(nq=8, debug=False):
    nc = bass.Bass("TRN2", target_bir_lowering=False)
    I = lambda n, shp: nc.dram_tensor(n, shp, F32, kind="ExternalInput").ap()
    D = {"xT": I("xT", [1024, S]), "wf": I("wf", [1024, 640]), "wt": I("wt", [1024, 140]), "w1": I("w1", [128, 32 * 128]), "peT": I("peT", [128, 32]), "w2": I("w2", [128, 192]), "btc": I("btc", [128, 4 * 256]), "mkc": I("mkc", [128, 256]), "bts": I("bts", [128, 4 * TW]), "mks": I("mks", [128, TW]), "mkw": I("mkw", [128, TW]), "m12": I("m12", [128, 256]), "rv": I("rv", [128, 1]), "c2s": I("c2s", [128, 128]), "exd": I("exd", [64, 32 * 128]), "hsel": I("hsel", [128, 256])}
    D["yT"] = nc.dram_tensor("yT", [256, S], F32, kind="ExternalOutput").ap()
    with ExitStack() as st:
        kb = KB(nc, st)
        a2_body(nc, kb, D, nq, debug)
        kb.emit()
    return nc


def a2_consts():
    i = np.arange(128)[:, None]
    j = np.arange(256)[None, :]
    dc = i - 16 * (j - 248) - 31
    mkc = np.where(dc >= 0, 0.0, NEG).astype(np.float32)
    w = np.arange(TW)[None, :]
    ds = w - i - 512
    mks = np.where(ds >= 0, 0.0, NEG).astype(np.float32)
    mkw = np.where((ds >= 0) & (ds < 512), 0.0, NEG).astype(np.float32)
    wv = np.arange(128)[None, :] - 64
    cur = (i >= 64).astype(np.int64)
    forced = (wv == cur) | (wv == cur - 1)
    valid = wv <= cur
    m1 = (valid & ~forced).astype(np.float32)
    m2 = np.where(forced, 1.0e4, np.where(valid, 0.0, -1.0)).astype(np.float32)
    m12 = np.concatenate([m1, m2], axis=1)
    rv = (np.arange(128) >= 31).astype(np.float32)[:, None]
    cs = np.arange(255)[:, None] * 16; ss = np.arange(64)[None, :] * 64
    ov = np.clip(np.minimum(cs + 32, ss + 64) - np.maximum(cs, ss), 0, None).astype(np.float32) / 32
    c2s = np.zeros((256, 64), np.float32); c2s[:255] = ov
    c2s = c2s.reshape(2, 128, 64).transpose(1, 0, 2).reshape(128, 128)
    exd = np.zeros((64, 32, 128), np.float32)
    for m in range(32):
        exd[2 * m, m, 0:64] = 1.0; exd[2 * m + 1, m, 64:128] = 1.0
    hsel = np.zeros((128, 2, 128), np.float32); hsel[0:64, 0, :] = 1.0; hsel[64:128, 1, :] = 1.0
    return dict(mkc=mkc, mks=mks, mkw=mkw, m12=m12, rv=rv, c2s=c2s, exd=exd.reshape(64, 32 * 128), hsel=hsel.reshape(128, 256),
                dc=dc, ds=ds)


def a2_inputs(inp, l, g, consts):
    w_in = inp["w_in"][l]; zr = 1792
    q = w_in[:, zr + 256 * g: zr + 256 * g + 256]
    def kvc(off):
        return w_in[:, zr + off + 64 * g: zr + off + 64 * g + 64]
    kc, vc, ks, vs, kw, vw = (kvc(o) for o in (512, 640, 768, 896, 1024, 1152))
    gates = w_in[:, zr + 1280 + 12 * g: zr + 1280 + 12 * g + 12]
    wf = np.concatenate([q, ks, ks, kw, kw, kc, vc], axis=1)
    wt = np.concatenate([vs, vw, gates], axis=1)
    def w1r(w):
        return w.reshape(32, 64, 128).transpose(1, 0, 2)
    w1 = np.concatenate([w1r(inp["cmp_w1_k"][l]), w1r(inp["cmp_w1_v"][l])], axis=0).reshape(128, 32 * 128)
    peT = np.concatenate([inp["cmp_pe_k"][l].T, inp["cmp_pe_v"][l].T], axis=0)
    w2 = np.concatenate([inp["cmp_w2_k"][l], inp["cmp_w2_k"][l], inp["cmp_w2_v"][l]], axis=1)
    rb = inp["rel_bias"][:, 4 * g:4 * g + 4]
    btc = np.take(rb, t5_bucket(consts["dc"]), axis=0).transpose(0, 2, 1).reshape(128, 4 * 256)
    bts = np.take(rb, t5_bucket(consts["ds"]), axis=0).transpose(0, 2, 1).reshape(128, 4 * TW)
    out = dict(wf=wf, wt=wt, w1=w1, peT=peT, w2=w2, btc=btc, bts=bts)
    for k in ("mkc", "mks", "mkw", "m12", "rv", "c2s", "exd", "hsel"):
        out[k] = consts[k]
    return {k: np.ascontiguousarray(v, dtype=np.float32) for k, v in out.items()}


NT = 2048
ALPHA = 8 ** 0.25
LN_EPS = 1e-5


def layer_norm_fm(kb, em, gbank, R, RB, out_fn, g_ap, b_ap, GB, tmp, TMP, ones, ONES, sq, SQ, mean, MEAN, rstd, RSTD):
    pm, PM = gbank()
    for i in range(8):
        em.mm(pm[:, :], ones[:], R[:, i, :], i == 0, i == 7, [ONES, RB], [PM])
    em.act(mean[:], pm[:, :], AF.Copy, [PM], [MEAN], scale=1.0 / 1024)
    pv, PV = gbank()
    for i in range(8):
        em.tt("pool" if i % 2 else "dve", sq[i % 2][:], R[:, i, :], R[:, i, :], ALU.mult, [RB], [SQ[i % 2]])
        em.mm(pv[:, :], ones[:], sq[i % 2][:], i == 0, i == 7, [ONES, SQ[i % 2]], [PV])
    em.tt("dve", tmp[:], mean[:], mean[:], ALU.mult, [MEAN], [TMP])
    em.stt("dve", rstd[:], pv[:, :], 1.0 / 1024, tmp[:], ALU.mult, ALU.subtract, [PV, TMP], [RSTD])
    em.ts("dve", rstd[:], rstd[:], LN_EPS, None, ALU.add, None, [RSTD], [RSTD])
    em.act(rstd[:], rstd[:], AF.Sqrt, [RSTD], [RSTD])
    em.rcp(rstd[:], rstd[:], [RSTD], [RSTD])
    for i in range(8):
        eng = "pool" if i % 2 else "dve"
        em.tt(eng, tmp[:], R[:, i, :], mean[:], ALU.subtract, [RB, MEAN], [TMP])
        em.tt(eng, tmp[:], tmp[:], rstd[:], ALU.mult, [TMP, RSTD], [TMP])
        o, O = out_fn(i)
        em.ts(eng, o, tmp[:], g_ap[:, i:i + 1], b_ap[:, i:i + 1], ALU.mult, ALU.add, [TMP, GB], [O])


def b1_body(nc, kb, D):
    xT_d, yr_d, yn_d, wg_d, wur_d, wun_d, wo_d, ln_d, o_d = (D[k] for k in ("xT", "yrT", "ynT", "wg", "wur", "wun", "wo", "ln", "x1T"))
    v3 = lambda ap: ap.rearrange("(kc p) n -> p kc n", p=128)
    if True:
        em = Em(kb); sb, ps = kb.sb, kb.ps
        wg = sb("wg", [128, 8, 2048], BF16); WG = Buf()
        wur = sb("wur", [128, 4, 1024], BF16); WUR = Buf()
        wun = sb("wun", [128, 4, 1024], BF16); WUN = Buf()
        wo = sb("wo", [128, 8, 1024], BF16); WO = Buf()
        ln = sb("ln", [128, 16]); LN = Buf()
        ones = sb("ones", [128, 128]); ONES = Buf()
        xf = sb("xf", [128, 8, 512]); XF = Buf()
        xb = sb("xb", [128, 8, 512], BF16); XB = Buf()
        yr = sb("yr", [128, 4, 512], BF16); YR = Buf()
        yn = sb("yn", [128, 4, 512], BF16); YN = Buf()
        sg = [sb(f"sg{i}", [128, 512]) for i in range(2)]; SG = [Buf() for _ in range(2)]
        m1 = sb("m1", [128, 512]); M1 = Buf()
        mg = sb("mg", [128, 8, 512], BF16); MG = Buf()
        R = sb("R", [128, 8, 512]); RB = Buf()
        ob = sb("ob", [128, 8, 512]); OB = Buf()
        tmp = sb("tmp", [128, 512]); TMP = Buf()
        sq = [sb(f"sq{i}", [128, 512]) for i in range(2)]; SQ = [Buf() for _ in range(2)]
        mean = sb("mean", [128, 512]); MEAN = Buf()
        rstd = sb("rstd", [128, 512]); RSTD = Buf()
        pg = [ps(f"pg{i}", [128, 512]) for i in range(6)]; PG = [Buf(excl=True) for _ in range(6)]
        gi = [0]

        def gbank():
            i = gi[0] % 6; gi[0] += 1
            return pg[i], PG[i]
        for k0 in range(0, 8, 2):
            em.dma("pool", "wg", wg[:, k0:k0 + 2, :], v3(wg_d)[:, k0:k0 + 2, :], [], [WG])
        em.dma("pool", "wur", wur[:, :, :], v3(wur_d), [], [WUR])
        em.dma("pool", "wun", wun[:, :, :], v3(wun_d), [], [WUN])
        em.dma("pool", "wo", wo[:, :, :], v3(wo_d), [], [WO])
        em.dma("sp", "ln", ln[:], ln_d[:, :], [], [LN])
        em.ms("dve", ones[:], 1.0, [ONES])
        for tg in range(NT // 512):
            ts_ = slice(tg * 512, (tg + 1) * 512)
            em.dma("sp", "xf", xf[:, :, :], v3(xT_d)[:, :, ts_], [], [XF])
            em.dma("pool", "yr", yr[:, :, :], v3(yr_d)[:, :, ts_], [], [YR])
            em.dma("pool", "yn", yn[:, :, :], v3(yn_d)[:, :, ts_], [], [YN])
            for i in range(8):
                em.cp("pool" if i % 2 else "dve", xb[:, i, :], xf[:, i, :], [XF], [XB])
            for j in range(8):
                cs = slice(j * 128, (j + 1) * 128)
                for br, (wu, WU, yy, YY) in enumerate(((wur, WUR, yr, YR), (wun, WUN, yn, YN))):
                    p, P = gbank()
                    for k in range(8):
                        em.mm(p[:, :], wg[:, k, br * 1024 + j * 128: br * 1024 + (j + 1) * 128], xb[:, k, :], k == 0, k == 7, [WG, XB], [P])
                    em.act(sg[br][:], p[:, :], AF.Sigmoid, [P], [SG[br]])
                    p2, P2 = gbank()
                    for k in range(4):
                        em.mm(p2[:, :], wu[:, k, cs], yy[:, k, :], k == 0, k == 3, [WU, YY], [P2])
                    if br == 0:
                        em.tt("dve", m1[:], sg[0][:], p2[:, :], ALU.mult, [SG[0], P2], [M1])
                    else:
                        em.tt("dve", sg[1][:], sg[1][:], p2[:, :], ALU.mult, [SG[1], P2], [SG[1]])
                        em.tt("pool", mg[:, j, :], m1[:], sg[1][:], ALU.add, [M1, SG[1]], [MG])
            for i in range(8):
                p, P = gbank()
                for k in range(8):
                    em.mm(p[:, :], wo[:, k, i * 128:(i + 1) * 128], mg[:, k, :], k == 0, k == 7, [WO, MG], [P])
                em.stt("dve", R[:, i, :], xf[:, i, :], ALPHA, p[:, :], ALU.mult, ALU.add, [XF, P], [RB])
            layer_norm_fm(kb, em, gbank, R, RB, lambda i: (ob[:, i, :], OB), ln[:, 0:8], ln[:, 8:16], LN, tmp, TMP, ones, ONES, sq, SQ, mean, MEAN, rstd, RSTD)
            em.dma("sp", "ob", v3(o_d)[:, :, ts_], ob[:, :, :], [OB], [])
        kb.finish([OB])


def build_b1():
    nc = bass.Bass("TRN2", target_bir_lowering=False)
    I = lambda n, shp: nc.dram_tensor(n, shp, F32, kind="ExternalInput").ap()
    D = dict(xT=I("xT", [1024, NT]), yrT=I("yrT", [512, NT]), ynT=I("ynT", [512, NT]), wg=I("wg", [1024, 2048]), wur=I("wur", [512, 1024]),
             wun=I("wun", [512, 1024]), wo=I("wo", [1024, 1024]), ln=I("ln", [128, 16]),
             x1T=nc.dram_tensor("x1T", [1024, NT], F32, kind="ExternalOutput").ap())
    with ExitStack() as st:
        kb = KB(nc, st)
        b1_body(nc, kb, D)
        kb.emit()
    return nc


def b1_inputs(inp, l):
    w_in = inp["w_in"][l]
    lnp = np.concatenate([inp["ln1_g"][l].reshape(8, 128).T, inp["ln1_b"][l].reshape(8, 128).T], axis=1)
    return dict(wg=np.ascontiguousarray(w_in[:, 1792 + 1304: 1792 + 1304 + 2048]), wur=inp["w_up_rwkv"][l], wun=inp["w_up_nsa"][l],
                wo=inp["w_out"][l], ln=np.ascontiguousarray(lnp, dtype=np.float32))


def b2_body(nc, kb, D, nexp=32):
    x1_d, wr_d, br_d, w1_d, w3_d, w2_d, ln_d, selb_d, g2e_d, o_d = (D[k] for k in ("x1T", "wr", "brr", "ew1", "ew3", "ew2", "ln", "selb", "g2e", "x2T"))
    v3 = lambda ap: ap.rearrange("(kc p) n -> p kc n", p=128)
    if True:
        em = Em(kb); sb, ps = kb.sb, kb.ps
        wr = sb("wr", [128, 8, 64]); WR = Buf()
        brr = sb("brr", [128, 36]); BRR = Buf()
        ln = sb("ln", [128, 16]); LN = Buf()
        selb = sb("selb", [32, 32, 128], BF16); SELB = Buf()
        g2e = sb("g2e", [128, 4, 32]); G2E = Buf()
        ones = sb("ones", [128, 128]); ONES = Buf()
        ident = sb("ident", [128, 128]); IDENT = Buf()
        xf = sb("xf", [128, 8, 512]); XF = Buf()
        x1b = sb("x1b", [128, 8, NT], BF16); X1B = [Buf() for _ in range(4)]
        out = sb("out", [128, 8, NT]); OUT = [Buf() for _ in range(4)]
        cwt = sb("cwt", [32, NT], BF16); CWT = [Buf() for _ in range(4)]
        lgt = sb("lgt", [128, 36]); LGT = Buf()
        rs = sb("rs", [128, 64]); RS = Buf()
        em32 = sb("em32", [128, 32]); EM32 = Buf()
        em2 = sb("em2", [128, 32]); EM2 = Buf()
        cw = sb("cw", [128, 32]); CW = Buf()
        w1 = [sb(f"w1_{i}", [128, 8, 512], BF16) for i in range(2)]; W1 = [Buf() for _ in range(2)]
        w3 = [sb(f"w3_{i}", [128, 8, 512], BF16) for i in range(2)]; W3 = [Buf() for _ in range(2)]
        w2 = [sb(f"w2_{i}", [128, 4, 1024], BF16) for i in range(2)]; W2 = [Buf() for _ in range(2)]
        cwb = [sb(f"cwb{i}", [128, 512]) for i in range(2)]; CWB = [Buf() for _ in range(2)]
        sl_ = [sb(f"sl{i}", [128, 512]) for i in range(2)]; SL = [Buf() for _ in range(2)]
        hb = [sb(f"hb{i}", [128, 4, 512], BF16) for i in range(2)]; HB = [[Buf() for _ in range(4)] for _ in range(2)]
        ob = xf; OB = XF
        tmp = sb("tmp", [128, 512]); TMP = Buf()
        sq = [sb(f"sq{i}", [128, 512]) for i in range(2)]; SQ = [Buf() for _ in range(2)]
        mean = sb("mean", [128, 512]); MEAN = Buf()
        rstd = sb("rstd", [128, 512]); RSTD = Buf()
        pg = [ps(f"pg{i}", [128, 512]) for i in range(8)]; PG = [Buf(excl=True) for _ in range(8)]
        gi = [0]

        def gbank():
            i = gi[0] % 8; gi[0] += 1
            return pg[i], PG[i]
        em.ms("dve", wr[:], 0.0, [WR])
        em.dma("sp", "wr", wr[:, :, 0:36], v3(wr_d), [WR], [WR])
        em.dma("sp", "brr", brr[:], br_d[0:1, :].partition_broadcast(128), [], [BRR])
        em.dma("sp", "ln", ln[:], ln_d[:, :], [], [LN])
        em.dma("pool", "selb", selb[:].rearrange("p a b -> p (a b)"), selb_d[:, :], [], [SELB])
        em.dma("sp", "g2e", g2e[:].rearrange("p a b -> p (a b)"), g2e_d[:, :], [], [G2E])
        em.ms("dve", ones[:], 1.0, [ONES])
        em.ms("pool", ident[:], 1.0, [IDENT])
        kb.op("pool", lambda e: e.affine_select(out=ident[:], in_=ident[:], pattern=[[-1, 128]], compare_op=ALU.is_equal,
                                                fill=0.0, base=0, channel_multiplier=1), [IDENT], [IDENT])

        def load_w(e):
            s = e % 2
            for k0 in range(0, 8, 4):
                em.dma("pool", f"w1_{s}", w1[s][:, k0:k0 + 4, :], w1_d[e].rearrange("(kc p) n -> p kc n", p=128)[:, k0:k0 + 4, :], [], [W1[s]])
                em.dma("pool", f"w3_{s}", w3[s][:, k0:k0 + 4, :], w3_d[e].rearrange("(kc p) n -> p kc n", p=128)[:, k0:k0 + 4, :], [], [W3[s]])
            for k0 in range(0, 4, 2):
                em.dma("pool", f"w2_{s}", w2[s][:, k0:k0 + 2, :], w2_d[e].rearrange("(kc p) n -> p kc n", p=128)[:, k0:k0 + 2, :], [], [W2[s]])

        load_w(0)
        for tg in range(4):
            ts_ = slice(tg * 512, (tg + 1) * 512)
            em.dma("sp", "xf", xf[:, :, :], v3(x1_d)[:, :, ts_], [], [XF])
            for i in range(8):
                em.cp("pool" if i % 2 else "dve", x1b[:, i, ts_], xf[:, i, :], [XF], [X1B[tg]])
                em.ts("dve" if i % 2 else "pool", out[:, i, ts_], xf[:, i, :], ALPHA, None, ALU.mult, None, [XF], [OUT[tg]])
            for tt_ in range(4):
                p, P = gbank()
                for k in range(8):
                    em.mm(p[:, 0:36], xf[:, k, tt_ * 128:(tt_ + 1) * 128], wr[:, k, 0:36], k == 0, k == 7, [XF, WR], [P])
                em.tt("dve", lgt[:], p[:, 0:36], brr[:], ALU.add, [P, BRR], [LGT])
                em.red(rs[:, 0:1], lgt[:, 0:4], ALU.max, [LGT], [RS])
                em.ts("dve", rs[:, 1:2], rs[:, 0:1], -1.0, None, ALU.mult, None, [RS], [RS])
                em.ms("dve", rs[:, 2:3], 0.0, [RS])
                em.act(rs[:, 4:8], lgt[:, 0:4], AF.Exp, [LGT, RS], [RS], bias=rs[:, 1:2], accum_out=rs[:, 2:3])
                em.rcp(rs[:, 3:4], rs[:, 2:3], [RS], [RS])
                em.ts("dve", rs[:, 8:12], lgt[:, 0:4], rs[:, 0:1], None, ALU.is_ge, None, [LGT, RS], [RS])
                em.ts("dve", em32[:], g2e[:, 0, :], rs[:, 8:9], None, ALU.mult, None, [G2E, RS], [EM32])
                for g in range(1, 4):
                    em.stt("dve", em32[:], g2e[:, g, :], rs[:, 8 + g:9 + g], em32[:], ALU.mult, ALU.add, [G2E, RS, EM32], [EM32])
                em.tt("dve", em2[:], lgt[:, 4:36], em32[:], ALU.mult, [LGT, EM32], [EM2])
                em.ts("dve", em32[:], em32[:], -1.0, 1.0e9, ALU.add, ALU.mult, [EM32], [EM32])
                em.tt("dve", em2[:], em2[:], em32[:], ALU.add, [EM2, EM32], [EM2])
                em.red(rs[:, 12:13], em2[:], ALU.max, [EM2], [RS])
                em.ts("dve", cw[:], em2[:], rs[:, 12:13], None, ALU.is_ge, None, [EM2, RS], [CW])
                em.stt("dve", em32[:], cw[:], -2.0e9, em2[:], ALU.mult, ALU.add, [CW, EM2], [EM32])
                em.red(rs[:, 13:14], em32[:], ALU.max, [EM32], [RS])
                em.ts("dve", em32[:], em32[:], rs[:, 13:14], None, ALU.is_ge, None, [EM32, RS], [EM32])
                em.tt("dve", rs[:, 14:15], rs[:, 13:14], rs[:, 12:13], ALU.subtract, [RS], [RS])
                em.act(rs[:, 15:16], rs[:, 14:15], AF.Exp, [RS], [RS])
                em.ts("dve", rs[:, 15:16], rs[:, 15:16], 1.0, None, ALU.add, None, [RS], [RS])
                em.rcp(rs[:, 16:17], rs[:, 15:16], [RS], [RS])
                em.tt("dve", rs[:, 17:18], rs[:, 16:17], rs[:, 3:4], ALU.mult, [RS], [RS])
                em.tt("dve", rs[:, 18:19], rs[:, 3:4], rs[:, 17:18], ALU.subtract, [RS], [RS])
                em.ts("dve", cw[:], cw[:], rs[:, 17:18], None, ALU.mult, None, [CW, RS], [CW])
                em.stt("dve", cw[:], em32[:], rs[:, 18:19], cw[:], ALU.mult, ALU.add, [EM32, RS, CW], [CW])
                pt_, PT_ = gbank()
                em.tr(pt_[0:32, 0:128], cw[:], ident[:], [CW, IDENT], [PT_])
                em.cp("dve", cwt[:, tg * 512 + tt_ * 128: tg * 512 + (tt_ + 1) * 128], pt_[0:32, 0:128], [PT_], [CWT[tg]])
        tiles = [(e, tg) for e in range(nexp) for tg in range(4)]

        def stage1(ti):
            e, tg = tiles[ti]
            s = e % 2
            hbuf = ti % 2
            ts_ = slice(tg * 512, (tg + 1) * 512)
            pc_, PC_ = gbank()
            em.mm(pc_[:, :], selb[:, e, :], cwt[:, ts_], True, True, [SELB, CWT[tg]], [PC_])
            em.act(cwb[hbuf][:], pc_[:, :], AF.Copy, [PC_], [CWB[hbuf]])
            for f in range(4):
                fs = slice(f * 128, (f + 1) * 128)
                pa, PA = gbank()
                for k in range(8):
                    em.mm(pa[:, :], w1[s][:, k, fs], x1b[:, k, ts_], k == 0, k == 7, [W1[s], X1B[tg]], [PA])
                pb, PB = gbank()
                for k in range(8):
                    em.mm(pb[:, :], w3[s][:, k, fs], x1b[:, k, ts_], k == 0, k == 7, [W3[s], X1B[tg]], [PB])
                em.act(sl_[f % 2][:], pa[:, :], AF.Silu, [PA], [SL[f % 2]])
                em.tt("dve", sl_[f % 2][:], sl_[f % 2][:], pb[:, :], ALU.mult, [SL[f % 2], PB], [SL[f % 2]])
                em.tt("pool", hb[hbuf][:, f, :], sl_[f % 2][:], cwb[hbuf][:], ALU.mult, [SL[f % 2], CWB[hbuf]], [HB[hbuf][f]])

        def stage2(ti):
            e, tg = tiles[ti]
            s = e % 2
            hbuf = ti % 2
            ts_ = slice(tg * 512, (tg + 1) * 512)
            for i in range(8):
                po, PO = gbank()
                for f in range(4):
                    em.mm(po[:, :], w2[s][:, f, i * 128:(i + 1) * 128], hb[hbuf][:, f, :], f == 0, f == 3, [W2[s], HB[hbuf][f]], [PO])
                em.tt("dve", out[:, i, ts_], out[:, i, ts_], po[:, :], ALU.add, [OUT[tg], PO], [OUT[tg]])
        if nexp > 1:
            load_w(1)
        if tiles:
            stage1(0)
        for ti in range(len(tiles)):
            if ti + 1 < len(tiles):
                stage1(ti + 1)
            stage2(ti)
            e_, tg_ = tiles[ti]
            if tg_ == 3 and e_ + 2 < nexp:
                load_w(e_ + 2)
        for tg in range(4):
            ts_ = slice(tg * 512, (tg + 1) * 512)
            layer_norm_fm(kb, em, gbank, out[:, :, ts_], OUT[tg], lambda i: (ob[:, i, :], OB), ln[:, 0:8], ln[:, 8:16], LN, tmp, TMP,
                          ones, ONES, sq, SQ, mean, MEAN, rstd, RSTD)
            em.dma("sp", "ob", v3(o_d)[:, :, ts_], ob[:, :, :], [OB], [])
        kb.finish([OB])


def build_b2(nexp=32):
    nc = bass.Bass("TRN2", target_bir_lowering=False)
    I = lambda n, shp: nc.dram_tensor(n, shp, F32, kind="ExternalInput").ap()
    D = dict(x1T=I("x1T", [1024, NT]), wr=I("wr", [1024, 36]), brr=I("brr", [1, 36]), ew1=I("ew1", [32, 1024, 512]), ew3=I("ew3", [32, 1024, 512]),
             ew2=I("ew2", [32, 512, 1024]), ln=I("ln", [128, 16]), selb=I("selb", [32, 32 * 128]), g2e=I("g2e", [128, 4 * 32]),
             x2T=nc.dram_tensor("x2T", [1024, NT], F32, kind="ExternalOutput").ap())
    with ExitStack() as st:
        kb = KB(nc, st)
        b2_body(nc, kb, D, nexp)
        kb.emit()
    return nc


def b2_consts():
    selb = np.zeros((32, 32, 128), np.float32)
    for e in range(32):
        selb[e, e, :] = 1.0
    g2e = np.zeros((128, 4, 32), np.float32)
    for g in range(4):
        g2e[:, g, g * 8:(g + 1) * 8] = 1.0
    return dict(selb=selb.reshape(32, 32 * 128), g2e=g2e.reshape(128, 128))


def b2_inputs(inp, l, consts):
    wr = np.concatenate([inp["router_group_w"][l], inp["router_expert_w"][l]], axis=1)
    brr = np.concatenate([inp["router_group_b"][l], inp["router_expert_b"][l]])[None, :]
    lnp = np.concatenate([inp["ln2_g"][l].reshape(8, 128).T, inp["ln2_b"][l].reshape(8, 128).T], axis=1)
    return dict(wr=np.ascontiguousarray(wr), brr=np.ascontiguousarray(brr), ew1=inp["exp_w1"][l], ew3=inp["exp_w3"][l], ew2=inp["exp_w2"][l],
                ln=np.ascontiguousarray(lnp, dtype=np.float32), selb=consts["selb"], g2e=consts["g2e"])


L_ = 4


def build_fused(nl=L_):
    nc = bass.Bass("TRN2", target_bir_lowering=False)

    def I(n, shp):
        return nc.dram_tensor(n, list(shp), F32, kind="ExternalInput").ap()

    def T(n, shp):
        return nc.dram_tensor(n, list(shp), F32, kind="Internal").ap()
    x0T = I("x0T", [1024, S])
    a1w = I("a1_w", [nl, 2, 1024, 1024]); a1vec = I("a1_vec", [nl, 2, 128, 22]); a1lw = I("a1_lw", [nl, 2, 128, 256])
    a1g2 = I("a1_g2", [nl, 2, 128, 256]); a1cst = I("a1_cst", [128, 1280])
    a2wf = I("a2_wf", [nl, 2, 1024, 640]); a2wt = I("a2_wt", [nl, 2, 1024, 140]); a2w1 = I("a2_w1", [nl, 128, 32 * 128])
    a2pe = I("a2_peT", [nl, 128, 32]); a2w2 = I("a2_w2", [nl, 128, 192]); a2btc = I("a2_btc", [2, 128, 4 * 256]); a2bts = I("a2_bts", [2, 128, 4 * TW])
    a2c = {k: I("a2_" + k, shp) for k, shp in (("mkc", [128, 256]), ("mks", [128, TW]), ("mkw", [128, TW]), ("m12", [128, 256]), ("rv", [128, 1]),
                                                 ("c2s", [128, 128]), ("exd", [64, 32 * 128]), ("hsel", [128, 256]))}
    b1wg = I("b1_wg", [nl, 1024, 2048]); b1wur = I("b1_wur", [nl, 512, 1024]); b1wun = I("b1_wun", [nl, 512, 1024]); b1wo = I("b1_wo", [nl, 1024, 1024])
    b1ln = I("b1_ln", [nl, 128, 16])
    b2wr = I("b2_wr", [nl, 1024, 36]); b2br = I("b2_brr", [nl, 1, 36]); b2e1 = I("b2_ew1", [nl, 32, 1024, 512]); b2e3 = I("b2_ew3", [nl, 32, 1024, 512])
    b2e2 = I("b2_ew2", [nl, 32, 512, 1024]); b2ln = I("b2_ln", [nl, 128, 16]); b2selb = I("b2_selb", [32, 32 * 128]); b2g2e = I("b2_g2e", [128, 128])
    outT = nc.dram_tensor("outT", [1024, S], F32, kind="ExternalOutput").ap()
    XT = [T("xt0", [1024, S]), T("xt1", [1024, S])]
    YR = T("yr", [512, S]); YN = T("yn", [512, S]); X1 = T("x1", [1024, S])

    with ExitStack() as st:
        kb = KB(nc, st)
        pn = [0]

        def phase(fn, D, *args):
            with ExitStack() as pst:
                kb.pstack = pst
                kb.prefix = f"p{pn[0]}_"
                pn[0] += 1
                fn(nc, kb, D, *args)
                kb.emit()
            kb.pstack = st
        for l in range(nl):
            xin = x0T if l == 0 else XT[l % 2]
            xout = outT if l == nl - 1 else XT[(l + 1) % 2]
            for hh in range(2):
                phase(a1_body, dict(xT=xin, w=a1w[l, hh], vec=a1vec[l, hh], lw=a1lw[l, hh], g2=a1g2[l, hh], cst=a1cst,
                                    yT=YR[hh * 256:(hh + 1) * 256, :]))
            for g in range(2):
                D = dict(xT=xin, wf=a2wf[l, g], wt=a2wt[l, g], w1=a2w1[l], peT=a2pe[l], w2=a2w2[l], btc=a2btc[g], bts=a2bts[g],
                         yT=YN[g * 256:(g + 1) * 256, :])
                D.update(a2c)
                phase(a2_body, D)
            for hf in range(2):
                ts = slice(hf * NT, (hf + 1) * NT)
                phase(b1_body, dict(xT=xin[:, ts], yrT=YR[:, ts], ynT=YN[:, ts], wg=b1wg[l], wur=b1wur[l], wun=b1wun[l], wo=b1wo[l], ln=b1ln[l],
                                    x1T=X1[:, ts]))
            for hf in range(2):
                ts = slice(hf * NT, (hf + 1) * NT)
                phase(b2_body, dict(x1T=X1[:, ts], wr=b2wr[l], brr=b2br[l], ew1=b2e1[l], ew3=b2e3[l], ew2=b2e2[l], ln=b2ln[l], selb=b2selb,
                                    g2e=b2g2e, x2T=xout[:, ts]))
        print("FUSED instructions:", kb.n_ins, kb.cnt)
    return nc


def fused_inputs(inp, nl=L_):
    c2 = a2_consts(); cb2 = b2_consts()
    a1 = [[a1_inputs(inp, l, 0, hh) for hh in range(2)] for l in range(nl)]
    a2 = [[a2_inputs(inp, l, g, c2) for g in range(2)] for l in range(nl)]
    b1 = [b1_inputs(inp, l) for l in range(nl)]
    b2 = [b2_inputs(inp, l, cb2) for l in range(nl)]
    st = lambda f: np.ascontiguousarray(np.stack(f, axis=0), dtype=np.float32)
    m = {}
    for k, nm in (("w", "a1_w"), ("vec", "a1_vec"), ("lw", "a1_lw"), ("g2", "a1_g2")):
        m[nm] = st([st([a1[l][hh][k] for hh in range(2)]) for l in range(nl)])
    m["a1_cst"] = a1[0][0]["cst"]
    for k, nm in (("wf", "a2_wf"), ("wt", "a2_wt")):
        m[nm] = st([st([a2[l][g][k] for g in range(2)]) for l in range(nl)])
    for k, nm in (("w1", "a2_w1"), ("peT", "a2_peT"), ("w2", "a2_w2")):
        m[nm] = st([a2[l][0][k] for l in range(nl)])
    m["a2_btc"] = st([a2[0][g]["btc"] for g in range(2)]); m["a2_bts"] = st([a2[0][g]["bts"] for g in range(2)])
    for k in ("mkc", "mks", "mkw", "m12", "rv", "c2s", "exd", "hsel"):
        m["a2_" + k] = a2[0][0][k]
    for k, nm in (("wg", "b1_wg"), ("wur", "b1_wur"), ("wun", "b1_wun"), ("wo", "b1_wo"), ("ln", "b1_ln")):
        m[nm] = st([b1[l][k] for l in range(nl)])
    for k, nm in (("wr", "b2_wr"), ("brr", "b2_brr"), ("ln", "b2_ln")):
        m[nm] = st([b2[l][k] for l in range(nl)])
    m["b2_ew1"] = np.ascontiguousarray(inp["exp_w1"][:nl], dtype=np.float32)
    m["b2_ew3"] = np.ascontiguousarray(inp["exp_w3"][:nl], dtype=np.float32)
    m["b2_ew2"] = np.ascontiguousarray(inp["exp_w2"][:nl], dtype=np.float32)
    m["b2_selb"] = cb2["selb"]; m["b2_g2e"] = cb2["g2e"]
    return m

_NC = {}
_CORE_OF = [0, 1, 4, 5]


def kernel(**inputs):
    inp = {k: np.asarray(v) for k, v in inputs.items()}
    if "nc" not in _NC:
        _NC["nc"] = build_fused(L_)
    m = fused_inputs(inp, L_)
    x = inp["x"].astype(np.float32, copy=False)
    B = x.shape[0]
    xT = [np.ascontiguousarray(x[b].T) for b in range(B)]
    zeros = {k: np.zeros_like(v) for k, v in m.items()}
    zeros["x0T"] = np.zeros_like(xT[0])
    maps = [zeros] * 8
    for b in range(B):
        mm_ = dict(m); mm_["x0T"] = xT[b]
        maps[_CORE_OF[b]] = mm_
    res = run_bass_kernel_spmd(_NC["nc"], maps, core_ids=list(range(8)))
    out = np.stack([res.results[_CORE_OF[b]]["outT"].T for b in range(B)], axis=0)
    return np.ascontiguousarray(out, dtype=np.float32)
```

```python
import numpy as np
from contextlib import ExitStack
import concourse.bass as bass
import concourse.mybir as mybir
from concourse.bass_utils import run_bass_kernel_spmd

F32 = mybir.dt.float32
BF16 = mybir.dt.bfloat16
AF = mybir.ActivationFunctionType
ALU = mybir.AluOpType
AX = mybir.AxisListType


class Buf:
    __slots__ = ("name", "w", "r", "excl")

    def __init__(self, name="", excl=False):
        self.name = name
        self.excl = excl
        self.w = None
        self.r = {}


class KB:
    def __init__(self, nc, stack):
        self.nc = nc
        self.stack = stack
        self.pstack = stack
        self.prefix = ""
        self.names = ["pe", "act", "dve", "pool", "sp"]
        self.prog = {e: [] for e in self.names}
        self.sem = {e: stack.enter_context(nc.semaphore("s_" + e)) for e in self.names}
        self.cnt = {e: 0 for e in self.names}
        self.seen = {e: {} for e in self.names}
        self.dsem = {}
        self.n_ins = 0
        self.nw = {e: 0 for e in self.names}

    def sb(self, name, shape, dt=F32):
        return self.pstack.enter_context(self.nc.sbuf_tensor(self.prefix + "sb_" + name, list(shape), dt))

    def ps(self, name, shape, dt=F32):
        return self.pstack.enter_context(self.nc.psum_tensor(self.prefix + "ps_" + name, list(shape), dt))

    def _semh(self, key):
        if key in self.sem:
            return self.sem[key]
        return self.dsem[key][0]

    def _waits(self, eng, reads, writes):
        need = {}

        def add(d):
            if d is None:
                return
            k, v = d
            if need.get(k, 0) < v:
                need[k] = v
        for b in reads:
            add(b.w)
        for b in writes:
            add(b.w)
            for k, v in b.r.items():
                add((k, v))
        out = []
        seen = self.seen[eng]
        for k, v in need.items():
            if k == "pe" and eng == "pe":
                continue
            if seen.get(k, 0) >= v:
                continue
            seen[k] = v
            out.append((self._semh(k), v))
        return out

    def _mark(self, tok, reads, writes):
        for b in writes:
            b.w = tok
            b.r = {}
        k, v = tok
        for b in reads:
            if b.r.get(k, 0) < v:
                b.r[k] = v

    def op(self, eng, fn, reads=(), writes=()):
        ex = [b for b in reads if b.excl]
        if ex:
            writes = list(writes) + ex
        waits = self._waits(eng, reads, writes)
        self.nw[eng] += len(waits)
        self.cnt[eng] += 1
        tok = (eng, self.cnt[eng])
        sem = self.sem[eng]

        def run(e, waits=waits, fn=fn, sem=sem):
            for s, v in waits:
                e.wait_ge(s, v)
            fn(e).then_inc(sem, 1)
        self.prog[eng].append(run)
        self._mark(tok, reads, writes)
        self.n_ins += 1

    def dma(self, q, key, fn, reads=(), writes=(), n=1):
        key = "d_" + key
        if key not in self.dsem:
            self.dsem[key] = [self.stack.enter_context(self.nc.semaphore(key)), 0]
        waits = self._waits(q, reads, writes)
        self.dsem[key][1] += 16 * n
        tok = (key, self.dsem[key][1])
        sem = self.dsem[key][0]

        def run(e, waits=waits, fn=fn, sem=sem):
            for s, v in waits:
                e.wait_ge(s, v)
            fn(e, sem)
        self.prog[q].append(run)
        self._mark(tok, reads, writes)
        self.n_ins += n

    def finish(self, bufs):
        waits = self._waits("sp", bufs, bufs)

        def run(e, waits=waits):
            for s, v in waits:
                e.wait_ge(s, v)
        self.prog["sp"].append(run)

    def emit(self):
        nc = self.nc
        prog = self.prog
        self.prog = {e: [] for e in self.names}
        with nc.Block() as block:
            @block.sync
            def _(e):
                for f in prog["sp"]:
                    f(e)

            @block.tensor
            def _(e):
                for f in prog["pe"]:
                    f(e)

            @block.scalar
            def _(e):
                for f in prog["act"]:
                    f(e)

            @block.vector
            def _(e):
                for f in prog["dve"]:
                    f(e)

            @block.gpsimd
            def _(e):
                for f in prog["pool"]:
                    f(e)


S = 4096
NS = 512
NSEG = S // NS
CH = 128
NCH = NS // CH
GN_EPS = 64e-5


def a1_body(nc, kb, D, nseg=NSEG, debug=False):
    Em_ = globals().get('Em')
    if Em_ is None:
        from a2 import Em as Em_
    xT_d, w_d, vec_d, lw_d, g2_d, cst_d, yT_d = (D[k] for k in ("xT", "w", "vec", "lw", "g2", "cst", "yT"))
    xT_v = xT_d.rearrange("(kc p) t -> p kc t", p=128)
    w_v = w_d.rearrange("(kc p) n -> p kc n", p=128)
    if True:
        sb, ps = kb.sb, kb.ps
        NXS = 3
        xs = [sb(f"xs{i}", [128, 8, NS], BF16) for i in range(NXS)]; XS = [Buf() for _ in range(NXS)]
        wsb = sb("wsb", [128, 8, 1024], BF16); WSB = Buf()
        vec = sb("vec", [128, 22]); VEC = Buf()
        vx = sb("vx", [128, 8]); VX = Buf()
        lw = sb("lw", [128, 256]); LW = Buf()
        g2 = sb("g2", [128, 256]); G2 = Buf()
        cst = sb("cst", [128, 1280]); CST = Buf()
        MASK1 = cst[:, 0:512]; MASK4 = cst[:, 512:1024]; MSL = cst[:, 1024:1152]; BONES = cst[:, 1152:1280]
        ident = sb("ident", [128, 128]); IDENT = Buf()
        car = sb("car", [128, 8]); CAR = Buf()
        zr = [sb(f"zr{i}", [128, NS + 1]) for i in range(2)]; ZR = [Buf() for _ in range(2)]
        dtmp = sb("dtmp", [128, NS]); DTMP = Buf()
        zs = [sb(f"zs{j}", [128, NS]) for j in range(8)]; ZS = [Buf() for _ in range(8)]
        tw = sb("tw", [128, NS]); TW = Buf()
        sg = sb("sg", [128, NS]); SG = Buf()
        tnames = ["nld", "cw", "ew", "ewi", "ewx", "aa", "kkn", "sq", "t1", "k2", "bh", "kh", "e1"]
        T = {n: sb("t_" + n, [128, NS]) for n in tnames}; TB = {n: Buf() for n in tnames}
        ar = [sb(f"ar{h}", [128, NCH, 2 * CH]) for h in range(2)]; AR = [Buf() for _ in range(2)]
        bt = [sb(f"bt{h}", [128, NS]) for h in range(2)]; BT = [Buf() for _ in range(2)]
        kt = [sb(f"kt{h}", [128, NS]) for h in range(2)]; KT = [Buf() for _ in range(2)]
        gg = [sb(f"gg{h}", [128, NS]) for h in range(2)]; GG = [Buf() for _ in range(2)]
        bon = [sb(f"bon{h}", [128, NS]) for h in range(2)]; BON = [Buf() for _ in range(2)]
        yf = [sb(f"yf{h}", [128, NS]) for h in range(2)]; YF = [Buf() for _ in range(2)]
        wc = [sb(f"wc{h}", [128, NCH]) for h in range(2)]; WC = [Buf() for _ in range(2)]
        bhT = sb("bhT", [128, NCH, 256]); BHT = [Buf() for _ in range(NCH)]
        khT = sb("khT", [128, NCH, 256]); KHT = [Buf() for _ in range(NCH)]
        vT = sb("vT", [128, NCH, 256]); VT = [Buf() for _ in range(NCH)]
        mabk = [sb(f"mabk{h}", [128, 512]) for h in range(4)]; MABK = [Buf() for _ in range(4)]
        nm = [[sb(f"nm{h}_{i}", [128, 256], BF16) for i in range(2)] for h in range(4)]; NM = [[Buf(), Buf()] for _ in range(4)]
        qq = [[sb(f"qq{h}_{i}", [128, 128], BF16) for i in range(2)] for h in range(4)]; QQ = [[Buf(), Buf()] for _ in range(4)]
        xsb = [sb(f"xsb{h}", [128, 64], BF16) for h in range(4)]; XSB = [Buf() for _ in range(4)]
        usb = [sb(f"usb{h}", [128, 64]) for h in range(4)]; USB = [Buf() for _ in range(4)]
        stt = [[sb(f"st{hp}_{i}", [128, 64]) for i in range(2)] for hp in range(2)]
        STT = [[[Buf(), Buf()] for _ in range(2)] for hp in range(2)]
        ytok = sb("ytok", [128, 256]); YTOK = Buf()
        ysq = sb("ysq", [128, 256]); YSQ = Buf()
        yn = sb("yn", [128, 256]); YN = Buf()
        sts = sb("sts", [128, 32]); STS = Buf()
        osb = [sb(f"osb{h}", [128, NS]) for h in range(2)]; OSB = [Buf() for _ in range(2)]
        NPA = 4
        pa = [ps(f"pa{i}", [128, 512]) for i in range(NPA)]; PA = [Buf(excl=True) for _ in range(NPA)]
        pq = [ps(f"pq{i}", [128, 512]) for i in range(4)]; PQB = [Buf(excl=True) for _ in range(4)]

        def ld(q, key, out, in_, B):
            kb.dma(q, key, lambda e, s: e.dma_start(out=out, in_=in_).then_inc(s, 16), writes=[B])
        ld("sp", "vec", vec[:], vec_d[:, :], VEC)
        ld("sp", "lw", lw[:], lw_d[:, :], LW)
        ld("sp", "g2", g2[:], g2_d[:, :], G2)
        ld("sp", "cst", cst[:], cst_d[:, :], CST)
        for kc in range(0, 8, 4):
            kb.dma("pool", "wsb", lambda e, s, kc=kc: e.dma_start(out=wsb[:, kc:kc + 4, :], in_=w_v[:, kc:kc + 4, :]).then_inc(s, 16), writes=[WSB])
        kb.op("pool", lambda e: e.memset(ident[:], 1.0), writes=[IDENT])
        kb.op("pool", lambda e: e.affine_select(out=ident[:], in_=ident[:], pattern=[[-1, 128]], compare_op=ALU.is_equal,
                                                fill=0.0, base=0, channel_multiplier=1), reads=[IDENT], writes=[IDENT])
        kb.op("dve", lambda e: e.memset(car[:], 0.0), writes=[CAR])
        kb.op("dve", lambda e: e.tensor_scalar(vx[:, 0:2], vec[:, 8:10], -1.0, None, ALU.mult), reads=[VEC], writes=[VX])
        kb.op("dve", lambda e: e.tensor_scalar(vx[:, 2:4], vec[:, 14:16], -1.0, 1.0, ALU.mult, ALU.add), reads=[VEC, VX], writes=[VX])
        for hp in range(2):
            for i in range(2):
                kb.op("dve", lambda e, hp=hp, i=i: e.memset(stt[hp][i][:], 0.0), writes=STT[hp][i])

        def load_x(sgi):
            sl = sgi % NXS
            kb.dma("pool", f"xs{sl}", lambda e, s, sl=sl, sgi=sgi: e.dma_start(
                out=xs[sl][:, :, :], in_=xT_v[:, :, sgi * NS:(sgi + 1) * NS]).then_inc(s, 16), writes=[XS[sl]])

        load_x(0)
        pai = [0]

        def next_pa():
            i = pai[0] % NPA
            pai[0] += 1
            return pa[i], PA[i]

        def mm512(lhsT_fn, rhs_fn, nk, reads, M=128):
            p, P = next_pa()
            for k in range(nk):
                a_, b_ = lhsT_fn(k), rhs_fn(k)
                kb.op("pe", lambda e, k=k, p=p, a_=a_, b_=b_: e.matmul(p[0:M, :], a_, b_, start=(k == 0), stop=(k == nk - 1)),
                      reads=reads, writes=[P])
            return p, P

        ping = [0, 0]
        dbg_n = [0]

        def dbg(name, ap, B):
            if not debug:
                return
            shp = list(ap.shape)
            d = nc.dram_tensor("dbg_" + name, shp, F32, kind="ExternalOutput").ap()
            dbg_n[0] += 1
            cntv = dbg_n[0] * 16

            def f(e, s, d=d, ap=ap, cntv=cntv):
                e.dma_start(out=d, in_=ap).then_inc(s, 16)
                e.wait_ge(s, cntv)
            kb.dma("sp", "dbg", f, reads=[B])
        for sgi in range(nseg):
            sl = sgi % NXS
            if sgi + 1 < nseg:
                load_x(sgi + 1)
            for j in range(8):
                p, P = mm512(lambda k, j=j: wsb[:, k, j * 128:(j + 1) * 128], lambda k, sl=sl: xs[sl][:, k, :], 8, [WSB, XS[sl]])
                z, Z = zr[j % 2], ZR[j % 2]
                kb.op("pool", lambda e, z=z, j=j: e.tensor_copy(z[:, 0:1], car[:, j:j + 1]), reads=[CAR], writes=[Z])
                kb.op("act", lambda e, z=z, p=p: e.activation(out=z[:, 1:NS + 1], in_=p[:, :], func=AF.Copy), reads=[P], writes=[Z])
                kb.op("pool", lambda e, z=z, j=j: e.tensor_copy(car[:, j:j + 1], z[:, NS:NS + 1]), reads=[Z], writes=[CAR])
                kb.op("dve", lambda e, z=z: e.tensor_tensor(dtmp[:], z[:, 0:NS], z[:, 1:NS + 1], ALU.subtract), reads=[Z], writes=[DTMP])
                kb.op("dve", lambda e, z=z, j=j: e.scalar_tensor_tensor(zs[j][:], dtmp[:], vec[:, j:j + 1], z[:, 1:NS + 1], ALU.mult, ALU.add),
                      reads=[DTMP, Z, VEC], writes=[ZS[j]])
            L1, L2 = zs[6], zs[7]
            for j in range(8):
                dbg(f"zs{j}", zs[j][:], ZS[j])
            kb.op("act", lambda e: e.activation(out=tw[0:64, :], in_=L1[0:64, :], func=AF.Tanh), reads=[ZS[6]], writes=[TW])
            kb.op("act", lambda e: e.activation(out=sg[:], in_=L2[:], func=AF.Sigmoid), reads=[ZS[7]], writes=[SG])
            for hp in range(2):
                Rz, Kz, Vz = zs[0 + hp], zs[2 + hp], zs[4 + hp]
                RZ, KZ, VZ = ZS[0 + hp], ZS[2 + hp], ZS[4 + hp]
                cs = slice(hp * 128, (hp + 1) * 128)
                p, P = mm512(lambda k: lw[64:128, cs], lambda k: L1[64:128, :], 1, [LW, ZS[6]])
                kb.op("act", lambda e, p=p, hp=hp: e.activation(out=T["aa"][:], in_=p[:, :], func=AF.Sigmoid, bias=vec[:, 10 + hp:11 + hp]),
                      reads=[P, VEC], writes=[TB["aa"]])
                p, P = mm512(lambda k: g2[:, cs], lambda k: sg[:], 1, [G2, SG])
                kb.op("act", lambda e, p=p, hp=hp: e.activation(out=gg[hp][:], in_=p[:, :], func=AF.Copy), reads=[P], writes=[GG[hp]])
                p, P = mm512(lambda k: lw[0:64, cs], lambda k: tw[0:64, :], 1, [LW, TW])
                kb.op("act", lambda e, p=p, hp=hp: e.activation(out=T["e1"][:], in_=p[:, :], func=AF.Exp, bias=vx[:, hp:hp + 1], scale=-1.0),
                      reads=[P, VX], writes=[TB["e1"]])
                kb.op("act", lambda e: e.activation(out=T["e1"][:], in_=T["e1"][:], func=AF.Ln, bias=1.0), reads=[TB["e1"]], writes=[TB["e1"]])
                kb.op("act", lambda e: e.activation(out=T["nld"][:], in_=T["e1"][:], func=AF.Exp, bias=-0.5, scale=-1.0),
                      reads=[TB["e1"]], writes=[TB["nld"]])
                kb.op("dve", lambda e: e.tensor_tensor_scan(T["cw"][:], MASK1, T["nld"][:], 0.0, ALU.mult, ALU.add),
                      reads=[CST, TB["nld"]], writes=[TB["cw"]])
                kb.op("act", lambda e: e.activation(out=T["ew"][:], in_=T["cw"][:], func=AF.Exp, scale=-1.0), reads=[TB["cw"]], writes=[TB["ew"]])
                kb.op("act", lambda e: e.activation(out=T["ewi"][:], in_=T["cw"][:], func=AF.Exp), reads=[TB["cw"]], writes=[TB["ewi"]])
                kb.op("pool", lambda e: e.tensor_tensor(T["ewx"][:], T["cw"][:], T["nld"][:], ALU.subtract), reads=[TB["cw"], TB["nld"]], writes=[TB["ewx"]])
                kb.op("act", lambda e: e.activation(out=T["ewx"][:], in_=T["ewx"][:], func=AF.Exp, scale=-1.0), reads=[TB["ewx"]], writes=[TB["ewx"]])
                kb.op("pool", lambda e, hp=hp: e.tensor_copy(wc[hp][:], T["ew"][:].rearrange("p (c t) -> p c t", t=CH)[:, :, CH - 1]),
                      reads=[TB["ew"]], writes=[WC[hp]])
                kb.op("dve", lambda e, hp=hp, Kz=Kz: e.tensor_scalar(T["kkn"][:], Kz[:], vec[:, 12 + hp:13 + hp], None, ALU.mult),
                      reads=[KZ, VEC], writes=[TB["kkn"]])
                kb.op("pool", lambda e: e.tensor_tensor(T["sq"][:], T["kkn"][:], T["kkn"][:], ALU.mult), reads=[TB["kkn"]], writes=[TB["sq"]])
                p, P = mm512(lambda k: BONES, lambda k: T["sq"][:], 1, [CST, TB["sq"]])
                kb.op("act", lambda e, p=p: e.activation(out=T["sq"][:], in_=p[:, :], func=AF.Sqrt), reads=[P], writes=[TB["sq"]])
                kb.op("dve", lambda e: e.tensor_scalar(T["sq"][:], T["sq"][:], 1e-12, None, ALU.max), reads=[TB["sq"]], writes=[TB["sq"]])
                kb.op("dve", lambda e: e.reciprocal(T["sq"][:], T["sq"][:]), reads=[TB["sq"]], writes=[TB["sq"]])
                kb.op("dve", lambda e: e.tensor_tensor(T["kkn"][:], T["kkn"][:], T["sq"][:], ALU.mult), reads=[TB["kkn"], TB["sq"]], writes=[TB["kkn"]])
                kb.op("pool", lambda e, hp=hp: e.tensor_scalar(T["t1"][:], T["aa"][:], vec[:, 14 + hp:15 + hp], vx[:, 2 + hp:3 + hp], ALU.mult, ALU.add),
                      reads=[TB["aa"], VEC, VX], writes=[TB["t1"]])
                kb.op("pool", lambda e, Kz=Kz: e.tensor_tensor(T["k2"][:], Kz[:], T["t1"][:], ALU.mult), reads=[KZ, TB["t1"]], writes=[TB["k2"]])
                arv = ar[hp]
                kb.op("dve", lambda e, arv=arv: e.scalar_tensor_tensor(arv[:, :, 0:CH], T["kkn"][:].rearrange("p (c t) -> p c t", t=CH), -1.0,
                                                                      T["ewx"][:].rearrange("p (c t) -> p c t", t=CH), ALU.mult, ALU.mult),
                      reads=[TB["kkn"], TB["ewx"]], writes=[AR[hp]])
                kb.op("pool", lambda e, arv=arv, Rz=Rz: e.tensor_tensor(arv[:, :, CH:2 * CH], Rz[:].rearrange("p (c t) -> p c t", t=CH),
                                                                       T["ew"][:].rearrange("p (c t) -> p c t", t=CH), ALU.mult),
                      reads=[RZ, TB["ew"]], writes=[AR[hp]])
                kb.op("dve", lambda e: e.tensor_tensor(T["t1"][:], T["kkn"][:], T["aa"][:], ALU.mult), reads=[TB["kkn"], TB["aa"]], writes=[TB["t1"]])
                kb.op("dve", lambda e, hp=hp: e.tensor_tensor(bt[hp][:], T["t1"][:], T["ewi"][:], ALU.mult), reads=[TB["t1"], TB["ewi"]], writes=[BT[hp]])
                kb.op("pool", lambda e, hp=hp: e.tensor_tensor(kt[hp][:], T["k2"][:], T["ewi"][:], ALU.mult), reads=[TB["k2"], TB["ewi"]], writes=[KT[hp]])
                wcb = wc[hp][:].unsqueeze(2).to_broadcast([128, NCH, CH])
                kb.op("dve", lambda e, hp=hp, wcb=wcb: e.tensor_tensor(T["bh"][:].rearrange("p (c t) -> p c t", t=CH),
                                                                      bt[hp][:].rearrange("p (c t) -> p c t", t=CH), wcb, ALU.mult),
                      reads=[BT[hp], WC[hp]], writes=[TB["bh"]])
                kb.op("pool", lambda e, hp=hp, wcb=wcb: e.tensor_tensor(T["kh"][:].rearrange("p (c t) -> p c t", t=CH),
                                                                       kt[hp][:].rearrange("p (c t) -> p c t", t=CH), wcb, ALU.mult),
                      reads=[KT[hp], WC[hp]], writes=[TB["kh"]])
                kb.op("dve", lambda e, hp=hp, Rz=Rz: e.scalar_tensor_tensor(T["t1"][:], Rz[:], vec[:, 16 + hp:17 + hp], T["k2"][:], ALU.mult, ALU.mult),
                      reads=[RZ, VEC, TB["k2"], TB["t1"]], writes=[TB["t1"]])
                p, P = mm512(lambda k: BONES, lambda k: T["t1"][:], 1, [CST, TB["t1"]])
                kb.op("dve", lambda e, p=p, hp=hp, Vz=Vz: e.tensor_tensor(bon[hp][:], p[:, :], Vz[:], ALU.mult), reads=[P, VZ], writes=[BON[hp]])
                for n_ in ("nld", "cw", "ew", "ewi", "ewx", "aa", "kkn", "k2", "bh", "kh"):
                    dbg(f"{n_}{hp}", T[n_][:], TB[n_])
                dbg(f"bt{hp}", bt[hp][:], BT[hp]); dbg(f"kt{hp}", kt[hp][:], KT[hp]); dbg(f"ar{hp}", ar[hp][:].rearrange("p c t -> p (c t)"), AR[hp])
                dbg(f"gg{hp}", gg[hp][:], GG[hp]); dbg(f"bon{hp}", bon[hp][:], BON[hp]); dbg(f"wc{hp}", wc[hp][:], WC[hp])
                for c in range(NCH):
                    for (src, SRC, dst, DST) in ((T["bh"], TB["bh"], bhT, BHT), (T["kh"], TB["kh"], khT, KHT), (Vz, VZ, vT, VT)):
                        pbank, PT = next_pa()
                        pt = pbank[:, 0:128]
                        kb.op("pe", lambda e, pt=pt, src=src, c=c: e.transpose(pt, src[:, c * CH:(c + 1) * CH], ident[:]),
                              reads=[SRC, IDENT], writes=[PT])
                        kb.op("act", lambda e, pt=pt, dst=dst, c=c, cs=cs: e.activation(out=dst[:, c, cs], in_=pt, func=AF.Copy),
                              reads=[PT], writes=[DST[c]])
            em = Em_(kb)
            for c in range(NCH):
                csl = slice(c * CH, (c + 1) * CH)
                HD = []
                for h in range(4):
                    hp, hh = h // 2, h % 2
                    HD.append(dict(h=h, hp=hp, hh=hh, rows=slice(64 * hh, 64 * hh + 64), hc=slice(h * 64, (h + 1) * 64), q=pq[h], Q=PQB[h]))
                for d in HD:
                    em.mm(d["q"][:, 0:256], bt[d["hp"]][d["rows"], csl], ar[d["hp"]][d["rows"], c, :], True, True, [BT[d["hp"]], AR[d["hp"]]], [d["Q"]])
                    em.mm(d["q"][:, 256:512], kt[d["hp"]][d["rows"], csl], ar[d["hp"]][d["rows"], c, :], True, True, [KT[d["hp"]], AR[d["hp"]]], [d["Q"]])
                for d in HD:
                    h = d["h"]
                    em.tt("dve", mabk[h][:], d["q"][:, :], MASK4, ALU.mult, [d["Q"], CST], [MABK[h]])
                for d in HD:
                    em.mm(d["q"][:, 0:128], ar[d["hp"]][d["rows"], c, 0:CH], bt[d["hp"]][d["rows"], csl], True, True, [BT[d["hp"]], AR[d["hp"]]], [d["Q"]])
                for d in HD:
                    h = d["h"]
                    em.tt("dve", nm[h][0][:, 0:128], d["q"][:, 0:128], MSL, ALU.mult, [d["Q"], CST], [NM[h][0]])
                    em.cp("pool", nm[h][0][:, 128:256], mabk[h][:, 0:128], [MABK[h]], [NM[h][0]])
                    em.tt("pool", qq[h][0][:], mabk[h][:, 0:128], ident[:], ALU.add, [MABK[h], IDENT], [QQ[h][0]])
                for k in range(6):
                    a_, b_ = k % 2, (k + 1) % 2
                    wdt = 256 if k < 5 else 128
                    for d in HD:
                        h = d["h"]
                        em.mm(d["q"][:, 0:128], nm[h][a_][:, 128:256], nm[h][a_][:, 0:128], True, True, [NM[h][a_]], [d["Q"]])
                        if k < 5:
                            em.mm(d["q"][:, 128:256], nm[h][a_][:, 0:128], nm[h][a_][:, 128:256], True, True, [NM[h][a_]], [d["Q"]])
                    for d in HD:
                        h = d["h"]
                        if h % 2 == 0:
                            em.cp("dve", nm[h][b_][:, 0:wdt], d["q"][:, 0:wdt], [d["Q"]], [NM[h][b_]])
                        else:
                            em.act(nm[h][b_][:, 0:wdt], d["q"][:, 0:wdt], AF.Copy, [d["Q"]], [NM[h][b_]])
                    for d in HD:
                        h = d["h"]
                        em.mm(d["q"][:, 256:384], nm[h][b_][:, 0:128], qq[h][a_][:], True, True, [NM[h][b_], QQ[h][a_]], [d["Q"]])
                    for d in HD:
                        h = d["h"]
                        em.tt("dve", qq[h][b_][:], d["q"][:, 256:384], qq[h][a_][:], ALU.add, [d["Q"], QQ[h][a_]], [QQ[h][b_]])
                qf = 0
                so = [ping[0], ping[1]]
                for d in HD:
                    h, hp, rows, hc = d["h"], d["hp"], d["rows"], d["hc"]
                    So = STT[hp][so[hp]][d["hh"]]
                    em.mm(d["q"][:, 0:64], mabk[h][:, 256:384], vT[:, c, hc], True, False, [MABK[h], VT[c]], [d["Q"]])
                    em.mm(d["q"][:, 0:64], ar[hp][rows, c, 0:CH], stt[hp][so[hp]][rows, :], False, True, [AR[hp], So], [d["Q"]])
                for d in HD:
                    h = d["h"]
                    if h % 2 == 0:
                        em.act(xsb[h][:], d["q"][:, 0:64], AF.Copy, [d["Q"]], [XSB[h]])
                    else:
                        em.cp("dve", xsb[h][:], d["q"][:, 0:64], [d["Q"]], [XSB[h]])
                for d in HD:
                    h = d["h"]
                    em.mm(d["q"][:, 64:128], qq[h][qf][:], xsb[h][:], True, True, [QQ[h][qf], XSB[h]], [d["Q"]])
                for d in HD:
                    h = d["h"]
                    if h % 2 == 0:
                        em.act(usb[h][:], d["q"][:, 64:128], AF.Copy, [d["Q"]], [USB[h]])
                    else:
                        em.cp("dve", usb[h][:], d["q"][:, 64:128], [d["Q"]], [USB[h]])
                for d in HD:
                    h, hp, rows, hc = d["h"], d["hp"], d["rows"], d["hc"]
                    So = STT[hp][so[hp]][d["hh"]]
                    em.mm(d["q"][:, 256:320], ar[hp][rows, c, CH:2 * CH], stt[hp][so[hp]][rows, :], True, False, [AR[hp], So], [d["Q"]])
                    em.mm(d["q"][:, 256:320], mabk[h][:, 128:256], usb[h][:], False, False, [MABK[h], USB[h]], [d["Q"]])
                    em.mm(d["q"][:, 256:320], mabk[h][:, 384:512], vT[:, c, hc], False, True, [MABK[h], VT[c]], [d["Q"]])
                    em.mm(d["q"][rows, 128:192], bhT[:, c, hc], usb[h][:], True, False, [BHT[c], USB[h]], [d["Q"]])
                    em.mm(d["q"][rows, 128:192], khT[:, c, hc], vT[:, c, hc], False, True, [KHT[c], VT[c]], [d["Q"]])
                for d in HD:
                    h, hp, rows, hc = d["h"], d["hp"], d["rows"], d["hc"]
                    sn = 1 - so[hp]
                    em.stt("dve", stt[hp][sn][rows, :], stt[hp][so[hp]][rows, :], wc[hp][rows, c:c + 1], d["q"][rows, 128:192], ALU.mult, ALU.add,
                           [STT[hp][so[hp]][d["hh"]], WC[hp], d["Q"]], [STT[hp][sn][d["hh"]]])
                    em.act(ytok[:, hc], d["q"][:, 256:320], AF.Copy, [d["Q"]], [YTOK])
                    em.act(ysq[:, hc], d["q"][:, 256:320], AF.Square, [d["Q"]], [YSQ])
                ping[0], ping[1] = 1 - so[0], 1 - so[1]
                kb.op("dve", lambda e: e.tensor_reduce(sts[:, 0:4], ytok[:].rearrange("p (h v) -> p h v", v=64), AX.X, ALU.add), reads=[YTOK], writes=[STS])
                kb.op("dve", lambda e: e.tensor_reduce(sts[:, 4:8], ysq[:].rearrange("p (h v) -> p h v", v=64), AX.X, ALU.add), reads=[YSQ, STS], writes=[STS])
                kb.op("dve", lambda e: e.tensor_scalar(sts[:, 8:12], sts[:, 0:4], 1.0 / 64, None, ALU.mult), reads=[STS], writes=[STS])
                kb.op("dve", lambda e: e.tensor_tensor(sts[:, 12:16], sts[:, 8:12], sts[:, 8:12], ALU.mult), reads=[STS], writes=[STS])
                kb.op("dve", lambda e: e.scalar_tensor_tensor(sts[:, 16:20], sts[:, 4:8], 1.0 / 64, sts[:, 12:16], ALU.mult, ALU.subtract),
                      reads=[STS], writes=[STS])
                kb.op("dve", lambda e: e.tensor_scalar(sts[:, 16:20], sts[:, 16:20], GN_EPS, None, ALU.add), reads=[STS], writes=[STS])
                kb.op("act", lambda e: e.activation(out=sts[:, 20:24], in_=sts[:, 16:20], func=AF.Sqrt), reads=[STS], writes=[STS])
                kb.op("dve", lambda e: e.reciprocal(sts[:, 24:28], sts[:, 20:24]), reads=[STS], writes=[STS])
                kb.op("dve", lambda e: e.tensor_tensor(yn[:].rearrange("p (h v) -> p h v", v=64), ytok[:].rearrange("p (h v) -> p h v", v=64),
                                                       sts[:, 8:12].unsqueeze(2).to_broadcast([128, 4, 64]), ALU.subtract), reads=[YTOK, STS], writes=[YN])
                kb.op("dve", lambda e: e.tensor_tensor(yn[:].rearrange("p (h v) -> p h v", v=64), yn[:].rearrange("p (h v) -> p h v", v=64),
                                                       sts[:, 24:28].unsqueeze(2).to_broadcast([128, 4, 64]), ALU.mult), reads=[YN, STS], writes=[YN])
                for hp in range(2):
                    pbank, PT = next_pa()
                    pt = pbank[:, 0:128]
                    em.tr(pt, yn[:, hp * 128:(hp + 1) * 128], ident[:], [YN, IDENT], [PT])
                    em.ts("dve", yf[hp][:, csl], pt, vec[:, 18 + hp:19 + hp], vec[:, 20 + hp:21 + hp], ALU.mult, ALU.add, [PT, VEC], [YF[hp]])
            for hp in range(2):
                kb.op("pool", lambda e, hp=hp: e.tensor_tensor(osb[hp][:], yf[hp][:], bon[hp][:], ALU.add), reads=[YF[hp], BON[hp]], writes=[OSB[hp]])
                kb.op("pool", lambda e, hp=hp: e.tensor_tensor(osb[hp][:], osb[hp][:], gg[hp][:], ALU.mult), reads=[OSB[hp], GG[hp]], writes=[OSB[hp]])
                kb.dma("sp", f"out{hp}", lambda e, s, hp=hp, sgi=sgi: e.dma_start(out=yT_d[hp * 128:(hp + 1) * 128, sgi * NS:(sgi + 1) * NS], in_=osb[hp][:]).then_inc(s, 16),
                       reads=[OSB[hp]])
        kb.finish(OSB)


def build_a1(nseg=NSEG, debug=False):
    nc = bass.Bass("TRN2", target_bir_lowering=False)
    I = lambda n, shp: nc.dram_tensor(n, shp, F32, kind="ExternalInput").ap()
    D = dict(xT=I("xT", [1024, S]), w=I("w", [1024, 1024]), vec=I("vec", [128, 22]), lw=I("lw", [128, 256]), g2=I("g2", [128, 256]),
             cst=I("cst", [128, 1280]), yT=nc.dram_tensor("yT", [256, S], F32, kind="ExternalOutput").ap())
    with ExitStack() as st:
        kb = KB(nc, st)
        a1_body(nc, kb, D, nseg, debug)
        kb.emit()
    return nc


def a1_consts():
    m1 = np.ones((128, 512), np.float32); m1[:, ::CH] = 0.0
    s = np.arange(128)[:, None]; t = np.arange(128)[None, :]
    msu = (t > s).astype(np.float32); miu = (t >= s).astype(np.float32)
    msl = (t < s).astype(np.float32)
    bones = (s // 64 == t // 64).astype(np.float32)
    return np.concatenate([m1, msu, miu, msu, miu, msl, bones], axis=1)


def a1_inputs(inp, l, b, hh):
    ch = slice(256 * hh, 256 * hh + 256)
    w_in = inp["w_in"][l]
    w = np.concatenate([w_in[:, 0:512][:, ch], w_in[:, 512:1024][:, ch], w_in[:, 1024:1536][:, ch], w_in[:, 1536:1792]], axis=1)
    mu = inp["shift_mu"][l]
    mu_cols = np.concatenate([mu[0:512][ch], mu[512:1024][ch], mu[1024:1536][ch], mu[1536:1792]])
    vec = np.zeros((128, 22), np.float32)
    vec[:, 0:8] = mu_cols.reshape(8, 128).T
    def two(v):
        return v[ch].reshape(2, 128).T
    vec[:, 8:10] = two(inp["rw_w0"][l]); vec[:, 10:12] = two(inp["rw_a0"][l]); vec[:, 12:14] = two(inp["rw_kk"][l])
    vec[:, 14:16] = two(inp["rw_ka"][l]); vec[:, 16:18] = two(inp["rw_rk"][l].reshape(512))
    vec[:, 18:20] = two(inp["rw_ln_g"][l]); vec[:, 20:22] = two(inp["rw_ln_b"][l])
    lw = np.concatenate([inp["rw_w2"][l][:, ch], inp["rw_a2"][l][:, ch]], axis=0)
    g2 = inp["rw_g2"][l][:, ch]
    return dict(w=np.ascontiguousarray(w), vec=vec, lw=np.ascontiguousarray(lw), g2=np.ascontiguousarray(g2), cst=a1_consts())


import math

S = 4096
NEG = -30000.0
TW = 2304
DCL = 1280


def t5_bucket(n):
    n = np.maximum(n, 0); me = 16
    nf = np.maximum(n, 1).astype(np.float32)
    large = me + (np.log(nf / np.float32(me)) / np.float32(math.log(1024 / me)) * np.float32(32 - me)).astype(np.int32)
    large = np.minimum(large, 31)
    return np.where(n < me, n, large)


class Em:
    def __init__(self, kb):
        self.kb = kb

    def mm(self, out, lhsT, rhs, start, stop, reads, writes):
        self.kb.op("pe", lambda e, o=out, a=lhsT, b=rhs, s=start, t=stop: e.matmul(o, a, b, start=s, stop=t), reads, writes)

    def tr(self, out, in_, ident, reads, writes):
        self.kb.op("pe", lambda e, o=out, a=in_, b=ident: e.transpose(o, a, b), reads, writes)

    def act(self, out, in_, func, reads, writes, **kw):
        self.kb.op("act", lambda e, o=out, i=in_, f=func, kw=kw: e.activation(out=o, in_=i, func=f, **kw), reads, writes)

    def tt(self, eng, out, a, b, op, reads, writes):
        self.kb.op(eng, lambda e, o=out, a=a, b=b, op=op: e.tensor_tensor(o, a, b, op), reads, writes)

    def ts(self, eng, out, a, s1, s2, op0, op1, reads, writes):
        if op1 is None:
            self.kb.op(eng, lambda e, o=out, a=a, s1=s1, op0=op0: e.tensor_scalar(o, a, s1, None, op0), reads, writes)
        else:
            self.kb.op(eng, lambda e, o=out, a=a, s1=s1, s2=s2, op0=op0, op1=op1: e.tensor_scalar(o, a, s1, s2, op0, op1), reads, writes)

    def stt(self, eng, out, a, s, b, op0, op1, reads, writes):
        self.kb.op(eng, lambda e, o=out, a=a, s=s, b=b, op0=op0, op1=op1: e.scalar_tensor_tensor(o, a, s, b, op0, op1), reads, writes)

    def cp(self, eng, out, in_, reads, writes):
        self.kb.op(eng, lambda e, o=out, i=in_: e.tensor_copy(o, i), reads, writes)

    def ms(self, eng, out, val, writes):
        self.kb.op(eng, lambda e, o=out, v=val: e.memset(o, v), (), writes)

    def red(self, out, in_, op, reads, writes):
        self.kb.op("dve", lambda e, o=out, i=in_, op=op: e.tensor_reduce(o, i, AX.X, op), reads, writes)

    def rcp(self, out, in_, reads, writes):
        self.kb.op("dve", lambda e, o=out, i=in_: e.reciprocal(o, i), reads, writes)

    def dma(self, q, key, out, in_, reads, writes):
        self.kb.dma(q, key, lambda e, s, o=out, i=in_: e.dma_start(out=o, in_=i).then_inc(s, 16), reads, writes)


def a2_body(nc, kb, D, nq=8, debug=False):
    xT_d, wf_d, wt_d, w1_d, pe_d, w2_d, btc_d, mkc_d, bts_d, mks_d, mkw_d, m12_d, rv_d, c2s_d, exd_d, hsel_d = (D[k] for k in ['xT', 'wf', 'wt', 'w1', 'peT', 'w2', 'btc', 'mkc', 'bts', 'mks', 'mkw', 'm12', 'rv', 'c2s', 'exd', 'hsel'])
    yT_d = D["yT"]
    xT_v = xT_d.rearrange("(kc p) t -> p kc t", p=128)
    if True:
        em = Em(kb)
        sb, ps = kb.sb, kb.ps
        xs = [sb(f"xs{i}", [128, 8, 512], BF16) for i in range(2)]; XS = [Buf() for _ in range(2)]
        wf = sb("wf", [128, 8, 640], BF16); WF = Buf()
        wt = sb("wt", [128, 8, 256], BF16); WT = Buf()
        w1 = sb("w1", [128, 32, 128], BF16); W1 = Buf()
        peT = sb("peT", [128, 32], BF16); PET = Buf()
        w2f = sb("w2f", [128, 192]); w2 = sb("w2", [128, 192], BF16); W2 = Buf()
        qT = [sb(f"qT{i}", [128, S], BF16) for i in range(2)]; QT = [Buf() for _ in range(2)]
        ksT = sb("ksT", [128, S], BF16); KST = Buf()
        kwT = sb("kwT", [128, S], BF16); KWT = Buf()
        kcv = sb("kcv", [128, S], BF16); KCV = Buf()
        vs = sb("vs", [128, 32, 96], BF16); VS = Buf()
        vw = sb("vw", [128, 32, 96], BF16); VW = Buf()
        gt = sb("gt", [128, 32, 16]); GT = Buf()
        btc = sb("btc", [128, 4, 256]); BTC = Buf()
        mkc = sb("mkc", [128, 256]); MKC = Buf()
        HW_ = TW // 2
        stg = sb("stg", [128, HW_]); STG = Buf()
        stg2 = sb("stg2", [128, HW_]); STG2 = Buf()
        mks = sb("mks", [128, HW_]); MKS = Buf()
        mkw = sb("mkw", [128, HW_]); MKW = Buf()
        ebs = [sb(f"ebs{h}", [128, TW], BF16) for h in range(4)]; EBS = [Buf() for _ in range(4)]
        ebw = [sb(f"ebw{h}", [128, TW], BF16) for h in range(4)]; EBW = [Buf() for _ in range(4)]
        m12 = sb("m12", [128, 256]); M12 = Buf()
        rv = sb("rv", [128, 1]); RV = Buf()
        vca = sb("vca", [128, 2, 128], BF16); VCA = Buf()
        c2sf = sb("c2sf", [128, 128]); C2SF = Buf()
        exd = sb("exd", [128, 32, 128], BF16); EXD = Buf()
        hself = sb("hself", [128, 256]); hsel = sb("hsel", [128, 2, 128], BF16); HSEL = Buf()
        identf = sb("identf", [128, 128]); ident = sb("ident", [128, 128], BF16); IDENT = Buf()
        kct = sb("kct", [128, 256], BF16); KCT = Buf()
        gh = [sb(f"gh{i}", [128, 256], BF16) for i in range(2)]; GH = [Buf() for _ in range(2)]
        hx = sb("hx", [128, 256]); HX = Buf()
        hy = sb("hy", [128, 256]); HY = Buf()
        hb = sb("hb", [128, 2]); HB = Buf()
        sm = sb("sm", [128, 64]); SM = Buf()
        cst = sb("cst", [128, 16]); CST = Buf()
        qsq = sb("qsq", [128, 512], BF16); QSQ = Buf()
        mxc = sb("mxc", [128, 4, 8]); MXC = Buf()
        kxc = sb("kxc", [128, 2, 8]); KXC = Buf()
        lgs = [sb(f"lg{h}", [128, 256]) for h in range(4)]; LGS = [Buf() for _ in range(4)]
        pcs = [sb(f"pc{h}", [128, 256], BF16) for h in range(4)]; PCS = [Buf() for _ in range(4)]
        pcts = [sb(f"pct{h}", [128, 2, 128], BF16) for h in range(4)]; PCTS = [Buf() for _ in range(4)]
        smh = sb("smh", [128, 32]); SMH = [Buf() for _ in range(4)]
        sc = sb("sc", [128, 64]); SC = Buf()
        sc2 = sb("sc2", [128, 64]); SC2 = Buf()
        mx8 = sb("mx8", [128, 16]); MX8 = Buf()
        nst = sb("nst", [128, 64], BF16); NST = Buf()
        nsh = sb("nsh", [128, 512], BF16); NSH = Buf()
        yg = sb("yg", [128, 4, 256]); YG = Buf()
        ygt = sb("ygt", [128, 2, 512]); YGT = Buf()
        pt = [sb(f"pt{i}", [128, 512], BF16) for i in range(3)]; PT = [Buf() for _ in range(3)]
        p2 = [sb(f"p2{i}", [128, 512], BF16) for i in range(3)]; P2 = [Buf() for _ in range(3)]
        rin = sb("rin", [128, 8]); RIN = Buf()
        zb = sb("zb", [128, 512], BF16); ZB = Buf()
        otmp = sb("otmp", [128, 4, 64]); OTMP = Buf()
        pg = [ps(f"pg{i}", [128, 512]) for i in range(3)]; PG = [Buf(excl=True) for _ in range(3)]
        ptb = ps("ptb", [128, 1024], BF16); PTB = Buf(excl=True)
        pl = [ps(f"pl{i}", [128, 512]) for i in range(2)]; PL = [Buf(excl=True) for _ in range(2)]
        pacc = [ps(f"pacc{i}", [128, 4, 128]) for i in range(2)]; PACC = [Buf(excl=True) for _ in range(2)]

        gi = [0]

        def gbank():
            i = gi[0] % 3; gi[0] += 1
            return pg[i], PG[i]

        dbg_n = [0]

        def dbg(name, ap, B):
            if not debug:
                return
            d = nc.dram_tensor("dbg_" + name, list(ap.shape), F32, kind="ExternalOutput").ap()
            dbg_n[0] += 1
            cntv = dbg_n[0] * 16

            def f(e, s, d=d, ap=ap, cntv=cntv):
                e.dma_start(out=d, in_=ap).then_inc(s, 16)
                e.wait_ge(s, cntv)
            kb.dma("pool", "dbg", f, reads=[B])

        em.dma("pool", "wf", wf[:, :, :], wf_d.rearrange("(kc p) n -> p kc n", p=128), [], [WF])
        em.dma("pool", "wt", wt[:, :, 0:140], wt_d.rearrange("(kc p) n -> p kc n", p=128), [], [WT])
        em.dma("pool", "w1", w1[:, :, :], w1_d.rearrange("p (a b) -> p a b", b=128), [], [W1])
        em.dma("pool", "pe", peT[:], pe_d[:, :], [], [PET])
        em.dma("sp", "w2", w2f[:], w2_d[:, :], [], [W2])
        em.dma("sp", "btc", btc[:].rearrange("p h w -> p (h w)"), btc_d[:, :], [], [BTC])
        em.dma("sp", "mkc", mkc[:], mkc_d[:, :], [], [MKC])
        em.dma("sp", "m12", m12[:], m12_d[:, :], [], [M12])
        em.dma("sp", "rv", rv[:], rv_d[:, :], [], [RV])
        em.dma("sp", "c2s", c2sf[:], c2s_d[:, :], [], [C2SF])
        em.ms("pool", exd[:], 0.0, [EXD])
        em.dma("pool", "exd", exd[0:64, :, :].rearrange("p a b -> p (a b)"), exd_d[:, :], [EXD], [EXD])
        em.dma("sp", "hsel", hself[:], hsel_d[:, :], [], [HSEL])
        em.cp("dve", w2[:], w2f[:], [W2], [W2])
        em.cp("dve", hsel[:].rearrange("p a b -> p (a b)"), hself[:], [HSEL], [HSEL])
        em.ms("pool", identf[:], 1.0, [IDENT])
        kb.op("pool", lambda e: e.affine_select(out=identf[:], in_=identf[:], pattern=[[-1, 128]], compare_op=ALU.is_equal,
                                                fill=0.0, base=0, channel_multiplier=1), [IDENT], [IDENT])
        em.cp("pool", ident[:], identf[:], [IDENT], [IDENT])
        em.ms("dve", vs[:, :, 64:65], 1.0, [VS])
        em.ms("dve", vw[:, :, 64:65], 1.0, [VW])
        em.ms("dve", vca[:], 0.0, [VCA])
        em.ms("dve", zb[:], 0.0, [ZB])
        em.ms("dve", nsh[:], 0.0, [NSH])
        for h_ in range(4):
            em.ms("dve", pcs[h_][:], 0.0, [PCS[h_]])
        em.ms("dve", kct[:], 0.0, [KCT])
        for h in range(4):
            em.tt("dve", btc[:, h, :], btc[:, h, :], mkc[:], ALU.add, [BTC, MKC], [BTC])
        first = True
        for hf in range(2):
            cs_ = slice(hf * HW_, (hf + 1) * HW_)
            em.dma("sp", "mks", mks[:], mks_d[:, cs_], [], [MKS])
            em.dma("sp", "mkw", mkw[:], mkw_d[:, cs_], [], [MKW])
            for h in range(4):
                em.dma("sp", "stg", stg[:], bts_d[:, h * TW + hf * HW_:h * TW + (hf + 1) * HW_], [], [STG])
                if first:
                    em.red(cst[:, 8:9], stg[:], ALU.max, [STG], [CST])
                    first = False
                else:
                    em.red(cst[:, 9:10], stg[:], ALU.max, [STG], [CST])
                    em.tt("dve", cst[:, 8:9], cst[:, 8:9], cst[:, 9:10], ALU.max, [CST], [CST])
                em.tt("dve", stg2[:], stg[:], mks[:], ALU.add, [STG, MKS], [STG2])
                em.act(ebs[h][:, cs_], stg2[:], AF.Exp, [STG2], [EBS[h]])
                em.tt("dve", stg2[:], stg[:], mkw[:], ALU.add, [STG, MKW], [STG2])
                em.act(ebw[h][:, cs_], stg2[:], AF.Exp, [STG2], [EBW[h]])

        import os as _os
        PH = int(_os.environ.get('PH', '9'))
        def load_x(sg):
            sl = sg % 2
            em.dma("pool", f"xs{sl}", xs[sl][:, :, :], xT_v[:, :, sg * 512:(sg + 1) * 512], [], [XS[sl]])

        load_x(0)
        for sg in range(8 if PH >= 2 else 0):
            sl = sg % 2
            if sg + 1 < 8:
                load_x(sg + 1)
            seg = slice(sg * 512, (sg + 1) * 512)
            dsts = [(qT[0], QT[0], 0.125), (qT[1], QT[1], 0.125), (ksT, KST, 1.0), (kwT, KWT, 1.0), (kcv, KCV, 1.0)]
            for j, (dst, DST, scl) in enumerate(dsts):
                p, P = gbank()
                for k in range(8):
                    em.mm(p[:, :], wf[:, k, j * 128:(j + 1) * 128], xs[sl][:, k, :], k == 0, k == 7, [WF, XS[sl]], [P])
                em.act(dst[:, seg], p[:, :], AF.Copy, [P], [DST], scale=scl)
            PJ = _os.environ.get('PJ', 'abc12')
            for tt_ in range(4 if 'b' in PJ else 0):
                tile_i = sg * 4 + tt_
                p, P = gbank()
                for k in range(8):
                    em.mm(p[:, 0:140], xs[sl][:, k, tt_ * 128:(tt_ + 1) * 128], wt[:, k, 0:140], k == 0, k == 7, [WT, XS[sl]], [P])
                if '1' in PJ:
                    em.cp("dve", vs[:, tile_i, 0:64], p[:, 0:64], [P], [VS])
                    em.cp("dve", vw[:, tile_i, 0:64], p[:, 64:128], [P], [VW])
                if '2' in PJ:
                    em.act(gt[:, tile_i, 0:12], p[:, 128:140], AF.Sigmoid, [P], [GT])
            for i in range(2 if 'c' in PJ else 0):
                em.tt("dve", qsq[:], qT[i][:, seg], qT[i][:, seg], ALU.mult, [QT[i]], [QSQ])
                for hh in range(2):
                    p, P = gbank()
                    em.mm(p[:, :], hsel[:, hh, :], qsq[:], True, True, [HSEL, QSQ], [P])
                    em.red(mxc[:, 2 * i + hh, sg:sg + 1], p[:, :], ALU.max, [P], [MXC])
            for i, (src, SRC) in enumerate(((ksT, KST), (kwT, KWT)) if 'c' in PJ else ()):
                em.tt("dve", qsq[:], src[:, seg], src[:, seg], ALU.mult, [SRC], [QSQ])
                p, P = gbank()
                em.mm(p[:, :], hsel[:, 0, :], qsq[:], True, True, [HSEL, QSQ], [P])
                em.red(kxc[:, i, sg:sg + 1], p[:, :], ALU.max, [P], [KXC])
        if PH < 3:
            nq = 0
        em.red(sm[:, 0:4], mxc[:], ALU.max, [MXC], [SM])
        em.red(sm[:, 4:6], kxc[:], ALU.max, [KXC], [SM])
        for br in range(2):
            em.ts("dve", sm[:, 8 + 4 * br:12 + 4 * br], sm[:, 0:4], sm[:, 4 + br:5 + br], None, ALU.mult, None, [SM], [SM])
        em.act(sm[:, 16:24], sm[:, 8:16], AF.Sqrt, [SM], [SM])
        em.ts("dve", cst[:, 0:8], sm[:, 16:24], cst[:, 8:9], -1.0, ALU.add, ALU.mult, [SM, CST], [CST])

        for br in range(2 if PH >= 3 else 0):
            rows = slice(64 * br, 64 * br + 64)
            p, P = gbank()
            for pp in range(32):
                em.mm(p[:, 0:255], w1[rows, pp, :], kcv[rows, pp:pp + 16 * 254 + 1:16], pp == 0, pp == 31, [W1, KCV], [P])
            pb, PB = gbank()
            for pp in range(32):
                em.mm(pb[:, 0:1], w1[rows, pp, :], peT[rows, pp:pp + 1], pp == 0, pp == 31, [W1, PET], [PB])
            em.cp("dve", hb[:, br:br + 1], pb[:, 0:1], [PB], [HB])
            em.ts("dve", hx[:, 0:255], p[:, 0:255], hb[:, br:br + 1], None, ALU.add, None, [P, HB], [HX])
            em.tt("dve", hy[:, 0:255], hx[:, 0:255], hx[:, 0:255], ALU.mult, [HX], [HY])
            em.ts("dve", hy[:, 0:255], hy[:, 0:255], 0.044715, 1.0, ALU.mult, ALU.add, [HY], [HY])
            em.tt("dve", hy[:, 0:255], hy[:, 0:255], hx[:, 0:255], ALU.mult, [HY, HX], [HY])
            em.act(hy[:, 0:255], hy[:, 0:255], AF.Tanh, [HY], [HY], scale=0.7978845608028654)
            em.ts("dve", hy[:, 0:255], hy[:, 0:255], 1.0, 0.5, ALU.add, ALU.mult, [HY], [HY])
            em.tt("dve", gh[br][:, 0:255], hy[:, 0:255], hx[:, 0:255], ALU.mult, [HY, HX], [GH[br]])
        p, P = gbank()
        em.mm(p[:, 0:255], w2[:, 0:128], gh[0][:, 0:255], True, True, [W2, GH[0]], [P])
        em.act(kct[:, 0:255], p[:, 0:255], AF.Copy, [P], [KCT])
        for ct in range(2):
            ncs = 128 if ct == 0 else 127
            p, P = gbank()
            em.mm(p[0:ncs, 0:64], gh[1][:, ct * 128:ct * 128 + ncs], w2[:, 128:192], True, True, [W2, GH[1]], [P])
            em.act(vca[0:ncs, ct, 0:64], p[0:ncs, 0:64], AF.Copy, [P], [VCA])
        em.cp("dve", vca[:, :, 64:128], c2sf[:].rearrange("p (a b) -> p a b", b=64), [C2SF], [VCA])
        dbg("kct", kct[:, 0:255], KCT); dbg("vca", vca[:].rearrange("p a b -> p (a b)"), VCA)
        dbg("cst", cst[:, 0:9], CST)

        pti = [0]
        for Q in range(nq):
            qs = slice(Q * 512, (Q + 1) * 512)
            for a in range(4):
                n = 4 * Q + a
                t0 = 128 * n
                ncol = min(255, 8 * n + 7)
                off = 248 - 8 * n
                ncp = min(256, (ncol + 31) // 32 * 32)
                nct = 1 if ncol <= 128 else 2
                for hpair in ((0, 1), (2, 3)):
                    HP = {}
                    for h in hpair:
                        rows = slice(64 * (h % 2), 64 * (h % 2) + 64)
                        p, P = gbank()
                        em.mm(p[:, 0:ncp], qT[h // 2][rows, t0:t0 + 128], kct[rows, 0:ncp], True, True, [QT[h // 2], KCT], [P])
                        HP[h] = (p, P)
                    for h in hpair:
                        p, P = HP[h]
                        s0 = 8 * h
                        em.tt("dve", lgs[h][:, 0:ncol], p[:, 0:ncol], btc[:, h, off:off + ncol], ALU.add, [P, BTC], [LGS[h]])
                        em.red(smh[:, s0:s0 + 1], lgs[h][:, 0:ncol], ALU.max, [LGS[h]], [SMH[h]])
                        em.ts("dve", smh[:, s0 + 1:s0 + 2], smh[:, s0:s0 + 1], -1.0, None, ALU.mult, None, [SMH[h]], [SMH[h]])
                        em.ms("dve", smh[:, s0 + 2:s0 + 3], 0.0, [SMH[h]])
                    for h in hpair:
                        s0 = 8 * h
                        em.act(pcs[h][:, 0:ncol], lgs[h][:, 0:ncol], AF.Exp, [LGS[h], SMH[h]], [PCS[h], SMH[h]], bias=smh[:, s0 + 1:s0 + 2], accum_out=smh[:, s0 + 2:s0 + 3])
                    for h in hpair:
                        for ct in range(nct):
                            em.tr(ptb[:, ct * 128:(ct + 1) * 128], pcs[h][:, ct * 128:(ct + 1) * 128], ident[:], [PCS[h], IDENT], [PTB])
                        em.cp("dve", pcts[h][:, 0:nct, :], ptb[:, 0:nct * 128].rearrange("p (a b) -> p a b", b=128), [PTB], [PCTS[h]])
                    PO_ = {}
                    for h in hpair:
                        po, PO = gbank()
                        for ct in range(nct):
                            em.mm(po[:, 0:128], pcts[h][:, ct, :], vca[:, ct, :], ct == 0, ct == nct - 1, [PCTS[h], VCA], [PO])
                        PO_[h] = (po, PO)
                    for h in hpair:
                        po, PO = PO_[h]
                        s0 = 8 * h
                        em.rcp(smh[:, s0 + 3:s0 + 4], smh[:, s0 + 2:s0 + 3], [SMH[h]], [SMH[h]])
                        if n == 0:
                            em.tt("dve", smh[:, s0 + 3:s0 + 4], smh[:, s0 + 3:s0 + 4], rv[:], ALU.mult, [SMH[h], RV], [SMH[h]])
                        em.tt("dve", smh[:, s0 + 4:s0 + 5], smh[:, s0 + 3:s0 + 4], gt[:, n, 3 * h:3 * h + 1], ALU.mult, [SMH[h], GT], [SMH[h]])
                        em.ts("dve", yg[:, a, h * 64:(h + 1) * 64], po[:, 0:64], smh[:, s0 + 4:s0 + 5], None, ALU.mult, None, [PO, SMH[h]], [YG])
                        if h == 0:
                            em.ts("dve", sc[:], po[:, 64:128], smh[:, s0 + 3:s0 + 4], None, ALU.mult, None, [PO, SMH[h]], [SC])
                        else:
                            em.stt("dve", sc[:], po[:, 64:128], smh[:, s0 + 3:s0 + 4], sc[:], ALU.mult, ALU.add, [PO, SMH[h], SC], [SC])
                w0 = 64 - 2 * n
                em.tt("dve", sc[:], sc[:], m12[:, w0:w0 + 64], ALU.mult, [SC, M12], [SC])
                em.tt("dve", sc[:], sc[:], m12[:, 128 + w0:128 + w0 + 64], ALU.add, [SC, M12], [SC])
                em.ms("dve", sc[:, 0:1], 1.0e4, [SC])
                kb.op("dve", lambda e: e.max(out=mx8[:, 0:8], in_=sc[:]), [SC], [MX8])
                kb.op("dve", lambda e: e.match_replace(out=sc2[:], in_to_replace=mx8[:, 0:8], in_values=sc[:], imm_value=-1.0e9), [SC, MX8], [SC2])
                kb.op("dve", lambda e: e.max(out=mx8[:, 8:16], in_=sc2[:]), [SC2], [MX8])
                em.red(sm[:, 40:41], mx8[:, 8:16], ALU.min, [MX8], [SM])
                em.ts("dve", nst[:], sc[:], sm[:, 40:41], NEG, ALU.is_lt, ALU.mult, [SC, SM], [NST])
                em.tr(ptb[0:64, 256:384], nst[:], ident[:], [NST, IDENT], [PTB])
                em.cp("dve", nsh[0:64, a * 128:(a + 1) * 128], ptb[0:64, 256:384], [PTB], [NSH])
                if Q == 0 and a == 1:
                    dbg("sc", sc[:], SC); dbg("yg1", yg[:, 1, :], YG)
            QP = _os.environ.get('QP', 'abc')
            for br in [b_ for b_ in range(2) if 'bc'[b_] in QP]:
                kT, KT_, vv, VV, eb, EB = (ksT, KST, vs, VS, ebs, EBS) if br == 0 else (kwT, KWT, vw, VW, ebw, EBW)
                m_lo = 0 if br == 0 else max(0, 4 * Q - 4)
                m_hi = 4 * Q + 3
                tiles = [(h, m) for h in range(4) for m in range(m_lo, m_hi + 1)]
                slot = {}

                def stage1(ti):
                    h, m = tiles[ti]
                    rows = slice(64 * (h % 2), 64 * (h % 2) + 64)
                    pli = pti[0] % 2
                    bi = pti[0] % 3
                    pti[0] += 1
                    slot[ti] = (pli, bi)
                    pl_, PL_ = pl[pli], PL[pli]
                    em.mm(pl_[:, :], kT[rows, m * 128:(m + 1) * 128], qT[h // 2][rows, qs], True, br == 1, [KT_, QT[h // 2]], [PL_])
                    if br == 0:
                        em.mm(pl_[:, :], exd[:, m, :], nsh[:, :], False, True, [EXD, NSH], [PL_])

                def stage23(ti):
                    h, m = tiles[ti]
                    pli, bi = slot.pop(ti)
                    pl_, PL_ = pl[pli], PL[pli]
                    acc, ACC = pacc[h % 2], PACC[h % 2]
                    D0 = 512 * Q - 128 * m
                    wst = min(D0, DCL) + 512
                    em.act(pt[bi][:], pl_[:, :], AF.Exp, [PL_, CST], [PT[bi]], bias=cst[:, 4 * br + h:4 * br + h + 1])
                    em.tt("dve", p2[bi][:], pt[bi][:], eb[h][:, wst:wst + 512], ALU.mult, [PT[bi], EB[h]], [P2[bi]])
                    if m == m_lo:
                        em.mm(acc[:].rearrange("p a b -> p (a b)"), zb[:, 0:128], zb[:, 0:512], True, False, [ZB], [ACC])
                    for a in range(4):
                        last_m = min(m_hi, 4 * Q + a)
                        if m > last_m:
                            continue
                        a_lo = m_lo if br == 0 else max(m_lo, 4 * Q + a - 4)
                        if m < a_lo:
                            continue
                        em.mm(acc[:, a, 0:65], p2[bi][:, a * 128:(a + 1) * 128], vv[:, m, 0:65], False, (m == m_hi and a == 3), [P2[bi], VV], [ACC])
                    if m == m_hi:
                        em.rcp(rin[:, 0:4], acc[:, :, 64], [ACC], [RIN])
                        em.tt("dve", rin[:, 4:8], rin[:, 0:4], gt[:, 4 * Q:4 * Q + 4, 3 * h + 1 + br], ALU.mult, [RIN, GT], [RIN])
                        em.tt("dve", otmp[:], acc[:, :, 0:64], rin[:, 4:8].unsqueeze(2).to_broadcast([128, 4, 64]), ALU.mult, [ACC, RIN], [OTMP])
                        em.tt("pool", yg[:, :, h * 64:(h + 1) * 64], yg[:, :, h * 64:(h + 1) * 64], otmp[:], ALU.add, [YG, OTMP], [YG])
                stage1(0)
                for ti in range(len(tiles)):
                    if ti + 1 < len(tiles):
                        stage1(ti + 1)
                    stage23(ti)
            for a in range(4):
                for hp in range(2):
                    pz, PZ = gbank()
                    em.tr(pz[:, 0:128], yg[:, a, hp * 128:(hp + 1) * 128], identf[:], [YG, IDENT], [PZ])
                    em.cp("dve" if hp else "pool_never", ygt[:, hp, a * 128:(a + 1) * 128], pz[:, 0:128], [PZ], [YGT]) if False else em.cp("dve", ygt[:, hp, a * 128:(a + 1) * 128], pz[:, 0:128], [PZ], [YGT])
            for hp in range(2):
                em.dma("sp", "y", yT_d[hp * 128:(hp + 1) * 128, Q * 512:(Q + 1) * 512], ygt[:, hp, :], [YGT], [])
        kb.finish([YG, YGT])


def build_a2(nq=8, debug=False):
    nc = bass.Bass("TRN2", target_bir_lowering=False)
    I = lambda n, shp: nc.dram_tensor(n, shp, F32, kind="ExternalInput").ap()
    D = {"xT": I("xT", [1024, S]), "wf": I("wf", [1024, 640]), "wt": I("wt", [1024, 140]), "w1": I("w1", [128, 32 * 128]), "peT": I("peT", [128, 32]), "w2": I("w2", [128, 192]), "btc": I("btc", [128, 4 * 256]), "mkc": I("mkc", [128, 256]), "bts": I("bts", [128, 4 * TW]), "mks": I("mks", [128, TW]), "mkw": I("mkw", [128, TW]), "m12": I("m12", [128, 256]), "rv": I("rv", [128, 1]), "c2s": I("c2s", [128, 128]), "exd": I("exd", [64, 32 * 128]), "hsel": I("hsel", [128, 256])}
    D["yT"] = nc.dram_tensor("yT", [256, S], F32, kind="ExternalOutput").ap()
    with ExitStack() as st:
        kb = KB(nc, st)
        a2_body(nc, kb, D, nq, debug)
        kb.emit()
    return nc


def a2_consts():
    i = np.arange(128)[:, None]
    j = np.arange(256)[None, :]
    dc = i - 16 * (j - 248) - 31
    mkc = np.where(dc >= 0, 0.0, NEG).astype(np.float32)
    w = np.arange(TW)[None, :]
    ds = w - i - 512
    mks = np.where(ds >= 0, 0.0, NEG).astype(np.float32)
    mkw = np.where((ds >= 0) & (ds < 512), 0.0, NEG).astype(np.float32)
    wv = np.arange(128)[None, :] - 64
    cur = (i >= 64).astype(np.int64)
    forced = (wv == cur) | (wv == cur - 1)
    valid = wv <= cur
    m1 = (valid & ~forced).astype(np.float32)
    m2 = np.where(forced, 1.0e4, np.where(valid, 0.0, -1.0)).astype(np.float32)
    m12 = np.concatenate([m1, m2], axis=1)
    rv = (np.arange(128) >= 31).astype(np.float32)[:, None]
    cs = np.arange(255)[:, None] * 16; ss = np.arange(64)[None, :] * 64
    ov = np.clip(np.minimum(cs + 32, ss + 64) - np.maximum(cs, ss), 0, None).astype(np.float32) / 32
    c2s = np.zeros((256, 64), np.float32); c2s[:255] = ov
    c2s = c2s.reshape(2, 128, 64).transpose(1, 0, 2).reshape(128, 128)
    exd = np.zeros((64, 32, 128), np.float32)
    for m in range(32):
        exd[2 * m, m, 0:64] = 1.0; exd[2 * m + 1, m, 64:128] = 1.0
    hsel = np.zeros((128, 2, 128), np.float32); hsel[0:64, 0, :] = 1.0; hsel[64:128, 1, :] = 1.0
    return dict(mkc=mkc, mks=mks, mkw=mkw, m12=m12, rv=rv, c2s=c2s, exd=exd.reshape(64, 32 * 128), hsel=hsel.reshape(128, 256),
                dc=dc, ds=ds)


def a2_inputs(inp, l, g, consts):
    w_in = inp["w_in"][l]; zr = 1792
    q = w_in[:, zr + 256 * g: zr + 256 * g + 256]
    def kvc(off):
        return w_in[:, zr + off + 64 * g: zr + off + 64 * g + 64]
    kc, vc, ks, vs, kw, vw = (kvc(o) for o in (512, 640, 768, 896, 1024, 1152))
    gates = w_in[:, zr + 1280 + 12 * g: zr + 1280 + 12 * g + 12]
    wf = np.concatenate([q, ks, ks, kw, kw, kc, vc], axis=1)
    wt = np.concatenate([vs, vw, gates], axis=1)
    def w1r(w):
        return w.reshape(32, 64, 128).transpose(1, 0, 2)
    w1 = np.concatenate([w1r(inp["cmp_w1_k"][l]), w1r(inp["cmp_w1_v"][l])], axis=0).reshape(128, 32 * 128)
    peT = np.concatenate([inp["cmp_pe_k"][l].T, inp["cmp_pe_v"][l].T], axis=0)
    w2 = np.concatenate([inp["cmp_w2_k"][l], inp["cmp_w2_k"][l], inp["cmp_w2_v"][l]], axis=1)
    rb = inp["rel_bias"][:, 4 * g:4 * g + 4]
    btc = np.take(rb, t5_bucket(consts["dc"]), axis=0).transpose(0, 2, 1).reshape(128, 4 * 256)
    bts = np.take(rb, t5_bucket(consts["ds"]), axis=0).transpose(0, 2, 1).reshape(128, 4 * TW)
    out = dict(wf=wf, wt=wt, w1=w1, peT=peT, w2=w2, btc=btc, bts=bts)
    for k in ("mkc", "mks", "mkw", "m12", "rv", "c2s", "exd", "hsel"):
        out[k] = consts[k]
    return {k: np.ascontiguousarray(v, dtype=np.float32) for k, v in out.items()}


NT = 2048
ALPHA = 8 ** 0.25
LN_EPS = 1e-5


def layer_norm_fm(kb, em, gbank, R, RB, out_fn, g_ap, b_ap, GB, tmp, TMP, ones, ONES, sq, SQ, mean, MEAN, rstd, RSTD):
    pm, PM = gbank()
    for i in range(8):
        em.mm(pm[:, :], ones[:], R[:, i, :], i == 0, i == 7, [ONES, RB], [PM])
    em.act(mean[:], pm[:, :], AF.Copy, [PM], [MEAN], scale=1.0 / 1024)
    pv, PV = gbank()
    for i in range(8):
        em.tt("pool" if i % 2 else "dve", sq[i % 2][:], R[:, i, :], R[:, i, :], ALU.mult, [RB], [SQ[i % 2]])
        em.mm(pv[:, :], ones[:], sq[i % 2][:], i == 0, i == 7, [ONES, SQ[i % 2]], [PV])
    em.tt("dve", tmp[:], mean[:], mean[:], ALU.mult, [MEAN], [TMP])
    em.stt("dve", rstd[:], pv[:, :], 1.0 / 1024, tmp[:], ALU.mult, ALU.subtract, [PV, TMP], [RSTD])
    em.ts("dve", rstd[:], rstd[:], LN_EPS, None, ALU.add, None, [RSTD], [RSTD])
    em.act(rstd[:], rstd[:], AF.Sqrt, [RSTD], [RSTD])
    em.rcp(rstd[:], rstd[:], [RSTD], [RSTD])
    for i in range(8):
        eng = "pool" if i % 2 else "dve"
        em.tt(eng, tmp[:], R[:, i, :], mean[:], ALU.subtract, [RB, MEAN], [TMP])
        em.tt(eng, tmp[:], tmp[:], rstd[:], ALU.mult, [TMP, RSTD], [TMP])
        o, O = out_fn(i)
        em.ts(eng, o, tmp[:], g_ap[:, i:i + 1], b_ap[:, i:i + 1], ALU.mult, ALU.add, [TMP, GB], [O])


def b1_body(nc, kb, D):
    xT_d, yr_d, yn_d, wg_d, wur_d, wun_d, wo_d, ln_d, o_d = (D[k] for k in ("xT", "yrT", "ynT", "wg", "wur", "wun", "wo", "ln", "x1T"))
    v3 = lambda ap: ap.rearrange("(kc p) n -> p kc n", p=128)
    if True:
        em = Em(kb); sb, ps = kb.sb, kb.ps
        wg = sb("wg", [128, 8, 2048], BF16); WG = Buf()
        wur = sb("wur", [128, 4, 1024], BF16); WUR = Buf()
        wun = sb("wun", [128, 4, 1024], BF16); WUN = Buf()
        wo = sb("wo", [128, 8, 1024], BF16); WO = Buf()
        ln = sb("ln", [128, 16]); LN = Buf()
        ones = sb("ones", [128, 128]); ONES = Buf()
        xf = sb("xf", [128, 8, 512]); XF = Buf()
        xb = sb("xb", [128, 8, 512], BF16); XB = Buf()
        yr = sb("yr", [128, 4, 512], BF16); YR = Buf()
        yn = sb("yn", [128, 4, 512], BF16); YN = Buf()
        sg = [sb(f"sg{i}", [128, 512]) for i in range(2)]; SG = [Buf() for _ in range(2)]
        m1 = sb("m1", [128, 512]); M1 = Buf()
        mg = sb("mg", [128, 8, 512], BF16); MG = Buf()
        R = sb("R", [128, 8, 512]); RB = Buf()
        ob = sb("ob", [128, 8, 512]); OB = Buf()
        tmp = sb("tmp", [128, 512]); TMP = Buf()
        sq = [sb(f"sq{i}", [128, 512]) for i in range(2)]; SQ = [Buf() for _ in range(2)]
        mean = sb("mean", [128, 512]); MEAN = Buf()
        rstd = sb("rstd", [128, 512]); RSTD = Buf()
        pg = [ps(f"pg{i}", [128, 512]) for i in range(6)]; PG = [Buf(excl=True) for _ in range(6)]
        gi = [0]

        def gbank():
            i = gi[0] % 6; gi[0] += 1
            return pg[i], PG[i]
        for k0 in range(0, 8, 2):
            em.dma("pool", "wg", wg[:, k0:k0 + 2, :], v3(wg_d)[:, k0:k0 + 2, :], [], [WG])
        em.dma("pool", "wur", wur[:, :, :], v3(wur_d), [], [WUR])
        em.dma("pool", "wun", wun[:, :, :], v3(wun_d), [], [WUN])
        em.dma("pool", "wo", wo[:, :, :], v3(wo_d), [], [WO])
        em.dma("sp", "ln", ln[:], ln_d[:, :], [], [LN])
        em.ms("dve", ones[:], 1.0, [ONES])
        for tg in range(NT // 512):
            ts_ = slice(tg * 512, (tg + 1) * 512)
            em.dma("sp", "xf", xf[:, :, :], v3(xT_d)[:, :, ts_], [], [XF])
            em.dma("pool", "yr", yr[:, :, :], v3(yr_d)[:, :, ts_], [], [YR])
            em.dma("pool", "yn", yn[:, :, :], v3(yn_d)[:, :, ts_], [], [YN])
            for i in range(8):
                em.cp("pool" if i % 2 else "dve", xb[:, i, :], xf[:, i, :], [XF], [XB])
            for j in range(8):
                cs = slice(j * 128, (j + 1) * 128)
                for br, (wu, WU, yy, YY) in enumerate(((wur, WUR, yr, YR), (wun, WUN, yn, YN))):
                    p, P = gbank()
                    for k in range(8):
                        em.mm(p[:, :], wg[:, k, br * 1024 + j * 128: br * 1024 + (j + 1) * 128], xb[:, k, :], k == 0, k == 7, [WG, XB], [P])
                    em.act(sg[br][:], p[:, :], AF.Sigmoid, [P], [SG[br]])
                    p2, P2 = gbank()
                    for k in range(4):
                        em.mm(p2[:, :], wu[:, k, cs], yy[:, k, :], k == 0, k == 3, [WU, YY], [P2])
                    if br == 0:
                        em.tt("dve", m1[:], sg[0][:], p2[:, :], ALU.mult, [SG[0], P2], [M1])
                    else:
                        em.tt("dve", sg[1][:], sg[1][:], p2[:, :], ALU.mult, [SG[1], P2], [SG[1]])
                        em.tt("pool", mg[:, j, :], m1[:], sg[1][:], ALU.add, [M1, SG[1]], [MG])
            for i in range(8):
                p, P = gbank()
                for k in range(8):
                    em.mm(p[:, :], wo[:, k, i * 128:(i + 1) * 128], mg[:, k, :], k == 0, k == 7, [WO, MG], [P])
                em.stt("dve", R[:, i, :], xf[:, i, :], ALPHA, p[:, :], ALU.mult, ALU.add, [XF, P], [RB])
            layer_norm_fm(kb, em, gbank, R, RB, lambda i: (ob[:, i, :], OB), ln[:, 0:8], ln[:, 8:16], LN, tmp, TMP, ones, ONES, sq, SQ, mean, MEAN, rstd, RSTD)
            em.dma("sp", "ob", v3(o_d)[:, :, ts_], ob[:, :, :], [OB], [])
        kb.finish([OB])


def build_b1():
    nc = bass.Bass("TRN2", target_bir_lowering=False)
    I = lambda n, shp: nc.dram_tensor(n, shp, F32, kind="ExternalInput").ap()
    D = dict(xT=I("xT", [1024, NT]), yrT=I("yrT", [512, NT]), ynT=I("ynT", [512, NT]), wg=I("wg", [1024, 2048]), wur=I("wur", [512, 1024]),
             wun=I("wun", [512, 1024]), wo=I("wo", [1024, 1024]), ln=I("ln", [128, 16]),
             x1T=nc.dram_tensor("x1T", [1024, NT], F32, kind="ExternalOutput").ap())
    with ExitStack() as st:
        kb = KB(nc, st)
        b1_body(nc, kb, D)
        kb.emit()
    return nc


def b1_inputs(inp, l):
    w_in = inp["w_in"][l]
    lnp = np.concatenate([inp["ln1_g"][l].reshape(8, 128).T, inp["ln1_b"][l].reshape(8, 128).T], axis=1)
    return dict(wg=np.ascontiguousarray(w_in[:, 1792 + 1304: 1792 + 1304 + 2048]), wur=inp["w_up_rwkv"][l], wun=inp["w_up_nsa"][l],
                wo=inp["w_out"][l], ln=np.ascontiguousarray(lnp, dtype=np.float32))


def b2_body(nc, kb, D, nexp=32):
    x1_d, wr_d, br_d, w1_d, w3_d, w2_d, ln_d, selb_d, g2e_d, o_d = (D[k] for k in ("x1T", "wr", "brr", "ew1", "ew3", "ew2", "ln", "selb", "g2e", "x2T"))
    v3 = lambda ap: ap.rearrange("(kc p) n -> p kc n", p=128)
    if True:
        em = Em(kb); sb, ps = kb.sb, kb.ps
        wr = sb("wr", [128, 8, 64]); WR = Buf()
        brr = sb("brr", [128, 36]); BRR = Buf()
        ln = sb("ln", [128, 16]); LN = Buf()
        selb = sb("selb", [32, 32, 128], BF16); SELB = Buf()
        g2e = sb("g2e", [128, 4, 32]); G2E = Buf()
        ones = sb("ones", [128, 128]); ONES = Buf()
        ident = sb("ident", [128, 128]); IDENT = Buf()
        xf = sb("xf", [128, 8, 512]); XF = Buf()
        x1b = sb("x1b", [128, 8, NT], BF16); X1B = [Buf() for _ in range(4)]
        out = sb("out", [128, 8, NT]); OUT = [Buf() for _ in range(4)]
        cwt = sb("cwt", [32, NT], BF16); CWT = [Buf() for _ in range(4)]
        lgt = sb("lgt", [128, 36]); LGT = Buf()
        rs = sb("rs", [128, 64]); RS = Buf()
        em32 = sb("em32", [128, 32]); EM32 = Buf()
        em2 = sb("em2", [128, 32]); EM2 = Buf()
        cw = sb("cw", [128, 32]); CW = Buf()
        w1 = [sb(f"w1_{i}", [128, 8, 512], BF16) for i in range(2)]; W1 = [Buf() for _ in range(2)]
        w3 = [sb(f"w3_{i}", [128, 8, 512], BF16) for i in range(2)]; W3 = [Buf() for _ in range(2)]
        w2 = [sb(f"w2_{i}", [128, 4, 1024], BF16) for i in range(2)]; W2 = [Buf() for _ in range(2)]
        cwb = [sb(f"cwb{i}", [128, 512]) for i in range(2)]; CWB = [Buf() for _ in range(2)]
        sl_ = [sb(f"sl{i}", [128, 512]) for i in range(2)]; SL = [Buf() for _ in range(2)]
        hb = [sb(f"hb{i}", [128, 4, 512], BF16) for i in range(2)]; HB = [[Buf() for _ in range(4)] for _ in range(2)]
        ob = xf; OB = XF
        tmp = sb("tmp", [128, 512]); TMP = Buf()
        sq = [sb(f"sq{i}", [128, 512]) for i in range(2)]; SQ = [Buf() for _ in range(2)]
        mean = sb("mean", [128, 512]); MEAN = Buf()
        rstd = sb("rstd", [128, 512]); RSTD = Buf()
        pg = [ps(f"pg{i}", [128, 512]) for i in range(8)]; PG = [Buf(excl=True) for _ in range(8)]
        gi = [0]

        def gbank():
            i = gi[0] % 8; gi[0] += 1
            return pg[i], PG[i]
        em.ms("dve", wr[:], 0.0, [WR])
        em.dma("sp", "wr", wr[:, :, 0:36], v3(wr_d), [WR], [WR])
        em.dma("sp", "brr", brr[:], br_d[0:1, :].partition_broadcast(128), [], [BRR])
        em.dma("sp", "ln", ln[:], ln_d[:, :], [], [LN])
        em.dma("pool", "selb", selb[:].rearrange("p a b -> p (a b)"), selb_d[:, :], [], [SELB])
        em.dma("sp", "g2e", g2e[:].rearrange("p a b -> p (a b)"), g2e_d[:, :], [], [G2E])
        em.ms("dve", ones[:], 1.0, [ONES])
        em.ms("pool", ident[:], 1.0, [IDENT])
        kb.op("pool", lambda e: e.affine_select(out=ident[:], in_=ident[:], pattern=[[-1, 128]], compare_op=ALU.is_equal,
                                                fill=0.0, base=0, channel_multiplier=1), [IDENT], [IDENT])

        def load_w(e):
            s = e % 2
            for k0 in range(0, 8, 4):
                em.dma("pool", f"w1_{s}", w1[s][:, k0:k0 + 4, :], w1_d[e].rearrange("(kc p) n -> p kc n", p=128)[:, k0:k0 + 4, :], [], [W1[s]])
                em.dma("pool", f"w3_{s}", w3[s][:, k0:k0 + 4, :], w3_d[e].rearrange("(kc p) n -> p kc n", p=128)[:, k0:k0 + 4, :], [], [W3[s]])
            for k0 in range(0, 4, 2):
                em.dma("pool", f"w2_{s}", w2[s][:, k0:k0 + 2, :], w2_d[e].rearrange("(kc p) n -> p kc n", p=128)[:, k0:k0 + 2, :], [], [W2[s]])

        load_w(0)
        for tg in range(4):
            ts_ = slice(tg * 512, (tg + 1) * 512)
            em.dma("sp", "xf", xf[:, :, :], v3(x1_d)[:, :, ts_], [], [XF])
            for i in range(8):
                em.cp("pool" if i % 2 else "dve", x1b[:, i, ts_], xf[:, i, :], [XF], [X1B[tg]])
                em.ts("dve" if i % 2 else "pool", out[:, i, ts_], xf[:, i, :], ALPHA, None, ALU.mult, None, [XF], [OUT[tg]])
            for tt_ in range(4):
                p, P = gbank()
                for k in range(8):
                    em.mm(p[:, 0:36], xf[:, k, tt_ * 128:(tt_ + 1) * 128], wr[:, k, 0:36], k == 0, k == 7, [XF, WR], [P])
                em.tt("dve", lgt[:], p[:, 0:36], brr[:], ALU.add, [P, BRR], [LGT])
                em.red(rs[:, 0:1], lgt[:, 0:4], ALU.max, [LGT], [RS])
                em.ts("dve", rs[:, 1:2], rs[:, 0:1], -1.0, None, ALU.mult, None, [RS], [RS])
                em.ms("dve", rs[:, 2:3], 0.0, [RS])
                em.act(rs[:, 4:8], lgt[:, 0:4], AF.Exp, [LGT, RS], [RS], bias=rs[:, 1:2], accum_out=rs[:, 2:3])
                em.rcp(rs[:, 3:4], rs[:, 2:3], [RS], [RS])
                em.ts("dve", rs[:, 8:12], lgt[:, 0:4], rs[:, 0:1], None, ALU.is_ge, None, [LGT, RS], [RS])
                em.ts("dve", em32[:], g2e[:, 0, :], rs[:, 8:9], None, ALU.mult, None, [G2E, RS], [EM32])
                for g in range(1, 4):
                    em.stt("dve", em32[:], g2e[:, g, :], rs[:, 8 + g:9 + g], em32[:], ALU.mult, ALU.add, [G2E, RS, EM32], [EM32])
                em.tt("dve", em2[:], lgt[:, 4:36], em32[:], ALU.mult, [LGT, EM32], [EM2])
                em.ts("dve", em32[:], em32[:], -1.0, 1.0e9, ALU.add, ALU.mult, [EM32], [EM32])
                em.tt("dve", em2[:], em2[:], em32[:], ALU.add, [EM2, EM32], [EM2])
                em.red(rs[:, 12:13], em2[:], ALU.max, [EM2], [RS])
                em.ts("dve", cw[:], em2[:], rs[:, 12:13], None, ALU.is_ge, None, [EM2, RS], [CW])
                em.stt("dve", em32[:], cw[:], -2.0e9, em2[:], ALU.mult, ALU.add, [CW, EM2], [EM32])
                em.red(rs[:, 13:14], em32[:], ALU.max, [EM32], [RS])
                em.ts("dve", em32[:], em32[:], rs[:, 13:14], None, ALU.is_ge, None, [EM32, RS], [EM32])
                em.tt("dve", rs[:, 14:15], rs[:, 13:14], rs[:, 12:13], ALU.subtract, [RS], [RS])
                em.act(rs[:, 15:16], rs[:, 14:15], AF.Exp, [RS], [RS])
                em.ts("dve", rs[:, 15:16], rs[:, 15:16], 1.0, None, ALU.add, None, [RS], [RS])
                em.rcp(rs[:, 16:17], rs[:, 15:16], [RS], [RS])
                em.tt("dve", rs[:, 17:18], rs[:, 16:17], rs[:, 3:4], ALU.mult, [RS], [RS])
                em.tt("dve", rs[:, 18:19], rs[:, 3:4], rs[:, 17:18], ALU.subtract, [RS], [RS])
                em.ts("dve", cw[:], cw[:], rs[:, 17:18], None, ALU.mult, None, [CW, RS], [CW])
                em.stt("dve", cw[:], em32[:], rs[:, 18:19], cw[:], ALU.mult, ALU.add, [EM32, RS, CW], [CW])
                pt_, PT_ = gbank()
                em.tr(pt_[0:32, 0:128], cw[:], ident[:], [CW, IDENT], [PT_])
                em.cp("dve", cwt[:, tg * 512 + tt_ * 128: tg * 512 + (tt_ + 1) * 128], pt_[0:32, 0:128], [PT_], [CWT[tg]])
        tiles = [(e, tg) for e in range(nexp) for tg in range(4)]

        def stage1(ti):
            e, tg = tiles[ti]
            s = e % 2
            hbuf = ti % 2
            ts_ = slice(tg * 512, (tg + 1) * 512)
            pc_, PC_ = gbank()
            em.mm(pc_[:, :], selb[:, e, :], cwt[:, ts_], True, True, [SELB, CWT[tg]], [PC_])
            em.act(cwb[hbuf][:], pc_[:, :], AF.Copy, [PC_], [CWB[hbuf]])
            for f in range(4):
                fs = slice(f * 128, (f + 1) * 128)
                pa, PA = gbank()
                for k in range(8):
                    em.mm(pa[:, :], w1[s][:, k, fs], x1b[:, k, ts_], k == 0, k == 7, [W1[s], X1B[tg]], [PA])
                pb, PB = gbank()
                for k in range(8):
                    em.mm(pb[:, :], w3[s][:, k, fs], x1b[:, k, ts_], k == 0, k == 7, [W3[s], X1B[tg]], [PB])
                em.act(sl_[f % 2][:], pa[:, :], AF.Silu, [PA], [SL[f % 2]])
                em.tt("dve", sl_[f % 2][:], sl_[f % 2][:], pb[:, :], ALU.mult, [SL[f % 2], PB], [SL[f % 2]])
                em.tt("pool", hb[hbuf][:, f, :], sl_[f % 2][:], cwb[hbuf][:], ALU.mult, [SL[f % 2], CWB[hbuf]], [HB[hbuf][f]])

        def stage2(ti):
            e, tg = tiles[ti]
            s = e % 2
            hbuf = ti % 2
            ts_ = slice(tg * 512, (tg + 1) * 512)
            for i in range(8):
                po, PO = gbank()
                for f in range(4):
                    em.mm(po[:, :], w2[s][:, f, i * 128:(i + 1) * 128], hb[hbuf][:, f, :], f == 0, f == 3, [W2[s], HB[hbuf][f]], [PO])
                em.tt("dve", out[:, i, ts_], out[:, i, ts_], po[:, :], ALU.add, [OUT[tg], PO], [OUT[tg]])
        if nexp > 1:
            load_w(1)
        if tiles:
            stage1(0)
        for ti in range(len(tiles)):
            if ti + 1 < len(tiles):
                stage1(ti + 1)
            stage2(ti)
            e_, tg_ = tiles[ti]
            if tg_ == 3 and e_ + 2 < nexp:
                load_w(e_ + 2)
        for tg in range(4):
            ts_ = slice(tg * 512, (tg + 1) * 512)
            layer_norm_fm(kb, em, gbank, out[:, :, ts_], OUT[tg], lambda i: (ob[:, i, :], OB), ln[:, 0:8], ln[:, 8:16], LN, tmp, TMP,
                          ones, ONES, sq, SQ, mean, MEAN, rstd, RSTD)
            em.dma("sp", "ob", v3(o_d)[:, :, ts_], ob[:, :, :], [OB], [])
        kb.finish([OB])


def build_b2(nexp=32):
    nc = bass.Bass("TRN2", target_bir_lowering=False)
    I = lambda n, shp: nc.dram_tensor(n, shp, F32, kind="ExternalInput").ap()
    D = dict(x1T=I("x1T", [1024, NT]), wr=I("wr", [1024, 36]), brr=I("brr", [1, 36]), ew1=I("ew1", [32, 1024, 512]), ew3=I("ew3", [32, 1024, 512]),
             ew2=I("ew2", [32, 512, 1024]), ln=I("ln", [128, 16]), selb=I("selb", [32, 32 * 128]), g2e=I("g2e", [128, 4 * 32]),
             x2T=nc.dram_tensor("x2T", [1024, NT], F32, kind="ExternalOutput").ap())
    with ExitStack() as st:
        kb = KB(nc, st)
        b2_body(nc, kb, D, nexp)
        kb.emit()
    return nc


def b2_consts():
    selb = np.zeros((32, 32, 128), np.float32)
    for e in range(32):
        selb[e, e, :] = 1.0
    g2e = np.zeros((128, 4, 32), np.float32)
    for g in range(4):
        g2e[:, g, g * 8:(g + 1) * 8] = 1.0
    return dict(selb=selb.reshape(32, 32 * 128), g2e=g2e.reshape(128, 128))


def b2_inputs(inp, l, consts):
    wr = np.concatenate([inp["router_group_w"][l], inp["router_expert_w"][l]], axis=1)
    brr = np.concatenate([inp["router_group_b"][l], inp["router_expert_b"][l]])[None, :]
    lnp = np.concatenate([inp["ln2_g"][l].reshape(8, 128).T, inp["ln2_b"][l].reshape(8, 128).T], axis=1)
    return dict(wr=np.ascontiguousarray(wr), brr=np.ascontiguousarray(brr), ew1=inp["exp_w1"][l], ew3=inp["exp_w3"][l], ew2=inp["exp_w2"][l],
                ln=np.ascontiguousarray(lnp, dtype=np.float32), selb=consts["selb"], g2e=consts["g2e"])


L_ = 4


def build_fused(nl=L_):
    nc = bass.Bass("TRN2", target_bir_lowering=False)

    def I(n, shp):
        return nc.dram_tensor(n, list(shp), F32, kind="ExternalInput").ap()

    def T(n, shp):
        return nc.dram_tensor(n, list(shp), F32, kind="Internal").ap()
    x0T = I("x0T", [1024, S])
    a1w = I("a1_w", [nl, 2, 1024, 1024]); a1vec = I("a1_vec", [nl, 2, 128, 22]); a1lw = I("a1_lw", [nl, 2, 128, 256])
    a1g2 = I("a1_g2", [nl, 2, 128, 256]); a1cst = I("a1_cst", [128, 1280])
    a2wf = I("a2_wf", [nl, 2, 1024, 640]); a2wt = I("a2_wt", [nl, 2, 1024, 140]); a2w1 = I("a2_w1", [nl, 128, 32 * 128])
    a2pe = I("a2_peT", [nl, 128, 32]); a2w2 = I("a2_w2", [nl, 128, 192]); a2btc = I("a2_btc", [2, 128, 4 * 256]); a2bts = I("a2_bts", [2, 128, 4 * TW])
    a2c = {k: I("a2_" + k, shp) for k, shp in (("mkc", [128, 256]), ("mks", [128, TW]), ("mkw", [128, TW]), ("m12", [128, 256]), ("rv", [128, 1]),
                                                 ("c2s", [128, 128]), ("exd", [64, 32 * 128]), ("hsel", [128, 256]))}
    b1wg = I("b1_wg", [nl, 1024, 2048]); b1wur = I("b1_wur", [nl, 512, 1024]); b1wun = I("b1_wun", [nl, 512, 1024]); b1wo = I("b1_wo", [nl, 1024, 1024])
    b1ln = I("b1_ln", [nl, 128, 16])
    b2wr = I("b2_wr", [nl, 1024, 36]); b2br = I("b2_brr", [nl, 1, 36]); b2e1 = I("b2_ew1", [nl, 32, 1024, 512]); b2e3 = I("b2_ew3", [nl, 32, 1024, 512])
    b2e2 = I("b2_ew2", [nl, 32, 512, 1024]); b2ln = I("b2_ln", [nl, 128, 16]); b2selb = I("b2_selb", [32, 32 * 128]); b2g2e = I("b2_g2e", [128, 128])
    outT = nc.dram_tensor("outT", [1024, S], F32, kind="ExternalOutput").ap()
    XT = [T("xt0", [1024, S]), T("xt1", [1024, S])]
    YR = T("yr", [512, S]); YN = T("yn", [512, S]); X1 = T("x1", [1024, S])

    with ExitStack() as st:
        kb = KB(nc, st)
        pn = [0]

        def phase(fn, D, *args):
            with ExitStack() as pst:
                kb.pstack = pst
                kb.prefix = f"p{pn[0]}_"
                pn[0] += 1
                fn(nc, kb, D, *args)
                kb.emit()
            kb.pstack = st
        for l in range(nl):
            xin = x0T if l == 0 else XT[l % 2]
            xout = outT if l == nl - 1 else XT[(l + 1) % 2]
            for hh in range(2):
                phase(a1_body, dict(xT=xin, w=a1w[l, hh], vec=a1vec[l, hh], lw=a1lw[l, hh], g2=a1g2[l, hh], cst=a1cst,
                                    yT=YR[hh * 256:(hh + 1) * 256, :]))
            for g in range(2):
                D = dict(xT=xin, wf=a2wf[l, g], wt=a2wt[l, g], w1=a2w1[l], peT=a2pe[l], w2=a2w2[l], btc=a2btc[g], bts=a2bts[g],
                         yT=YN[g * 256:(g + 1) * 256, :])
                D.update(a2c)
                phase(a2_body, D)
            for hf in range(2):
                ts = slice(hf * NT, (hf + 1) * NT)
                phase(b1_body, dict(xT=xin[:, ts], yrT=YR[:, ts], ynT=YN[:, ts], wg=b1wg[l], wur=b1wur[l], wun=b1wun[l], wo=b1wo[l], ln=b1ln[l],
                                    x1T=X1[:, ts]))
            for hf in range(2):
                ts = slice(hf * NT, (hf + 1) * NT)
                phase(b2_body, dict(x1T=X1[:, ts], wr=b2wr[l], brr=b2br[l], ew1=b2e1[l], ew3=b2e3[l], ew2=b2e2[l], ln=b2ln[l], selb=b2selb,
                                    g2e=b2g2e, x2T=xout[:, ts]))
        print("FUSED instructions:", kb.n_ins, kb.cnt)
    return nc


def fused_inputs(inp, nl=L_):
    c2 = a2_consts(); cb2 = b2_consts()
    a1 = [[a1_inputs(inp, l, 0, hh) for hh in range(2)] for l in range(nl)]
    a2 = [[a2_inputs(inp, l, g, c2) for g in range(2)] for l in range(nl)]
    b1 = [b1_inputs(inp, l) for l in range(nl)]
    b2 = [b2_inputs(inp, l, cb2) for l in range(nl)]
    st = lambda f: np.ascontiguousarray(np.stack(f, axis=0), dtype=np.float32)
    m = {}
    for k, nm in (("w", "a1_w"), ("vec", "a1_vec"), ("lw", "a1_lw"), ("g2", "a1_g2")):
        m[nm] = st([st([a1[l][hh][k] for hh in range(2)]) for l in range(nl)])
    m["a1_cst"] = a1[0][0]["cst"]
    for k, nm in (("wf", "a2_wf"), ("wt", "a2_wt")):
        m[nm] = st([st([a2[l][g][k] for g in range(2)]) for l in range(nl)])
    for k, nm in (("w1", "a2_w1"), ("peT", "a2_peT"), ("w2", "a2_w2")):
        m[nm] = st([a2[l][0][k] for l in range(nl)])
    m["a2_btc"] = st([a2[0][g]["btc"] for g in range(2)]); m["a2_bts"] = st([a2[0][g]["bts"] for g in range(2)])
    for k in ("mkc", "mks", "mkw", "m12", "rv", "c2s", "exd", "hsel"):
        m["a2_" + k] = a2[0][0][k]
    for k, nm in (("wg", "b1_wg"), ("wur", "b1_wur"), ("wun", "b1_wun"), ("wo", "b1_wo"), ("ln", "b1_ln")):
        m[nm] = st([b1[l][k] for l in range(nl)])
    for k, nm in (("wr", "b2_wr"), ("brr", "b2_brr"), ("ln", "b2_ln")):
        m[nm] = st([b2[l][k] for l in range(nl)])
    m["b2_ew1"] = np.ascontiguousarray(inp["exp_w1"][:nl], dtype=np.float32)
    m["b2_ew3"] = np.ascontiguousarray(inp["exp_w3"][:nl], dtype=np.float32)
    m["b2_ew2"] = np.ascontiguousarray(inp["exp_w2"][:nl], dtype=np.float32)
    m["b2_selb"] = cb2["selb"]; m["b2_g2e"] = cb2["g2e"]
    return m

_NC = {}


def kernel(**inputs):
    inp = {k: np.asarray(v) for k, v in inputs.items()}
    if "nc" not in _NC:
        _NC["nc"] = build_fused(L_)
    m = fused_inputs(inp, L_)
    x = inp["x"].astype(np.float32, copy=False)
    B = x.shape[0]
    xT = [np.ascontiguousarray(x[b].T) for b in range(B)]
    maps = []
    for c in range(8):
        mm_ = dict(m); mm_["x0T"] = xT[c // 2]; maps.append(mm_)
    res = run_bass_kernel_spmd(_NC["nc"], maps, core_ids=list(range(8)))
    out = np.stack([res.results[2 * b]["outT"].T for b in range(B)], axis=0)
    return np.ascontiguousarray(out, dtype=np.float32)
```

```python
import numpy as np
from contextlib import ExitStack
import concourse.bass as bass
import concourse.mybir as mybir
from concourse.bass_utils import run_bass_kernel_spmd

F32 = mybir.dt.float32
BF16 = mybir.dt.bfloat16
AF = mybir.ActivationFunctionType
ALU = mybir.AluOpType
AX = mybir.AxisListType


class Buf:
    __slots__ = ("name", "w", "r", "excl")

    def __init__(self, name="", excl=False):
        self.name = name
        self.excl = excl
        self.w = None
        self.r = {}


class KB:
    def __init__(self, nc, stack):
        self.nc = nc
        self.stack = stack
        self.pstack = stack
        self.prefix = ""
        self.names = ["pe", "act", "dve", "pool", "sp"]
        self.prog = {e: [] for e in self.names}
        self.sem = {e: stack.enter_context(nc.semaphore("s_" + e)) for e in self.names}
        self.cnt = {e: 0 for e in self.names}
        self.seen = {e: {} for e in self.names}
        self.dsem = {}
        self.n_ins = 0
        self.nw = {e: 0 for e in self.names}

    def sb(self, name, shape, dt=F32):
        return self.pstack.enter_context(self.nc.sbuf_tensor(self.prefix + "sb_" + name, list(shape), dt))

    def ps(self, name, shape, dt=F32):
        return self.pstack.enter_context(self.nc.psum_tensor(self.prefix + "ps_" + name, list(shape), dt))

    def _semh(self, key):
        if key in self.sem:
            return self.sem[key]
        return self.dsem[key][0]

    def _waits(self, eng, reads, writes):
        need = {}

        def add(d):
            if d is None:
                return
            k, v = d
            if need.get(k, 0) < v:
                need[k] = v
        for b in reads:
            add(b.w)
        for b in writes:
            add(b.w)
            for k, v in b.r.items():
                add((k, v))
        out = []
        seen = self.seen[eng]
        for k, v in need.items():
            if k == "pe" and eng == "pe":
                continue
            if seen.get(k, 0) >= v:
                continue
            seen[k] = v
            out.append((self._semh(k), v))
        return out

    def _mark(self, tok, reads, writes):
        for b in writes:
            b.w = tok
            b.r = {}
        k, v = tok
        for b in reads:
            if b.r.get(k, 0) < v:
                b.r[k] = v

    def op(self, eng, fn, reads=(), writes=()):
        ex = [b for b in reads if b.excl]
        if ex:
            writes = list(writes) + ex
        waits = self._waits(eng, reads, writes)
        self.nw[eng] += len(waits)
        self.cnt[eng] += 1
        tok = (eng, self.cnt[eng])
        sem = self.sem[eng]

        def run(e, waits=waits, fn=fn, sem=sem):
            for s, v in waits:
                e.wait_ge(s, v)
            fn(e).then_inc(sem, 1)
        self.prog[eng].append(run)
        self._mark(tok, reads, writes)
        self.n_ins += 1

    def dma(self, q, key, fn, reads=(), writes=(), n=1):
        key = "d_" + key
        if key not in self.dsem:
            self.dsem[key] = [self.stack.enter_context(self.nc.semaphore(key)), 0]
        waits = self._waits(q, reads, writes)
        self.dsem[key][1] += 16 * n
        tok = (key, self.dsem[key][1])
        sem = self.dsem[key][0]

        def run(e, waits=waits, fn=fn, sem=sem):
            for s, v in waits:
                e.wait_ge(s, v)
            fn(e, sem)
        self.prog[q].append(run)
        self._mark(tok, reads, writes)
        self.n_ins += n

    def finish(self, bufs):
        waits = self._waits("sp", bufs, bufs)

        def run(e, waits=waits):
            for s, v in waits:
                e.wait_ge(s, v)
        self.prog["sp"].append(run)

    def emit(self):
        nc = self.nc
        prog = self.prog
        self.prog = {e: [] for e in self.names}
        with nc.Block() as block:
            @block.sync
            def _(e):
                for f in prog["sp"]:
                    f(e)

            @block.tensor
            def _(e):
                for f in prog["pe"]:
                    f(e)

            @block.scalar
            def _(e):
                for f in prog["act"]:
                    f(e)

            @block.vector
            def _(e):
                for f in prog["dve"]:
                    f(e)

            @block.gpsimd
            def _(e):
                for f in prog["pool"]:
                    f(e)


S = 4096
NS = 512
NSEG = S // NS
CH = 128
NCH = NS // CH
GN_EPS = 64e-5


def a1_body(nc, kb, D, nseg=NSEG, debug=False):
    Em_ = globals().get('Em')
    if Em_ is None:
        from a2 import Em as Em_
    xT_d, w_d, vec_d, lw_d, g2_d, cst_d, yT_d = (D[k] for k in ("xT", "w", "vec", "lw", "g2", "cst", "yT"))
    xT_v = xT_d.rearrange("(kc p) t -> p kc t", p=128)
    w_v = w_d.rearrange("(kc p) n -> p kc n", p=128)
    if True:
        sb, ps = kb.sb, kb.ps
        NXS = 3
        xs = [sb(f"xs{i}", [128, 8, NS], BF16) for i in range(NXS)]; XS = [Buf() for _ in range(NXS)]
        wsb = sb("wsb", [128, 8, 1024], BF16); WSB = Buf()
        vec = sb("vec", [128, 22]); VEC = Buf()
        vx = sb("vx", [128, 8]); VX = Buf()
        lw = sb("lw", [128, 256]); LW = Buf()
        g2 = sb("g2", [128, 256]); G2 = Buf()
        cst = sb("cst", [128, 1280]); CST = Buf()
        MASK1 = cst[:, 0:512]; MASK4 = cst[:, 512:1024]; MSL = cst[:, 1024:1152]; BONES = cst[:, 1152:1280]
        ident = sb("ident", [128, 128]); IDENT = Buf()
        car = sb("car", [128, 8]); CAR = Buf()
        zr = [sb(f"zr{i}", [128, NS + 1]) for i in range(2)]; ZR = [Buf() for _ in range(2)]
        dtmp = sb("dtmp", [128, NS]); DTMP = Buf()
        zs = [sb(f"zs{j}", [128, NS]) for j in range(8)]; ZS = [Buf() for _ in range(8)]
        tw = sb("tw", [128, NS]); TW = Buf()
        sg = sb("sg", [128, NS]); SG = Buf()
        tnames = ["nld", "cw", "ew", "ewi", "ewx", "aa", "kkn", "sq", "t1", "k2", "bh", "kh", "e1"]
        T = {n: sb("t_" + n, [128, NS]) for n in tnames}; TB = {n: Buf() for n in tnames}
        ar = [sb(f"ar{h}", [128, NCH, 2 * CH]) for h in range(2)]; AR = [Buf() for _ in range(2)]
        bt = [sb(f"bt{h}", [128, NS]) for h in range(2)]; BT = [Buf() for _ in range(2)]
        kt = [sb(f"kt{h}", [128, NS]) for h in range(2)]; KT = [Buf() for _ in range(2)]
        gg = [sb(f"gg{h}", [128, NS]) for h in range(2)]; GG = [Buf() for _ in range(2)]
        bon = [sb(f"bon{h}", [128, NS]) for h in range(2)]; BON = [Buf() for _ in range(2)]
        yf = [sb(f"yf{h}", [128, NS]) for h in range(2)]; YF = [Buf() for _ in range(2)]
        wc = [sb(f"wc{h}", [128, NCH]) for h in range(2)]; WC = [Buf() for _ in range(2)]
        bhT = sb("bhT", [128, NCH, 256]); BHT = [Buf() for _ in range(NCH)]
        khT = sb("khT", [128, NCH, 256]); KHT = [Buf() for _ in range(NCH)]
        vT = sb("vT", [128, NCH, 256]); VT = [Buf() for _ in range(NCH)]
        mabk = [sb(f"mabk{h}", [128, 512]) for h in range(4)]; MABK = [Buf() for _ in range(4)]
        nm = [[sb(f"nm{h}_{i}", [128, 256]) for i in range(2)] for h in range(4)]; NM = [[Buf(), Buf()] for _ in range(4)]
        qq = [[sb(f"qq{h}_{i}", [128, 128]) for i in range(2)] for h in range(4)]; QQ = [[Buf(), Buf()] for _ in range(4)]
        xsb = [sb(f"xsb{h}", [128, 64]) for h in range(4)]; XSB = [Buf() for _ in range(4)]
        usb = [sb(f"usb{h}", [128, 64]) for h in range(4)]; USB = [Buf() for _ in range(4)]
        stt = [[sb(f"st{hp}_{i}", [128, 64]) for i in range(2)] for hp in range(2)]
        STT = [[[Buf(), Buf()] for _ in range(2)] for hp in range(2)]
        ytok = sb("ytok", [128, 256]); YTOK = Buf()
        ysq = sb("ysq", [128, 256]); YSQ = Buf()
        yn = sb("yn", [128, 256]); YN = Buf()
        sts = sb("sts", [128, 32]); STS = Buf()
        osb = [sb(f"osb{h}", [128, NS]) for h in range(2)]; OSB = [Buf() for _ in range(2)]
        NPA = 4
        pa = [ps(f"pa{i}", [128, 512]) for i in range(NPA)]; PA = [Buf(excl=True) for _ in range(NPA)]
        pq = [ps(f"pq{i}", [128, 512]) for i in range(4)]; PQB = [Buf(excl=True) for _ in range(4)]

        def ld(q, key, out, in_, B):
            kb.dma(q, key, lambda e, s: e.dma_start(out=out, in_=in_).then_inc(s, 16), writes=[B])
        ld("sp", "vec", vec[:], vec_d[:, :], VEC)
        ld("sp", "lw", lw[:], lw_d[:, :], LW)
        ld("sp", "g2", g2[:], g2_d[:, :], G2)
        ld("sp", "cst", cst[:], cst_d[:, :], CST)
        for kc in range(0, 8, 4):
            kb.dma("pool", "wsb", lambda e, s, kc=kc: e.dma_start(out=wsb[:, kc:kc + 4, :], in_=w_v[:, kc:kc + 4, :]).then_inc(s, 16), writes=[WSB])
        kb.op("pool", lambda e: e.memset(ident[:], 1.0), writes=[IDENT])
        kb.op("pool", lambda e: e.affine_select(out=ident[:], in_=ident[:], pattern=[[-1, 128]], compare_op=ALU.is_equal,
                                                fill=0.0, base=0, channel_multiplier=1), reads=[IDENT], writes=[IDENT])
        kb.op("dve", lambda e: e.memset(car[:], 0.0), writes=[CAR])
        kb.op("dve", lambda e: e.tensor_scalar(vx[:, 0:2], vec[:, 8:10], -1.0, None, ALU.mult), reads=[VEC], writes=[VX])
        kb.op("dve", lambda e: e.tensor_scalar(vx[:, 2:4], vec[:, 14:16], -1.0, 1.0, ALU.mult, ALU.add), reads=[VEC, VX], writes=[VX])
        for hp in range(2):
            for i in range(2):
                kb.op("dve", lambda e, hp=hp, i=i: e.memset(stt[hp][i][:], 0.0), writes=STT[hp][i])

        def load_x(sgi):
            sl = sgi % NXS
            kb.dma("pool", f"xs{sl}", lambda e, s, sl=sl, sgi=sgi: e.dma_start(
                out=xs[sl][:, :, :], in_=xT_v[:, :, sgi * NS:(sgi + 1) * NS]).then_inc(s, 16), writes=[XS[sl]])

        load_x(0)
        pai = [0]

        def next_pa():
            i = pai[0] % NPA
            pai[0] += 1
            return pa[i], PA[i]

        def mm512(lhsT_fn, rhs_fn, nk, reads, M=128):
            p, P = next_pa()
            for k in range(nk):
                a_, b_ = lhsT_fn(k), rhs_fn(k)
                kb.op("pe", lambda e, k=k, p=p, a_=a_, b_=b_: e.matmul(p[0:M, :], a_, b_, start=(k == 0), stop=(k == nk - 1)),
                      reads=reads, writes=[P])
            return p, P

        ping = [0, 0]
        dbg_n = [0]

        def dbg(name, ap, B):
            if not debug:
                return
            shp = list(ap.shape)
            d = nc.dram_tensor("dbg_" + name, shp, F32, kind="ExternalOutput").ap()
            dbg_n[0] += 1
            cntv = dbg_n[0] * 16

            def f(e, s, d=d, ap=ap, cntv=cntv):
                e.dma_start(out=d, in_=ap).then_inc(s, 16)
                e.wait_ge(s, cntv)
            kb.dma("sp", "dbg", f, reads=[B])
        for sgi in range(nseg):
            sl = sgi % NXS
            if sgi + 1 < nseg:
                load_x(sgi + 1)
            for j in range(8):
                p, P = mm512(lambda k, j=j: wsb[:, k, j * 128:(j + 1) * 128], lambda k, sl=sl: xs[sl][:, k, :], 8, [WSB, XS[sl]])
                z, Z = zr[j % 2], ZR[j % 2]
                kb.op("pool", lambda e, z=z, j=j: e.tensor_copy(z[:, 0:1], car[:, j:j + 1]), reads=[CAR], writes=[Z])
                kb.op("act", lambda e, z=z, p=p: e.activation(out=z[:, 1:NS + 1], in_=p[:, :], func=AF.Copy), reads=[P], writes=[Z])
                kb.op("pool", lambda e, z=z, j=j: e.tensor_copy(car[:, j:j + 1], z[:, NS:NS + 1]), reads=[Z], writes=[CAR])
                kb.op("dve", lambda e, z=z: e.tensor_tensor(dtmp[:], z[:, 0:NS], z[:, 1:NS + 1], ALU.subtract), reads=[Z], writes=[DTMP])
                kb.op("dve", lambda e, z=z, j=j: e.scalar_tensor_tensor(zs[j][:], dtmp[:], vec[:, j:j + 1], z[:, 1:NS + 1], ALU.mult, ALU.add),
                      reads=[DTMP, Z, VEC], writes=[ZS[j]])
            L1, L2 = zs[6], zs[7]
            for j in range(8):
                dbg(f"zs{j}", zs[j][:], ZS[j])
            kb.op("act", lambda e: e.activation(out=tw[0:64, :], in_=L1[0:64, :], func=AF.Tanh), reads=[ZS[6]], writes=[TW])
            kb.op("act", lambda e: e.activation(out=sg[:], in_=L2[:], func=AF.Sigmoid), reads=[ZS[7]], writes=[SG])
            for hp in range(2):
                Rz, Kz, Vz = zs[0 + hp], zs[2 + hp], zs[4 + hp]
                RZ, KZ, VZ = ZS[0 + hp], ZS[2 + hp], ZS[4 + hp]
                cs = slice(hp * 128, (hp + 1) * 128)
                p, P = mm512(lambda k: lw[64:128, cs], lambda k: L1[64:128, :], 1, [LW, ZS[6]])
                kb.op("act", lambda e, p=p, hp=hp: e.activation(out=T["aa"][:], in_=p[:, :], func=AF.Sigmoid, bias=vec[:, 10 + hp:11 + hp]),
                      reads=[P, VEC], writes=[TB["aa"]])
                p, P = mm512(lambda k: g2[:, cs], lambda k: sg[:], 1, [G2, SG])
                kb.op("act", lambda e, p=p, hp=hp: e.activation(out=gg[hp][:], in_=p[:, :], func=AF.Copy), reads=[P], writes=[GG[hp]])
                p, P = mm512(lambda k: lw[0:64, cs], lambda k: tw[0:64, :], 1, [LW, TW])
                kb.op("act", lambda e, p=p, hp=hp: e.activation(out=T["e1"][:], in_=p[:, :], func=AF.Exp, bias=vx[:, hp:hp + 1], scale=-1.0),
                      reads=[P, VX], writes=[TB["e1"]])
                kb.op("act", lambda e: e.activation(out=T["e1"][:], in_=T["e1"][:], func=AF.Ln, bias=1.0), reads=[TB["e1"]], writes=[TB["e1"]])
                kb.op("act", lambda e: e.activation(out=T["nld"][:], in_=T["e1"][:], func=AF.Exp, bias=-0.5, scale=-1.0),
                      reads=[TB["e1"]], writes=[TB["nld"]])
                kb.op("dve", lambda e: e.tensor_tensor_scan(T["cw"][:], MASK1, T["nld"][:], 0.0, ALU.mult, ALU.add),
                      reads=[CST, TB["nld"]], writes=[TB["cw"]])
                kb.op("act", lambda e: e.activation(out=T["ew"][:], in_=T["cw"][:], func=AF.Exp, scale=-1.0), reads=[TB["cw"]], writes=[TB["ew"]])
                kb.op("act", lambda e: e.activation(out=T["ewi"][:], in_=T["cw"][:], func=AF.Exp), reads=[TB["cw"]], writes=[TB["ewi"]])
                kb.op("pool", lambda e: e.tensor_tensor(T["ewx"][:], T["cw"][:], T["nld"][:], ALU.subtract), reads=[TB["cw"], TB["nld"]], writes=[TB["ewx"]])
                kb.op("act", lambda e: e.activation(out=T["ewx"][:], in_=T["ewx"][:], func=AF.Exp, scale=-1.0), reads=[TB["ewx"]], writes=[TB["ewx"]])
                kb.op("pool", lambda e, hp=hp: e.tensor_copy(wc[hp][:], T["ew"][:].rearrange("p (c t) -> p c t", t=CH)[:, :, CH - 1]),
                      reads=[TB["ew"]], writes=[WC[hp]])
                kb.op("dve", lambda e, hp=hp, Kz=Kz: e.tensor_scalar(T["kkn"][:], Kz[:], vec[:, 12 + hp:13 + hp], None, ALU.mult),
                      reads=[KZ, VEC], writes=[TB["kkn"]])
                kb.op("pool", lambda e: e.tensor_tensor(T["sq"][:], T["kkn"][:], T["kkn"][:], ALU.mult), reads=[TB["kkn"]], writes=[TB["sq"]])
                p, P = mm512(lambda k: BONES, lambda k: T["sq"][:], 1, [CST, TB["sq"]])
                kb.op("act", lambda e, p=p: e.activation(out=T["sq"][:], in_=p[:, :], func=AF.Sqrt), reads=[P], writes=[TB["sq"]])
                kb.op("dve", lambda e: e.tensor_scalar(T["sq"][:], T["sq"][:], 1e-12, None, ALU.max), reads=[TB["sq"]], writes=[TB["sq"]])
                kb.op("dve", lambda e: e.reciprocal(T["sq"][:], T["sq"][:]), reads=[TB["sq"]], writes=[TB["sq"]])
                kb.op("dve", lambda e: e.tensor_tensor(T["kkn"][:], T["kkn"][:], T["sq"][:], ALU.mult), reads=[TB["kkn"], TB["sq"]], writes=[TB["kkn"]])
                kb.op("pool", lambda e, hp=hp: e.tensor_scalar(T["t1"][:], T["aa"][:], vec[:, 14 + hp:15 + hp], vx[:, 2 + hp:3 + hp], ALU.mult, ALU.add),
                      reads=[TB["aa"], VEC, VX], writes=[TB["t1"]])
                kb.op("pool", lambda e, Kz=Kz: e.tensor_tensor(T["k2"][:], Kz[:], T["t1"][:], ALU.mult), reads=[KZ, TB["t1"]], writes=[TB["k2"]])
                arv = ar[hp]
                kb.op("dve", lambda e, arv=arv: e.scalar_tensor_tensor(arv[:, :, 0:CH], T["kkn"][:].rearrange("p (c t) -> p c t", t=CH), -1.0,
                                                                      T["ewx"][:].rearrange("p (c t) -> p c t", t=CH), ALU.mult, ALU.mult),
                      reads=[TB["kkn"], TB["ewx"]], writes=[AR[hp]])
                kb.op("pool", lambda e, arv=arv, Rz=Rz: e.tensor_tensor(arv[:, :, CH:2 * CH], Rz[:].rearrange("p (c t) -> p c t", t=CH),
                                                                       T["ew"][:].rearrange("p (c t) -> p c t", t=CH), ALU.mult),
                      reads=[RZ, TB["ew"]], writes=[AR[hp]])
                kb.op("dve", lambda e: e.tensor_tensor(T["t1"][:], T["kkn"][:], T["aa"][:], ALU.mult), reads=[TB["kkn"], TB["aa"]], writes=[TB["t1"]])
                kb.op("dve", lambda e, hp=hp: e.tensor_tensor(bt[hp][:], T["t1"][:], T["ewi"][:], ALU.mult), reads=[TB["t1"], TB["ewi"]], writes=[BT[hp]])
                kb.op("pool", lambda e, hp=hp: e.tensor_tensor(kt[hp][:], T["k2"][:], T["ewi"][:], ALU.mult), reads=[TB["k2"], TB["ewi"]], writes=[KT[hp]])
                wcb = wc[hp][:].unsqueeze(2).to_broadcast([128, NCH, CH])
                kb.op("dve", lambda e, hp=hp, wcb=wcb: e.tensor_tensor(T["bh"][:].rearrange("p (c t) -> p c t", t=CH),
                                                                      bt[hp][:].rearrange("p (c t) -> p c t", t=CH), wcb, ALU.mult),
                      reads=[BT[hp], WC[hp]], writes=[TB["bh"]])
                kb.op("pool", lambda e, hp=hp, wcb=wcb: e.tensor_tensor(T["kh"][:].rearrange("p (c t) -> p c t", t=CH),
                                                                       kt[hp][:].rearrange("p (c t) -> p c t", t=CH), wcb, ALU.mult),
                      reads=[KT[hp], WC[hp]], writes=[TB["kh"]])
                kb.op("dve", lambda e, hp=hp, Rz=Rz: e.scalar_tensor_tensor(T["t1"][:], Rz[:], vec[:, 16 + hp:17 + hp], T["k2"][:], ALU.mult, ALU.mult),
                      reads=[RZ, VEC, TB["k2"], TB["t1"]], writes=[TB["t1"]])
                p, P = mm512(lambda k: BONES, lambda k: T["t1"][:], 1, [CST, TB["t1"]])
                kb.op("dve", lambda e, p=p, hp=hp, Vz=Vz: e.tensor_tensor(bon[hp][:], p[:, :], Vz[:], ALU.mult), reads=[P, VZ], writes=[BON[hp]])
                for n_ in ("nld", "cw", "ew", "ewi", "ewx", "aa", "kkn", "k2", "bh", "kh"):
                    dbg(f"{n_}{hp}", T[n_][:], TB[n_])
                dbg(f"bt{hp}", bt[hp][:], BT[hp]); dbg(f"kt{hp}", kt[hp][:], KT[hp]); dbg(f"ar{hp}", ar[hp][:].rearrange("p c t -> p (c t)"), AR[hp])
                dbg(f"gg{hp}", gg[hp][:], GG[hp]); dbg(f"bon{hp}", bon[hp][:], BON[hp]); dbg(f"wc{hp}", wc[hp][:], WC[hp])
                for c in range(NCH):
                    for (src, SRC, dst, DST) in ((T["bh"], TB["bh"], bhT, BHT), (T["kh"], TB["kh"], khT, KHT), (Vz, VZ, vT, VT)):
                        pbank, PT = next_pa()
                        pt = pbank[:, 0:128]
                        kb.op("pe", lambda e, pt=pt, src=src, c=c: e.transpose(pt, src[:, c * CH:(c + 1) * CH], ident[:]),
                              reads=[SRC, IDENT], writes=[PT])
                        kb.op("act", lambda e, pt=pt, dst=dst, c=c, cs=cs: e.activation(out=dst[:, c, cs], in_=pt, func=AF.Copy),
                              reads=[PT], writes=[DST[c]])
            em = Em_(kb)
            for c in range(NCH):
                csl = slice(c * CH, (c + 1) * CH)
                HD = []
                for h in range(4):
                    hp, hh = h // 2, h % 2
                    HD.append(dict(h=h, hp=hp, hh=hh, rows=slice(64 * hh, 64 * hh + 64), hc=slice(h * 64, (h + 1) * 64), q=pq[h], Q=PQB[h]))
                for d in HD:
                    em.mm(d["q"][:, 0:256], bt[d["hp"]][d["rows"], csl], ar[d["hp"]][d["rows"], c, :], True, True, [BT[d["hp"]], AR[d["hp"]]], [d["Q"]])
                    em.mm(d["q"][:, 256:512], kt[d["hp"]][d["rows"], csl], ar[d["hp"]][d["rows"], c, :], True, True, [KT[d["hp"]], AR[d["hp"]]], [d["Q"]])
                for d in HD:
                    h = d["h"]
                    em.tt("dve", mabk[h][:], d["q"][:, :], MASK4, ALU.mult, [d["Q"], CST], [MABK[h]])
                for d in HD:
                    em.mm(d["q"][:, 0:128], ar[d["hp"]][d["rows"], c, 0:CH], bt[d["hp"]][d["rows"], csl], True, True, [BT[d["hp"]], AR[d["hp"]]], [d["Q"]])
                for d in HD:
                    h = d["h"]
                    em.tt("dve", nm[h][0][:, 0:128], d["q"][:, 0:128], MSL, ALU.mult, [d["Q"], CST], [NM[h][0]])
                    em.cp("pool", nm[h][0][:, 128:256], mabk[h][:, 0:128], [MABK[h]], [NM[h][0]])
                    em.tt("pool", qq[h][0][:], mabk[h][:, 0:128], ident[:], ALU.add, [MABK[h], IDENT], [QQ[h][0]])
                for k in range(6):
                    a_, b_ = k % 2, (k + 1) % 2
                    wdt = 256 if k < 5 else 128
                    for d in HD:
                        h = d["h"]
                        em.mm(d["q"][:, 0:128], nm[h][a_][:, 128:256], nm[h][a_][:, 0:128], True, True, [NM[h][a_]], [d["Q"]])
                        if k < 5:
                            em.mm(d["q"][:, 128:256], nm[h][a_][:, 0:128], nm[h][a_][:, 128:256], True, True, [NM[h][a_]], [d["Q"]])
                    for d in HD:
                        h = d["h"]
                        if h % 2 == 0:
                            em.cp("dve", nm[h][b_][:, 0:wdt], d["q"][:, 0:wdt], [d["Q"]], [NM[h][b_]])
                        else:
                            em.act(nm[h][b_][:, 0:wdt], d["q"][:, 0:wdt], AF.Copy, [d["Q"]], [NM[h][b_]])
                    for d in HD:
                        h = d["h"]
                        em.mm(d["q"][:, 256:384], nm[h][b_][:, 0:128], qq[h][a_][:], True, True, [NM[h][b_], QQ[h][a_]], [d["Q"]])
                    for d in HD:
                        h = d["h"]
                        em.tt("dve", qq[h][b_][:], d["q"][:, 256:384], qq[h][a_][:], ALU.add, [d["Q"], QQ[h][a_]], [QQ[h][b_]])
                qf = 0
                so = [ping[0], ping[1]]
                for d in HD:
                    h, hp, rows, hc = d["h"], d["hp"], d["rows"], d["hc"]
                    So = STT[hp][so[hp]][d["hh"]]
                    em.mm(d["q"][:, 0:64], mabk[h][:, 256:384], vT[:, c, hc], True, False, [MABK[h], VT[c]], [d["Q"]])
                    em.mm(d["q"][:, 0:64], ar[hp][rows, c, 0:CH], stt[hp][so[hp]][rows, :], False, True, [AR[hp], So], [d["Q"]])
                for d in HD:
                    h = d["h"]
                    if h % 2 == 0:
                        em.act(xsb[h][:], d["q"][:, 0:64], AF.Copy, [d["Q"]], [XSB[h]])
                    else:
                        em.cp("dve", xsb[h][:], d["q"][:, 0:64], [d["Q"]], [XSB[h]])
                for d in HD:
                    h = d["h"]
                    em.mm(d["q"][:, 64:128], qq[h][qf][:], xsb[h][:], True, True, [QQ[h][qf], XSB[h]], [d["Q"]])
                for d in HD:
                    h = d["h"]
                    if h % 2 == 0:
                        em.act(usb[h][:], d["q"][:, 64:128], AF.Copy, [d["Q"]], [USB[h]])
                    else:
                        em.cp("dve", usb[h][:], d["q"][:, 64:128], [d["Q"]], [USB[h]])
                for d in HD:
                    h, hp, rows, hc = d["h"], d["hp"], d["rows"], d["hc"]
                    So = STT[hp][so[hp]][d["hh"]]
                    em.mm(d["q"][:, 256:320], ar[hp][rows, c, CH:2 * CH], stt[hp][so[hp]][rows, :], True, False, [AR[hp], So], [d["Q"]])
                    em.mm(d["q"][:, 256:320], mabk[h][:, 128:256], usb[h][:], False, False, [MABK[h], USB[h]], [d["Q"]])
                    em.mm(d["q"][:, 256:320], mabk[h][:, 384:512], vT[:, c, hc], False, True, [MABK[h], VT[c]], [d["Q"]])
                    em.mm(d["q"][rows, 128:192], bhT[:, c, hc], usb[h][:], True, False, [BHT[c], USB[h]], [d["Q"]])
                    em.mm(d["q"][rows, 128:192], khT[:, c, hc], vT[:, c, hc], False, True, [KHT[c], VT[c]], [d["Q"]])
                for d in HD:
                    h, hp, rows, hc = d["h"], d["hp"], d["rows"], d["hc"]
                    sn = 1 - so[hp]
                    em.stt("dve", stt[hp][sn][rows, :], stt[hp][so[hp]][rows, :], wc[hp][rows, c:c + 1], d["q"][rows, 128:192], ALU.mult, ALU.add,
                           [STT[hp][so[hp]][d["hh"]], WC[hp], d["Q"]], [STT[hp][sn][d["hh"]]])
                    em.act(ytok[:, hc], d["q"][:, 256:320], AF.Copy, [d["Q"]], [YTOK])
                    em.act(ysq[:, hc], d["q"][:, 256:320], AF.Square, [d["Q"]], [YSQ])
                ping[0], ping[1] = 1 - so[0], 1 - so[1]
                kb.op("dve", lambda e: e.tensor_reduce(sts[:, 0:4], ytok[:].rearrange("p (h v) -> p h v", v=64), AX.X, ALU.add), reads=[YTOK], writes=[STS])
                kb.op("dve", lambda e: e.tensor_reduce(sts[:, 4:8], ysq[:].rearrange("p (h v) -> p h v", v=64), AX.X, ALU.add), reads=[YSQ, STS], writes=[STS])
                kb.op("dve", lambda e: e.tensor_scalar(sts[:, 8:12], sts[:, 0:4], 1.0 / 64, None, ALU.mult), reads=[STS], writes=[STS])
                kb.op("dve", lambda e: e.tensor_tensor(sts[:, 12:16], sts[:, 8:12], sts[:, 8:12], ALU.mult), reads=[STS], writes=[STS])
                kb.op("dve", lambda e: e.scalar_tensor_tensor(sts[:, 16:20], sts[:, 4:8], 1.0 / 64, sts[:, 12:16], ALU.mult, ALU.subtract),
                      reads=[STS], writes=[STS])
                kb.op("dve", lambda e: e.tensor_scalar(sts[:, 16:20], sts[:, 16:20], GN_EPS, None, ALU.add), reads=[STS], writes=[STS])
                kb.op("act", lambda e: e.activation(out=sts[:, 20:24], in_=sts[:, 16:20], func=AF.Sqrt), reads=[STS], writes=[STS])
                kb.op("dve", lambda e: e.reciprocal(sts[:, 24:28], sts[:, 20:24]), reads=[STS], writes=[STS])
                kb.op("dve", lambda e: e.tensor_tensor(yn[:].rearrange("p (h v) -> p h v", v=64), ytok[:].rearrange("p (h v) -> p h v", v=64),
                                                       sts[:, 8:12].unsqueeze(2).to_broadcast([128, 4, 64]), ALU.subtract), reads=[YTOK, STS], writes=[YN])
                kb.op("dve", lambda e: e.tensor_tensor(yn[:].rearrange("p (h v) -> p h v", v=64), yn[:].rearrange("p (h v) -> p h v", v=64),
                                                       sts[:, 24:28].unsqueeze(2).to_broadcast([128, 4, 64]), ALU.mult), reads=[YN, STS], writes=[YN])
                for hp in range(2):
                    pbank, PT = next_pa()
                    pt = pbank[:, 0:128]
                    em.tr(pt, yn[:, hp * 128:(hp + 1) * 128], ident[:], [YN, IDENT], [PT])
                    em.ts("dve", yf[hp][:, csl], pt, vec[:, 18 + hp:19 + hp], vec[:, 20 + hp:21 + hp], ALU.mult, ALU.add, [PT, VEC], [YF[hp]])
            for hp in range(2):
                kb.op("pool", lambda e, hp=hp: e.tensor_tensor(osb[hp][:], yf[hp][:], bon[hp][:], ALU.add), reads=[YF[hp], BON[hp]], writes=[OSB[hp]])
                kb.op("pool", lambda e, hp=hp: e.tensor_tensor(osb[hp][:], osb[hp][:], gg[hp][:], ALU.mult), reads=[OSB[hp], GG[hp]], writes=[OSB[hp]])
                kb.dma("sp", f"out{hp}", lambda e, s, hp=hp, sgi=sgi: e.dma_start(out=yT_d[hp * 128:(hp + 1) * 128, sgi * NS:(sgi + 1) * NS], in_=osb[hp][:]).then_inc(s, 16),
                       reads=[OSB[hp]])
        kb.finish(OSB)


def build_a1(nseg=NSEG, debug=False):
    nc = bass.Bass("TRN2", target_bir_lowering=False)
    I = lambda n, shp: nc.dram_tensor(n, shp, F32, kind="ExternalInput").ap()
    D = dict(xT=I("xT", [1024, S]), w=I("w", [1024, 1024]), vec=I("vec", [128, 22]), lw=I("lw", [128, 256]), g2=I("g2", [128, 256]),
             cst=I("cst", [128, 1280]), yT=nc.dram_tensor("yT", [256, S], F32, kind="ExternalOutput").ap())
    with ExitStack() as st:
        kb = KB(nc, st)
        a1_body(nc, kb, D, nseg, debug)
        kb.emit()
    return nc


def a1_consts():
    m1 = np.ones((128, 512), np.float32); m1[:, ::CH] = 0.0
    s = np.arange(128)[:, None]; t = np.arange(128)[None, :]
    msu = (t > s).astype(np.float32); miu = (t >= s).astype(np.float32)
    msl = (t < s).astype(np.float32)
    bones = (s // 64 == t // 64).astype(np.float32)
    return np.concatenate([m1, msu, miu, msu, miu, msl, bones], axis=1)


def a1_inputs(inp, l, b, hh):
    ch = slice(256 * hh, 256 * hh + 256)
    w_in = inp["w_in"][l]
    w = np.concatenate([w_in[:, 0:512][:, ch], w_in[:, 512:1024][:, ch], w_in[:, 1024:1536][:, ch], w_in[:, 1536:1792]], axis=1)
    mu = inp["shift_mu"][l]
    mu_cols = np.concatenate([mu[0:512][ch], mu[512:1024][ch], mu[1024:1536][ch], mu[1536:1792]])
    vec = np.zeros((128, 22), np.float32)
    vec[:, 0:8] = mu_cols.reshape(8, 128).T
    def two(v):
        return v[ch].reshape(2, 128).T
    vec[:, 8:10] = two(inp["rw_w0"][l]); vec[:, 10:12] = two(inp["rw_a0"][l]); vec[:, 12:14] = two(inp["rw_kk"][l])
    vec[:, 14:16] = two(inp["rw_ka"][l]); vec[:, 16:18] = two(inp["rw_rk"][l].reshape(512))
    vec[:, 18:20] = two(inp["rw_ln_g"][l]); vec[:, 20:22] = two(inp["rw_ln_b"][l])
    lw = np.concatenate([inp["rw_w2"][l][:, ch], inp["rw_a2"][l][:, ch]], axis=0)
    g2 = inp["rw_g2"][l][:, ch]
    return dict(w=np.ascontiguousarray(w), vec=vec, lw=np.ascontiguousarray(lw), g2=np.ascontiguousarray(g2), cst=a1_consts())


import math

S = 4096
NEG = -30000.0
TW = 2304
DCL = 1280


def t5_bucket(n):
    n = np.maximum(n, 0); me = 16
    nf = np.maximum(n, 1).astype(np.float32)
    large = me + (np.log(nf / np.float32(me)) / np.float32(math.log(1024 / me)) * np.float32(32 - me)).astype(np.int32)
    large = np.minimum(large, 31)
    return np.where(n < me, n, large)


class Em:
    def __init__(self, kb):
        self.kb = kb

    def mm(self, out, lhsT, rhs, start, stop, reads, writes):
        self.kb.op("pe", lambda e, o=out, a=lhsT, b=rhs, s=start, t=stop: e.matmul(o, a, b, start=s, stop=t), reads, writes)

    def tr(self, out, in_, ident, reads, writes):
        self.kb.op("pe", lambda e, o=out, a=in_, b=ident: e.transpose(o, a, b), reads, writes)

    def act(self, out, in_, func, reads, writes, **kw):
        self.kb.op("act", lambda e, o=out, i=in_, f=func, kw=kw: e.activation(out=o, in_=i, func=f, **kw), reads, writes)

    def tt(self, eng, out, a, b, op, reads, writes):
        self.kb.op(eng, lambda e, o=out, a=a, b=b, op=op: e.tensor_tensor(o, a, b, op), reads, writes)

    def ts(self, eng, out, a, s1, s2, op0, op1, reads, writes):
        if op1 is None:
            self.kb.op(eng, lambda e, o=out, a=a, s1=s1, op0=op0: e.tensor_scalar(o, a, s1, None, op0), reads, writes)
        else:
            self.kb.op(eng, lambda e, o=out, a=a, s1=s1, s2=s2, op0=op0, op1=op1: e.tensor_scalar(o, a, s1, s2, op0, op1), reads, writes)

    def stt(self, eng, out, a, s, b, op0, op1, reads, writes):
        self.kb.op(eng, lambda e, o=out, a=a, s=s, b=b, op0=op0, op1=op1: e.scalar_tensor_tensor(o, a, s, b, op0, op1), reads, writes)

    def cp(self, eng, out, in_, reads, writes):
        self.kb.op(eng, lambda e, o=out, i=in_: e.tensor_copy(o, i), reads, writes)

    def ms(self, eng, out, val, writes):
        self.kb.op(eng, lambda e, o=out, v=val: e.memset(o, v), (), writes)

    def red(self, out, in_, op, reads, writes):
        self.kb.op("dve", lambda e, o=out, i=in_, op=op: e.tensor_reduce(o, i, AX.X, op), reads, writes)

    def rcp(self, out, in_, reads, writes):
        self.kb.op("dve", lambda e, o=out, i=in_: e.reciprocal(o, i), reads, writes)

    def dma(self, q, key, out, in_, reads, writes):
        self.kb.dma(q, key, lambda e, s, o=out, i=in_: e.dma_start(out=o, in_=i).then_inc(s, 16), reads, writes)


def a2_body(nc, kb, D, nq=8, debug=False):
    xT_d, wf_d, wt_d, w1_d, pe_d, w2_d, btc_d, mkc_d, bts_d, mks_d, mkw_d, m12_d, rv_d, c2s_d, exd_d, hsel_d = (D[k] for k in ['xT', 'wf', 'wt', 'w1', 'peT', 'w2', 'btc', 'mkc', 'bts', 'mks', 'mkw', 'm12', 'rv', 'c2s', 'exd', 'hsel'])
    yT_d = D["yT"]
    xT_v = xT_d.rearrange("(kc p) t -> p kc t", p=128)
    if True:
        em = Em(kb)
        sb, ps = kb.sb, kb.ps
        xs = [sb(f"xs{i}", [128, 8, 512], BF16) for i in range(2)]; XS = [Buf() for _ in range(2)]
        wf = sb("wf", [128, 8, 640], BF16); WF = Buf()
        wt = sb("wt", [128, 8, 256], BF16); WT = Buf()
        w1 = sb("w1", [128, 32, 128], BF16); W1 = Buf()
        peT = sb("peT", [128, 32], BF16); PET = Buf()
        w2f = sb("w2f", [128, 192]); w2 = sb("w2", [128, 192], BF16); W2 = Buf()
        qT = [sb(f"qT{i}", [128, S], BF16) for i in range(2)]; QT = [Buf() for _ in range(2)]
        ksT = sb("ksT", [128, S], BF16); KST = Buf()
        kwT = sb("kwT", [128, S], BF16); KWT = Buf()
        kcv = sb("kcv", [128, S], BF16); KCV = Buf()
        vs = sb("vs", [128, 32, 96], BF16); VS = Buf()
        vw = sb("vw", [128, 32, 96], BF16); VW = Buf()
        gt = sb("gt", [128, 32, 16]); GT = Buf()
        btc = sb("btc", [128, 4, 256]); BTC = Buf()
        mkc = sb("mkc", [128, 256]); MKC = Buf()
        HW_ = TW // 2
        stg = sb("stg", [128, HW_]); STG = Buf()
        stg2 = sb("stg2", [128, HW_]); STG2 = Buf()
        mks = sb("mks", [128, HW_]); MKS = Buf()
        mkw = sb("mkw", [128, HW_]); MKW = Buf()
        ebs = [sb(f"ebs{h}", [128, TW], BF16) for h in range(4)]; EBS = [Buf() for _ in range(4)]
        ebw = [sb(f"ebw{h}", [128, TW], BF16) for h in range(4)]; EBW = [Buf() for _ in range(4)]
        m12 = sb("m12", [128, 256]); M12 = Buf()
        rv = sb("rv", [128, 1]); RV = Buf()
        vca = sb("vca", [128, 2, 128], BF16); VCA = Buf()
        c2sf = sb("c2sf", [128, 128]); C2SF = Buf()
        exd = sb("exd", [128, 32, 128], BF16); EXD = Buf()
        hself = sb("hself", [128, 256]); hsel = sb("hsel", [128, 2, 128], BF16); HSEL = Buf()
        identf = sb("identf", [128, 128]); ident = sb("ident", [128, 128], BF16); IDENT = Buf()
        kct = sb("kct", [128, 256], BF16); KCT = Buf()
        gh = [sb(f"gh{i}", [128, 256], BF16) for i in range(2)]; GH = [Buf() for _ in range(2)]
        hx = sb("hx", [128, 256]); HX = Buf()
        hy = sb("hy", [128, 256]); HY = Buf()
        hb = sb("hb", [128, 2]); HB = Buf()
        sm = sb("sm", [128, 64]); SM = Buf()
        cst = sb("cst", [128, 16]); CST = Buf()
        qsq = sb("qsq", [128, 512], BF16); QSQ = Buf()
        mxc = sb("mxc", [128, 4, 8]); MXC = Buf()
        kxc = sb("kxc", [128, 2, 8]); KXC = Buf()
        lgs = [sb(f"lg{h}", [128, 256]) for h in range(4)]; LGS = [Buf() for _ in range(4)]
        pcs = [sb(f"pc{h}", [128, 256], BF16) for h in range(4)]; PCS = [Buf() for _ in range(4)]
        pcts = [sb(f"pct{h}", [128, 2, 128], BF16) for h in range(4)]; PCTS = [Buf() for _ in range(4)]
        smh = sb("smh", [128, 32]); SMH = [Buf() for _ in range(4)]
        sc = sb("sc", [128, 64]); SC = Buf()
        sc2 = sb("sc2", [128, 64]); SC2 = Buf()
        mx8 = sb("mx8", [128, 16]); MX8 = Buf()
        nst = sb("nst", [128, 64], BF16); NST = Buf()
        nsh = sb("nsh", [128, 512], BF16); NSH = Buf()
        yg = sb("yg", [128, 4, 256]); YG = Buf()
        ygt = sb("ygt", [128, 2, 512]); YGT = Buf()
        pt = [sb(f"pt{i}", [128, 512], BF16) for i in range(3)]; PT = [Buf() for _ in range(3)]
        p2 = [sb(f"p2{i}", [128, 512], BF16) for i in range(3)]; P2 = [Buf() for _ in range(3)]
        rin = sb("rin", [128, 8]); RIN = Buf()
        zb = sb("zb", [128, 512], BF16); ZB = Buf()
        otmp = sb("otmp", [128, 4, 64]); OTMP = Buf()
        pg = [ps(f"pg{i}", [128, 512]) for i in range(2)]; PG = [Buf(excl=True) for _ in range(2)]
        ptb = ps("ptb", [128, 1024], BF16); PTB = Buf(excl=True)
        pl = [ps(f"pl{i}", [128, 512]) for i in range(3)]; PL = [Buf(excl=True) for _ in range(3)]
        pacc = [ps(f"pacc{i}", [128, 4, 128]) for i in range(2)]; PACC = [Buf(excl=True) for _ in range(2)]

        gi = [0]

        def gbank():
            i = gi[0] % 2; gi[0] += 1
            return pg[i], PG[i]

        dbg_n = [0]

        def dbg(name, ap, B):
            if not debug:
                return
            d = nc.dram_tensor("dbg_" + name, list(ap.shape), F32, kind="ExternalOutput").ap()
            dbg_n[0] += 1
            cntv = dbg_n[0] * 16

            def f(e, s, d=d, ap=ap, cntv=cntv):
                e.dma_start(out=d, in_=ap).then_inc(s, 16)
                e.wait_ge(s, cntv)
            kb.dma("pool", "dbg", f, reads=[B])

        em.dma("pool", "wf", wf[:, :, :], wf_d.rearrange("(kc p) n -> p kc n", p=128), [], [WF])
        em.dma("pool", "wt", wt[:, :, 0:140], wt_d.rearrange("(kc p) n -> p kc n", p=128), [], [WT])
        em.dma("pool", "w1", w1[:, :, :], w1_d.rearrange("p (a b) -> p a b", b=128), [], [W1])
        em.dma("pool", "pe", peT[:], pe_d[:, :], [], [PET])
        em.dma("sp", "w2", w2f[:], w2_d[:, :], [], [W2])
        em.dma("sp", "btc", btc[:].rearrange("p h w -> p (h w)"), btc_d[:, :], [], [BTC])
        em.dma("sp", "mkc", mkc[:], mkc_d[:, :], [], [MKC])
        em.dma("sp", "m12", m12[:], m12_d[:, :], [], [M12])
        em.dma("sp", "rv", rv[:], rv_d[:, :], [], [RV])
        em.dma("sp", "c2s", c2sf[:], c2s_d[:, :], [], [C2SF])
        em.ms("pool", exd[:], 0.0, [EXD])
        em.dma("pool", "exd", exd[0:64, :, :].rearrange("p a b -> p (a b)"), exd_d[:, :], [EXD], [EXD])
        em.dma("sp", "hsel", hself[:], hsel_d[:, :], [], [HSEL])
        em.cp("dve", w2[:], w2f[:], [W2], [W2])
        em.cp("dve", hsel[:].rearrange("p a b -> p (a b)"), hself[:], [HSEL], [HSEL])
        em.ms("pool", identf[:], 1.0, [IDENT])
        kb.op("pool", lambda e: e.affine_select(out=identf[:], in_=identf[:], pattern=[[-1, 128]], compare_op=ALU.is_equal,
                                                fill=0.0, base=0, channel_multiplier=1), [IDENT], [IDENT])
        em.cp("pool", ident[:], identf[:], [IDENT], [IDENT])
        em.ms("dve", vs[:, :, 64:65], 1.0, [VS])
        em.ms("dve", vw[:, :, 64:65], 1.0, [VW])
        em.ms("dve", vca[:], 0.0, [VCA])
        em.ms("dve", zb[:], 0.0, [ZB])
        em.ms("dve", nsh[:], 0.0, [NSH])
        for h_ in range(4):
            em.ms("dve", pcs[h_][:], 0.0, [PCS[h_]])
        em.ms("dve", kct[:], 0.0, [KCT])
        for h in range(4):
            em.tt("dve", btc[:, h, :], btc[:, h, :], mkc[:], ALU.add, [BTC, MKC], [BTC])
        first = True
        for hf in range(2):
            cs_ = slice(hf * HW_, (hf + 1) * HW_)
            em.dma("sp", "mks", mks[:], mks_d[:, cs_], [], [MKS])
            em.dma("sp", "mkw", mkw[:], mkw_d[:, cs_], [], [MKW])
            for h in range(4):
                em.dma("sp", "stg", stg[:], bts_d[:, h * TW + hf * HW_:h * TW + (hf + 1) * HW_], [], [STG])
                if first:
                    em.red(cst[:, 8:9], stg[:], ALU.max, [STG], [CST])
                    first = False
                else:
                    em.red(cst[:, 9:10], stg[:], ALU.max, [STG], [CST])
                    em.tt("dve", cst[:, 8:9], cst[:, 8:9], cst[:, 9:10], ALU.max, [CST], [CST])
                em.tt("dve", stg2[:], stg[:], mks[:], ALU.add, [STG, MKS], [STG2])
                em.act(ebs[h][:, cs_], stg2[:], AF.Exp, [STG2], [EBS[h]])
                em.tt("dve", stg2[:], stg[:], mkw[:], ALU.add, [STG, MKW], [STG2])
                em.act(ebw[h][:, cs_], stg2[:], AF.Exp, [STG2], [EBW[h]])

        import os as _os
        PH = int(_os.environ.get('PH', '9'))
        def load_x(sg):
            sl = sg % 2
            em.dma("pool", f"xs{sl}", xs[sl][:, :, :], xT_v[:, :, sg * 512:(sg + 1) * 512], [], [XS[sl]])

        load_x(0)
        for sg in range(8 if PH >= 2 else 0):
            sl = sg % 2
            if sg + 1 < 8:
                load_x(sg + 1)
            seg = slice(sg * 512, (sg + 1) * 512)
            dsts = [(qT[0], QT[0], 0.125), (qT[1], QT[1], 0.125), (ksT, KST, 1.0), (kwT, KWT, 1.0), (kcv, KCV, 1.0)]
            for j, (dst, DST, scl) in enumerate(dsts):
                p, P = gbank()
                for k in range(8):
                    em.mm(p[:, :], wf[:, k, j * 128:(j + 1) * 128], xs[sl][:, k, :], k == 0, k == 7, [WF, XS[sl]], [P])
                em.act(dst[:, seg], p[:, :], AF.Copy, [P], [DST], scale=scl)
            PJ = _os.environ.get('PJ', 'abc12')
            for tt_ in range(4 if 'b' in PJ else 0):
                tile_i = sg * 4 + tt_
                p, P = gbank()
                for k in range(8):
                    em.mm(p[:, 0:140], xs[sl][:, k, tt_ * 128:(tt_ + 1) * 128], wt[:, k, 0:140], k == 0, k == 7, [WT, XS[sl]], [P])
                if '1' in PJ:
                    em.cp("dve", vs[:, tile_i, 0:64], p[:, 0:64], [P], [VS])
                    em.cp("dve", vw[:, tile_i, 0:64], p[:, 64:128], [P], [VW])
                if '2' in PJ:
                    em.act(gt[:, tile_i, 0:12], p[:, 128:140], AF.Sigmoid, [P], [GT])
            for i in range(2 if 'c' in PJ else 0):
                em.tt("dve", qsq[:], qT[i][:, seg], qT[i][:, seg], ALU.mult, [QT[i]], [QSQ])
                for hh in range(2):
                    p, P = gbank()
                    em.mm(p[:, :], hsel[:, hh, :], qsq[:], True, True, [HSEL, QSQ], [P])
                    em.red(mxc[:, 2 * i + hh, sg:sg + 1], p[:, :], ALU.max, [P], [MXC])
            for i, (src, SRC) in enumerate(((ksT, KST), (kwT, KWT)) if 'c' in PJ else ()):
                em.tt("dve", qsq[:], src[:, seg], src[:, seg], ALU.mult, [SRC], [QSQ])
                p, P = gbank()
                em.mm(p[:, :], hsel[:, 0, :], qsq[:], True, True, [HSEL, QSQ], [P])
                em.red(kxc[:, i, sg:sg + 1], p[:, :], ALU.max, [P], [KXC])
        if PH < 3:
            nq = 0
        em.red(sm[:, 0:4], mxc[:], ALU.max, [MXC], [SM])
        em.red(sm[:, 4:6], kxc[:], ALU.max, [KXC], [SM])
        for br in range(2):
            em.ts("dve", sm[:, 8 + 4 * br:12 + 4 * br], sm[:, 0:4], sm[:, 4 + br:5 + br], None, ALU.mult, None, [SM], [SM])
        em.act(sm[:, 16:24], sm[:, 8:16], AF.Sqrt, [SM], [SM])
        em.ts("dve", cst[:, 0:8], sm[:, 16:24], cst[:, 8:9], -1.0, ALU.add, ALU.mult, [SM, CST], [CST])

        for br in range(2 if PH >= 3 else 0):
            rows = slice(64 * br, 64 * br + 64)
            p, P = gbank()
            for pp in range(32):
                em.mm(p[:, 0:255], w1[rows, pp, :], kcv[rows, pp:pp + 16 * 254 + 1:16], pp == 0, pp == 31, [W1, KCV], [P])
            pb, PB = gbank()
            for pp in range(32):
                em.mm(pb[:, 0:1], w1[rows, pp, :], peT[rows, pp:pp + 1], pp == 0, pp == 31, [W1, PET], [PB])
            em.cp("dve", hb[:, br:br + 1], pb[:, 0:1], [PB], [HB])
            em.ts("dve", hx[:, 0:255], p[:, 0:255], hb[:, br:br + 1], None, ALU.add, None, [P, HB], [HX])
            em.tt("dve", hy[:, 0:255], hx[:, 0:255], hx[:, 0:255], ALU.mult, [HX], [HY])
            em.ts("dve", hy[:, 0:255], hy[:, 0:255], 0.044715, 1.0, ALU.mult, ALU.add, [HY], [HY])
            em.tt("dve", hy[:, 0:255], hy[:, 0:255], hx[:, 0:255], ALU.mult, [HY, HX], [HY])
            em.act(hy[:, 0:255], hy[:, 0:255], AF.Tanh, [HY], [HY], scale=0.7978845608028654)
            em.ts("dve", hy[:, 0:255], hy[:, 0:255], 1.0, 0.5, ALU.add, ALU.mult, [HY], [HY])
            em.tt("dve", gh[br][:, 0:255], hy[:, 0:255], hx[:, 0:255], ALU.mult, [HY, HX], [GH[br]])
        p, P = gbank()
        em.mm(p[:, 0:255], w2[:, 0:128], gh[0][:, 0:255], True, True, [W2, GH[0]], [P])
        em.act(kct[:, 0:255], p[:, 0:255], AF.Copy, [P], [KCT])
        for ct in range(2):
            ncs = 128 if ct == 0 else 127
            p, P = gbank()
            em.mm(p[0:ncs, 0:64], gh[1][:, ct * 128:ct * 128 + ncs], w2[:, 128:192], True, True, [W2, GH[1]], [P])
            em.act(vca[0:ncs, ct, 0:64], p[0:ncs, 0:64], AF.Copy, [P], [VCA])
        em.cp("dve", vca[:, :, 64:128], c2sf[:].rearrange("p (a b) -> p a b", b=64), [C2SF], [VCA])
        dbg("kct", kct[:, 0:255], KCT); dbg("vca", vca[:].rearrange("p a b -> p (a b)"), VCA)
        dbg("cst", cst[:, 0:9], CST)

        pti = [0]
        for Q in range(nq):
            qs = slice(Q * 512, (Q + 1) * 512)
            for a in range(4):
                n = 4 * Q + a
                t0 = 128 * n
                ncol = min(255, 8 * n + 7)
                off = 248 - 8 * n
                ncp = min(256, (ncol + 31) // 32 * 32)
                nct = 1 if ncol <= 128 else 2
                for hpair in ((0, 1), (2, 3)):
                    HP = {}
                    for h in hpair:
                        rows = slice(64 * (h % 2), 64 * (h % 2) + 64)
                        p, P = gbank()
                        em.mm(p[:, 0:ncp], qT[h // 2][rows, t0:t0 + 128], kct[rows, 0:ncp], True, True, [QT[h // 2], KCT], [P])
                        HP[h] = (p, P)
                    for h in hpair:
                        p, P = HP[h]
                        s0 = 8 * h
                        em.tt("dve", lgs[h][:, 0:ncol], p[:, 0:ncol], btc[:, h, off:off + ncol], ALU.add, [P, BTC], [LGS[h]])
                        em.red(smh[:, s0:s0 + 1], lgs[h][:, 0:ncol], ALU.max, [LGS[h]], [SMH[h]])
                        em.ts("dve", smh[:, s0 + 1:s0 + 2], smh[:, s0:s0 + 1], -1.0, None, ALU.mult, None, [SMH[h]], [SMH[h]])
                        em.ms("dve", smh[:, s0 + 2:s0 + 3], 0.0, [SMH[h]])
                    for h in hpair:
                        s0 = 8 * h
                        em.act(pcs[h][:, 0:ncol], lgs[h][:, 0:ncol], AF.Exp, [LGS[h], SMH[h]], [PCS[h], SMH[h]], bias=smh[:, s0 + 1:s0 + 2], accum_out=smh[:, s0 + 2:s0 + 3])
                    for h in hpair:
                        for ct in range(nct):
                            em.tr(ptb[:, ct * 128:(ct + 1) * 128], pcs[h][:, ct * 128:(ct + 1) * 128], ident[:], [PCS[h], IDENT], [PTB])
                        em.cp("dve", pcts[h][:, 0:nct, :], ptb[:, 0:nct * 128].rearrange("p (a b) -> p a b", b=128), [PTB], [PCTS[h]])
                    PO_ = {}
                    for h in hpair:
                        po, PO = gbank()
                        for ct in range(nct):
                            em.mm(po[:, 0:128], pcts[h][:, ct, :], vca[:, ct, :], ct == 0, ct == nct - 1, [PCTS[h], VCA], [PO])
                        PO_[h] = (po, PO)
                    for h in hpair:
                        po, PO = PO_[h]
                        s0 = 8 * h
                        em.rcp(smh[:, s0 + 3:s0 + 4], smh[:, s0 + 2:s0 + 3], [SMH[h]], [SMH[h]])
                        if n == 0:
                            em.tt("dve", smh[:, s0 + 3:s0 + 4], smh[:, s0 + 3:s0 + 4], rv[:], ALU.mult, [SMH[h], RV], [SMH[h]])
                        em.tt("dve", smh[:, s0 + 4:s0 + 5], smh[:, s0 + 3:s0 + 4], gt[:, n, 3 * h:3 * h + 1], ALU.mult, [SMH[h], GT], [SMH[h]])
                        em.ts("dve", yg[:, a, h * 64:(h + 1) * 64], po[:, 0:64], smh[:, s0 + 4:s0 + 5], None, ALU.mult, None, [PO, SMH[h]], [YG])
                        if h == 0:
                            em.ts("dve", sc[:], po[:, 64:128], smh[:, s0 + 3:s0 + 4], None, ALU.mult, None, [PO, SMH[h]], [SC])
                        else:
                            em.stt("dve", sc[:], po[:, 64:128], smh[:, s0 + 3:s0 + 4], sc[:], ALU.mult, ALU.add, [PO, SMH[h], SC], [SC])
                w0 = 64 - 2 * n
                em.tt("dve", sc[:], sc[:], m12[:, w0:w0 + 64], ALU.mult, [SC, M12], [SC])
                em.tt("dve", sc[:], sc[:], m12[:, 128 + w0:128 + w0 + 64], ALU.add, [SC, M12], [SC])
                em.ms("dve", sc[:, 0:1], 1.0e4, [SC])
                kb.op("dve", lambda e: e.max(out=mx8[:, 0:8], in_=sc[:]), [SC], [MX8])
                kb.op("dve", lambda e: e.match_replace(out=sc2[:], in_to_replace=mx8[:, 0:8], in_values=sc[:], imm_value=-1.0e9), [SC, MX8], [SC2])
                kb.op("dve", lambda e: e.max(out=mx8[:, 8:16], in_=sc2[:]), [SC2], [MX8])
                em.red(sm[:, 40:41], mx8[:, 8:16], ALU.min, [MX8], [SM])
                em.ts("dve", nst[:], sc[:], sm[:, 40:41], NEG, ALU.is_lt, ALU.mult, [SC, SM], [NST])
                em.tr(ptb[0:64, 256:384], nst[:], ident[:], [NST, IDENT], [PTB])
                em.cp("dve", nsh[0:64, a * 128:(a + 1) * 128], ptb[0:64, 256:384], [PTB], [NSH])
                if Q == 0 and a == 1:
                    dbg("sc", sc[:], SC); dbg("yg1", yg[:, 1, :], YG)
            QP = _os.environ.get('QP', 'abc')
            for br in [b_ for b_ in range(2) if 'bc'[b_] in QP]:
                kT, KT_, vv, VV, eb, EB = (ksT, KST, vs, VS, ebs, EBS) if br == 0 else (kwT, KWT, vw, VW, ebw, EBW)
                m_lo = 0 if br == 0 else max(0, 4 * Q - 4)
                m_hi = 4 * Q + 3
                tiles = [(h, m) for h in range(4) for m in range(m_lo, m_hi + 1)]
                slot = {}

                def stage1(ti):
                    h, m = tiles[ti]
                    rows = slice(64 * (h % 2), 64 * (h % 2) + 64)
                    pli = pti[0] % 3
                    bi = pti[0] % 3
                    pti[0] += 1
                    slot[ti] = (pli, bi)
                    pl_, PL_ = pl[pli], PL[pli]
                    em.mm(pl_[:, :], kT[rows, m * 128:(m + 1) * 128], qT[h // 2][rows, qs], True, br == 1, [KT_, QT[h // 2]], [PL_])
                    if br == 0:
                        em.mm(pl_[:, :], exd[:, m, :], nsh[:, :], False, True, [EXD, NSH], [PL_])

                def stage23(ti):
                    h, m = tiles[ti]
                    pli, bi = slot.pop(ti)
                    pl_, PL_ = pl[pli], PL[pli]
                    acc, ACC = pacc[h % 2], PACC[h % 2]
                    D0 = 512 * Q - 128 * m
                    wst = min(D0, DCL) + 512
                    em.act(pt[bi][:], pl_[:, :], AF.Exp, [PL_, CST], [PT[bi]], bias=cst[:, 4 * br + h:4 * br + h + 1])
                    em.tt("dve", p2[bi][:], pt[bi][:], eb[h][:, wst:wst + 512], ALU.mult, [PT[bi], EB[h]], [P2[bi]])
                    if m == m_lo:
                        em.mm(acc[:].rearrange("p a b -> p (a b)"), zb[:, 0:128], zb[:, 0:512], True, False, [ZB], [ACC])
                    for a in range(4):
                        last_m = min(m_hi, 4 * Q + a)
                        if m > last_m:
                            continue
                        a_lo = m_lo if br == 0 else max(m_lo, 4 * Q + a - 4)
                        if m < a_lo:
                            continue
                        em.mm(acc[:, a, 0:65], p2[bi][:, a * 128:(a + 1) * 128], vv[:, m, 0:65], False, (m == m_hi and a == 3), [P2[bi], VV], [ACC])
                    if m == m_hi:
                        em.rcp(rin[:, 0:4], acc[:, :, 64], [ACC], [RIN])
                        em.tt("dve", rin[:, 4:8], rin[:, 0:4], gt[:, 4 * Q:4 * Q + 4, 3 * h + 1 + br], ALU.mult, [RIN, GT], [RIN])
                        em.tt("dve", otmp[:], acc[:, :, 0:64], rin[:, 4:8].unsqueeze(2).to_broadcast([128, 4, 64]), ALU.mult, [ACC, RIN], [OTMP])
                        em.tt("pool", yg[:, :, h * 64:(h + 1) * 64], yg[:, :, h * 64:(h + 1) * 64], otmp[:], ALU.add, [YG, OTMP], [YG])
                stage1(0)
                if len(tiles) > 1:
                    stage1(1)
                for ti in range(len(tiles)):
                    if ti + 2 < len(tiles):
                        stage1(ti + 2)
                    stage23(ti)
            for a in range(4):
                for hp in range(2):
                    pz, PZ = gbank()
                    em.tr(pz[:, 0:128], yg[:, a, hp * 128:(hp + 1) * 128], identf[:], [YG, IDENT], [PZ])
                    em.cp("dve" if hp else "pool_never", ygt[:, hp, a * 128:(a + 1) * 128], pz[:, 0:128], [PZ], [YGT]) if False else em.cp("dve", ygt[:, hp, a * 128:(a + 1) * 128], pz[:, 0:128], [PZ], [YGT])
            for hp in range(2):
                em.dma("sp", "y", yT_d[hp * 128:(hp + 1) * 128, Q * 512:(Q + 1) * 512], ygt[:, hp, :], [YGT], [])
        kb.finish([YG, YGT])


def build_a2(nq=8, debug=False):
    nc = bass.Bass("TRN2", target_bir_lowering=False)
    I = lambda n, shp: nc.dram_tensor(n, shp, F32, kind="ExternalInput").ap()
    D = {"xT": I("xT", [1024, S]), "wf": I("wf", [1024, 640]), "wt": I("wt", [1024, 140]), "w1": I("w1", [128, 32 * 128]), "peT": I("peT", [128, 32]), "w2": I("w2", [128, 192]), "btc": I("btc", [128, 4 * 256]), "mkc": I("mkc", [128, 256]), "bts": I("bts", [128, 4 * TW]), "mks": I("mks", [128, TW]), "mkw": I("mkw", [128, TW]), "m12": I("m12", [128, 256]), "rv": I("rv", [128, 1]), "c2s": I("c2s", [128, 128]), "exd": I("exd", [64, 32 * 128]), "hsel": I("hsel", [128, 256])}
    D["yT"] = nc.dram_tensor("yT", [256, S], F32, kind="ExternalOutput").ap()
    with ExitStack() as st:
        kb = KB(nc, st)
        a2_body(nc, kb, D, nq, debug)
        kb.emit()
    return nc


def a2_consts():
    i = np.arange(128)[:, None]
    j = np.arange(256)[None, :]
    dc = i - 16 * (j - 248) - 31
    mkc = np.where(dc >= 0, 0.0, NEG).astype(np.float32)
    w = np.arange(TW)[None, :]
    ds = w - i - 512
    mks = np.where(ds >= 0, 0.0, NEG).astype(np.float32)
    mkw = np.where((ds >= 0) & (ds < 512), 0.0, NEG).astype(np.float32)
    wv = np.arange(128)[None, :] - 64
    cur = (i >= 64).astype(np.int64)
    forced = (wv == cur) | (wv == cur - 1)
    valid = wv <= cur
    m1 = (valid & ~forced).astype(np.float32)
    m2 = np.where(forced, 1.0e4, np.where(valid, 0.0, -1.0)).astype(np.float32)
    m12 = np.concatenate([m1, m2], axis=1)
    rv = (np.arange(128) >= 31).astype(np.float32)[:, None]
    cs = np.arange(255)[:, None] * 16; ss = np.arange(64)[None, :] * 64
    ov = np.clip(np.minimum(cs + 32, ss + 64) - np.maximum(cs, ss), 0, None).astype(np.float32) / 32
    c2s = np.zeros((256, 64), np.float32); c2s[:255] = ov
    c2s = c2s.reshape(2, 128, 64).transpose(1, 0, 2).reshape(128, 128)
    exd = np.zeros((64, 32, 128), np.float32)
    for m in range(32):
        exd[2 * m, m, 0:64] = 1.0; exd[2 * m + 1, m, 64:128] = 1.0
    hsel = np.zeros((128, 2, 128), np.float32); hsel[0:64, 0, :] = 1.0; hsel[64:128, 1, :] = 1.0
    return dict(mkc=mkc, mks=mks, mkw=mkw, m12=m12, rv=rv, c2s=c2s, exd=exd.reshape(64, 32 * 128), hsel=hsel.reshape(128, 256),
                dc=dc, ds=ds)


def a2_inputs(inp, l, g, consts):
    w_in = inp["w_in"][l]; zr = 1792
    q = w_in[:, zr + 256 * g: zr + 256 * g + 256]
    def kvc(off):
        return w_in[:, zr + off + 64 * g: zr + off + 64 * g + 64]
    kc, vc, ks, vs, kw, vw = (kvc(o) for o in (512, 640, 768, 896, 1024, 1152))
    gates = w_in[:, zr + 1280 + 12 * g: zr + 1280 + 12 * g + 12]
    wf = np.concatenate([q, ks, ks, kw, kw, kc, vc], axis=1)
    wt = np.concatenate([vs, vw, gates], axis=1)
    def w1r(w):
        return w.reshape(32, 64, 128).transpose(1, 0, 2)
    w1 = np.concatenate([w1r(inp["cmp_w1_k"][l]), w1r(inp["cmp_w1_v"][l])], axis=0).reshape(128, 32 * 128)
    peT = np.concatenate([inp["cmp_pe_k"][l].T, inp["cmp_pe_v"][l].T], axis=0)
    w2 = np.concatenate([inp["cmp_w2_k"][l], inp["cmp_w2_k"][l], inp["cmp_w2_v"][l]], axis=1)
    rb = inp["rel_bias"][:, 4 * g:4 * g + 4]
    btc = np.take(rb, t5_bucket(consts["dc"]), axis=0).transpose(0, 2, 1).reshape(128, 4 * 256)
    bts = np.take(rb, t5_bucket(consts["ds"]), axis=0).transpose(0, 2, 1).reshape(128, 4 * TW)
    out = dict(wf=wf, wt=wt, w1=w1, peT=peT, w2=w2, btc=btc, bts=bts)
    for k in ("mkc", "mks", "mkw", "m12", "rv", "c2s", "exd", "hsel"):
        out[k] = consts[k]
    return {k: np.ascontiguousarray(v, dtype=np.float32) for k, v in out.items()}


NT = 2048
ALPHA = 8 ** 0.25
LN_EPS = 1e-5


def layer_norm_fm(kb, em, gbank, R, RB, out_fn, g_ap, b_ap, GB, tmp, TMP, ones, ONES, sq, SQ, mean, MEAN, rstd, RSTD):
    pm, PM = gbank()
    for i in range(8):
        em.mm(pm[:, :], ones[:], R[:, i, :], i == 0, i == 7, [ONES, RB], [PM])
    em.act(mean[:], pm[:, :], AF.Copy, [PM], [MEAN], scale=1.0 / 1024)
    pv, PV = gbank()
    for i in range(8):
        em.tt("pool" if i % 2 else "dve", sq[i % 2][:], R[:, i, :], R[:, i, :], ALU.mult, [RB], [SQ[i % 2]])
        em.mm(pv[:, :], ones[:], sq[i % 2][:], i == 0, i == 7, [ONES, SQ[i % 2]], [PV])
    em.tt("dve", tmp[:], mean[:], mean[:], ALU.mult, [MEAN], [TMP])
    em.stt("dve", rstd[:], pv[:, :], 1.0 / 1024, tmp[:], ALU.mult, ALU.subtract, [PV, TMP], [RSTD])
    em.ts("dve", rstd[:], rstd[:], LN_EPS, None, ALU.add, None, [RSTD], [RSTD])
    em.act(rstd[:], rstd[:], AF.Sqrt, [RSTD], [RSTD])
    em.rcp(rstd[:], rstd[:], [RSTD], [RSTD])
    for i in range(8):
        eng = "pool" if i % 2 else "dve"
        em.tt(eng, tmp[:], R[:, i, :], mean[:], ALU.subtract, [RB, MEAN], [TMP])
        em.tt(eng, tmp[:], tmp[:], rstd[:], ALU.mult, [TMP, RSTD], [TMP])
        o, O = out_fn(i)
        em.ts(eng, o, tmp[:], g_ap[:, i:i + 1], b_ap[:, i:i + 1], ALU.mult, ALU.add, [TMP, GB], [O])


def b1_body(nc, kb, D):
    xT_d, yr_d, yn_d, wg_d, wur_d, wun_d, wo_d, ln_d, o_d = (D[k] for k in ("xT", "yrT", "ynT", "wg", "wur", "wun", "wo", "ln", "x1T"))
    v3 = lambda ap: ap.rearrange("(kc p) n -> p kc n", p=128)
    if True:
        em = Em(kb); sb, ps = kb.sb, kb.ps
        wg = sb("wg", [128, 8, 2048], BF16); WG = Buf()
        wur = sb("wur", [128, 4, 1024], BF16); WUR = Buf()
        wun = sb("wun", [128, 4, 1024], BF16); WUN = Buf()
        wo = sb("wo", [128, 8, 1024], BF16); WO = Buf()
        ln = sb("ln", [128, 16]); LN = Buf()
        ones = sb("ones", [128, 128]); ONES = Buf()
        xf = sb("xf", [128, 8, 512]); XF = Buf()
        xb = sb("xb", [128, 8, 512], BF16); XB = Buf()
        yr = sb("yr", [128, 4, 512], BF16); YR = Buf()
        yn = sb("yn", [128, 4, 512], BF16); YN = Buf()
        sg = [sb(f"sg{i}", [128, 512]) for i in range(2)]; SG = [Buf() for _ in range(2)]
        m1 = sb("m1", [128, 512]); M1 = Buf()
        mg = sb("mg", [128, 8, 512], BF16); MG = Buf()
        R = sb("R", [128, 8, 512]); RB = Buf()
        ob = sb("ob", [128, 8, 512]); OB = Buf()
        tmp = sb("tmp", [128, 512]); TMP = Buf()
        sq = [sb(f"sq{i}", [128, 512]) for i in range(2)]; SQ = [Buf() for _ in range(2)]
        mean = sb("mean", [128, 512]); MEAN = Buf()
        rstd = sb("rstd", [128, 512]); RSTD = Buf()
        pg = [ps(f"pg{i}", [128, 512]) for i in range(6)]; PG = [Buf(excl=True) for _ in range(6)]
        gi = [0]

        def gbank():
            i = gi[0] % 6; gi[0] += 1
            return pg[i], PG[i]
        for k0 in range(0, 8, 2):
            em.dma("pool", "wg", wg[:, k0:k0 + 2, :], v3(wg_d)[:, k0:k0 + 2, :], [], [WG])
        em.dma("pool", "wur", wur[:, :, :], v3(wur_d), [], [WUR])
        em.dma("pool", "wun", wun[:, :, :], v3(wun_d), [], [WUN])
        em.dma("pool", "wo", wo[:, :, :], v3(wo_d), [], [WO])
        em.dma("sp", "ln", ln[:], ln_d[:, :], [], [LN])
        em.ms("dve", ones[:], 1.0, [ONES])
        for tg in range(NT // 512):
            ts_ = slice(tg * 512, (tg + 1) * 512)
            em.dma("sp", "xf", xf[:, :, :], v3(xT_d)[:, :, ts_], [], [XF])
            em.dma("pool", "yr", yr[:, :, :], v3(yr_d)[:, :, ts_], [], [YR])
            em.dma("pool", "yn", yn[:, :, :], v3(yn_d)[:, :, ts_], [], [YN])
            for i in range(8):
                em.cp("pool" if i % 2 else "dve", xb[:, i, :], xf[:, i, :], [XF], [XB])
            for j in range(8):
                cs = slice(j * 128, (j + 1) * 128)
                for br, (wu, WU, yy, YY) in enumerate(((wur, WUR, yr, YR), (wun, WUN, yn, YN))):
                    p, P = gbank()
                    for k in range(8):
                        em.mm(p[:, :], wg[:, k, br * 1024 + j * 128: br * 1024 + (j + 1) * 128], xb[:, k, :], k == 0, k == 7, [WG, XB], [P])
                    em.act(sg[br][:], p[:, :], AF.Sigmoid, [P], [SG[br]])
                    p2, P2 = gbank()
                    for k in range(4):
                        em.mm(p2[:, :], wu[:, k, cs], yy[:, k, :], k == 0, k == 3, [WU, YY], [P2])
                    if br == 0:
                        em.tt("dve", m1[:], sg[0][:], p2[:, :], ALU.mult, [SG[0], P2], [M1])
                    else:
                        em.tt("dve", sg[1][:], sg[1][:], p2[:, :], ALU.mult, [SG[1], P2], [SG[1]])
                        em.tt("pool", mg[:, j, :], m1[:], sg[1][:], ALU.add, [M1, SG[1]], [MG])
            for i in range(8):
                p, P = gbank()
                for k in range(8):
                    em.mm(p[:, :], wo[:, k, i * 128:(i + 1) * 128], mg[:, k, :], k == 0, k == 7, [WO, MG], [P])
                em.stt("dve", R[:, i, :], xf[:, i, :], ALPHA, p[:, :], ALU.mult, ALU.add, [XF, P], [RB])
            layer_norm_fm(kb, em, gbank, R, RB, lambda i: (ob[:, i, :], OB), ln[:, 0:8], ln[:, 8:16], LN, tmp, TMP, ones, ONES, sq, SQ, mean, MEAN, rstd, RSTD)
            em.dma("sp", "ob", v3(o_d)[:, :, ts_], ob[:, :, :], [OB], [])
        kb.finish([OB])


def build_b1():
    nc = bass.Bass("TRN2", target_bir_lowering=False)
    I = lambda n, shp: nc.dram_tensor(n, shp, F32, kind="ExternalInput").ap()
    D = dict(xT=I("xT", [1024, NT]), yrT=I("yrT", [512, NT]), ynT=I("ynT", [512, NT]), wg=I("wg", [1024, 2048]), wur=I("wur", [512, 1024]),
             wun=I("wun", [512, 1024]), wo=I("wo", [1024, 1024]), ln=I("ln", [128, 16]),
             x1T=nc.dram_tensor("x1T", [1024, NT], F32, kind="ExternalOutput").ap())
    with ExitStack() as st:
        kb = KB(nc, st)
        b1_body(nc, kb, D)
        kb.emit()
    return nc


def b1_inputs(inp, l):
    w_in = inp["w_in"][l]
    lnp = np.concatenate([inp["ln1_g"][l].reshape(8, 128).T, inp["ln1_b"][l].reshape(8, 128).T], axis=1)
    return dict(wg=np.ascontiguousarray(w_in[:, 1792 + 1304: 1792 + 1304 + 2048]), wur=inp["w_up_rwkv"][l], wun=inp["w_up_nsa"][l],
                wo=inp["w_out"][l], ln=np.ascontiguousarray(lnp, dtype=np.float32))


def b2_body(nc, kb, D, nexp=32):
    x1_d, wr_d, br_d, w1_d, w3_d, w2_d, ln_d, selb_d, g2e_d, o_d = (D[k] for k in ("x1T", "wr", "brr", "ew1", "ew3", "ew2", "ln", "selb", "g2e", "x2T"))
    v3 = lambda ap: ap.rearrange("(kc p) n -> p kc n", p=128)
    if True:
        em = Em(kb); sb, ps = kb.sb, kb.ps
        wr = sb("wr", [128, 8, 64]); WR = Buf()
        brr = sb("brr", [128, 36]); BRR = Buf()
        ln = sb("ln", [128, 16]); LN = Buf()
        selb = sb("selb", [32, 32, 128], BF16); SELB = Buf()
        g2e = sb("g2e", [128, 4, 32]); G2E = Buf()
        ones = sb("ones", [128, 128]); ONES = Buf()
        ident = sb("ident", [128, 128]); IDENT = Buf()
        xf = sb("xf", [128, 8, 512]); XF = Buf()
        x1b = sb("x1b", [128, 8, NT], BF16); X1B = [Buf() for _ in range(4)]
        out = sb("out", [128, 8, NT]); OUT = [Buf() for _ in range(4)]
        cwt = sb("cwt", [32, NT], BF16); CWT = [Buf() for _ in range(4)]
        lgt = sb("lgt", [128, 36]); LGT = Buf()
        rs = sb("rs", [128, 64]); RS = Buf()
        em32 = sb("em32", [128, 32]); EM32 = Buf()
        em2 = sb("em2", [128, 32]); EM2 = Buf()
        cw = sb("cw", [128, 32]); CW = Buf()
        w1 = [sb(f"w1_{i}", [128, 8, 512], BF16) for i in range(2)]; W1 = [Buf() for _ in range(2)]
        w3 = [sb(f"w3_{i}", [128, 8, 512], BF16) for i in range(2)]; W3 = [Buf() for _ in range(2)]
        w2 = [sb(f"w2_{i}", [128, 4, 1024], BF16) for i in range(2)]; W2 = [Buf() for _ in range(2)]
        cwb = [sb(f"cwb{i}", [128, 512]) for i in range(2)]; CWB = [Buf() for _ in range(2)]
        sl_ = [sb(f"sl{i}", [128, 512]) for i in range(2)]; SL = [Buf() for _ in range(2)]
        hb = [sb(f"hb{i}", [128, 4, 512], BF16) for i in range(2)]; HB = [[Buf() for _ in range(4)] for _ in range(2)]
        ob = xf; OB = XF
        tmp = sb("tmp", [128, 512]); TMP = Buf()
        sq = [sb(f"sq{i}", [128, 512]) for i in range(2)]; SQ = [Buf() for _ in range(2)]
        mean = sb("mean", [128, 512]); MEAN = Buf()
        rstd = sb("rstd", [128, 512]); RSTD = Buf()
        pg = [ps(f"pg{i}", [128, 512]) for i in range(8)]; PG = [Buf(excl=True) for _ in range(8)]
        gi = [0]

        def gbank():
            i = gi[0] % 8; gi[0] += 1
            return pg[i], PG[i]
        em.ms("dve", wr[:], 0.0, [WR])
        em.dma("sp", "wr", wr[:, :, 0:36], v3(wr_d), [WR], [WR])
        em.dma("sp", "brr", brr[:], br_d[0:1, :].partition_broadcast(128), [], [BRR])
        em.dma("sp", "ln", ln[:], ln_d[:, :], [], [LN])
        em.dma("pool", "selb", selb[:].rearrange("p a b -> p (a b)"), selb_d[:, :], [], [SELB])
        em.dma("sp", "g2e", g2e[:].rearrange("p a b -> p (a b)"), g2e_d[:, :], [], [G2E])
        em.ms("dve", ones[:], 1.0, [ONES])
        em.ms("pool", ident[:], 1.0, [IDENT])
        kb.op("pool", lambda e: e.affine_select(out=ident[:], in_=ident[:], pattern=[[-1, 128]], compare_op=ALU.is_equal,
                                                fill=0.0, base=0, channel_multiplier=1), [IDENT], [IDENT])

        def load_w(e):
            s = e % 2
            for k0 in range(0, 8, 4):
                em.dma("pool", f"w1_{s}", w1[s][:, k0:k0 + 4, :], w1_d[e].rearrange("(kc p) n -> p kc n", p=128)[:, k0:k0 + 4, :], [], [W1[s]])
                em.dma("pool", f"w3_{s}", w3[s][:, k0:k0 + 4, :], w3_d[e].rearrange("(kc p) n -> p kc n", p=128)[:, k0:k0 + 4, :], [], [W3[s]])
            for k0 in range(0, 4, 2):
                em.dma("pool", f"w2_{s}", w2[s][:, k0:k0 + 2, :], w2_d[e].rearrange("(kc p) n -> p kc n", p=128)[:, k0:k0 + 2, :], [], [W2[s]])

        load_w(0)
        for tg in range(4):
            ts_ = slice(tg * 512, (tg + 1) * 512)
            em.dma("sp", "xf", xf[:, :, :], v3(x1_d)[:, :, ts_], [], [XF])
            for i in range(8):
                em.cp("pool" if i % 2 else "dve", x1b[:, i, ts_], xf[:, i, :], [XF], [X1B[tg]])
                em.ts("dve" if i % 2 else "pool", out[:, i, ts_], xf[:, i, :], ALPHA, None, ALU.mult, None, [XF], [OUT[tg]])
            for tt_ in range(4):
                p, P = gbank()
                for k in range(8):
                    em.mm(p[:, 0:36], xf[:, k, tt_ * 128:(tt_ + 1) * 128], wr[:, k, 0:36], k == 0, k == 7, [XF, WR], [P])
                em.tt("dve", lgt[:], p[:, 0:36], brr[:], ALU.add, [P, BRR], [LGT])
                em.red(rs[:, 0:1], lgt[:, 0:4], ALU.max, [LGT], [RS])
                em.ts("dve", rs[:, 1:2], rs[:, 0:1], -1.0, None, ALU.mult, None, [RS], [RS])
                em.ms("dve", rs[:, 2:3], 0.0, [RS])
                em.act(rs[:, 4:8], lgt[:, 0:4], AF.Exp, [LGT, RS], [RS], bias=rs[:, 1:2], accum_out=rs[:, 2:3])
                em.rcp(rs[:, 3:4], rs[:, 2:3], [RS], [RS])
                em.ts("dve", rs[:, 8:12], lgt[:, 0:4], rs[:, 0:1], None, ALU.is_ge, None, [LGT, RS], [RS])
                em.ts("dve", em32[:], g2e[:, 0, :], rs[:, 8:9], None, ALU.mult, None, [G2E, RS], [EM32])
                for g in range(1, 4):
                    em.stt("dve", em32[:], g2e[:, g, :], rs[:, 8 + g:9 + g], em32[:], ALU.mult, ALU.add, [G2E, RS, EM32], [EM32])
                em.tt("dve", em2[:], lgt[:, 4:36], em32[:], ALU.mult, [LGT, EM32], [EM2])
                em.ts("dve", em32[:], em32[:], -1.0, 1.0e9, ALU.add, ALU.mult, [EM32], [EM32])
                em.tt("dve", em2[:], em2[:], em32[:], ALU.add, [EM2, EM32], [EM2])
                em.red(rs[:, 12:13], em2[:], ALU.max, [EM2], [RS])
                em.ts("dve", cw[:], em2[:], rs[:, 12:13], None, ALU.is_ge, None, [EM2, RS], [CW])
                em.stt("dve", em32[:], cw[:], -2.0e9, em2[:], ALU.mult, ALU.add, [CW, EM2], [EM32])
                em.red(rs[:, 13:14], em32[:], ALU.max, [EM32], [RS])
                em.ts("dve", em32[:], em32[:], rs[:, 13:14], None, ALU.is_ge, None, [EM32, RS], [EM32])
                em.tt("dve", rs[:, 14:15], rs[:, 13:14], rs[:, 12:13], ALU.subtract, [RS], [RS])
                em.act(rs[:, 15:16], rs[:, 14:15], AF.Exp, [RS], [RS])
                em.ts("dve", rs[:, 15:16], rs[:, 15:16], 1.0, None, ALU.add, None, [RS], [RS])
                em.rcp(rs[:, 16:17], rs[:, 15:16], [RS], [RS])
                em.tt("dve", rs[:, 17:18], rs[:, 16:17], rs[:, 3:4], ALU.mult, [RS], [RS])
                em.tt("dve", rs[:, 18:19], rs[:, 3:4], rs[:, 17:18], ALU.subtract, [RS], [RS])
                em.ts("dve", cw[:], cw[:], rs[:, 17:18], None, ALU.mult, None, [CW, RS], [CW])
                em.stt("dve", cw[:], em32[:], rs[:, 18:19], cw[:], ALU.mult, ALU.add, [EM32, RS, CW], [CW])
                pt_, PT_ = gbank()
                em.tr(pt_[0:32, 0:128], cw[:], ident[:], [CW, IDENT], [PT_])
                em.cp("dve", cwt[:, tg * 512 + tt_ * 128: tg * 512 + (tt_ + 1) * 128], pt_[0:32, 0:128], [PT_], [CWT[tg]])
        tiles = [(e, tg) for e in range(nexp) for tg in range(4)]

        def stage1(ti):
            e, tg = tiles[ti]
            s = e % 2
            hbuf = ti % 2
            ts_ = slice(tg * 512, (tg + 1) * 512)
            pc_, PC_ = gbank()
            em.mm(pc_[:, :], selb[:, e, :], cwt[:, ts_], True, True, [SELB, CWT[tg]], [PC_])
            em.act(cwb[hbuf][:], pc_[:, :], AF.Copy, [PC_], [CWB[hbuf]])
            for f in range(4):
                fs = slice(f * 128, (f + 1) * 128)
                pa, PA = gbank()
                for k in range(8):
                    em.mm(pa[:, :], w1[s][:, k, fs], x1b[:, k, ts_], k == 0, k == 7, [W1[s], X1B[tg]], [PA])
                pb, PB = gbank()
                for k in range(8):
                    em.mm(pb[:, :], w3[s][:, k, fs], x1b[:, k, ts_], k == 0, k == 7, [W3[s], X1B[tg]], [PB])
                em.act(sl_[f % 2][:], pa[:, :], AF.Silu, [PA], [SL[f % 2]])
                em.tt("dve", sl_[f % 2][:], sl_[f % 2][:], pb[:, :], ALU.mult, [SL[f % 2], PB], [SL[f % 2]])
                em.tt("pool", hb[hbuf][:, f, :], sl_[f % 2][:], cwb[hbuf][:], ALU.mult, [SL[f % 2], CWB[hbuf]], [HB[hbuf][f]])

        def stage2(ti):
            e, tg = tiles[ti]
            s = e % 2
            hbuf = ti % 2
            ts_ = slice(tg * 512, (tg + 1) * 512)
            for i in range(8):
                po, PO = gbank()
                for f in range(4):
                    em.mm(po[:, :], w2[s][:, f, i * 128:(i + 1) * 128], hb[hbuf][:, f, :], f == 0, f == 3, [W2[s], HB[hbuf][f]], [PO])
                em.tt("dve", out[:, i, ts_], out[:, i, ts_], po[:, :], ALU.add, [OUT[tg], PO], [OUT[tg]])
        if nexp > 1:
            load_w(1)
        if tiles:
            stage1(0)
        for ti in range(len(tiles)):
            if ti + 1 < len(tiles):
                stage1(ti + 1)
            stage2(ti)
            e_, tg_ = tiles[ti]
            if tg_ == 3 and e_ + 2 < nexp:
                load_w(e_ + 2)
        for tg in range(4):
            ts_ = slice(tg * 512, (tg + 1) * 512)
            layer_norm_fm(kb, em, gbank, out[:, :, ts_], OUT[tg], lambda i: (ob[:, i, :], OB), ln[:, 0:8], ln[:, 8:16], LN, tmp, TMP,
                          ones, ONES, sq, SQ, mean, MEAN, rstd, RSTD)
            em.dma("sp", "ob", v3(o_d)[:, :, ts_], ob[:, :, :], [OB], [])
        kb.finish([OB])


def build_b2(nexp=32):
    nc = bass.Bass("TRN2", target_bir_lowering=False)
    I = lambda n, shp: nc.dram_tensor(n, shp, F32, kind="ExternalInput").ap()
    D = dict(x1T=I("x1T", [1024, NT]), wr=I("wr", [1024, 36]), brr=I("brr", [1, 36]), ew1=I("ew1", [32, 1024, 512]), ew3=I("ew3", [32, 1024, 512]),
             ew2=I("ew2", [32, 512, 1024]), ln=I("ln", [128, 16]), selb=I("selb", [32, 32 * 128]), g2e=I("g2e", [128, 4 * 32]),
             x2T=nc.dram_tensor("x2T", [1024, NT], F32, kind="ExternalOutput").ap())
    with ExitStack() as st:
        kb = KB(nc, st)
        b2_body(nc, kb, D, nexp)
        kb.emit()
    return nc


def b2_consts():
    selb = np.zeros((32, 32, 128), np.float32)
    for e in range(32):
        selb[e, e, :] = 1.0
    g2e = np.zeros((128, 4, 32), np.float32)
    for g in range(4):
        g2e[:, g, g * 8:(g + 1) * 8] = 1.0
    return dict(selb=selb.reshape(32, 32 * 128), g2e=g2e.reshape(128, 128))


def b2_inputs(inp, l, consts):
    wr = np.concatenate([inp["router_group_w"][l], inp["router_expert_w"][l]], axis=1)
    brr = np.concatenate([inp["router_group_b"][l], inp["router_expert_b"][l]])[None, :]
    lnp = np.concatenate([inp["ln2_g"][l].reshape(8, 128).T, inp["ln2_b"][l].reshape(8, 128).T], axis=1)
    return dict(wr=np.ascontiguousarray(wr), brr=np.ascontiguousarray(brr), ew1=inp["exp_w1"][l], ew3=inp["exp_w3"][l], ew2=inp["exp_w2"][l],
                ln=np.ascontiguousarray(lnp, dtype=np.float32), selb=consts["selb"], g2e=consts["g2e"])


L_ = 4


def build_fused(nl=L_):
    nc = bass.Bass("TRN2", target_bir_lowering=False)

    def I(n, shp):
        return nc.dram_tensor(n, list(shp), F32, kind="ExternalInput").ap()

    def T(n, shp):
        return nc.dram_tensor(n, list(shp), F32, kind="Internal").ap()
    x0T = I("x0T", [1024, S])
    a1w = I("a1_w", [nl, 2, 1024, 1024]); a1vec = I("a1_vec", [nl, 2, 128, 22]); a1lw = I("a1_lw", [nl, 2, 128, 256])
    a1g2 = I("a1_g2", [nl, 2, 128, 256]); a1cst = I("a1_cst", [128, 1280])
    a2wf = I("a2_wf", [nl, 2, 1024, 640]); a2wt = I("a2_wt", [nl, 2, 1024, 140]); a2w1 = I("a2_w1", [nl, 128, 32 * 128])
    a2pe = I("a2_peT", [nl, 128, 32]); a2w2 = I("a2_w2", [nl, 128, 192]); a2btc = I("a2_btc", [2, 128, 4 * 256]); a2bts = I("a2_bts", [2, 128, 4 * TW])
    a2c = {k: I("a2_" + k, shp) for k, shp in (("mkc", [128, 256]), ("mks", [128, TW]), ("mkw", [128, TW]), ("m12", [128, 256]), ("rv", [128, 1]),
                                                 ("c2s", [128, 128]), ("exd", [64, 32 * 128]), ("hsel", [128, 256]))}
    b1wg = I("b1_wg", [nl, 1024, 2048]); b1wur = I("b1_wur", [nl, 512, 1024]); b1wun = I("b1_wun", [nl, 512, 1024]); b1wo = I("b1_wo", [nl, 1024, 1024])
    b1ln = I("b1_ln", [nl, 128, 16])
    b2wr = I("b2_wr", [nl, 1024, 36]); b2br = I("b2_brr", [nl, 1, 36]); b2e1 = I("b2_ew1", [nl, 32, 1024, 512]); b2e3 = I("b2_ew3", [nl, 32, 1024, 512])
    b2e2 = I("b2_ew2", [nl, 32, 512, 1024]); b2ln = I("b2_ln", [nl, 128, 16]); b2selb = I("b2_selb", [32, 32 * 128]); b2g2e = I("b2_g2e", [128, 128])
    outT = nc.dram_tensor("outT", [1024, S], F32, kind="ExternalOutput").ap()
    XT = [T("xt0", [1024, S]), T("xt1", [1024, S])]
    YR = T("yr", [512, S]); YN = T("yn", [512, S]); X1 = T("x1", [1024, S])

    with ExitStack() as st:
        kb = KB(nc, st)
        pn = [0]

        def phase(fn, D, *args):
            with ExitStack() as pst:
                kb.pstack = pst
                kb.prefix = f"p{pn[0]}_"
                pn[0] += 1
                fn(nc, kb, D, *args)
                kb.emit()
            kb.pstack = st
        for l in range(nl):
            xin = x0T if l == 0 else XT[l % 2]
            xout = outT if l == nl - 1 else XT[(l + 1) % 2]
            for hh in range(2):
                phase(a1_body, dict(xT=xin, w=a1w[l, hh], vec=a1vec[l, hh], lw=a1lw[l, hh], g2=a1g2[l, hh], cst=a1cst,
                                    yT=YR[hh * 256:(hh + 1) * 256, :]))
            for g in range(2):
                D = dict(xT=xin, wf=a2wf[l, g], wt=a2wt[l, g], w1=a2w1[l], peT=a2pe[l], w2=a2w2[l], btc=a2btc[g], bts=a2bts[g],
                         yT=YN[g * 256:(g + 1) * 256, :])
                D.update(a2c)
                phase(a2_body, D)
            for hf in range(2):
                ts = slice(hf * NT, (hf + 1) * NT)
                phase(b1_body, dict(xT=xin[:, ts], yrT=YR[:, ts], ynT=YN[:, ts], wg=b1wg[l], wur=b1wur[l], wun=b1wun[l], wo=b1wo[l], ln=b1ln[l],
                                    x1T=X1[:, ts]))
            for hf in range(2):
                ts = slice(hf * NT, (hf + 1) * NT)
                phase(b2_body, dict(x1T=X1[:, ts], wr=b2wr[l], brr=b2br[l], ew1=b2e1[l], ew3=b2e3[l], ew2=b2e2[l], ln=b2ln[l], selb=b2selb,
                                    g2e=b2g2e, x2T=xout[:, ts]))
        print("FUSED instructions:", kb.n_ins, kb.cnt)
    return nc


def fused_inputs(inp, nl=L_):
    c2 = a2_consts(); cb2 = b2_consts()
    a1 = [[a1_inputs(inp, l, 0, hh) for hh in range(2)] for l in range(nl)]
    a2 = [[a2_inputs(inp, l, g, c2) for g in range(2)] for l in range(nl)]
    b1 = [b1_inputs(inp, l) for l in range(nl)]
    b2 = [b2_inputs(inp, l, cb2) for l in range(nl)]
    st = lambda f: np.ascontiguousarray(np.stack(f, axis=0), dtype=np.float32)
    m = {}
    for k, nm in (("w", "a1_w"), ("vec", "a1_vec"), ("lw", "a1_lw"), ("g2", "a1_g2")):
        m[nm] = st([st([a1[l][hh][k] for hh in range(2)]) for l in range(nl)])
    m["a1_cst"] = a1[0][0]["cst"]
    for k, nm in (("wf", "a2_wf"), ("wt", "a2_wt")):
        m[nm] = st([st([a2[l][g][k] for g in range(2)]) for l in range(nl)])
    for k, nm in (("w1", "a2_w1"), ("peT", "a2_peT"), ("w2", "a2_w2")):
        m[nm] = st([a2[l][0][k] for l in range(nl)])
    m["a2_btc"] = st([a2[0][g]["btc"] for g in range(2)]); m["a2_bts"] = st([a2[0][g]["bts"] for g in range(2)])
    for k in ("mkc", "mks", "mkw", "m12", "rv", "c2s", "exd", "hsel"):
        m["a2_" + k] = a2[0][0][k]
    for k, nm in (("wg", "b1_wg"), ("wur", "b1_wur"), ("wun", "b1_wun"), ("wo", "b1_wo"), ("ln", "b1_ln")):
        m[nm] = st([b1[l][k] for l in range(nl)])
    for k, nm in (("wr", "b2_wr"), ("brr", "b2_brr"), ("ln", "b2_ln")):
        m[nm] = st([b2[l][k] for l in range(nl)])
    m["b2_ew1"] = np.ascontiguousarray(inp["exp_w1"][:nl], dtype=np.float32)
    m["b2_ew3"] = np.ascontiguousarray(inp["exp_w3"][:nl], dtype=np.float32)
    m["b2_ew2"] = np.ascontiguousarray(inp["exp_w2"][:nl], dtype=np.float32)
    m["b2_selb"] = cb2["selb"]; m["b2_g2e"] = cb2["g2e"]
    return m

_NC = {}


def kernel(**inputs):
    inp = {k: np.asarray(v) for k, v in inputs.items()}
    if "nc" not in _NC:
        _NC["nc"] = build_fused(L_)
    m = fused_inputs(inp, L_)
    x = inp["x"].astype(np.float32, copy=False)
    B = x.shape[0]
    xT = [np.ascontiguousarray(x[b].T) for b in range(B)]
    maps = []
    for c in range(8):
        mm_ = dict(m); mm_["x0T"] = xT[c // 2]; maps.append(mm_)
    res = run_bass_kernel_spmd(_NC["nc"], maps, core_ids=list(range(8)))
    out = np.stack([res.results[2 * b]["outT"].T for b in range(B)], axis=0)
    return np.ascontiguousarray(out, dtype=np.float32)
```

```python
import numpy as np
from contextlib import ExitStack
import concourse.bass as bass
import concourse.mybir as mybir
from concourse.bass_utils import run_bass_kernel_spmd

F32 = mybir.dt.float32
BF16 = mybir.dt.bfloat16
AF = mybir.ActivationFunctionType
ALU = mybir.AluOpType
AX = mybir.AxisListType


class Buf:
    __slots__ = ("name", "w", "r", "excl")

    def __init__(self, name="", excl=False):
        self.name = name
        self.excl = excl
        self.w = None
        self.r = {}


class KB:
    def __init__(self, nc, stack):
        self.nc = nc
        self.stack = stack
        self.pstack = stack
        self.prefix = ""
        self.names = ["pe", "act", "dve", "pool", "sp"]
        self.prog = {e: [] for e in self.names}
        self.sem = {e: stack.enter_context(nc.semaphore("s_" + e)) for e in self.names}
        self.cnt = {e: 0 for e in self.names}
        self.seen = {e: {} for e in self.names}
        self.dsem = {}
        self.n_ins = 0
        self.nw = {e: 0 for e in self.names}

    def sb(self, name, shape, dt=F32):
        return self.pstack.enter_context(self.nc.sbuf_tensor(self.prefix + "sb_" + name, list(shape), dt))

    def ps(self, name, shape, dt=F32):
        return self.pstack.enter_context(self.nc.psum_tensor(self.prefix + "ps_" + name, list(shape), dt))

    def _semh(self, key):
        if key in self.sem:
            return self.sem[key]
        return self.dsem[key][0]

    def _waits(self, eng, reads, writes):
        need = {}

        def add(d):
            if d is None:
                return
            k, v = d
            if need.get(k, 0) < v:
                need[k] = v
        for b in reads:
            add(b.w)
        for b in writes:
            add(b.w)
            for k, v in b.r.items():
                add((k, v))
        out = []
        seen = self.seen[eng]
        for k, v in need.items():
            if k == "pe" and eng == "pe":
                continue
            if seen.get(k, 0) >= v:
                continue
            seen[k] = v
            out.append((self._semh(k), v))
        return out

    def _mark(self, tok, reads, writes):
        for b in writes:
            b.w = tok
            b.r = {}
        k, v = tok
        for b in reads:
            if b.r.get(k, 0) < v:
                b.r[k] = v

    def op(self, eng, fn, reads=(), writes=()):
        ex = [b for b in reads if b.excl]
        if ex:
            writes = list(writes) + ex
        waits = self._waits(eng, reads, writes)
        self.nw[eng] += len(waits)
        self.cnt[eng] += 1
        tok = (eng, self.cnt[eng])
        sem = self.sem[eng]

        def run(e, waits=waits, fn=fn, sem=sem):
            for s, v in waits:
                e.wait_ge(s, v)
            fn(e).then_inc(sem, 1)
        self.prog[eng].append(run)
        self._mark(tok, reads, writes)
        self.n_ins += 1

    def dma(self, q, key, fn, reads=(), writes=(), n=1):
        key = "d_" + key
        if key not in self.dsem:
            self.dsem[key] = [self.stack.enter_context(self.nc.semaphore(key)), 0]
        waits = self._waits(q, reads, writes)
        self.dsem[key][1] += 16 * n
        tok = (key, self.dsem[key][1])
        sem = self.dsem[key][0]

        def run(e, waits=waits, fn=fn, sem=sem):
            for s, v in waits:
                e.wait_ge(s, v)
            fn(e, sem)
        self.prog[q].append(run)
        self._mark(tok, reads, writes)
        self.n_ins += n

    def finish(self, bufs):
        waits = self._waits("sp", bufs, bufs)

        def run(e, waits=waits):
            for s, v in waits:
                e.wait_ge(s, v)
        self.prog["sp"].append(run)

    def emit(self):
        nc = self.nc
        prog = self.prog
        self.prog = {e: [] for e in self.names}
        with nc.Block() as block:
            @block.sync
            def _(e):
                for f in prog["sp"]:
                    f(e)

            @block.tensor
            def _(e):
                for f in prog["pe"]:
                    f(e)

            @block.scalar
            def _(e):
                for f in prog["act"]:
                    f(e)

            @block.vector
            def _(e):
                for f in prog["dve"]:
                    f(e)

            @block.gpsimd
            def _(e):
                for f in prog["pool"]:
                    f(e)


S = 4096
NS = 512
NSEG = S // NS
CH = 128
NCH = NS // CH
GN_EPS = 64e-5


def a1_body(nc, kb, D, nseg=NSEG, debug=False):
    Em_ = globals().get('Em')
    if Em_ is None:
        from a2 import Em as Em_
    xT_d, w_d, vec_d, lw_d, g2_d, cst_d, yT_d = (D[k] for k in ("xT", "w", "vec", "lw", "g2", "cst", "yT"))
    xT_v = xT_d.rearrange("(kc p) t -> p kc t", p=128)
    w_v = w_d.rearrange("(kc p) n -> p kc n", p=128)
    if True:
        sb, ps = kb.sb, kb.ps
        NXS = 3
        xs = [sb(f"xs{i}", [128, 8, NS], BF16) for i in range(NXS)]; XS = [Buf() for _ in range(NXS)]
        wsb = sb("wsb", [128, 8, 1024], BF16); WSB = Buf()
        vec = sb("vec", [128, 22]); VEC = Buf()
        vx = sb("vx", [128, 8]); VX = Buf()
        lw = sb("lw", [128, 256]); LW = Buf()
        g2 = sb("g2", [128, 256]); G2 = Buf()
        cst = sb("cst", [128, 1280]); CST = Buf()
        MASK1 = cst[:, 0:512]; MASK4 = cst[:, 512:1024]; MSL = cst[:, 1024:1152]; BONES = cst[:, 1152:1280]
        ident = sb("ident", [128, 128]); IDENT = Buf()
        car = sb("car", [128, 8]); CAR = Buf()
        zr = [sb(f"zr{i}", [128, NS + 1]) for i in range(2)]; ZR = [Buf() for _ in range(2)]
        dtmp = sb("dtmp", [128, NS]); DTMP = Buf()
        zs = [sb(f"zs{j}", [128, NS]) for j in range(8)]; ZS = [Buf() for _ in range(8)]
        tw = sb("tw", [128, NS]); TW = Buf()
        sg = sb("sg", [128, NS]); SG = Buf()
        tnames = ["nld", "cw", "ew", "ewi", "ewx", "aa", "kkn", "sq", "t1", "k2", "bh", "kh", "e1"]
        T = {n: sb("t_" + n, [128, NS]) for n in tnames}; TB = {n: Buf() for n in tnames}
        ar = [sb(f"ar{h}", [128, NCH, 2 * CH]) for h in range(2)]; AR = [Buf() for _ in range(2)]
        bt = [sb(f"bt{h}", [128, NS]) for h in range(2)]; BT = [Buf() for _ in range(2)]
        kt = [sb(f"kt{h}", [128, NS]) for h in range(2)]; KT = [Buf() for _ in range(2)]
        gg = [sb(f"gg{h}", [128, NS]) for h in range(2)]; GG = [Buf() for _ in range(2)]
        bon = [sb(f"bon{h}", [128, NS]) for h in range(2)]; BON = [Buf() for _ in range(2)]
        yf = [sb(f"yf{h}", [128, NS]) for h in range(2)]; YF = [Buf() for _ in range(2)]
        wc = [sb(f"wc{h}", [128, NCH]) for h in range(2)]; WC = [Buf() for _ in range(2)]
        bhT = sb("bhT", [128, NCH, 256]); BHT = [Buf() for _ in range(NCH)]
        khT = sb("khT", [128, NCH, 256]); KHT = [Buf() for _ in range(NCH)]
        vT = sb("vT", [128, NCH, 256]); VT = [Buf() for _ in range(NCH)]
        mabk = [sb(f"mabk{h}", [128, 512]) for h in range(4)]; MABK = [Buf() for _ in range(4)]
        nm = [[sb(f"nm{h}_{i}", [128, 256]) for i in range(2)] for h in range(4)]; NM = [[Buf(), Buf()] for _ in range(4)]
        qq = [[sb(f"qq{h}_{i}", [128, 128]) for i in range(2)] for h in range(4)]; QQ = [[Buf(), Buf()] for _ in range(4)]
        xsb = [sb(f"xsb{h}", [128, 64]) for h in range(4)]; XSB = [Buf() for _ in range(4)]
        usb = [sb(f"usb{h}", [128, 64]) for h in range(4)]; USB = [Buf() for _ in range(4)]
        stt = [[sb(f"st{hp}_{i}", [128, 64]) for i in range(2)] for hp in range(2)]
        STT = [[[Buf(), Buf()] for _ in range(2)] for hp in range(2)]
        ytok = sb("ytok", [128, 256]); YTOK = Buf()
        ysq = sb("ysq", [128, 256]); YSQ = Buf()
        yn = sb("yn", [128, 256]); YN = Buf()
        sts = sb("sts", [128, 32]); STS = Buf()
        osb = [sb(f"osb{h}", [128, NS]) for h in range(2)]; OSB = [Buf() for _ in range(2)]
        NPA = 4
        pa = [ps(f"pa{i}", [128, 512]) for i in range(NPA)]; PA = [Buf(excl=True) for _ in range(NPA)]
        pq = [ps(f"pq{i}", [128, 512]) for i in range(4)]; PQB = [Buf(excl=True) for _ in range(4)]

        def ld(q, key, out, in_, B):
            kb.dma(q, key, lambda e, s: e.dma_start(out=out, in_=in_).then_inc(s, 16), writes=[B])
        ld("sp", "vec", vec[:], vec_d[:, :], VEC)
        ld("sp", "lw", lw[:], lw_d[:, :], LW)
        ld("sp", "g2", g2[:], g2_d[:, :], G2)
        ld("sp", "cst", cst[:], cst_d[:, :], CST)
        for kc in range(0, 8, 4):
            kb.dma("pool", "wsb", lambda e, s, kc=kc: e.dma_start(out=wsb[:, kc:kc + 4, :], in_=w_v[:, kc:kc + 4, :]).then_inc(s, 16), writes=[WSB])
        kb.op("pool", lambda e: e.memset(ident[:], 1.0), writes=[IDENT])
        kb.op("pool", lambda e: e.affine_select(out=ident[:], in_=ident[:], pattern=[[-1, 128]], compare_op=ALU.is_equal,
                                                fill=0.0, base=0, channel_multiplier=1), reads=[IDENT], writes=[IDENT])
        kb.op("dve", lambda e: e.memset(car[:], 0.0), writes=[CAR])
        kb.op("dve", lambda e: e.tensor_scalar(vx[:, 0:2], vec[:, 8:10], -1.0, None, ALU.mult), reads=[VEC], writes=[VX])
        kb.op("dve", lambda e: e.tensor_scalar(vx[:, 2:4], vec[:, 14:16], -1.0, 1.0, ALU.mult, ALU.add), reads=[VEC, VX], writes=[VX])
        for hp in range(2):
            for i in range(2):
                kb.op("dve", lambda e, hp=hp, i=i: e.memset(stt[hp][i][:], 0.0), writes=STT[hp][i])

        def load_x(sgi):
            sl = sgi % NXS
            kb.dma("pool", f"xs{sl}", lambda e, s, sl=sl, sgi=sgi: e.dma_start(
                out=xs[sl][:, :, :], in_=xT_v[:, :, sgi * NS:(sgi + 1) * NS]).then_inc(s, 16), writes=[XS[sl]])

        load_x(0)
        pai = [0]

        def next_pa():
            i = pai[0] % NPA
            pai[0] += 1
            return pa[i], PA[i]

        def mm512(lhsT_fn, rhs_fn, nk, reads, M=128):
            p, P = next_pa()
            for k in range(nk):
                a_, b_ = lhsT_fn(k), rhs_fn(k)
                kb.op("pe", lambda e, k=k, p=p, a_=a_, b_=b_: e.matmul(p[0:M, :], a_, b_, start=(k == 0), stop=(k == nk - 1)),
                      reads=reads, writes=[P])
            return p, P

        ping = [0, 0]
        dbg_n = [0]

        def dbg(name, ap, B):
            if not debug:
                return
            shp = list(ap.shape)
            d = nc.dram_tensor("dbg_" + name, shp, F32, kind="ExternalOutput").ap()
            dbg_n[0] += 1
            cntv = dbg_n[0] * 16

            def f(e, s, d=d, ap=ap, cntv=cntv):
                e.dma_start(out=d, in_=ap).then_inc(s, 16)
                e.wait_ge(s, cntv)
            kb.dma("sp", "dbg", f, reads=[B])
        for sgi in range(nseg):
            sl = sgi % NXS
            if sgi + 1 < nseg:
                load_x(sgi + 1)
            for j in range(8):
                p, P = mm512(lambda k, j=j: wsb[:, k, j * 128:(j + 1) * 128], lambda k, sl=sl: xs[sl][:, k, :], 8, [WSB, XS[sl]])
                z, Z = zr[j % 2], ZR[j % 2]
                kb.op("pool", lambda e, z=z, j=j: e.tensor_copy(z[:, 0:1], car[:, j:j + 1]), reads=[CAR], writes=[Z])
                kb.op("act", lambda e, z=z, p=p: e.activation(out=z[:, 1:NS + 1], in_=p[:, :], func=AF.Copy), reads=[P], writes=[Z])
                kb.op("pool", lambda e, z=z, j=j: e.tensor_copy(car[:, j:j + 1], z[:, NS:NS + 1]), reads=[Z], writes=[CAR])
                kb.op("dve", lambda e, z=z: e.tensor_tensor(dtmp[:], z[:, 0:NS], z[:, 1:NS + 1], ALU.subtract), reads=[Z], writes=[DTMP])
                kb.op("dve", lambda e, z=z, j=j: e.scalar_tensor_tensor(zs[j][:], dtmp[:], vec[:, j:j + 1], z[:, 1:NS + 1], ALU.mult, ALU.add),
                      reads=[DTMP, Z, VEC], writes=[ZS[j]])
            L1, L2 = zs[6], zs[7]
            for j in range(8):
                dbg(f"zs{j}", zs[j][:], ZS[j])
            kb.op("act", lambda e: e.activation(out=tw[0:64, :], in_=L1[0:64, :], func=AF.Tanh), reads=[ZS[6]], writes=[TW])
            kb.op("act", lambda e: e.activation(out=sg[:], in_=L2[:], func=AF.Sigmoid), reads=[ZS[7]], writes=[SG])
            for hp in range(2):
                Rz, Kz, Vz = zs[0 + hp], zs[2 + hp], zs[4 + hp]
                RZ, KZ, VZ = ZS[0 + hp], ZS[2 + hp], ZS[4 + hp]
                cs = slice(hp * 128, (hp + 1) * 128)
                p, P = mm512(lambda k: lw[64:128, cs], lambda k: L1[64:128, :], 1, [LW, ZS[6]])
                kb.op("act", lambda e, p=p, hp=hp: e.activation(out=T["aa"][:], in_=p[:, :], func=AF.Sigmoid, bias=vec[:, 10 + hp:11 + hp]),
                      reads=[P, VEC], writes=[TB["aa"]])
                p, P = mm512(lambda k: g2[:, cs], lambda k: sg[:], 1, [G2, SG])
                kb.op("act", lambda e, p=p, hp=hp: e.activation(out=gg[hp][:], in_=p[:, :], func=AF.Copy), reads=[P], writes=[GG[hp]])
                p, P = mm512(lambda k: lw[0:64, cs], lambda k: tw[0:64, :], 1, [LW, TW])
                kb.op("act", lambda e, p=p, hp=hp: e.activation(out=T["e1"][:], in_=p[:, :], func=AF.Exp, bias=vx[:, hp:hp + 1], scale=-1.0),
                      reads=[P, VX], writes=[TB["e1"]])
                kb.op("act", lambda e: e.activation(out=T["e1"][:], in_=T["e1"][:], func=AF.Ln, bias=1.0), reads=[TB["e1"]], writes=[TB["e1"]])
                kb.op("act", lambda e: e.activation(out=T["nld"][:], in_=T["e1"][:], func=AF.Exp, bias=-0.5, scale=-1.0),
                      reads=[TB["e1"]], writes=[TB["nld"]])
                kb.op("dve", lambda e: e.tensor_tensor_scan(T["cw"][:], MASK1, T["nld"][:], 0.0, ALU.mult, ALU.add),
                      reads=[CST, TB["nld"]], writes=[TB["cw"]])
                kb.op("act", lambda e: e.activation(out=T["ew"][:], in_=T["cw"][:], func=AF.Exp, scale=-1.0), reads=[TB["cw"]], writes=[TB["ew"]])
                kb.op("act", lambda e: e.activation(out=T["ewi"][:], in_=T["cw"][:], func=AF.Exp), reads=[TB["cw"]], writes=[TB["ewi"]])
                kb.op("pool", lambda e: e.tensor_tensor(T["ewx"][:], T["cw"][:], T["nld"][:], ALU.subtract), reads=[TB["cw"], TB["nld"]], writes=[TB["ewx"]])
                kb.op("act", lambda e: e.activation(out=T["ewx"][:], in_=T["ewx"][:], func=AF.Exp, scale=-1.0), reads=[TB["ewx"]], writes=[TB["ewx"]])
                kb.op("pool", lambda e, hp=hp: e.tensor_copy(wc[hp][:], T["ew"][:].rearrange("p (c t) -> p c t", t=CH)[:, :, CH - 1]),
                      reads=[TB["ew"]], writes=[WC[hp]])
                kb.op("dve", lambda e, hp=hp, Kz=Kz: e.tensor_scalar(T["kkn"][:], Kz[:], vec[:, 12 + hp:13 + hp], None, ALU.mult),
                      reads=[KZ, VEC], writes=[TB["kkn"]])
                kb.op("pool", lambda e: e.tensor_tensor(T["sq"][:], T["kkn"][:], T["kkn"][:], ALU.mult), reads=[TB["kkn"]], writes=[TB["sq"]])
                p, P = mm512(lambda k: BONES, lambda k: T["sq"][:], 1, [CST, TB["sq"]])
                kb.op("act", lambda e, p=p: e.activation(out=T["sq"][:], in_=p[:, :], func=AF.Sqrt), reads=[P], writes=[TB["sq"]])
                kb.op("dve", lambda e: e.tensor_scalar(T["sq"][:], T["sq"][:], 1e-12, None, ALU.max), reads=[TB["sq"]], writes=[TB["sq"]])
                kb.op("dve", lambda e: e.reciprocal(T["sq"][:], T["sq"][:]), reads=[TB["sq"]], writes=[TB["sq"]])
                kb.op("dve", lambda e: e.tensor_tensor(T["kkn"][:], T["kkn"][:], T["sq"][:], ALU.mult), reads=[TB["kkn"], TB["sq"]], writes=[TB["kkn"]])
                kb.op("pool", lambda e, hp=hp: e.tensor_scalar(T["t1"][:], T["aa"][:], vec[:, 14 + hp:15 + hp], vx[:, 2 + hp:3 + hp], ALU.mult, ALU.add),
                      reads=[TB["aa"], VEC, VX], writes=[TB["t1"]])
                kb.op("pool", lambda e, Kz=Kz: e.tensor_tensor(T["k2"][:], Kz[:], T["t1"][:], ALU.mult), reads=[KZ, TB["t1"]], writes=[TB["k2"]])
                arv = ar[hp]
                kb.op("dve", lambda e, arv=arv: e.scalar_tensor_tensor(arv[:, :, 0:CH], T["kkn"][:].rearrange("p (c t) -> p c t", t=CH), -1.0,
                                                                      T["ewx"][:].rearrange("p (c t) -> p c t", t=CH), ALU.mult, ALU.mult),
                      reads=[TB["kkn"], TB["ewx"]], writes=[AR[hp]])
                kb.op("pool", lambda e, arv=arv, Rz=Rz: e.tensor_tensor(arv[:, :, CH:2 * CH], Rz[:].rearrange("p (c t) -> p c t", t=CH),
                                                                       T["ew"][:].rearrange("p (c t) -> p c t", t=CH), ALU.mult),
                      reads=[RZ, TB["ew"]], writes=[AR[hp]])
                kb.op("dve", lambda e: e.tensor_tensor(T["t1"][:], T["kkn"][:], T["aa"][:], ALU.mult), reads=[TB["kkn"], TB["aa"]], writes=[TB["t1"]])
                kb.op("dve", lambda e, hp=hp: e.tensor_tensor(bt[hp][:], T["t1"][:], T["ewi"][:], ALU.mult), reads=[TB["t1"], TB["ewi"]], writes=[BT[hp]])
                kb.op("pool", lambda e, hp=hp: e.tensor_tensor(kt[hp][:], T["k2"][:], T["ewi"][:], ALU.mult), reads=[TB["k2"], TB["ewi"]], writes=[KT[hp]])
                wcb = wc[hp][:].unsqueeze(2).to_broadcast([128, NCH, CH])
                kb.op("dve", lambda e, hp=hp, wcb=wcb: e.tensor_tensor(T["bh"][:].rearrange("p (c t) -> p c t", t=CH),
                                                                      bt[hp][:].rearrange("p (c t) -> p c t", t=CH), wcb, ALU.mult),
                      reads=[BT[hp], WC[hp]], writes=[TB["bh"]])
                kb.op("pool", lambda e, hp=hp, wcb=wcb: e.tensor_tensor(T["kh"][:].rearrange("p (c t) -> p c t", t=CH),
                                                                       kt[hp][:].rearrange("p (c t) -> p c t", t=CH), wcb, ALU.mult),
                      reads=[KT[hp], WC[hp]], writes=[TB["kh"]])
                kb.op("dve", lambda e, hp=hp, Rz=Rz: e.scalar_tensor_tensor(T["t1"][:], Rz[:], vec[:, 16 + hp:17 + hp], T["k2"][:], ALU.mult, ALU.mult),
                      reads=[RZ, VEC, TB["k2"], TB["t1"]], writes=[TB["t1"]])
                p, P = mm512(lambda k: BONES, lambda k: T["t1"][:], 1, [CST, TB["t1"]])
                kb.op("dve", lambda e, p=p, hp=hp, Vz=Vz: e.tensor_tensor(bon[hp][:], p[:, :], Vz[:], ALU.mult), reads=[P, VZ], writes=[BON[hp]])
                for n_ in ("nld", "cw", "ew", "ewi", "ewx", "aa", "kkn", "k2", "bh", "kh"):
                    dbg(f"{n_}{hp}", T[n_][:], TB[n_])
                dbg(f"bt{hp}", bt[hp][:], BT[hp]); dbg(f"kt{hp}", kt[hp][:], KT[hp]); dbg(f"ar{hp}", ar[hp][:].rearrange("p c t -> p (c t)"), AR[hp])
                dbg(f"gg{hp}", gg[hp][:], GG[hp]); dbg(f"bon{hp}", bon[hp][:], BON[hp]); dbg(f"wc{hp}", wc[hp][:], WC[hp])
                for c in range(NCH):
                    for (src, SRC, dst, DST) in ((T["bh"], TB["bh"], bhT, BHT), (T["kh"], TB["kh"], khT, KHT), (Vz, VZ, vT, VT)):
                        pbank, PT = next_pa()
                        pt = pbank[:, 0:128]
                        kb.op("pe", lambda e, pt=pt, src=src, c=c: e.transpose(pt, src[:, c * CH:(c + 1) * CH], ident[:]),
                              reads=[SRC, IDENT], writes=[PT])
                        kb.op("act", lambda e, pt=pt, dst=dst, c=c, cs=cs: e.activation(out=dst[:, c, cs], in_=pt, func=AF.Copy),
                              reads=[PT], writes=[DST[c]])
            em = Em_(kb)
            for c in range(NCH):
                csl = slice(c * CH, (c + 1) * CH)
                HD = []
                for h in range(4):
                    hp, hh = h // 2, h % 2
                    HD.append(dict(h=h, hp=hp, hh=hh, rows=slice(64 * hh, 64 * hh + 64), hc=slice(h * 64, (h + 1) * 64), q=pq[h], Q=PQB[h]))
                for d in HD:
                    em.mm(d["q"][:, 0:256], bt[d["hp"]][d["rows"], csl], ar[d["hp"]][d["rows"], c, :], True, True, [BT[d["hp"]], AR[d["hp"]]], [d["Q"]])
                    em.mm(d["q"][:, 256:512], kt[d["hp"]][d["rows"], csl], ar[d["hp"]][d["rows"], c, :], True, True, [KT[d["hp"]], AR[d["hp"]]], [d["Q"]])
                for d in HD:
                    h = d["h"]
                    em.tt("dve", mabk[h][:], d["q"][:, :], MASK4, ALU.mult, [d["Q"], CST], [MABK[h]])
                for d in HD:
                    em.mm(d["q"][:, 0:128], ar[d["hp"]][d["rows"], c, 0:CH], bt[d["hp"]][d["rows"], csl], True, True, [BT[d["hp"]], AR[d["hp"]]], [d["Q"]])
                for d in HD:
                    h = d["h"]
                    em.tt("dve", nm[h][0][:, 0:128], d["q"][:, 0:128], MSL, ALU.mult, [d["Q"], CST], [NM[h][0]])
                    em.cp("pool", nm[h][0][:, 128:256], mabk[h][:, 0:128], [MABK[h]], [NM[h][0]])
                    em.tt("pool", qq[h][0][:], mabk[h][:, 0:128], ident[:], ALU.add, [MABK[h], IDENT], [QQ[h][0]])
                for k in range(6):
                    a_, b_ = k % 2, (k + 1) % 2
                    wdt = 256 if k < 5 else 128
                    for d in HD:
                        h = d["h"]
                        em.mm(d["q"][:, 0:128], nm[h][a_][:, 128:256], nm[h][a_][:, 0:128], True, True, [NM[h][a_]], [d["Q"]])
                        if k < 5:
                            em.mm(d["q"][:, 128:256], nm[h][a_][:, 0:128], nm[h][a_][:, 128:256], True, True, [NM[h][a_]], [d["Q"]])
                    for d in HD:
                        h = d["h"]
                        if h % 2 == 0:
                            em.cp("dve", nm[h][b_][:, 0:wdt], d["q"][:, 0:wdt], [d["Q"]], [NM[h][b_]])
                        else:
                            em.act(nm[h][b_][:, 0:wdt], d["q"][:, 0:wdt], AF.Copy, [d["Q"]], [NM[h][b_]])
                    for d in HD:
                        h = d["h"]
                        em.mm(d["q"][:, 256:384], nm[h][b_][:, 0:128], qq[h][a_][:], True, True, [NM[h][b_], QQ[h][a_]], [d["Q"]])
                    for d in HD:
                        h = d["h"]
                        em.tt("dve", qq[h][b_][:], d["q"][:, 256:384], qq[h][a_][:], ALU.add, [d["Q"], QQ[h][a_]], [QQ[h][b_]])
                qf = 0
                so = [ping[0], ping[1]]
                for d in HD:
                    h, hp, rows, hc = d["h"], d["hp"], d["rows"], d["hc"]
                    So = STT[hp][so[hp]][d["hh"]]
                    em.mm(d["q"][:, 0:64], mabk[h][:, 256:384], vT[:, c, hc], True, False, [MABK[h], VT[c]], [d["Q"]])
                    em.mm(d["q"][:, 0:64], ar[hp][rows, c, 0:CH], stt[hp][so[hp]][rows, :], False, True, [AR[hp], So], [d["Q"]])
                for d in HD:
                    h = d["h"]
                    if h % 2 == 0:
                        em.act(xsb[h][:], d["q"][:, 0:64], AF.Copy, [d["Q"]], [XSB[h]])
                    else:
                        em.cp("dve", xsb[h][:], d["q"][:, 0:64], [d["Q"]], [XSB[h]])
                for d in HD:
                    h = d["h"]
                    em.mm(d["q"][:, 64:128], qq[h][qf][:], xsb[h][:], True, True, [QQ[h][qf], XSB[h]], [d["Q"]])
                for d in HD:
                    h = d["h"]
                    if h % 2 == 0:
                        em.act(usb[h][:], d["q"][:, 64:128], AF.Copy, [d["Q"]], [USB[h]])
                    else:
                        em.cp("dve", usb[h][:], d["q"][:, 64:128], [d["Q"]], [USB[h]])
                for d in HD:
                    h, hp, rows, hc = d["h"], d["hp"], d["rows"], d["hc"]
                    So = STT[hp][so[hp]][d["hh"]]
                    em.mm(d["q"][:, 256:320], ar[hp][rows, c, CH:2 * CH], stt[hp][so[hp]][rows, :], True, False, [AR[hp], So], [d["Q"]])
                    em.mm(d["q"][:, 256:320], mabk[h][:, 128:256], usb[h][:], False, False, [MABK[h], USB[h]], [d["Q"]])
                    em.mm(d["q"][:, 256:320], mabk[h][:, 384:512], vT[:, c, hc], False, True, [MABK[h], VT[c]], [d["Q"]])
                    em.mm(d["q"][rows, 128:192], bhT[:, c, hc], usb[h][:], True, False, [BHT[c], USB[h]], [d["Q"]])
                    em.mm(d["q"][rows, 128:192], khT[:, c, hc], vT[:, c, hc], False, True, [KHT[c], VT[c]], [d["Q"]])
                for d in HD:
                    h, hp, rows, hc = d["h"], d["hp"], d["rows"], d["hc"]
                    sn = 1 - so[hp]
                    em.stt("dve", stt[hp][sn][rows, :], stt[hp][so[hp]][rows, :], wc[hp][rows, c:c + 1], d["q"][rows, 128:192], ALU.mult, ALU.add,
                           [STT[hp][so[hp]][d["hh"]], WC[hp], d["Q"]], [STT[hp][sn][d["hh"]]])
                    em.act(ytok[:, hc], d["q"][:, 256:320], AF.Copy, [d["Q"]], [YTOK])
                    em.act(ysq[:, hc], d["q"][:, 256:320], AF.Square, [d["Q"]], [YSQ])
                ping[0], ping[1] = 1 - so[0], 1 - so[1]
                kb.op("dve", lambda e: e.tensor_reduce(sts[:, 0:4], ytok[:].rearrange("p (h v) -> p h v", v=64), AX.X, ALU.add), reads=[YTOK], writes=[STS])
                kb.op("dve", lambda e: e.tensor_reduce(sts[:, 4:8], ysq[:].rearrange("p (h v) -> p h v", v=64), AX.X, ALU.add), reads=[YSQ, STS], writes=[STS])
                kb.op("dve", lambda e: e.tensor_scalar(sts[:, 8:12], sts[:, 0:4], 1.0 / 64, None, ALU.mult), reads=[STS], writes=[STS])
                kb.op("dve", lambda e: e.tensor_tensor(sts[:, 12:16], sts[:, 8:12], sts[:, 8:12], ALU.mult), reads=[STS], writes=[STS])
                kb.op("dve", lambda e: e.scalar_tensor_tensor(sts[:, 16:20], sts[:, 4:8], 1.0 / 64, sts[:, 12:16], ALU.mult, ALU.subtract),
                      reads=[STS], writes=[STS])
                kb.op("dve", lambda e: e.tensor_scalar(sts[:, 16:20], sts[:, 16:20], GN_EPS, None, ALU.add), reads=[STS], writes=[STS])
                kb.op("act", lambda e: e.activation(out=sts[:, 20:24], in_=sts[:, 16:20], func=AF.Sqrt), reads=[STS], writes=[STS])
                kb.op("dve", lambda e: e.reciprocal(sts[:, 24:28], sts[:, 20:24]), reads=[STS], writes=[STS])
                kb.op("dve", lambda e: e.tensor_tensor(yn[:].rearrange("p (h v) -> p h v", v=64), ytok[:].rearrange("p (h v) -> p h v", v=64),
                                                       sts[:, 8:12].unsqueeze(2).to_broadcast([128, 4, 64]), ALU.subtract), reads=[YTOK, STS], writes=[YN])
                kb.op("dve", lambda e: e.tensor_tensor(yn[:].rearrange("p (h v) -> p h v", v=64), yn[:].rearrange("p (h v) -> p h v", v=64),
                                                       sts[:, 24:28].unsqueeze(2).to_broadcast([128, 4, 64]), ALU.mult), reads=[YN, STS], writes=[YN])
                for hp in range(2):
                    pbank, PT = next_pa()
                    pt = pbank[:, 0:128]
                    em.tr(pt, yn[:, hp * 128:(hp + 1) * 128], ident[:], [YN, IDENT], [PT])
                    em.ts("dve", yf[hp][:, csl], pt, vec[:, 18 + hp:19 + hp], vec[:, 20 + hp:21 + hp], ALU.mult, ALU.add, [PT, VEC], [YF[hp]])
            for hp in range(2):
                kb.op("pool", lambda e, hp=hp: e.tensor_tensor(osb[hp][:], yf[hp][:], bon[hp][:], ALU.add), reads=[YF[hp], BON[hp]], writes=[OSB[hp]])
                kb.op("pool", lambda e, hp=hp: e.tensor_tensor(osb[hp][:], osb[hp][:], gg[hp][:], ALU.mult), reads=[OSB[hp], GG[hp]], writes=[OSB[hp]])
                kb.dma("sp", f"out{hp}", lambda e, s, hp=hp, sgi=sgi: e.dma_start(out=yT_d[hp * 128:(hp + 1) * 128, sgi * NS:(sgi + 1) * NS], in_=osb[hp][:]).then_inc(s, 16),
                       reads=[OSB[hp]])
        kb.finish(OSB)


def build_a1(nseg=NSEG, debug=False):
    nc = bass.Bass("TRN2", target_bir_lowering=False)
    I = lambda n, shp: nc.dram_tensor(n, shp, F32, kind="ExternalInput").ap()
    D = dict(xT=I("xT", [1024, S]), w=I("w", [1024, 1024]), vec=I("vec", [128, 22]), lw=I("lw", [128, 256]), g2=I("g2", [128, 256]),
             cst=I("cst", [128, 1280]), yT=nc.dram_tensor("yT", [256, S], F32, kind="ExternalOutput").ap())
    with ExitStack() as st:
        kb = KB(nc, st)
        a1_body(nc, kb, D, nseg, debug)
        kb.emit()
    return nc


def a1_consts():
    m1 = np.ones((128, 512), np.float32); m1[:, ::CH] = 0.0
    s = np.arange(128)[:, None]; t = np.arange(128)[None, :]
    msu = (t > s).astype(np.float32); miu = (t >= s).astype(np.float32)
    msl = (t < s).astype(np.float32)
    bones = (s // 64 == t // 64).astype(np.float32)
    return np.concatenate([m1, msu, miu, msu, miu, msl, bones], axis=1)


def a1_inputs(inp, l, b, hh):
    ch = slice(256 * hh, 256 * hh + 256)
    w_in = inp["w_in"][l]
    w = np.concatenate([w_in[:, 0:512][:, ch], w_in[:, 512:1024][:, ch], w_in[:, 1024:1536][:, ch], w_in[:, 1536:1792]], axis=1)
    mu = inp["shift_mu"][l]
    mu_cols = np.concatenate([mu[0:512][ch], mu[512:1024][ch], mu[1024:1536][ch], mu[1536:1792]])
    vec = np.zeros((128, 22), np.float32)
    vec[:, 0:8] = mu_cols.reshape(8, 128).T
    def two(v):
        return v[ch].reshape(2, 128).T
    vec[:, 8:10] = two(inp["rw_w0"][l]); vec[:, 10:12] = two(inp["rw_a0"][l]); vec[:, 12:14] = two(inp["rw_kk"][l])
    vec[:, 14:16] = two(inp["rw_ka"][l]); vec[:, 16:18] = two(inp["rw_rk"][l].reshape(512))
    vec[:, 18:20] = two(inp["rw_ln_g"][l]); vec[:, 20:22] = two(inp["rw_ln_b"][l])
    lw = np.concatenate([inp["rw_w2"][l][:, ch], inp["rw_a2"][l][:, ch]], axis=0)
    g2 = inp["rw_g2"][l][:, ch]
    return dict(w=np.ascontiguousarray(w), vec=vec, lw=np.ascontiguousarray(lw), g2=np.ascontiguousarray(g2), cst=a1_consts())


import math

S = 4096
NEG = -30000.0
TW = 2304
DCL = 1280


def t5_bucket(n):
    n = np.maximum(n, 0); me = 16
    nf = np.maximum(n, 1).astype(np.float32)
    large = me + (np.log(nf / np.float32(me)) / np.float32(math.log(1024 / me)) * np.float32(32 - me)).astype(np.int32)
    large = np.minimum(large, 31)
    return np.where(n < me, n, large)


class Em:
    def __init__(self, kb):
        self.kb = kb

    def mm(self, out, lhsT, rhs, start, stop, reads, writes):
        self.kb.op("pe", lambda e, o=out, a=lhsT, b=rhs, s=start, t=stop: e.matmul(o, a, b, start=s, stop=t), reads, writes)

    def tr(self, out, in_, ident, reads, writes):
        self.kb.op("pe", lambda e, o=out, a=in_, b=ident: e.transpose(o, a, b), reads, writes)

    def act(self, out, in_, func, reads, writes, **kw):
        self.kb.op("act", lambda e, o=out, i=in_, f=func, kw=kw: e.activation(out=o, in_=i, func=f, **kw), reads, writes)

    def tt(self, eng, out, a, b, op, reads, writes):
        self.kb.op(eng, lambda e, o=out, a=a, b=b, op=op: e.tensor_tensor(o, a, b, op), reads, writes)

    def ts(self, eng, out, a, s1, s2, op0, op1, reads, writes):
        if op1 is None:
            self.kb.op(eng, lambda e, o=out, a=a, s1=s1, op0=op0: e.tensor_scalar(o, a, s1, None, op0), reads, writes)
        else:
            self.kb.op(eng, lambda e, o=out, a=a, s1=s1, s2=s2, op0=op0, op1=op1: e.tensor_scalar(o, a, s1, s2, op0, op1), reads, writes)

    def stt(self, eng, out, a, s, b, op0, op1, reads, writes):
        self.kb.op(eng, lambda e, o=out, a=a, s=s, b=b, op0=op0, op1=op1: e.scalar_tensor_tensor(o, a, s, b, op0, op1), reads, writes)

    def cp(self, eng, out, in_, reads, writes):
        self.kb.op(eng, lambda e, o=out, i=in_: e.tensor_copy(o, i), reads, writes)

    def ms(self, eng, out, val, writes):
        self.kb.op(eng, lambda e, o=out, v=val: e.memset(o, v), (), writes)

    def red(self, out, in_, op, reads, writes):
        self.kb.op("dve", lambda e, o=out, i=in_, op=op: e.tensor_reduce(o, i, AX.X, op), reads, writes)

    def rcp(self, out, in_, reads, writes):
        self.kb.op("dve", lambda e, o=out, i=in_: e.reciprocal(o, i), reads, writes)

    def dma(self, q, key, out, in_, reads, writes):
        self.kb.dma(q, key, lambda e, s, o=out, i=in_: e.dma_start(out=o, in_=i).then_inc(s, 16), reads, writes)


def a2_body(nc, kb, D, nq=8, debug=False):
    xT_d, wf_d, wt_d, w1_d, pe_d, w2_d, btc_d, mkc_d, bts_d, mks_d, mkw_d, m12_d, rv_d, c2s_d, exd_d, hsel_d = (D[k] for k in ['xT', 'wf', 'wt', 'w1', 'peT', 'w2', 'btc', 'mkc', 'bts', 'mks', 'mkw', 'm12', 'rv', 'c2s', 'exd', 'hsel'])
    yT_d = D["yT"]
    xT_v = xT_d.rearrange("(kc p) t -> p kc t", p=128)
    if True:
        em = Em(kb)
        sb, ps = kb.sb, kb.ps
        xs = [sb(f"xs{i}", [128, 8, 512], BF16) for i in range(2)]; XS = [Buf() for _ in range(2)]
        wf = sb("wf", [128, 8, 640], BF16); WF = Buf()
        wt = sb("wt", [128, 8, 256], BF16); WT = Buf()
        w1 = sb("w1", [128, 32, 128], BF16); W1 = Buf()
        peT = sb("peT", [128, 32], BF16); PET = Buf()
        w2f = sb("w2f", [128, 192]); w2 = sb("w2", [128, 192], BF16); W2 = Buf()
        qT = [sb(f"qT{i}", [128, S], BF16) for i in range(2)]; QT = [Buf() for _ in range(2)]
        ksT = sb("ksT", [128, S], BF16); KST = Buf()
        kwT = sb("kwT", [128, S], BF16); KWT = Buf()
        kcv = sb("kcv", [128, S], BF16); KCV = Buf()
        vs = sb("vs", [128, 32, 96], BF16); VS = Buf()
        vw = sb("vw", [128, 32, 96], BF16); VW = Buf()
        gt = sb("gt", [128, 32, 16]); GT = Buf()
        btc = sb("btc", [128, 4, 256]); BTC = Buf()
        mkc = sb("mkc", [128, 256]); MKC = Buf()
        HW_ = TW // 2
        stg = sb("stg", [128, HW_]); STG = Buf()
        stg2 = sb("stg2", [128, HW_]); STG2 = Buf()
        mks = sb("mks", [128, HW_]); MKS = Buf()
        mkw = sb("mkw", [128, HW_]); MKW = Buf()
        ebs = [sb(f"ebs{h}", [128, TW], BF16) for h in range(4)]; EBS = [Buf() for _ in range(4)]
        ebw = [sb(f"ebw{h}", [128, TW], BF16) for h in range(4)]; EBW = [Buf() for _ in range(4)]
        m12 = sb("m12", [128, 256]); M12 = Buf()
        rv = sb("rv", [128, 1]); RV = Buf()
        vca = sb("vca", [128, 2, 128], BF16); VCA = Buf()
        c2sf = sb("c2sf", [128, 128]); C2SF = Buf()
        exd = sb("exd", [128, 32, 128], BF16); EXD = Buf()
        hself = sb("hself", [128, 256]); hsel = sb("hsel", [128, 2, 128], BF16); HSEL = Buf()
        identf = sb("identf", [128, 128]); ident = sb("ident", [128, 128], BF16); IDENT = Buf()
        kct = sb("kct", [128, 256], BF16); KCT = Buf()
        gh = [sb(f"gh{i}", [128, 256], BF16) for i in range(2)]; GH = [Buf() for _ in range(2)]
        hx = sb("hx", [128, 256]); HX = Buf()
        hy = sb("hy", [128, 256]); HY = Buf()
        hb = sb("hb", [128, 2]); HB = Buf()
        sm = sb("sm", [128, 64]); SM = Buf()
        cst = sb("cst", [128, 16]); CST = Buf()
        qsq = sb("qsq", [128, 512], BF16); QSQ = Buf()
        mxc = sb("mxc", [128, 4, 8]); MXC = Buf()
        kxc = sb("kxc", [128, 2, 8]); KXC = Buf()
        lgs = [sb(f"lg{h}", [128, 256]) for h in range(4)]; LGS = [Buf() for _ in range(4)]
        pcs = [sb(f"pc{h}", [128, 256], BF16) for h in range(4)]; PCS = [Buf() for _ in range(4)]
        pcts = [sb(f"pct{h}", [128, 2, 128], BF16) for h in range(4)]; PCTS = [Buf() for _ in range(4)]
        smh = sb("smh", [128, 32]); SMH = [Buf() for _ in range(4)]
        sc = sb("sc", [128, 64]); SC = Buf()
        sc2 = sb("sc2", [128, 64]); SC2 = Buf()
        mx8 = sb("mx8", [128, 16]); MX8 = Buf()
        nst = sb("nst", [128, 64], BF16); NST = Buf()
        nsh = sb("nsh", [128, 512], BF16); NSH = Buf()
        yg = sb("yg", [128, 4, 256]); YG = Buf()
        ygt = sb("ygt", [128, 2, 512]); YGT = Buf()
        pt = [sb(f"pt{i}", [128, 512], BF16) for i in range(3)]; PT = [Buf() for _ in range(3)]
        p2 = [sb(f"p2{i}", [128, 512], BF16) for i in range(3)]; P2 = [Buf() for _ in range(3)]
        rin = sb("rin", [128, 8]); RIN = Buf()
        zb = sb("zb", [128, 512], BF16); ZB = Buf()
        otmp = sb("otmp", [128, 4, 64]); OTMP = Buf()
        pg = [ps(f"pg{i}", [128, 512]) for i in range(2)]; PG = [Buf(excl=True) for _ in range(2)]
        ptb = ps("ptb", [128, 1024], BF16); PTB = Buf(excl=True)
        pl = [ps(f"pl{i}", [128, 512]) for i in range(3)]; PL = [Buf(excl=True) for _ in range(3)]
        pacc = [ps(f"pacc{i}", [128, 4, 128]) for i in range(2)]; PACC = [Buf(excl=True) for _ in range(2)]

        gi = [0]

        def gbank():
            i = gi[0] % 2; gi[0] += 1
            return pg[i], PG[i]

        dbg_n = [0]

        def dbg(name, ap, B):
            if not debug:
                return
            d = nc.dram_tensor("dbg_" + name, list(ap.shape), F32, kind="ExternalOutput").ap()
            dbg_n[0] += 1
            cntv = dbg_n[0] * 16

            def f(e, s, d=d, ap=ap, cntv=cntv):
                e.dma_start(out=d, in_=ap).then_inc(s, 16)
                e.wait_ge(s, cntv)
            kb.dma("pool", "dbg", f, reads=[B])

        em.dma("pool", "wf", wf[:, :, :], wf_d.rearrange("(kc p) n -> p kc n", p=128), [], [WF])
        em.dma("pool", "wt", wt[:, :, 0:140], wt_d.rearrange("(kc p) n -> p kc n", p=128), [], [WT])
        em.dma("pool", "w1", w1[:, :, :], w1_d.rearrange("p (a b) -> p a b", b=128), [], [W1])
        em.dma("pool", "pe", peT[:], pe_d[:, :], [], [PET])
        em.dma("sp", "w2", w2f[:], w2_d[:, :], [], [W2])
        em.dma("sp", "btc", btc[:].rearrange("p h w -> p (h w)"), btc_d[:, :], [], [BTC])
        em.dma("sp", "mkc", mkc[:], mkc_d[:, :], [], [MKC])
        em.dma("sp", "m12", m12[:], m12_d[:, :], [], [M12])
        em.dma("sp", "rv", rv[:], rv_d[:, :], [], [RV])
        em.dma("sp", "c2s", c2sf[:], c2s_d[:, :], [], [C2SF])
        em.ms("pool", exd[:], 0.0, [EXD])
        em.dma("pool", "exd", exd[0:64, :, :].rearrange("p a b -> p (a b)"), exd_d[:, :], [EXD], [EXD])
        em.dma("sp", "hsel", hself[:], hsel_d[:, :], [], [HSEL])
        em.cp("dve", w2[:], w2f[:], [W2], [W2])
        em.cp("dve", hsel[:].rearrange("p a b -> p (a b)"), hself[:], [HSEL], [HSEL])
        em.ms("pool", identf[:], 1.0, [IDENT])
        kb.op("pool", lambda e: e.affine_select(out=identf[:], in_=identf[:], pattern=[[-1, 128]], compare_op=ALU.is_equal,
                                                fill=0.0, base=0, channel_multiplier=1), [IDENT], [IDENT])
        em.cp("pool", ident[:], identf[:], [IDENT], [IDENT])
        em.ms("dve", vs[:, :, 64:65], 1.0, [VS])
        em.ms("dve", vw[:, :, 64:65], 1.0, [VW])
        em.ms("dve", vca[:], 0.0, [VCA])
        em.ms("dve", zb[:], 0.0, [ZB])
        em.ms("dve", nsh[:], 0.0, [NSH])
        for h_ in range(4):
            em.ms("dve", pcs[h_][:], 0.0, [PCS[h_]])
        em.ms("dve", kct[:], 0.0, [KCT])
        for h in range(4):
            em.tt("dve", btc[:, h, :], btc[:, h, :], mkc[:], ALU.add, [BTC, MKC], [BTC])
        first = True
        for hf in range(2):
            cs_ = slice(hf * HW_, (hf + 1) * HW_)
            em.dma("sp", "mks", mks[:], mks_d[:, cs_], [], [MKS])
            em.dma("sp", "mkw", mkw[:], mkw_d[:, cs_], [], [MKW])
            for h in range(4):
                em.dma("sp", "stg", stg[:], bts_d[:, h * TW + hf * HW_:h * TW + (hf + 1) * HW_], [], [STG])
                if first:
                    em.red(cst[:, 8:9], stg[:], ALU.max, [STG], [CST])
                    first = False
                else:
                    em.red(cst[:, 9:10], stg[:], ALU.max, [STG], [CST])
                    em.tt("dve", cst[:, 8:9], cst[:, 8:9], cst[:, 9:10], ALU.max, [CST], [CST])
                em.tt("dve", stg2[:], stg[:], mks[:], ALU.add, [STG, MKS], [STG2])
                em.act(ebs[h][:, cs_], stg2[:], AF.Exp, [STG2], [EBS[h]])
                em.tt("dve", stg2[:], stg[:], mkw[:], ALU.add, [STG, MKW], [STG2])
                em.act(ebw[h][:, cs_], stg2[:], AF.Exp, [STG2], [EBW[h]])

        import os as _os
        PH = int(_os.environ.get('PH', '9'))
        def load_x(sg):
            sl = sg % 2
            em.dma("pool", f"xs{sl}", xs[sl][:, :, :], xT_v[:, :, sg * 512:(sg + 1) * 512], [], [XS[sl]])

        load_x(0)
        for sg in range(8 if PH >= 2 else 0):
            sl = sg % 2
            if sg + 1 < 8:
                load_x(sg + 1)
            seg = slice(sg * 512, (sg + 1) * 512)
            dsts = [(qT[0], QT[0], 0.125), (qT[1], QT[1], 0.125), (ksT, KST, 1.0), (kwT, KWT, 1.0), (kcv, KCV, 1.0)]
            for j, (dst, DST, scl) in enumerate(dsts):
                p, P = gbank()
                for k in range(8):
                    em.mm(p[:, :], wf[:, k, j * 128:(j + 1) * 128], xs[sl][:, k, :], k == 0, k == 7, [WF, XS[sl]], [P])
                em.act(dst[:, seg], p[:, :], AF.Copy, [P], [DST], scale=scl)
            PJ = _os.environ.get('PJ', 'abc12')
            for tt_ in range(4 if 'b' in PJ else 0):
                tile_i = sg * 4 + tt_
                p, P = gbank()
                for k in range(8):
                    em.mm(p[:, 0:140], xs[sl][:, k, tt_ * 128:(tt_ + 1) * 128], wt[:, k, 0:140], k == 0, k == 7, [WT, XS[sl]], [P])
                if '1' in PJ:
                    em.cp("dve", vs[:, tile_i, 0:64], p[:, 0:64], [P], [VS])
                    em.cp("dve", vw[:, tile_i, 0:64], p[:, 64:128], [P], [VW])
                if '2' in PJ:
                    em.act(gt[:, tile_i, 0:12], p[:, 128:140], AF.Sigmoid, [P], [GT])
            for i in range(2 if 'c' in PJ else 0):
                em.tt("dve", qsq[:], qT[i][:, seg], qT[i][:, seg], ALU.mult, [QT[i]], [QSQ])
                for hh in range(2):
                    p, P = gbank()
                    em.mm(p[:, :], hsel[:, hh, :], qsq[:], True, True, [HSEL, QSQ], [P])
                    em.red(mxc[:, 2 * i + hh, sg:sg + 1], p[:, :], ALU.max, [P], [MXC])
            for i, (src, SRC) in enumerate(((ksT, KST), (kwT, KWT)) if 'c' in PJ else ()):
                em.tt("dve", qsq[:], src[:, seg], src[:, seg], ALU.mult, [SRC], [QSQ])
                p, P = gbank()
                em.mm(p[:, :], hsel[:, 0, :], qsq[:], True, True, [HSEL, QSQ], [P])
                em.red(kxc[:, i, sg:sg + 1], p[:, :], ALU.max, [P], [KXC])
        if PH < 3:
            nq = 0
        em.red(sm[:, 0:4], mxc[:], ALU.max, [MXC], [SM])
        em.red(sm[:, 4:6], kxc[:], ALU.max, [KXC], [SM])
        for br in range(2):
            em.ts("dve", sm[:, 8 + 4 * br:12 + 4 * br], sm[:, 0:4], sm[:, 4 + br:5 + br], None, ALU.mult, None, [SM], [SM])
        em.act(sm[:, 16:24], sm[:, 8:16], AF.Sqrt, [SM], [SM])
        em.ts("dve", cst[:, 0:8], sm[:, 16:24], cst[:, 8:9], -1.0, ALU.add, ALU.mult, [SM, CST], [CST])

        for br in range(2 if PH >= 3 else 0):
            rows = slice(64 * br, 64 * br + 64)
            p, P = gbank()
            for pp in range(32):
                em.mm(p[:, 0:255], w1[rows, pp, :], kcv[rows, pp:pp + 16 * 254 + 1:16], pp == 0, pp == 31, [W1, KCV], [P])
            pb, PB = gbank()
            for pp in range(32):
                em.mm(pb[:, 0:1], w1[rows, pp, :], peT[rows, pp:pp + 1], pp == 0, pp == 31, [W1, PET], [PB])
            em.cp("dve", hb[:, br:br + 1], pb[:, 0:1], [PB], [HB])
            em.ts("dve", hx[:, 0:255], p[:, 0:255], hb[:, br:br + 1], None, ALU.add, None, [P, HB], [HX])
            em.tt("dve", hy[:, 0:255], hx[:, 0:255], hx[:, 0:255], ALU.mult, [HX], [HY])
            em.ts("dve", hy[:, 0:255], hy[:, 0:255], 0.044715, 1.0, ALU.mult, ALU.add, [HY], [HY])
            em.tt("dve", hy[:, 0:255], hy[:, 0:255], hx[:, 0:255], ALU.mult, [HY, HX], [HY])
            em.act(hy[:, 0:255], hy[:, 0:255], AF.Tanh, [HY], [HY], scale=0.7978845608028654)
            em.ts("dve", hy[:, 0:255], hy[:, 0:255], 1.0, 0.5, ALU.add, ALU.mult, [HY], [HY])
            em.tt("dve", gh[br][:, 0:255], hy[:, 0:255], hx[:, 0:255], ALU.mult, [HY, HX], [GH[br]])
        p, P = gbank()
        em.mm(p[:, 0:255], w2[:, 0:128], gh[0][:, 0:255], True, True, [W2, GH[0]], [P])
        em.act(kct[:, 0:255], p[:, 0:255], AF.Copy, [P], [KCT])
        for ct in range(2):
            ncs = 128 if ct == 0 else 127
            p, P = gbank()
            em.mm(p[0:ncs, 0:64], gh[1][:, ct * 128:ct * 128 + ncs], w2[:, 128:192], True, True, [W2, GH[1]], [P])
            em.act(vca[0:ncs, ct, 0:64], p[0:ncs, 0:64], AF.Copy, [P], [VCA])
        em.cp("dve", vca[:, :, 64:128], c2sf[:].rearrange("p (a b) -> p a b", b=64), [C2SF], [VCA])
        dbg("kct", kct[:, 0:255], KCT); dbg("vca", vca[:].rearrange("p a b -> p (a b)"), VCA)
        dbg("cst", cst[:, 0:9], CST)

        pti = [0]
        for Q in range(nq):
            qs = slice(Q * 512, (Q + 1) * 512)
            for a in range(4):
                n = 4 * Q + a
                t0 = 128 * n
                ncol = min(255, 8 * n + 7)
                off = 248 - 8 * n
                ncp = min(256, (ncol + 31) // 32 * 32)
                nct = 1 if ncol <= 128 else 2
                for hpair in ((0, 1), (2, 3)):
                    HP = {}
                    for h in hpair:
                        rows = slice(64 * (h % 2), 64 * (h % 2) + 64)
                        p, P = gbank()
                        em.mm(p[:, 0:ncp], qT[h // 2][rows, t0:t0 + 128], kct[rows, 0:ncp], True, True, [QT[h // 2], KCT], [P])
                        HP[h] = (p, P)
                    for h in hpair:
                        p, P = HP[h]
                        s0 = 8 * h
                        em.tt("dve", lgs[h][:, 0:ncol], p[:, 0:ncol], btc[:, h, off:off + ncol], ALU.add, [P, BTC], [LGS[h]])
                        em.red(smh[:, s0:s0 + 1], lgs[h][:, 0:ncol], ALU.max, [LGS[h]], [SMH[h]])
                        em.ts("dve", smh[:, s0 + 1:s0 + 2], smh[:, s0:s0 + 1], -1.0, None, ALU.mult, None, [SMH[h]], [SMH[h]])
                        em.ms("dve", smh[:, s0 + 2:s0 + 3], 0.0, [SMH[h]])
                    for h in hpair:
                        s0 = 8 * h
                        em.act(pcs[h][:, 0:ncol], lgs[h][:, 0:ncol], AF.Exp, [LGS[h], SMH[h]], [PCS[h], SMH[h]], bias=smh[:, s0 + 1:s0 + 2], accum_out=smh[:, s0 + 2:s0 + 3])
                    for h in hpair:
                        for ct in range(nct):
                            em.tr(ptb[:, ct * 128:(ct + 1) * 128], pcs[h][:, ct * 128:(ct + 1) * 128], ident[:], [PCS[h], IDENT], [PTB])
                        em.cp("dve", pcts[h][:, 0:nct, :], ptb[:, 0:nct * 128].rearrange("p (a b) -> p a b", b=128), [PTB], [PCTS[h]])
                    PO_ = {}
                    for h in hpair:
                        po, PO = gbank()
                        for ct in range(nct):
                            em.mm(po[:, 0:128], pcts[h][:, ct, :], vca[:, ct, :], ct == 0, ct == nct - 1, [PCTS[h], VCA], [PO])
                        PO_[h] = (po, PO)
                    for h in hpair:
                        po, PO = PO_[h]
                        s0 = 8 * h
                        em.rcp(smh[:, s0 + 3:s0 + 4], smh[:, s0 + 2:s0 + 3], [SMH[h]], [SMH[h]])
                        if n == 0:
                            em.tt("dve", smh[:, s0 + 3:s0 + 4], smh[:, s0 + 3:s0 + 4], rv[:], ALU.mult, [SMH[h], RV], [SMH[h]])
                        em.tt("dve", smh[:, s0 + 4:s0 + 5], smh[:, s0 + 3:s0 + 4], gt[:, n, 3 * h:3 * h + 1], ALU.mult, [SMH[h], GT], [SMH[h]])
                        em.ts("dve", yg[:, a, h * 64:(h + 1) * 64], po[:, 0:64], smh[:, s0 + 4:s0 + 5], None, ALU.mult, None, [PO, SMH[h]], [YG])
                        if h == 0:
                            em.ts("dve", sc[:], po[:, 64:128], smh[:, s0 + 3:s0 + 4], None, ALU.mult, None, [PO, SMH[h]], [SC])
                        else:
                            em.stt("dve", sc[:], po[:, 64:128], smh[:, s0 + 3:s0 + 4], sc[:], ALU.mult, ALU.add, [PO, SMH[h], SC], [SC])
                w0 = 64 - 2 * n
                em.tt("dve", sc[:], sc[:], m12[:, w0:w0 + 64], ALU.mult, [SC, M12], [SC])
                em.tt("dve", sc[:], sc[:], m12[:, 128 + w0:128 + w0 + 64], ALU.add, [SC, M12], [SC])
                em.ms("dve", sc[:, 0:1], 1.0e4, [SC])
                kb.op("dve", lambda e: e.max(out=mx8[:, 0:8], in_=sc[:]), [SC], [MX8])
                kb.op("dve", lambda e: e.match_replace(out=sc2[:], in_to_replace=mx8[:, 0:8], in_values=sc[:], imm_value=-1.0e9), [SC, MX8], [SC2])
                kb.op("dve", lambda e: e.max(out=mx8[:, 8:16], in_=sc2[:]), [SC2], [MX8])
                em.red(sm[:, 40:41], mx8[:, 8:16], ALU.min, [MX8], [SM])
                em.ts("dve", nst[:], sc[:], sm[:, 40:41], NEG, ALU.is_lt, ALU.mult, [SC, SM], [NST])
                em.tr(ptb[0:64, 256:384], nst[:], ident[:], [NST, IDENT], [PTB])
                em.cp("dve", nsh[0:64, a * 128:(a + 1) * 128], ptb[0:64, 256:384], [PTB], [NSH])
                if Q == 0 and a == 1:
                    dbg("sc", sc[:], SC); dbg("yg1", yg[:, 1, :], YG)
            QP = _os.environ.get('QP', 'abc')
            for br in [b_ for b_ in range(2) if 'bc'[b_] in QP]:
                kT, KT_, vv, VV, eb, EB = (ksT, KST, vs, VS, ebs, EBS) if br == 0 else (kwT, KWT, vw, VW, ebw, EBW)
                m_lo = 0 if br == 0 else max(0, 4 * Q - 4)
                m_hi = 4 * Q + 3
                tiles = [(h, m) for h in range(4) for m in range(m_lo, m_hi + 1)]
                slot = {}

                def stage1(ti):
                    h, m = tiles[ti]
                    rows = slice(64 * (h % 2), 64 * (h % 2) + 64)
                    pli = pti[0] % 3
                    bi = pti[0] % 3
                    pti[0] += 1
                    slot[ti] = (pli, bi)
                    pl_, PL_ = pl[pli], PL[pli]
                    em.mm(pl_[:, :], kT[rows, m * 128:(m + 1) * 128], qT[h // 2][rows, qs], True, br == 1, [KT_, QT[h // 2]], [PL_])
                    if br == 0:
                        em.mm(pl_[:, :], exd[:, m, :], nsh[:, :], False, True, [EXD, NSH], [PL_])

                def stage23(ti):
                    h, m = tiles[ti]
                    pli, bi = slot.pop(ti)
                    pl_, PL_ = pl[pli], PL[pli]
                    acc, ACC = pacc[h % 2], PACC[h % 2]
                    D0 = 512 * Q - 128 * m
                    wst = min(D0, DCL) + 512
                    em.act(pt[bi][:], pl_[:, :], AF.Exp, [PL_, CST], [PT[bi]], bias=cst[:, 4 * br + h:4 * br + h + 1])
                    em.tt("dve", p2[bi][:], pt[bi][:], eb[h][:, wst:wst + 512], ALU.mult, [PT[bi], EB[h]], [P2[bi]])
                    if m == m_lo:
                        em.mm(acc[:].rearrange("p a b -> p (a b)"), zb[:, 0:128], zb[:, 0:512], True, False, [ZB], [ACC])
                    for a in range(4):
                        last_m = min(m_hi, 4 * Q + a)
                        if m > last_m:
                            continue
                        a_lo = m_lo if br == 0 else max(m_lo, 4 * Q + a - 4)
                        if m < a_lo:
                            continue
                        em.mm(acc[:, a, 0:65], p2[bi][:, a * 128:(a + 1) * 128], vv[:, m, 0:65], False, (m == m_hi and a == 3), [P2[bi], VV], [ACC])
                    if m == m_hi:
                        em.rcp(rin[:, 0:4], acc[:, :, 64], [ACC], [RIN])
                        em.tt("dve", rin[:, 4:8], rin[:, 0:4], gt[:, 4 * Q:4 * Q + 4, 3 * h + 1 + br], ALU.mult, [RIN, GT], [RIN])
                        em.tt("dve", otmp[:], acc[:, :, 0:64], rin[:, 4:8].unsqueeze(2).to_broadcast([128, 4, 64]), ALU.mult, [ACC, RIN], [OTMP])
                        em.tt("pool", yg[:, :, h * 64:(h + 1) * 64], yg[:, :, h * 64:(h + 1) * 64], otmp[:], ALU.add, [YG, OTMP], [YG])
                stage1(0)
                if len(tiles) > 1:
                    stage1(1)
                for ti in range(len(tiles)):
                    if ti + 2 < len(tiles):
                        stage1(ti + 2)
                    stage23(ti)
            for a in range(4):
                for hp in range(2):
                    pz, PZ = gbank()
                    em.tr(pz[:, 0:128], yg[:, a, hp * 128:(hp + 1) * 128], identf[:], [YG, IDENT], [PZ])
                    em.cp("dve" if hp else "pool_never", ygt[:, hp, a * 128:(a + 1) * 128], pz[:, 0:128], [PZ], [YGT]) if False else em.cp("dve", ygt[:, hp, a * 128:(a + 1) * 128], pz[:, 0:128], [PZ], [YGT])
            for hp in range(2):
                em.dma("sp", "y", yT_d[hp * 128:(hp + 1) * 128, Q * 512:(Q + 1) * 512], ygt[:, hp, :], [YGT], [])
        kb.finish([YG, YGT])


def build_a2(nq=8, debug=False):
    nc = bass.Bass("TRN2", target_bir_lowering=False)
    I = lambda n, shp: nc.dram_tensor(n, shp, F32, kind="ExternalInput").ap()
    D = {"xT": I("xT", [1024, S]), "wf": I("wf", [1024, 640]), "wt": I("wt", [1024, 140]), "w1": I("w1", [128, 32 * 128]), "peT": I("peT", [128, 32]), "w2": I("w2", [128, 192]), "btc": I("btc", [128, 4 * 256]), "mkc": I("mkc", [128, 256]), "bts": I("bts", [128, 4 * TW]), "mks": I("mks", [128, TW]), "mkw": I("mkw", [128, TW]), "m12": I("m12", [128, 256]), "rv": I("rv", [128, 1]), "c2s": I("c2s", [128, 128]), "exd": I("exd", [64, 32 * 128]), "hsel": I("hsel", [128, 256])}
    D["yT"] = nc.dram_tensor("yT", [256, S], F32, kind="ExternalOutput").ap()
    with ExitStack() as st:
        kb = KB(nc, st)
        a2_body(nc, kb, D, nq, debug)
        kb.emit()
    return nc


def a2_consts():
    i = np.arange(128)[:, None]
    j = np.arange(256)[None, :]
    dc = i - 16 * (j - 248) - 31
    mkc = np.where(dc >= 0, 0.0, NEG).astype(np.float32)
    w = np.arange(TW)[None, :]
    ds = w - i - 512
    mks = np.where(ds >= 0, 0.0, NEG).astype(np.float32)
    mkw = np.where((ds >= 0) & (ds < 512), 0.0, NEG).astype(np.float32)
    wv = np.arange(128)[None, :] - 64
    cur = (i >= 64).astype(np.int64)
    forced = (wv == cur) | (wv == cur - 1)
    valid = wv <= cur
    m1 = (valid & ~forced).astype(np.float32)
    m2 = np.where(forced, 1.0e4, np.where(valid, 0.0, -1.0)).astype(np.float32)
    m12 = np.concatenate([m1, m2], axis=1)
    rv = (np.arange(128) >= 31).astype(np.float32)[:, None]
    cs = np.arange(255)[:, None] * 16; ss = np.arange(64)[None, :] * 64
    ov = np.clip(np.minimum(cs + 32, ss + 64) - np.maximum(cs, ss), 0, None).astype(np.float32) / 32
    c2s = np.zeros((256, 64), np.float32); c2s[:255] = ov
    c2s = c2s.reshape(2, 128, 64).transpose(1, 0, 2).reshape(128, 128)
    exd = np.zeros((64, 32, 128), np.float32)
    for m in range(32):
        exd[2 * m, m, 0:64] = 1.0; exd[2 * m + 1, m, 64:128] = 1.0
    hsel = np.zeros((128, 2, 128), np.float32); hsel[0:64, 0, :] = 1.0; hsel[64:128, 1, :] = 1.0
    return dict(mkc=mkc, mks=mks, mkw=mkw, m12=m12, rv=rv, c2s=c2s, exd=exd.reshape(64, 32 * 128), hsel=hsel.reshape(128, 256),
                dc=dc, ds=ds)


def a2_inputs(inp, l, g, consts):
    w_in = inp["w_in"][l]; zr = 1792
    q = w_in[:, zr + 256 * g: zr + 256 * g + 256]
    def kvc(off):
        return w_in[:, zr + off + 64 * g: zr + off + 64 * g + 64]
    kc, vc, ks, vs, kw, vw = (kvc(o) for o in (512, 640, 768, 896, 1024, 1152))
    gates = w_in[:, zr + 1280 + 12 * g: zr + 1280 + 12 * g + 12]
    wf = np.concatenate([q, ks, ks, kw, kw, kc, vc], axis=1)
    wt = np.concatenate([vs, vw, gates], axis=1)
    def w1r(w):
        return w.reshape(32, 64, 128).transpose(1, 0, 2)
    w1 = np.concatenate([w1r(inp["cmp_w1_k"][l]), w1r(inp["cmp_w1_v"][l])], axis=0).reshape(128, 32 * 128)
    peT = np.concatenate([inp["cmp_pe_k"][l].T, inp["cmp_pe_v"][l].T], axis=0)
    w2 = np.concatenate([inp["cmp_w2_k"][l], inp["cmp_w2_k"][l], inp["cmp_w2_v"][l]], axis=1)
    rb = inp["rel_bias"][:, 4 * g:4 * g + 4]
    btc = np.take(rb, t5_bucket(consts["dc"]), axis=0).transpose(0, 2, 1).reshape(128, 4 * 256)
    bts = np.take(rb, t5_bucket(consts["ds"]), axis=0).transpose(0, 2, 1).reshape(128, 4 * TW)
    out = dict(wf=wf, wt=wt, w1=w1, peT=peT, w2=w2, btc=btc, bts=bts)
    for k in ("mkc", "mks", "mkw", "m12", "rv", "c2s", "exd", "hsel"):
        out[k] = consts[k]
    return {k: np.ascontiguousarray(v, dtype=np.float32) for k, v in out.items()}


NT = 2048
ALPHA = 8 ** 0.25
LN_EPS = 1e-5


def layer_norm_fm(kb, em, gbank, R, RB, out_fn, g_ap, b_ap, GB, tmp, TMP, ones, ONES, sq, SQ, mean, MEAN, rstd, RSTD):
    pm, PM = gbank()
    for i in range(8):
        em.mm(pm[:, :], ones[:], R[:, i, :], i == 0, i == 7, [ONES, RB], [PM])
    em.act(mean[:], pm[:, :], AF.Copy, [PM], [MEAN], scale=1.0 / 1024)
    pv, PV = gbank()
    for i in range(8):
        em.tt("pool" if i % 2 else "dve", sq[i % 2][:], R[:, i, :], R[:, i, :], ALU.mult, [RB], [SQ[i % 2]])
        em.mm(pv[:, :], ones[:], sq[i % 2][:], i == 0, i == 7, [ONES, SQ[i % 2]], [PV])
    em.tt("dve", tmp[:], mean[:], mean[:], ALU.mult, [MEAN], [TMP])
    em.stt("dve", rstd[:], pv[:, :], 1.0 / 1024, tmp[:], ALU.mult, ALU.subtract, [PV, TMP], [RSTD])
    em.ts("dve", rstd[:], rstd[:], LN_EPS, None, ALU.add, None, [RSTD], [RSTD])
    em.act(rstd[:], rstd[:], AF.Sqrt, [RSTD], [RSTD])
    em.rcp(rstd[:], rstd[:], [RSTD], [RSTD])
    for i in range(8):
        eng = "pool" if i % 2 else "dve"
        em.tt(eng, tmp[:], R[:, i, :], mean[:], ALU.subtract, [RB, MEAN], [TMP])
        em.tt(eng, tmp[:], tmp[:], rstd[:], ALU.mult, [TMP, RSTD], [TMP])
        o, O = out_fn(i)
        em.ts(eng, o, tmp[:], g_ap[:, i:i + 1], b_ap[:, i:i + 1], ALU.mult, ALU.add, [TMP, GB], [O])


def b1_body(nc, kb, D):
    xT_d, yr_d, yn_d, wg_d, wur_d, wun_d, wo_d, ln_d, o_d = (D[k] for k in ("xT", "yrT", "ynT", "wg", "wur", "wun", "wo", "ln", "x1T"))
    v3 = lambda ap: ap.rearrange("(kc p) n -> p kc n", p=128)
    if True:
        em = Em(kb); sb, ps = kb.sb, kb.ps
        wg = sb("wg", [128, 8, 2048], BF16); WG = Buf()
        wur = sb("wur", [128, 4, 1024], BF16); WUR = Buf()
        wun = sb("wun", [128, 4, 1024], BF16); WUN = Buf()
        wo = sb("wo", [128, 8, 1024], BF16); WO = Buf()
        ln = sb("ln", [128, 16]); LN = Buf()
        ones = sb("ones", [128, 128]); ONES = Buf()
        xf2 = [sb(f"xf{i}", [128, 8, 512]) for i in range(2)]; XF2 = [Buf() for _ in range(2)]
        xb2 = [sb(f"xb{i}", [128, 8, 512], BF16) for i in range(2)]; XB2 = [Buf() for _ in range(2)]
        yr2 = [sb(f"yr{i}", [128, 4, 512], BF16) for i in range(2)]; YR2 = [Buf() for _ in range(2)]
        yn2 = [sb(f"yn{i}", [128, 4, 512], BF16) for i in range(2)]; YN2 = [Buf() for _ in range(2)]
        sg = [sb(f"sg{i}", [128, 512]) for i in range(2)]; SG = [Buf() for _ in range(2)]
        m1 = sb("m1", [128, 512]); M1 = Buf()
        mg = sb("mg", [128, 8, 512], BF16); MG = Buf()
        R = sb("R", [128, 8, 512]); RB = Buf()
        ob = sb("ob", [128, 8, 512]); OB = Buf()
        tmp = sb("tmp", [128, 512]); TMP = Buf()
        sq = [sb(f"sq{i}", [128, 512]) for i in range(2)]; SQ = [Buf() for _ in range(2)]
        mean = sb("mean", [128, 512]); MEAN = Buf()
        rstd = sb("rstd", [128, 512]); RSTD = Buf()
        pg = [ps(f"pg{i}", [128, 512]) for i in range(6)]; PG = [Buf(excl=True) for _ in range(6)]
        gi = [0]

        def gbank():
            i = gi[0] % 6; gi[0] += 1
            return pg[i], PG[i]
        for k0 in range(0, 8, 2):
            em.dma("pool", "wg", wg[:, k0:k0 + 2, :], v3(wg_d)[:, k0:k0 + 2, :], [], [WG])
        em.dma("pool", "wur", wur[:, :, :], v3(wur_d), [], [WUR])
        em.dma("pool", "wun", wun[:, :, :], v3(wun_d), [], [WUN])
        em.dma("pool", "wo", wo[:, :, :], v3(wo_d), [], [WO])
        em.dma("sp", "ln", ln[:], ln_d[:, :], [], [LN])
        em.ms("dve", ones[:], 1.0, [ONES])
        def load_tg(t):
            bb = t % 2
            tsl = slice(t * 512, (t + 1) * 512)
            em.dma("sp", f"xf{bb}", xf2[bb][:, :, :], v3(xT_d)[:, :, tsl], [], [XF2[bb]])
            em.dma("pool", f"yr{bb}", yr2[bb][:, :, :], v3(yr_d)[:, :, tsl], [], [YR2[bb]])
            em.dma("pool", f"yn{bb}", yn2[bb][:, :, :], v3(yn_d)[:, :, tsl], [], [YN2[bb]])

        def cast_tg(t):
            bb = t % 2
            for i in range(8):
                em.cp("pool" if i % 2 else "dve", xb2[bb][:, i, :], xf2[bb][:, i, :], [XF2[bb]], [XB2[bb]])
        load_tg(0)
        cast_tg(0)
        for tg in range(NT // 512):
            ts_ = slice(tg * 512, (tg + 1) * 512)
            bb_ = tg % 2
            xf, XF, xb, XB, yr, YR, yn, YN = xf2[bb_], XF2[bb_], xb2[bb_], XB2[bb_], yr2[bb_], YR2[bb_], yn2[bb_], YN2[bb_]
            if tg + 1 < NT // 512:
                load_tg(tg + 1)
            for j in range(8):
                cs = slice(j * 128, (j + 1) * 128)
                for br, (wu, WU, yy, YY) in enumerate(((wur, WUR, yr, YR), (wun, WUN, yn, YN))):
                    p, P = gbank()
                    for k in range(8):
                        em.mm(p[:, :], wg[:, k, br * 1024 + j * 128: br * 1024 + (j + 1) * 128], xb[:, k, :], k == 0, k == 7, [WG, XB], [P])
                    em.act(sg[br][:], p[:, :], AF.Sigmoid, [P], [SG[br]])
                    p2, P2 = gbank()
                    for k in range(4):
                        em.mm(p2[:, :], wu[:, k, cs], yy[:, k, :], k == 0, k == 3, [WU, YY], [P2])
                    if br == 0:
                        em.tt("dve", m1[:], sg[0][:], p2[:, :], ALU.mult, [SG[0], P2], [M1])
                    else:
                        em.tt("dve", sg[1][:], sg[1][:], p2[:, :], ALU.mult, [SG[1], P2], [SG[1]])
                        em.tt("pool", mg[:, j, :], m1[:], sg[1][:], ALU.add, [M1, SG[1]], [MG])
            for i in range(8):
                p, P = gbank()
                for k in range(8):
                    em.mm(p[:, :], wo[:, k, i * 128:(i + 1) * 128], mg[:, k, :], k == 0, k == 7, [WO, MG], [P])
                em.stt("dve", R[:, i, :], xf[:, i, :], ALPHA, p[:, :], ALU.mult, ALU.add, [XF, P], [RB])
            if tg + 1 < NT // 512:
                cast_tg(tg + 1)
            layer_norm_fm(kb, em, gbank, R, RB, lambda i: (ob[:, i, :], OB), ln[:, 0:8], ln[:, 8:16], LN, tmp, TMP, ones, ONES, sq, SQ, mean, MEAN, rstd, RSTD)
            em.dma("sp", "ob", v3(o_d)[:, :, ts_], ob[:, :, :], [OB], [])
        kb.finish([OB])


def build_b1():
    nc = bass.Bass("TRN2", target_bir_lowering=False)
    I = lambda n, shp: nc.dram_tensor(n, shp, F32, kind="ExternalInput").ap()
    D = dict(xT=I("xT", [1024, NT]), yrT=I("yrT", [512, NT]), ynT=I("ynT", [512, NT]), wg=I("wg", [1024, 2048]), wur=I("wur", [512, 1024]),
             wun=I("wun", [512, 1024]), wo=I("wo", [1024, 1024]), ln=I("ln", [128, 16]),
             x1T=nc.dram_tensor("x1T", [1024, NT], F32, kind="ExternalOutput").ap())
    with ExitStack() as st:
        kb = KB(nc, st)
        b1_body(nc, kb, D)
        kb.emit()
    return nc


def b1_inputs(inp, l):
    w_in = inp["w_in"][l]
    lnp = np.concatenate([inp["ln1_g"][l].reshape(8, 128).T, inp["ln1_b"][l].reshape(8, 128).T], axis=1)
    return dict(wg=np.ascontiguousarray(w_in[:, 1792 + 1304: 1792 + 1304 + 2048]), wur=inp["w_up_rwkv"][l], wun=inp["w_up_nsa"][l],
                wo=inp["w_out"][l], ln=np.ascontiguousarray(lnp, dtype=np.float32))


def b2_body(nc, kb, D, nexp=32):
    x1_d, wr_d, br_d, w1_d, w3_d, w2_d, ln_d, selb_d, g2e_d, o_d = (D[k] for k in ("x1T", "wr", "brr", "ew1", "ew3", "ew2", "ln", "selb", "g2e", "x2T"))
    v3 = lambda ap: ap.rearrange("(kc p) n -> p kc n", p=128)
    if True:
        em = Em(kb); sb, ps = kb.sb, kb.ps
        wr = sb("wr", [128, 8, 64]); WR = Buf()
        brr = sb("brr", [128, 36]); BRR = Buf()
        ln = sb("ln", [128, 16]); LN = Buf()
        selb = sb("selb", [32, 32, 128], BF16); SELB = Buf()
        g2e = sb("g2e", [128, 4, 32]); G2E = Buf()
        ones = sb("ones", [128, 128]); ONES = Buf()
        ident = sb("ident", [128, 128]); IDENT = Buf()
        xf = sb("xf", [128, 8, 512]); XF = Buf()
        x1b = sb("x1b", [128, 8, NT], BF16); X1B = [Buf() for _ in range(4)]
        out = sb("out", [128, 8, NT]); OUT = [Buf() for _ in range(4)]
        cwt = sb("cwt", [32, NT], BF16); CWT = [Buf() for _ in range(4)]
        lgt = sb("lgt", [128, 36]); LGT = Buf()
        rs = sb("rs", [128, 64]); RS = Buf()
        em32 = sb("em32", [128, 32]); EM32 = Buf()
        em2 = sb("em2", [128, 32]); EM2 = Buf()
        cw = sb("cw", [128, 32]); CW = Buf()
        w1 = [sb(f"w1_{i}", [128, 8, 512], BF16) for i in range(2)]; W1 = [Buf() for _ in range(2)]
        w3 = [sb(f"w3_{i}", [128, 8, 512], BF16) for i in range(2)]; W3 = [Buf() for _ in range(2)]
        w2 = [sb(f"w2_{i}", [128, 4, 1024], BF16) for i in range(2)]; W2 = [Buf() for _ in range(2)]
        cwb = [sb(f"cwb{i}", [128, 512]) for i in range(2)]; CWB = [Buf() for _ in range(2)]
        sl_ = [sb(f"sl{i}", [128, 512]) for i in range(2)]; SL = [Buf() for _ in range(2)]
        hb = [sb(f"hb{i}", [128, 4, 512], BF16) for i in range(2)]; HB = [[Buf() for _ in range(4)] for _ in range(2)]
        ob = xf; OB = XF
        tmp = sb("tmp", [128, 512]); TMP = Buf()
        sq = [sb(f"sq{i}", [128, 512]) for i in range(2)]; SQ = [Buf() for _ in range(2)]
        mean = sb("mean", [128, 512]); MEAN = Buf()
        rstd = sb("rstd", [128, 512]); RSTD = Buf()
        pg = [ps(f"pg{i}", [128, 512]) for i in range(8)]; PG = [Buf(excl=True) for _ in range(8)]
        gi = [0]

        def gbank():
            i = gi[0] % 8; gi[0] += 1
            return pg[i], PG[i]
        em.ms("dve", wr[:], 0.0, [WR])
        em.dma("sp", "wr", wr[:, :, 0:36], v3(wr_d), [WR], [WR])
        em.dma("sp", "brr", brr[:], br_d[0:1, :].partition_broadcast(128), [], [BRR])
        em.dma("sp", "ln", ln[:], ln_d[:, :], [], [LN])
        em.dma("pool", "selb", selb[:].rearrange("p a b -> p (a b)"), selb_d[:, :], [], [SELB])
        em.dma("sp", "g2e", g2e[:].rearrange("p a b -> p (a b)"), g2e_d[:, :], [], [G2E])
        em.ms("dve", ones[:], 1.0, [ONES])
        em.ms("pool", ident[:], 1.0, [IDENT])
        kb.op("pool", lambda e: e.affine_select(out=ident[:], in_=ident[:], pattern=[[-1, 128]], compare_op=ALU.is_equal,
                                                fill=0.0, base=0, channel_multiplier=1), [IDENT], [IDENT])

        def load_w(e):
            s = e % 2
            for k0 in range(0, 8, 4):
                em.dma("pool", f"w1_{s}", w1[s][:, k0:k0 + 4, :], w1_d[e].rearrange("(kc p) n -> p kc n", p=128)[:, k0:k0 + 4, :], [], [W1[s]])
                em.dma("pool", f"w3_{s}", w3[s][:, k0:k0 + 4, :], w3_d[e].rearrange("(kc p) n -> p kc n", p=128)[:, k0:k0 + 4, :], [], [W3[s]])
            for k0 in range(0, 4, 2):
                em.dma("pool", f"w2_{s}", w2[s][:, k0:k0 + 2, :], w2_d[e].rearrange("(kc p) n -> p kc n", p=128)[:, k0:k0 + 2, :], [], [W2[s]])

        load_w(0)
        for tg in range(4):
            ts_ = slice(tg * 512, (tg + 1) * 512)
            em.dma("sp", "xf", xf[:, :, :], v3(x1_d)[:, :, ts_], [], [XF])
            for i in range(8):
                em.cp("pool" if i % 2 else "dve", x1b[:, i, ts_], xf[:, i, :], [XF], [X1B[tg]])
                em.ts("dve" if i % 2 else "pool", out[:, i, ts_], xf[:, i, :], ALPHA, None, ALU.mult, None, [XF], [OUT[tg]])
            for tt_ in range(4):
                p, P = gbank()
                for k in range(8):
                    em.mm(p[:, 0:36], xf[:, k, tt_ * 128:(tt_ + 1) * 128], wr[:, k, 0:36], k == 0, k == 7, [XF, WR], [P])
                em.tt("dve", lgt[:], p[:, 0:36], brr[:], ALU.add, [P, BRR], [LGT])
                em.red(rs[:, 0:1], lgt[:, 0:4], ALU.max, [LGT], [RS])
                em.ts("dve", rs[:, 1:2], rs[:, 0:1], -1.0, None, ALU.mult, None, [RS], [RS])
                em.ms("dve", rs[:, 2:3], 0.0, [RS])
                em.act(rs[:, 4:8], lgt[:, 0:4], AF.Exp, [LGT, RS], [RS], bias=rs[:, 1:2], accum_out=rs[:, 2:3])
                em.rcp(rs[:, 3:4], rs[:, 2:3], [RS], [RS])
                em.ts("dve", rs[:, 8:12], lgt[:, 0:4], rs[:, 0:1], None, ALU.is_ge, None, [LGT, RS], [RS])
                em.ts("dve", em32[:], g2e[:, 0, :], rs[:, 8:9], None, ALU.mult, None, [G2E, RS], [EM32])
                for g in range(1, 4):
                    em.stt("dve", em32[:], g2e[:, g, :], rs[:, 8 + g:9 + g], em32[:], ALU.mult, ALU.add, [G2E, RS, EM32], [EM32])
                em.tt("dve", em2[:], lgt[:, 4:36], em32[:], ALU.mult, [LGT, EM32], [EM2])
                em.ts("dve", em32[:], em32[:], -1.0, 1.0e9, ALU.add, ALU.mult, [EM32], [EM32])
                em.tt("dve", em2[:], em2[:], em32[:], ALU.add, [EM2, EM32], [EM2])
                em.red(rs[:, 12:13], em2[:], ALU.max, [EM2], [RS])
                em.ts("dve", cw[:], em2[:], rs[:, 12:13], None, ALU.is_ge, None, [EM2, RS], [CW])
                em.stt("dve", em32[:], cw[:], -2.0e9, em2[:], ALU.mult, ALU.add, [CW, EM2], [EM32])
                em.red(rs[:, 13:14], em32[:], ALU.max, [EM32], [RS])
                em.ts("dve", em32[:], em32[:], rs[:, 13:14], None, ALU.is_ge, None, [EM32, RS], [EM32])
                em.tt("dve", rs[:, 14:15], rs[:, 13:14], rs[:, 12:13], ALU.subtract, [RS], [RS])
                em.act(rs[:, 15:16], rs[:, 14:15], AF.Exp, [RS], [RS])
                em.ts("dve", rs[:, 15:16], rs[:, 15:16], 1.0, None, ALU.add, None, [RS], [RS])
                em.rcp(rs[:, 16:17], rs[:, 15:16], [RS], [RS])
                em.tt("dve", rs[:, 17:18], rs[:, 16:17], rs[:, 3:4], ALU.mult, [RS], [RS])
                em.tt("dve", rs[:, 18:19], rs[:, 3:4], rs[:, 17:18], ALU.subtract, [RS], [RS])
                em.ts("dve", cw[:], cw[:], rs[:, 17:18], None, ALU.mult, None, [CW, RS], [CW])
                em.stt("dve", cw[:], em32[:], rs[:, 18:19], cw[:], ALU.mult, ALU.add, [EM32, RS, CW], [CW])
                pt_, PT_ = gbank()
                em.tr(pt_[0:32, 0:128], cw[:], ident[:], [CW, IDENT], [PT_])
                em.cp("dve", cwt[:, tg * 512 + tt_ * 128: tg * 512 + (tt_ + 1) * 128], pt_[0:32, 0:128], [PT_], [CWT[tg]])
        tiles = [(e, tg) for e in range(nexp) for tg in range(4)]

        def stage1(ti):
            e, tg = tiles[ti]
            s = e % 2
            hbuf = ti % 2
            ts_ = slice(tg * 512, (tg + 1) * 512)
            pc_, PC_ = gbank()
            em.mm(pc_[:, :], selb[:, e, :], cwt[:, ts_], True, True, [SELB, CWT[tg]], [PC_])
            em.act(cwb[hbuf][:], pc_[:, :], AF.Copy, [PC_], [CWB[hbuf]])
            for f in range(4):
                fs = slice(f * 128, (f + 1) * 128)
                pa, PA = gbank()
                for k in range(8):
                    em.mm(pa[:, :], w1[s][:, k, fs], x1b[:, k, ts_], k == 0, k == 7, [W1[s], X1B[tg]], [PA])
                pb, PB = gbank()
                for k in range(8):
                    em.mm(pb[:, :], w3[s][:, k, fs], x1b[:, k, ts_], k == 0, k == 7, [W3[s], X1B[tg]], [PB])
                em.act(sl_[f % 2][:], pa[:, :], AF.Silu, [PA], [SL[f % 2]])
                em.tt("dve", sl_[f % 2][:], sl_[f % 2][:], pb[:, :], ALU.mult, [SL[f % 2], PB], [SL[f % 2]])
                em.tt("pool", hb[hbuf][:, f, :], sl_[f % 2][:], cwb[hbuf][:], ALU.mult, [SL[f % 2], CWB[hbuf]], [HB[hbuf][f]])

        def stage2(ti):
            e, tg = tiles[ti]
            s = e % 2
            hbuf = ti % 2
            ts_ = slice(tg * 512, (tg + 1) * 512)
            for i in range(8):
                po, PO = gbank()
                for f in range(4):
                    em.mm(po[:, :], w2[s][:, f, i * 128:(i + 1) * 128], hb[hbuf][:, f, :], f == 0, f == 3, [W2[s], HB[hbuf][f]], [PO])
                em.tt("dve", out[:, i, ts_], out[:, i, ts_], po[:, :], ALU.add, [OUT[tg], PO], [OUT[tg]])
        if nexp > 1:
            load_w(1)
        if tiles:
            stage1(0)
        for ti in range(len(tiles)):
            if ti + 1 < len(tiles):
                stage1(ti + 1)
            stage2(ti)
            e_, tg_ = tiles[ti]
            if tg_ == 3 and e_ + 2 < nexp:
                load_w(e_ + 2)
        for tg in range(4):
            ts_ = slice(tg * 512, (tg + 1) * 512)
            layer_norm_fm(kb, em, gbank, out[:, :, ts_], OUT[tg], lambda i: (ob[:, i, :], OB), ln[:, 0:8], ln[:, 8:16], LN, tmp, TMP,
                          ones, ONES, sq, SQ, mean, MEAN, rstd, RSTD)
            em.dma("sp", "ob", v3(o_d)[:, :, ts_], ob[:, :, :], [OB], [])
        kb.finish([OB])


def build_b2(nexp=32):
    nc = bass.Bass("TRN2", target_bir_lowering=False)
    I = lambda n, shp: nc.dram_tensor(n, shp, F32, kind="ExternalInput").ap()
    D = dict(x1T=I("x1T", [1024, NT]), wr=I("wr", [1024, 36]), brr=I("brr", [1, 36]), ew1=I("ew1", [32, 1024, 512]), ew3=I("ew3", [32, 1024, 512]),
             ew2=I("ew2", [32, 512, 1024]), ln=I("ln", [128, 16]), selb=I("selb", [32, 32 * 128]), g2e=I("g2e", [128, 4 * 32]),
             x2T=nc.dram_tensor("x2T", [1024, NT], F32, kind="ExternalOutput").ap())
    with ExitStack() as st:
        kb = KB(nc, st)
        b2_body(nc, kb, D, nexp)
        kb.emit()
    return nc


def b2_consts():
    selb = np.zeros((32, 32, 128), np.float32)
    for e in range(32):
        selb[e, e, :] = 1.0
    g2e = np.zeros((128, 4, 32), np.float32)
    for g in range(4):
        g2e[:, g, g * 8:(g + 1) * 8] = 1.0
    return dict(selb=selb.reshape(32, 32 * 128), g2e=g2e.reshape(128, 128))


def b2_inputs(inp, l, consts):
    wr = np.concatenate([inp["router_group_w"][l], inp["router_expert_w"][l]], axis=1)
    brr = np.concatenate([inp["router_group_b"][l], inp["router_expert_b"][l]])[None, :]
    lnp = np.concatenate([inp["ln2_g"][l].reshape(8, 128).T, inp["ln2_b"][l].reshape(8, 128).T], axis=1)
    return dict(wr=np.ascontiguousarray(wr), brr=np.ascontiguousarray(brr), ew1=inp["exp_w1"][l], ew3=inp["exp_w3"][l], ew2=inp["exp_w2"][l],
                ln=np.ascontiguousarray(lnp, dtype=np.float32), selb=consts["selb"], g2e=consts["g2e"])


L_ = 4


def build_fused(nl=L_):
    nc = bass.Bass("TRN2", target_bir_lowering=False)

    def I(n, shp):
        return nc.dram_tensor(n, list(shp), F32, kind="ExternalInput").ap()

    def T(n, shp):
        return nc.dram_tensor(n, list(shp), F32, kind="Internal").ap()
    x0T = I("x0T", [1024, S])
    a1w = I("a1_w", [nl, 2, 1024, 1024]); a1vec = I("a1_vec", [nl, 2, 128, 22]); a1lw = I("a1_lw", [nl, 2, 128, 256])
    a1g2 = I("a1_g2", [nl, 2, 128, 256]); a1cst = I("a1_cst", [128, 1280])
    a2wf = I("a2_wf", [nl, 2, 1024, 640]); a2wt = I("a2_wt", [nl, 2, 1024, 140]); a2w1 = I("a2_w1", [nl, 128, 32 * 128])
    a2pe = I("a2_peT", [nl, 128, 32]); a2w2 = I("a2_w2", [nl, 128, 192]); a2btc = I("a2_btc", [2, 128, 4 * 256]); a2bts = I("a2_bts", [2, 128, 4 * TW])
    a2c = {k: I("a2_" + k, shp) for k, shp in (("mkc", [128, 256]), ("mks", [128, TW]), ("mkw", [128, TW]), ("m12", [128, 256]), ("rv", [128, 1]),
                                                 ("c2s", [128, 128]), ("exd", [64, 32 * 128]), ("hsel", [128, 256]))}
    b1wg = I("b1_wg", [nl, 1024, 2048]); b1wur = I("b1_wur", [nl, 512, 1024]); b1wun = I("b1_wun", [nl, 512, 1024]); b1wo = I("b1_wo", [nl, 1024, 1024])
    b1ln = I("b1_ln", [nl, 128, 16])
    b2wr = I("b2_wr", [nl, 1024, 36]); b2br = I("b2_brr", [nl, 1, 36]); b2e1 = I("b2_ew1", [nl, 32, 1024, 512]); b2e3 = I("b2_ew3", [nl, 32, 1024, 512])
    b2e2 = I("b2_ew2", [nl, 32, 512, 1024]); b2ln = I("b2_ln", [nl, 128, 16]); b2selb = I("b2_selb", [32, 32 * 128]); b2g2e = I("b2_g2e", [128, 128])
    outT = nc.dram_tensor("outT", [1024, S], F32, kind="ExternalOutput").ap()
    XT = [T("xt0", [1024, S]), T("xt1", [1024, S])]
    YR = T("yr", [512, S]); YN = T("yn", [512, S]); X1 = T("x1", [1024, S])

    with ExitStack() as st:
        kb = KB(nc, st)
        pn = [0]

        def phase(fn, D, *args):
            with ExitStack() as pst:
                kb.pstack = pst
                kb.prefix = f"p{pn[0]}_"
                pn[0] += 1
                fn(nc, kb, D, *args)
                kb.emit()
            kb.pstack = st
        for l in range(nl):
            xin = x0T if l == 0 else XT[l % 2]
            xout = outT if l == nl - 1 else XT[(l + 1) % 2]
            for hh in range(2):
                phase(a1_body, dict(xT=xin, w=a1w[l, hh], vec=a1vec[l, hh], lw=a1lw[l, hh], g2=a1g2[l, hh], cst=a1cst,
                                    yT=YR[hh * 256:(hh + 1) * 256, :]))
            for g in range(2):
                D = dict(xT=xin, wf=a2wf[l, g], wt=a2wt[l, g], w1=a2w1[l], peT=a2pe[l], w2=a2w2[l], btc=a2btc[g], bts=a2bts[g],
                         yT=YN[g * 256:(g + 1) * 256, :])
                D.update(a2c)
                phase(a2_body, D)
            for hf in range(2):
                ts = slice(hf * NT, (hf + 1) * NT)
                phase(b1_body, dict(xT=xin[:, ts], yrT=YR[:, ts], ynT=YN[:, ts], wg=b1wg[l], wur=b1wur[l], wun=b1wun[l], wo=b1wo[l], ln=b1ln[l],
                                    x1T=X1[:, ts]))
            for hf in range(2):
                ts = slice(hf * NT, (hf + 1) * NT)
                phase(b2_body, dict(x1T=X1[:, ts], wr=b2wr[l], brr=b2br[l], ew1=b2e1[l], ew3=b2e3[l], ew2=b2e2[l], ln=b2ln[l], selb=b2selb,
                                    g2e=b2g2e, x2T=xout[:, ts]))
        print("FUSED instructions:", kb.n_ins, kb.cnt)
    return nc


def fused_inputs(inp, nl=L_):
    c2 = a2_consts(); cb2 = b2_consts()
    a1 = [[a1_inputs(inp, l, 0, hh) for hh in range(2)] for l in range(nl)]
    a2 = [[a2_inputs(inp, l, g, c2) for g in range(2)] for l in range(nl)]
    b1 = [b1_inputs(inp, l) for l in range(nl)]
    b2 = [b2_inputs(inp, l, cb2) for l in range(nl)]
    st = lambda f: np.ascontiguousarray(np.stack(f, axis=0), dtype=np.float32)
    m = {}
    for k, nm in (("w", "a1_w"), ("vec", "a1_vec"), ("lw", "a1_lw"), ("g2", "a1_g2")):
        m[nm] = st([st([a1[l][hh][k] for hh in range(2)]) for l in range(nl)])
    m["a1_cst"] = a1[0][0]["cst"]
    for k, nm in (("wf", "a2_wf"), ("wt", "a2_wt")):
        m[nm] = st([st([a2[l][g][k] for g in range(2)]) for l in range(nl)])
    for k, nm in (("w1", "a2_w1"), ("peT", "a2_peT"), ("w2", "a2_w2")):
        m[nm] = st([a2[l][0][k] for l in range(nl)])
    m["a2_btc"] = st([a2[0][g]["btc"] for g in range(2)]); m["a2_bts"] = st([a2[0][g]["bts"] for g in range(2)])
    for k in ("mkc", "mks", "mkw", "m12", "rv", "c2s", "exd", "hsel"):
        m["a2_" + k] = a2[0][0][k]
    for k, nm in (("wg", "b1_wg"), ("wur", "b1_wur"), ("wun", "b1_wun"), ("wo", "b1_wo"), ("ln", "b1_ln")):
        m[nm] = st([b1[l][k] for l in range(nl)])
    for k, nm in (("wr", "b2_wr"), ("brr", "b2_brr"), ("ln", "b2_ln")):
        m[nm] = st([b2[l][k] for l in range(nl)])
    m["b2_ew1"] = np.ascontiguousarray(inp["exp_w1"][:nl], dtype=np.float32)
    m["b2_ew3"] = np.ascontiguousarray(inp["exp_w3"][:nl], dtype=np.float32)
    m["b2_ew2"] = np.ascontiguousarray(inp["exp_w2"][:nl], dtype=np.float32)
    m["b2_selb"] = cb2["selb"]; m["b2_g2e"] = cb2["g2e"]
    return m

_NC = {}


def kernel(**inputs):
    inp = {k: np.asarray(v) for k, v in inputs.items()}
    if "nc" not in _NC:
        _NC["nc"] = build_fused(L_)
    m = fused_inputs(inp, L_)
    x = inp["x"].astype(np.float32, copy=False)
    B = x.shape[0]
    xT = [np.ascontiguousarray(x[b].T) for b in range(B)]
    maps = []
    for c in range(8):
        mm_ = dict(m); mm_["x0T"] = xT[c // 2]; maps.append(mm_)
    res = run_bass_kernel_spmd(_NC["nc"], maps, core_ids=list(range(8)))
    out = np.stack([res.results[2 * b]["outT"].T for b in range(B)], axis=0)
    return np.ascontiguousarray(out, dtype=np.float32)
```
